# Optimizing a Trainium2 kernel written in Bass

```python
import jax, jax.numpy as jnp
from jax import lax
import numpy as np

D_MODEL = 1024
BATCH = 8
SEQ = 4096
DEPTH = 1

D_MIX = D_MODEL
D_RET = D_MIX // 2
D_POOL = D_MIX - D_RET
RET_HEADS = 8
RET_HEAD_DIM = D_RET // RET_HEADS
RET_CHUNK = 128
ROPE_BASE = 10000.0
POOL_WINDOWS = (2, 4, 8, 16)
POOL_GROUPS = len(POOL_WINDOWS)
POOL_GROUP_DIM = D_POOL // POOL_GROUPS
D_IN_PROJ = 4 * D_RET + D_POOL
N_EXPERTS = 32
TOP_K = 4
D_EXPERT = D_MODEL
SWIGLU_LIMIT = 7.0
SWIGLU_ALPHA = 1.702
PLE_DIM = 256
NORM_EPS = 1e-5
GN_EPS = 1e-5

kernel_name = "hybrid_retention_pool_moe_block"


def rms_norm(x, g):
    xf = x.astype(jnp.float32)
    y = xf * lax.rsqrt(jnp.mean(xf * xf, axis=-1, keepdims=True) + NORM_EPS)
    return (y * g.astype(jnp.float32)).astype(x.dtype)


def rope(x, positions):
    half = x.shape[-1] // 2
    inv_freq = ROPE_BASE ** (-jnp.arange(half, dtype=jnp.float32) / half)
    ang = positions.astype(jnp.float32)[..., None] * inv_freq
    cos = jnp.cos(ang)[:, :, None, :]
    sin = jnp.sin(ang)[:, :, None, :]
    xf = x.astype(jnp.float32)
    x1, x2 = xf[..., :half], xf[..., half:]
    out = jnp.concatenate([x1 * cos - x2 * sin, x2 * cos + x1 * sin], axis=-1)
    return out.astype(x.dtype)


def retention(q, k, v, gn_w):
    B, S, H, d = q.shape
    C = RET_CHUNK
    N = S // C
    log_gamma = jnp.log1p(-jnp.power(2.0, -5.0 - jnp.arange(H, dtype=jnp.float32)))
    qc = q.reshape(B, N, C, H, d)
    kc = (k * (d ** -0.5)).reshape(B, N, C, H, d)
    vc = v.reshape(B, N, C, H, d)
    idx = jnp.arange(C, dtype=jnp.float32)
    rel = idx[:, None] - idx[None, :]
    causal = rel >= 0
    decay = jnp.where(causal[None], jnp.exp(jnp.where(causal, rel, 0.0)[None] * log_gamma[:, None, None]), 0.0)
    scores = jnp.einsum('bnchd,bnmhd->bnhcm', qc, kc) * decay
    intra = jnp.einsum('bnhcm,bnmhd->bnchd', scores, vc)
    zeta = jnp.exp((C - 1 - idx)[None, :] * log_gamma[:, None])
    kv = jnp.einsum('bnmhd,hm,bnmhe->bnhde', kc, zeta, vc)
    chunk_decay = jnp.exp(C * log_gamma)[:, None, None]

    def step(state, kv_i):
        return chunk_decay * state + kv_i, state

    init = jnp.zeros((B, H, d, d), dtype=kv.dtype)
    _, states = lax.scan(step, init, jnp.moveaxis(kv, 1, 0))
    states = jnp.moveaxis(states, 0, 1)
    xi = jnp.exp((idx + 1.0)[None, :] * log_gamma[:, None])
    cross = jnp.einsum('bnchd,bnhde,hc->bnche', qc, states, xi)
    y = (intra + cross).reshape(B, S, H, d).astype(jnp.float32)
    mu = jnp.mean(y, axis=-1, keepdims=True)
    var = jnp.mean(jnp.square(y - mu), axis=-1, keepdims=True)
    y = (y - mu) * lax.rsqrt(var + GN_EPS)
    y = y.reshape(B, S, H * d) * gn_w.astype(jnp.float32)
    return y.astype(q.dtype)


def pool_mixer(u, w_pool, pool_scale):
    B, S, _ = u.shape
    ug = u.reshape(B, S, POOL_GROUPS, POOL_GROUP_DIM).astype(jnp.float32)
    P = jnp.pad(jnp.cumsum(ug, axis=1), ((0, 0), (1, 0), (0, 0), (0, 0)))
    t = jnp.arange(S, dtype=jnp.float32)
    outs = []
    for gi, w in enumerate(POOL_WINDOWS):
        Pg = P[:, :, gi]
        lag = jnp.pad(Pg, ((0, 0), (w, 0), (0, 0)))[:, :S + 1]
        window_sum = Pg[:, 1:] - lag[:, 1:]
        count = jnp.minimum(t + 1.0, float(w))
        outs.append(window_sum / count[None, :, None] - ug[:, :, gi])
    pooled = jnp.stack(outs, axis=2).astype(u.dtype)
    mixed = jnp.einsum('bsgc,gcd->bsgd', pooled, w_pool)
    return mixed.reshape(B, S, D_POOL) * pool_scale


def moe(hn, router_w, router_b, w_gu, b_gu, w_down, b_down):
    B, S, D = hn.shape
    xt = hn.reshape(-1, D)
    logits = xt.astype(jnp.float32) @ router_w.astype(jnp.float32) + router_b.astype(jnp.float32)
    top_vals, top_idx = lax.top_k(logits, TOP_K)
    gates = jax.nn.softmax(top_vals, axis=-1)
    flat_e = top_idx.reshape(-1)
    order = jnp.argsort(flat_e)
    sorted_e = flat_e[order]
    tok = order // TOP_K
    xs = xt[tok]
    group_sizes = jnp.bincount(flat_e, length=N_EXPERTS).astype(jnp.int32)
    gu = lax.ragged_dot(xs, w_gu, group_sizes) + b_gu[sorted_e]
    gate = jnp.minimum(gu[:, ::2], SWIGLU_LIMIT)
    up = jnp.clip(gu[:, 1::2], -SWIGLU_LIMIT, SWIGLU_LIMIT)
    act = (up + 1.0) * (gate * jax.nn.sigmoid(SWIGLU_ALPHA * gate))
    y = lax.ragged_dot(act.astype(w_down.dtype), w_down, group_sizes) + b_down[sorted_e]
    y = y * gates.reshape(-1)[order][:, None].astype(y.dtype)
    out = jnp.zeros_like(xt).at[tok].add(y.astype(xt.dtype))
    return out.reshape(B, S, D)


def setup_inputs(seed: int = 0) -> dict:
    key = jax.random.key(seed)
    ks = jax.random.split(key, 24)
    f32 = jnp.float32
    L = DEPTH

    def nrm(k, shape, scale):
        return jax.random.normal(k, shape, dtype=f32) * scale

    offsets = jax.random.randint(ks[2], (BATCH, 1), 0, 1024, dtype=jnp.int32)
    positions = (jnp.arange(SEQ, dtype=jnp.int32)[None, :] + offsets).astype(jnp.int32)
    return {
        "x": nrm(ks[0], (BATCH, SEQ, D_MODEL), 1.0),
        "p": nrm(ks[1], (DEPTH, BATCH, SEQ, PLE_DIM), 1.0),
        "positions": positions,
        "w_in": nrm(ks[3], (L, D_MODEL, D_IN_PROJ), D_MODEL ** -0.5),
        "w_out": nrm(ks[4], (L, D_MIX, D_MODEL), D_MIX ** -0.5),
        "ret_gn_w": 1.0 + nrm(ks[5], (L, D_RET), 0.02),
        "pool_w": nrm(ks[6], (L, POOL_GROUPS, POOL_GROUP_DIM, POOL_GROUP_DIM), POOL_GROUP_DIM ** -0.5),
        "pool_scale": 1.0 + nrm(ks[7], (L, D_POOL), 0.02),
        "norm_mix_w": 1.0 + nrm(ks[8], (L, D_MODEL), 0.02),
        "norm_moe_w": 1.0 + nrm(ks[9], (L, D_MODEL), 0.02),
        "router_w": nrm(ks[10], (L, D_MODEL, N_EXPERTS), D_MODEL ** -0.5),
        "router_b": nrm(ks[11], (L, N_EXPERTS), 0.01),
        "expert_w_gate_up": nrm(ks[12], (L, N_EXPERTS, D_MODEL, 2 * D_EXPERT), D_MODEL ** -0.5),
        "expert_b_gate_up": nrm(ks[13], (L, N_EXPERTS, 2 * D_EXPERT), 0.02),
        "expert_w_down": nrm(ks[14], (L, N_EXPERTS, D_EXPERT, D_MODEL), D_EXPERT ** -0.5),
        "expert_b_down": nrm(ks[15], (L, N_EXPERTS, D_MODEL), 0.02),
        "norm_ple_w": 1.0 + nrm(ks[16], (L, D_MODEL), 0.02),
        "ple_gate_w": nrm(ks[17], (L, D_MODEL, D_MODEL), D_MODEL ** -0.5),
        "ple_proj_w": nrm(ks[18], (L, PLE_DIM, D_MODEL), PLE_DIM ** -0.5),
        "final_norm_w": 1.0 + nrm(ks[19], (D_MODEL,), 0.02),
    }


def reference(x, p, positions, w_in, w_out, ret_gn_w, pool_w, pool_scale, norm_mix_w,
              norm_moe_w, router_w, router_b, expert_w_gate_up, expert_b_gate_up,
              expert_w_down, expert_b_down, norm_ple_w, ple_gate_w, ple_proj_w, final_norm_w):
    B, S, _ = x.shape
    h = x
    for l in range(DEPTH):
        hn = rms_norm(h, norm_mix_w[l])
        proj = hn @ w_in[l]
        q = proj[..., 0 * D_RET:1 * D_RET].reshape(B, S, RET_HEADS, RET_HEAD_DIM)
        k = proj[..., 1 * D_RET:2 * D_RET].reshape(B, S, RET_HEADS, RET_HEAD_DIM)
        v = proj[..., 2 * D_RET:3 * D_RET].reshape(B, S, RET_HEADS, RET_HEAD_DIM)
        g = proj[..., 3 * D_RET:4 * D_RET]
        u = proj[..., 4 * D_RET:]
        q = rope(q, positions)
        k = rope(k, positions)
        ret = jax.nn.silu(g) * retention(q, k, v, ret_gn_w[l])
        pool = pool_mixer(u, pool_w[l], pool_scale[l])
        h = h + jnp.concatenate([ret, pool], axis=-1) @ w_out[l]
        h = h + moe(rms_norm(h, norm_moe_w[l]), router_w[l], router_b[l],
                    expert_w_gate_up[l], expert_b_gate_up[l], expert_w_down[l], expert_b_down[l])
        gate = jax.nn.sigmoid(rms_norm(h, norm_ple_w[l]) @ ple_gate_w[l])
        h = h + gate * (p[l] @ ple_proj_w[l])
    return rms_norm(h, final_norm_w)
```

```python
import numpy as np
from contextlib import ExitStack
import concourse.bass as bass
import concourse.mybir as mybir
from concourse.bass_utils import run_bass_kernel_spmd

F32 = mybir.dt.float32
BF16 = mybir.dt.bfloat16
I32 = mybir.dt.int32
U32 = mybir.dt.uint32
AF = mybir.ActivationFunctionType
ALU = mybir.AluOpType
AX = mybir.AxisListType

ENGS = ("pe", "dve", "act", "pool", "sp")


class Tok:
    __slots__ = ("name", "w", "r", "sem", "cnt", "multi")

    def __init__(self, name):
        self.name = name
        self.multi = False
        self.w = None
        self.r = {}
        self.sem = None
        self.cnt = 0


class Op:
    __slots__ = ("eng", "fn", "deps", "signal", "sigval", "dma", "idx", "dur", "xfer", "start", "fin", "prev_dma", "delay")

    def __init__(self, eng, fn, deps):
        self.eng = eng
        self.fn = fn
        self.deps = deps
        self.signal = False
        self.sigval = None
        self.dma = None
        self.dur = 0.5
        self.xfer = 0.0
        self.start = 0.0
        self.fin = 0.0
        self.prev_dma = None
        self.delay = 0.0


class DmaDep:
    __slots__ = ("tok", "val", "op")

    def __init__(self, tok, val, op=None):
        self.tok = tok
        self.val = val
        self.op = op


import os as _os
_OPT = {"est": int(_os.environ.get("KEST", "1")), "slack": float(_os.environ.get("KSLACK", "1.2")), "sdel": float(_os.environ.get("KSDEL", "25")), "crit": int(_os.environ.get("KCRIT", "1")), "run": int(_os.environ.get("KRUN", "2500"))}


class _Probe:
    def __getattr__(self, name):
        def f(*a, **k):
            self.__dict__["call"] = (name, a, k)
            return self
        return f


def _estimate(eng, fn):
    pr = _Probe()
    try:
        fn(pr)
        name, a, k = pr.call
    except Exception:
        return None

    def free(ap):
        n = 1
        for d in list(ap.shape)[1:]:
            n *= int(d)
        return n
    try:
        if eng == "pe":
            if name == "matmul":
                rhs = k.get("rhs", a[2] if len(a) > 2 else None)
                cols = free(rhs)
                mult = 4.0 if rhs.dtype == F32 else 1.0
                return 0.05 + 1.15 * mult * max(cols, 64) / 2400.0
            if name == "transpose":
                src = k.get("in_", a[1] if len(a) > 1 else None)
                mult = 3.0 if src.dtype == F32 else 1.0
                return 0.05 + mult * 128 / 2400.0 * 1.2
            return 0.1
        out = k.get("out", a[0] if a else None)
        n = free(out)
        if eng == "dve":
            return 0.12 + n / 960.0
        if eng == "act":
            return 0.22 + n / 1150.0
        if eng == "pool":
            if name == "tensor_scalar":
                return 0.5 + n / 70.0
            return 0.3 + n / 560.0
    except Exception:
        return None
    return None


class Prog:
    def __init__(self, nc, stack):
        self.nc = nc
        self.stack = stack
        self.ops = {e: [] for e in ENGS}
        self.nsem = 0
        self.limit = None
        self.count = 0
        self.segs = [[]]
        self.last_dma = {}
        self.sched = True
        self.slack = _OPT["slack"]

    def tok(self, name):
        return Tok(name)

    def toks(self, name, n):
        return [Tok(f"{name}{i}") for i in range(n)]

    def _collect(self, eng, reads, writes, dma_semtok=None):
        deps = []
        for t in reads:
            if t.multi:
                deps.extend(t.w.values())
            elif t.w is not None:
                deps.append(t.w)
        for t in writes:
            if t.multi:
                deps.extend(t.r.values())
                continue
            if t.w is not None:
                w = t.w
                skip = False
                if isinstance(w, DmaDep) and dma_semtok is not None and w.tok is dma_semtok:
                    skip = True
                if not skip:
                    deps.append(w)
            deps.extend(t.r.values())
        return deps

    def _commit(self, dep, key, reads, writes):
        for t in reads:
            if isinstance(dep, Op):
                t.r[id(dep)] = dep
            else:
                t.r[key] = dep
        for t in writes:
            if t.multi:
                t.w[key] = dep
                continue
            t.w = dep
            t.r = {}

    def op(self, eng, fn, reads=(), writes=(), n=None):
        self.count += 1
        if self.limit is not None and self.count > self.limit:
            return None
        deps = self._collect(eng, reads, writes)
        o = Op(eng, fn, deps)
        if n is not None:
            if eng == "pe":
                o.dur = 0.03 + n / 2400.0
            elif eng == "dve":
                o.dur = 0.12 + n / 960.0
            elif eng == "act":
                o.dur = 0.22 + n / 1200.0
            elif eng == "pool":
                o.dur = 0.25 + n / 450.0
        else:
            est = _estimate(eng, fn) if _OPT["est"] else None
            o.dur = est if est is not None else {"pe": 0.12, "dve": 0.5, "act": 0.6, "pool": 1.15, "sp": 0.1}[eng]
        self.ops[eng].append(o)
        self.segs[-1].append(o)
        self._commit(o, eng, reads, writes)
        return o

    def dma(self, eng, fn, semtok, reads=(), writes=(), nbytes=None, delay=0.0):
        self.count += 1
        if self.limit is not None and self.count > self.limit:
            return None
        deps = self._collect(eng, reads, writes, dma_semtok=semtok)
        o = Op(eng, fn, deps)
        if semtok.sem is None:
            semtok.sem = self.stack.enter_context(self.nc.semaphore(f"d{self.nsem}_{semtok.name}"))
            self.nsem += 1
        semtok.cnt += 16
        o.dma = DmaDep(semtok, semtok.cnt, o)
        o.dur = 1.2 if eng == "pool" else 0.15
        o.xfer = 2.0 + (nbytes or 0) / 150e3
        o.delay = delay
        o.prev_dma = self.last_dma.get(id(semtok))
        self.last_dma[id(semtok)] = o
        self.ops[eng].append(o)
        self.segs[-1].append(o)
        self._commit(o.dma, ("dma", id(semtok)), reads, writes)
        return o

    def wait_all(self, eng, toks):
        deps = self._collect(eng, (), toks)
        o = Op(eng, None, deps)
        o.dur = 0.05
        self.ops[eng].append(o)
        self.segs[-1].append(o)
        return o

    def barrier(self, all_toks):
        for e in ENGS:
            self.wait_all(e, all_toks)
        self.segs.append([])

    def schedule(self, runahead=None):
        runahead = runahead or _OPT["run"]
        new_ops = {e: [] for e in ENGS}
        t_base = 0.0
        for seg in self.segs:
            if not seg:
                continue
            n = len(seg)
            pos = {id(o): i for i, o in enumerate(seg)}
            succ = [[] for _ in range(n)]
            ndep = [0] * n
            preds = [None] * n
            for i, o in enumerate(seg):
                ps_ = []
                for d in o.deps:
                    p = d if isinstance(d, Op) else d.op
                    k = pos.get(id(p))
                    if k is None:
                        continue
                    ps_.append((k, isinstance(d, Op)))
                if o.prev_dma is not None:
                    k = pos.get(id(o.prev_dma))
                    if k is not None:
                        ps_.append((k, None))
                preds[i] = ps_
                ks = set(k for k, _ in ps_)
                ndep[i] = len(ks)
                for k in ks:
                    succ[k].append(i)
            bott = [0.0] * n
            for i in range(n - 1, -1, -1):
                m = 0.0
                for k in succ[i]:
                    if bott[k] > m:
                        m = bott[k]
                bott[i] = seg[i].dur + seg[i].xfer + m
            ready = {e: [] for e in ENGS}
            free = {e: t_base for e in ENGS}
            scheduled = [False] * n
            low = 0

            def make_ready(i):
                o = seg[i]
                t = t_base
                for k, kind in preds[i]:
                    p = seg[k]
                    if kind is None:
                        ft = p.start
                    elif kind:
                        ft = p.fin
                    else:
                        ft = p.fin + p.xfer
                    if kind is not None and p.eng != o.eng:
                        ft += 0.06
                    if ft > t:
                        t = ft
                ready[o.eng].append((t + o.delay, i))

            for i in range(n):
                if ndep[i] == 0:
                    make_ready(i)
            nleft = n
            while nleft > 0:
                while low < n and scheduled[low]:
                    low += 1
                lim = low + runahead
                best = None
                cands = []
                tmin = None
                for e in ENGS:
                    fr = free[e]
                    for (rt, i) in ready[e]:
                        if i >= lim:
                            continue
                        st_ = rt if rt > fr else fr
                        cands.append((st_, i, e, rt))
                        if tmin is None or st_ < tmin:
                            tmin = st_
                if cands:
                    slack = self.slack
                    if _OPT["crit"]:
                        best = max((c for c in cands if c[0] <= tmin + slack), key=lambda c: (bott[c[1]], -c[1]))
                    else:
                        best = min((c for c in cands if c[0] <= tmin + slack), key=lambda c: c[1])
                if best is None:
                    for e in ENGS:
                        for (rt, i) in ready[e]:
                            st_ = max(rt, free[e])
                            if best is None or i < best[1]:
                                best = (st_, i, e, rt)
                assert best is not None, "scheduler stuck"
                st_, i, e, rt = best
                ready[e].remove((rt, i))
                o = seg[i]
                o.start = st_
                o.fin = st_ + o.dur
                free[e] = o.fin
                scheduled[i] = True
                new_ops[e].append(o)
                nleft -= 1
                for k in succ[i]:
                    ndep[k] -= 1
                    if ndep[k] == 0:
                        make_ready(k)
            t_base = max(max(free.values()), max((o.fin + o.xfer) for o in seg))
        self.ops = new_ops
        self.est_total = t_base

    def emit(self):
        nc = self.nc
        if self.sched:
            self.schedule()
        for e in ENGS:
            for o in self.ops[e]:
                for d in o.deps:
                    if isinstance(d, Op) and not (d.eng == "pe" and e == "pe"):
                        d.signal = True
        esem = {}
        for e in ENGS:
            n = 0
            for o in self.ops[e]:
                if o.signal:
                    n += 1
                    o.sigval = n
            esem[e] = self.stack.enter_context(nc.semaphore(f"eng_{e}"))
        self.esem = esem
        stats = {}
        with nc.Block() as block:
            def run(ename, engine):
                waited = {}
                nw = 0
                for o in self.ops[ename]:
                    for d in o.deps:
                        if isinstance(d, Op):
                            if d.eng == "pe" and ename == "pe":
                                continue
                            sem, val, key = esem[d.eng], d.sigval, d.eng
                        else:
                            sem, val, key = d.tok.sem, d.val, id(d.tok)
                        if waited.get(key, 0) >= val:
                            continue
                        waited[key] = val
                        engine.wait_ge(sem, val)
                        nw += 1
                    if o.fn is None:
                        continue
                    inst = o.fn(engine)
                    if o.dma is not None:
                        inst.then_inc(o.dma.tok.sem, 16)
                    elif o.signal:
                        inst.then_inc(esem[ename], 1)
                stats[ename] = (len(self.ops[ename]), nw)

            @block.tensor
            def _(e):
                run("pe", e)

            @block.vector
            def _(e):
                run("dve", e)

            @block.scalar
            def _(e):
                run("act", e)

            @block.gpsimd
            def _(e):
                run("pool", e)

            @block.sync
            def _(e):
                run("sp", e)
        self.stats = stats
        return stats


D = 1024
SEQ = 4096
NT = SEQ // 128
H = 8
DH = 64
NE = 32
TOPK = 4
CAP = 768
NSLOT = NE * CAP
PLE = 256
EPS = 1e-5
LIMIT = 7.0
ALPHA = 1.702
BIGPOS = 1.0e6
WINS = (2, 4, 8, 16)

_off = {}
_n = 0
for _name, _w in (("ident", 128), ("mask", 128), ("ustrict", 128), ("ones", 128), ("invf", 64),
                  ("xiq", 8), ("xik", 8), ("gc", 512), ("bands", 12 * 128), ("ecap", 32),
                  ("nmix", 8), ("nple", 8), ("pscale", 4), ("bgate", 256), ("bup", 256)):
    _off[_name] = (_n, _w)
    _n += _w
CST_N = _n
_roff = {}
_n = 0
for _name, _w in (("gnw", 512), ("nmoe", 1024), ("rb", 32), ("fnw", 1024)):
    _roff[_name] = (_n, _w)
    _n += _w
ROW_N = _n


def host_consts(inp):
    c = np.zeros((128, CST_N), np.float32)

    def put(name, arr):
        o, w = _off[name]
        c[:, o:o + w] = np.asarray(arr, np.float32).reshape(128, w)

    idx = np.arange(128)
    put("ident", np.eye(128))
    put("mask", (idx[None, :] >= idx[:, None]).astype(np.float32))
    put("ustrict", (idx[:, None] < idx[None, :]).astype(np.float32))
    put("ones", np.ones((128, 128)))
    half = DH // 2
    invf = (10000.0 ** (-(np.arange(half, dtype=np.float32)) / np.float32(half))).astype(np.float32)
    put("invf", np.tile(np.concatenate([invf, invf])[None, :], (128, 1)))
    lg = np.log1p(-np.power(2.0, -5.0 - np.arange(H, dtype=np.float64)))
    cpos = (idx + 1.0)[:, None]
    put("xiq", np.exp(cpos * lg[None, :]))
    put("xik", np.exp(-cpos * lg[None, :]) * (DH ** -0.5))
    gc = np.zeros((128, 512))
    for p in range(128):
        for h in range(H):
            if p // 64 == h % 2:
                gc[p, h * 64:(h + 1) * 64] = np.exp(128.0 * lg[h])
    put("gc", gc)
    bands = np.zeros((128, 12, 128))
    for g, w in enumerate(WINS):
        tp = idx[:, None]
        t = idx[None, :]
        inwin = (tp <= t) & (tp > t - w)
        bands[:, 3 * g + 0, :] = inwin / float(w) - (tp == t)
        cnt0 = np.minimum(t + 1.0, float(w))
        bands[:, 3 * g + 1, :] = inwin / cnt0 - (tp == t)
        bands[:, 3 * g + 2, :] = (tp >= 129 + t - w) / float(w)
    put("bands", bands)
    put("ecap", np.tile((np.arange(NE) * CAP)[None, :], (128, 1)))
    put("nmix", inp["norm_mix_w"][0].reshape(8, 128).T)
    put("nple", inp["norm_ple_w"][0].reshape(8, 128).T)
    put("pscale", inp["pool_scale"][0].reshape(4, 128).T)
    bgu = inp["expert_b_gate_up"][0]
    put("bgate", bgu[:, 0::2].reshape(NE, 8, 128).transpose(2, 0, 1))
    put("bup", bgu[:, 1::2].reshape(NE, 8, 128).transpose(2, 0, 1))
    r = np.zeros((1, ROW_N), np.float32)
    for name, v in (("gnw", inp["ret_gn_w"][0]), ("nmoe", inp["norm_moe_w"][0]),
                    ("rb", inp["router_b"][0]), ("fnw", inp["final_norm_w"])):
        o, w = _roff[name]
        r[0, o:o + w] = v
    return c, r


def _bc(ap, shape):
    return ap.broadcast_to(shape)


_BCREG = {}


def _bcreg(e, nc):
    if _BCREG.get("nc") is not nc:
        r = e.alloc_register("slot_bound")
        e.reg_mov(r, NSLOT - 1)
        _BCREG["nc"] = nc
        _BCREG["r"] = r
    return _BCREG["r"]


class Ctx:
    pass


def build(nt=NT, phases=4, debug=False, limit=None, n_exp=NE, sched=True):
    nc = bass.Bass("TRN2", target_bir_lowering=False)
    dt = lambda name, shape, dtype, kind: nc.dram_tensor(name, shape, dtype, kind=kind).ap()
    x_d = dt("x", [SEQ, D], F32, "ExternalInput")
    p_d = dt("p", [SEQ, PLE], F32, "ExternalInput")
    pos_d = dt("pos", [128, NT], I32, "ExternalInput")
    cst_d = dt("cst", [128, CST_N], F32, "ExternalInput")
    rows_d = dt("rows", [1, ROW_N], F32, "ExternalInput")
    win_d = dt("w_in", [D, 2560], F32, "ExternalInput")
    wout_d = dt("w_out", [D, D], F32, "ExternalInput")
    poolw_d = dt("pool_w", [4, 128, 128], F32, "ExternalInput")
    rw_d = dt("router_w", [D, NE], F32, "ExternalInput")
    wg_d = dt("w_g", [NE, D, D], F32, "ExternalInput")
    wu_d = dt("w_u", [NE, D, D], F32, "ExternalInput")
    wd_d = dt("w_d", [NE, D, D], F32, "ExternalInput")
    bd_d = dt("b_d", [NE, D], F32, "ExternalInput")
    pgw_d = dt("ple_gate_w", [D, D], F32, "ExternalInput")
    ppw_d = dt("ple_proj_w", [PLE, D], F32, "ExternalInput")
    out_d = dt("out", [SEQ, D], F32, "ExternalOutput")
    xs_d = nc.dram_tensor("xs_scr", [NSLOT, D], BF16).ap()
    ys_d = nc.dram_tensor("ys_scr", [NSLOT, D], F32).ap()
    h_d = nc.dram_tensor("h_scr", [SEQ, D], F32).ap()
    dbg = {}
    if debug:
        dbg["h1"] = dt("dbg_h1", [SEQ, D], F32, "ExternalOutput")
        dbg["gate"] = dt("dbg_gate", [128, NT * 4], F32, "ExternalOutput")
        dbg["pos"] = dt("dbg_pos", [128, NT * 4], I32, "ExternalOutput")
        dbg["h2"] = dt("dbg_h2", [SEQ, D], F32, "ExternalOutput")

    with ExitStack() as gst:
        P = Prog(nc, gst)
        P.limit = limit
        P.sched = sched
        alltoks = []

        def T(name):
            t = P.tok(name)
            alltoks.append(t)
            return t

        def sbuf(st, name, shape, dtype):
            return st.enter_context(nc.sbuf_tensor("s_" + name, shape, dtype))

        V = lambda fn, r=(), w=(), n=None: P.op("dve", fn, r, w, n)
        A = lambda fn, r=(), w=(), n=None: P.op("act", fn, r, w, n)
        G = lambda fn, r=(), w=(), n=None: P.op("pool", fn, r, w, n)
        M = lambda fn, r=(), w=(), n=None: P.op("pe", fn, r, w, n)

        cst = sbuf(gst, "cst", [128, CST_N], F32)
        t_cst = T("cst")
        identb = sbuf(gst, "identb", [128, 128], BF16)
        t_identb = T("identb")
        gates = sbuf(gst, "gates", [128, NT, 4], F32)
        posi = sbuf(gst, "posi", [128, NT, 4], I32)
        t_gates = [T(f"gates{j}") for j in range(NT)]
        t_posi = [T(f"posi{j}") for j in range(NT)]
        nhalf = sbuf(gst, "nhalf", [128, 8], F32)
        t_nhalf = T("nhalf")
        wgu_buf, wd_buf, t_wgu, t_wd = [], [], [], []
        psb = [gst.enter_context(nc.psum_tensor(f"ps{i}", [128, 512], F32)) for i in range(8)]
        t_ps = [T(f"ps{i}") for i in range(8)]

        def C(name):
            o, w = _off[name]
            return cst[:, o:o + w]

        t_hd = [T(f"hd{j}") for j in range(NT)]
        t_dbgs = [T(f"dbgs{i}") for i in range(16)] if debug else []
        t_xsd = T("xsd")
        t_xsd.multi = True
        t_xsd.w = {}
        t_zfd = T("zfd")
        t_ysd = T("ysd")
        t_ysd.multi = True
        t_ysd.w = {}

        P.dma("sp", lambda e: e.dma_start(out=cst[:], in_=cst_d), t_cst, writes=[t_cst])
        V(lambda e: e.tensor_copy(identb[:], C("ident")), [t_cst], [t_identb])
        G(lambda e: e.memset(nhalf[:], -0.5), (), [t_nhalf])
        if debug:
            G(lambda e: e.memset(gates[:], 0.0), (), t_gates)
            G(lambda e: e.memset(posi[:], 0), (), t_posi)
        ident = C("ident")

        def load_expert(e_idx, slot):
            wg_v = wg_d[e_idx].rearrange("(c p) n -> p c n", p=128)
            wu_v = wu_d[e_idx].rearrange("(c p) n -> p c n", p=128)
            wd_v = wd_d[e_idx].rearrange("(c p) n -> p c n", p=128)
            for hc in range(2):
                cs = slice(hc * 4, hc * 4 + 4)
                P.dma("pool", lambda e, cs=cs: e.dma_start(out=wgu_buf[slot][:, cs, 0:1024], in_=wg_v[:, cs, :]),
                      t_wgu[slot], writes=[t_wgu[slot]])
                P.dma("pool", lambda e, cs=cs: e.dma_start(out=wgu_buf[slot][:, cs, 1024:2048], in_=wu_v[:, cs, :]),
                      t_wgu[slot], writes=[t_wgu[slot]])
                P.dma("pool", lambda e, cs=cs: e.dma_start(out=wd_buf[slot][:, cs, :], in_=wd_v[:, cs, :]),
                      t_wd[slot], writes=[t_wd[slot]])

        with ExitStack() as st:
            RN1 = _roff["fnw"][0]
            rows = sbuf(st, "rows", [128, RN1], F32)
            t_rows = T("rows")
            P.dma("sp", lambda e: e.dma_start(out=rows[:], in_=rows_d[0, 0:RN1].partition_broadcast(128)), t_rows, writes=[t_rows])

            def R(name):
                o, w = _roff[name]
                return rows[:, o:o + w]

            w_in = sbuf(st, "w_in", [128, 8, 2560], BF16)
            t_win = T("w_in")
            w_out = sbuf(st, "w_out", [128, 8, 1024], BF16)
            t_wout = T("w_out")
            poolw = sbuf(st, "poolw", [128, 4, 128], BF16)
            t_poolw = T("poolw")
            rw = sbuf(st, "rw", [128, 8, NE], F32)
            t_rw = T("rw")
            posi_t = sbuf(st, "pos_i", [128, NT], I32)
            posf = sbuf(st, "pos_f", [128, NT], F32)
            CC = sbuf(st, "CC", [128, NT, 64], F32)
            SS = sbuf(st, "SS", [128, NT, 64], F32)
            st_setup = ExitStack()
            ang = sbuf(st_setup, "ang", [128, NT, 64], F32)
            tmpa = sbuf(st_setup, "tmpa", [128, NT, 64], F32)
            stage = [sbuf(st_setup, f"stage{i}", [128, 1280], F32) for i in range(2)]
            t_stage = [T(f"stage{i}") for i in range(2)]
            win_v = win_d.rearrange("(c p) n -> p c n", p=128)
            for c2 in range(16):
                s = c2 % 2
                c, hf = c2 // 2, c2 % 2
                P.dma("sp", lambda e, c=c, s=s, hf=hf: e.dma_start(out=stage[s][:], in_=win_v[:, c, hf * 1280:(hf + 1) * 1280]), t_stage[s], writes=[t_stage[s]])
                o, _ = _off["nmix"]
                if c2 % 2 == 0:
                    V(lambda e, c=c, s=s, o=o, hf=hf: e.tensor_scalar(out=w_in[:, c, hf * 1280:(hf + 1) * 1280], in0=stage[s][:], scalar1=cst[:, o + c:o + c + 1],
                                                                    scalar2=None, op0=ALU.mult), [t_stage[s], t_cst], [t_win], n=1280)
                else:
                    A(lambda e, c=c, s=s, o=o, hf=hf: e.activation(out=w_in[:, c, hf * 1280:(hf + 1) * 1280], in_=stage[s][:], func=AF.Copy, scale=cst[:, o + c:o + c + 1]),
                      [t_stage[s], t_cst], [t_win], n=1280)
            P.dma("pool", lambda e: e.dma_start(out=w_out[:], in_=wout_d.rearrange("(c p) n -> p c n", p=128)), t_wout, writes=[t_wout])
            P.dma("pool", lambda e: e.dma_start(out=poolw[:], in_=poolw_d.rearrange("g c d -> c g d")), t_poolw, writes=[t_poolw])
            P.dma("sp", lambda e: e.dma_start(out=rw[:], in_=rw_d.rearrange("(c p) n -> p c n", p=128)), t_rw, writes=[t_rw])

            t_pos, t_ang, t_tmpa, t_CC, t_SS = T("pos"), T("ang"), T("tmpa"), T("CC"), T("SS")
            P.dma("sp", lambda e: e.dma_start(out=posi_t[:], in_=pos_d), t_pos, writes=[t_pos])
            V(lambda e: e.tensor_copy(posf[:], posi_t[:]), [t_pos], [t_pos])
            V(lambda e: e.tensor_tensor(out=ang[:], in0=_bc(posf[:].unsqueeze(2), [128, NT, 64]),
                                        in1=_bc(C("invf").unsqueeze(1), [128, NT, 64]), op=ALU.mult), [t_pos, t_cst], [t_ang])
            TWO_PI = float(2.0 * np.pi)

            def sin_table(dst, t_dst, shift):
                V(lambda e: e.tensor_scalar(out=tmpa[:], in0=ang[:], scalar1=shift, scalar2=None, op0=ALU.add), [t_ang], [t_tmpa])
                V(lambda e: e.tensor_scalar(out=dst[:].bitcast(I32), in0=tmpa[:], scalar1=1.0 / TWO_PI, scalar2=None, op0=ALU.mult), [t_tmpa], [t_dst])
                V(lambda e: e.tensor_copy(dst[:], dst[:].bitcast(I32)), [t_dst], [t_dst])
                V(lambda e: e.scalar_tensor_tensor(out=tmpa[:], in0=dst[:], scalar=-TWO_PI, in1=tmpa[:], op0=ALU.mult, op1=ALU.add),
                  [t_dst, t_tmpa], [t_tmpa])
                V(lambda e: e.tensor_scalar(out=dst[:], in0=tmpa[:], scalar1=float(np.pi), scalar2=-TWO_PI, op0=ALU.is_gt, op1=ALU.mult),
                  [t_tmpa], [t_dst])
                V(lambda e: e.tensor_tensor(out=tmpa[:], in0=tmpa[:], in1=dst[:], op=ALU.add), [t_tmpa, t_dst], [t_tmpa])
                V(lambda e: e.tensor_scalar(out=dst[:], in0=tmpa[:], scalar1=-float(np.pi), scalar2=TWO_PI, op0=ALU.is_lt, op1=ALU.mult),
                  [t_tmpa], [t_dst])
                V(lambda e: e.tensor_tensor(out=tmpa[:], in0=tmpa[:], in1=dst[:], op=ALU.add), [t_tmpa, t_dst], [t_tmpa])
                V(lambda e: e.tensor_scalar(out=tmpa[:], in0=tmpa[:], scalar1=-3.1415925, scalar2=3.1415925, op0=ALU.max, op1=ALU.min),
                  [t_tmpa], [t_tmpa])
                A(lambda e: e.activation(out=dst[:], in_=tmpa[:], func=AF.Sin), [t_tmpa], [t_dst])

            sin_table(CC, t_CC, float(np.pi / 2))
            sin_table(SS, t_SS, 0.0)
            V(lambda e: e.tensor_scalar(out=SS[:, :, 0:32], in0=SS[:, :, 0:32], scalar1=-1.0, scalar2=None, op0=ALU.mult), [t_SS], [t_SS])

            P.barrier(alltoks)
            st_setup.close()
            state = sbuf(st, "state", [128, 512], F32)
            stateb = sbuf(st, "stateb", [128, 512], BF16)
            t_state, t_stateb = T("state"), T("stateb")
            V(lambda e: e.memset(state[:], 0.0), (), [t_state])
            V(lambda e: e.memset(stateb[:], 0.0), (), [t_stateb])
            macc = sbuf(st, "macc", [128, NE], F32)
            t_macc = T("macc")
            V(lambda e: e.memset(macc[:], 0.0), (), [t_macc])
            us = [sbuf(st, f"us{i}", [128, 512], F32) for i in range(2)]
            t_us = [T(f"us{i}") for i in range(2)]

            NB = 2
            def mk(name, shape, dtype, n=NB):
                return [sbuf(st, f"{name}{i}", shape, dtype) for i in range(n)], [T(f"{name}{i}") for i in range(n)]
            xt, t_xt = mk("xt", [128, D], F32)
            junk, t_junk = mk("junk", [128, D], BF16, 1)
            ssq, t_ssq = mk("ssq", [128, 8], F32)
            xT, t_xT = mk("xT", [128, 8, 128], BF16)
            qs, t_qs = mk("qs", [128, 512], F32)
            ks, t_ks = mk("ks", [128, 512], F32)
            vb, t_vb = mk("vb", [128, 512], BF16)
            sg, t_sg = mk("sg", [128, 512], F32)
            qa, t_qa = mk("qa", [128, 512], F32)
            qb, t_qb = mk("qb", [128, 512], F32)
            ka, t_ka, kb, t_kb = qa, t_qa, qb, t_qb
            qt, t_qt = mk("qt", [128, 512], BF16)
            kt, t_kt = mk("kt", [128, 512], BF16)
            qkT, t_qkT = mk("qkT", [128, 1536], BF16)
            for i_ in range(NB):
                V(lambda e, i_=i_: e.memset(qkT[i_][:], 0.0), (), [t_qkT[i_]])
            PT, t_PT = mk("PT", [128, 1024], BF16)
            ysb, t_ysb = mk("ysb", [128, 512], F32)
            ysq, t_ysq = mk("ysq", [128, 512], F32, 1)
            ysq, t_ysq = ysq * 2, t_ysq * 2
            gst8, t_gst8 = mk("gst8", [128, 6, 8], F32)
            gsg, t_gsg = mk("gsg", [128, 512], F32)
            ret, t_ret = mk("ret", [128, 512], BF16)
            mixT, t_mixT = mk("mixT", [128, 8, 128], BF16)
            pooledT, t_pooledT = mk("pooledT", [128, 4, 128], BF16)
            ht, t_ht = mk("ht", [128, D], F32)
            xn2, t_xn2 = mk("xn2", [128, D], F32, 1)
            xn2, t_xn2 = xn2 * 2, t_xn2 * 2
            NBX = 5
            xn2b, t_xn2b = mk("xn2b", [128, D], BF16, NBX)
            pend = []
            zt = sbuf(st, "zt", [128, 2048], BF16)
            t_zt, t_zf = T("zt"), T("zf")
            G(lambda e: e.memset(zt[:], 0.0), (), [t_zt])
            assert NSLOT % 256 == 0
            for kz in range(NSLOT // 256):
                P.dma("act" if kz % 2 else "sp", lambda e, kz=kz: e.dma_start(out=xs_d[kz * 256:(kz + 1) * 256, :].rearrange("(p r) d -> p (r d)", p=128), in_=zt[:]),
                      t_zf, reads=[t_zt], writes=[t_zfd], nbytes=512 * 1024, delay=40.0 + 1.5 * kz)
            xn2T, t_xn2T = mk("xn2T", [128, 8, 128], F32, 1)
            xn2T, t_xn2T = xn2T * 2, t_xn2T * 2
            rt, t_rt = mk("rt", [128, 8, 32], F32)
            top8, t_top8 = mk("top8", [128, 16], F32)
            posk, t_posk = mk("posk", [128, 4], F32)

            bank_ctr = [0]

            def nxt():
                v = bank_ctr[0] % 8
                bank_ctr[0] += 1
                return v

            for j in range(nt):
                b = j % NB
                P.dma("sp", lambda e, j=j, b=b: e.dma_start(out=xt[b][:], in_=x_d[j * 128:(j + 1) * 128, :]), t_xt[b], writes=[t_xt[b]])
                A(lambda e, b=b: e.activation(out=junk[0][:], in_=xt[b][:], func=AF.Square, accum_out=ssq[b][:, 0:1]),
                  [t_xt[b]], [t_junk[0], t_ssq[b]])
                V(lambda e, b=b: e.tensor_scalar(out=ssq[b][:, 1:2], in0=ssq[b][:, 0:1], scalar1=1.0 / D, scalar2=EPS, op0=ALU.mult, op1=ALU.add),
                  [t_ssq[b]], [t_ssq[b]])
                G(lambda e, b=b: e.tensor_tensor(out=ssq[b][:, 2:3], in0=ssq[b][:, 1:2], in1=nhalf[:, 0:1], op=ALU.pow),
                  [t_ssq[b], t_nhalf], [t_ssq[b]])
                rstd = ssq[b][:, 2:3]
                bx0, bx1 = nxt(), nxt()
                for c in range(8):
                    bank = (bx0, bx1)[c // 4]
                    M(lambda e, b=b, c=c, bank=bank: e.transpose(psb[bank][:, (c % 4) * 128:(c % 4 + 1) * 128], xt[b][:, c * 128:(c + 1) * 128], ident),
                      [t_xt[b], t_cst], [t_ps[bank]])
                V(lambda e, b=b, bx0=bx0: e.tensor_copy(xT[b][:, 0:4, :], psb[bx0][:].rearrange("p (c t) -> p c t", c=4)), [t_ps[bx0]], [t_xT[b]])
                A(lambda e, b=b, bx1=bx1: e.copy(xT[b][:, 4:8, :], psb[bx1][:].rearrange("p (c t) -> p c t", c=4)), [t_ps[bx1]], [t_xT[b]])
                bp = [nxt() for _ in range(5)]
                for nb in range(5):
                    for c in range(8):
                        M(lambda e, b=b, nb=nb, c=c, bk=bp[nb]: e.matmul(psb[bk][:], lhsT=xT[b][:, c, :], rhs=w_in[:, c, nb * 512:(nb + 1) * 512],
                                                             start=(c == 0), stop=(c == 7)),
                          [t_xT[b], t_win], [t_ps[bp[nb]]], n=530)
                ub = j % 2
                A(lambda e, bp=bp, b=b: e.activation(out=qs[b][:], in_=psb[bp[0]][:], func=AF.Copy, scale=ssq[b][:, 2:3]), [t_ps[bp[0]], t_ssq[b]], [t_qs[b]])
                A(lambda e, bp=bp, b=b: e.activation(out=ks[b][:], in_=psb[bp[1]][:], func=AF.Copy, scale=ssq[b][:, 2:3]), [t_ps[bp[1]], t_ssq[b]], [t_ks[b]])
                A(lambda e, bp=bp, b=b: e.activation(out=vb[b][:], in_=psb[bp[2]][:], func=AF.Copy, scale=ssq[b][:, 2:3]), [t_ps[bp[2]], t_ssq[b]], [t_vb[b]])
                A(lambda e, bp=bp, b=b: e.activation(out=sg[b][:], in_=psb[bp[3]][:], func=AF.Silu, scale=ssq[b][:, 2:3]), [t_ps[bp[3]], t_ssq[b]], [t_sg[b]])
                A(lambda e, bp=bp, b=b, ub=ub: e.activation(out=us[ub][:], in_=psb[bp[4]][:], func=AF.Copy, scale=ssq[b][:, 2:3]), [t_ps[bp[4]], t_ssq[b]], [t_us[ub]])
                ccj = _bc(CC[:, j, :].unsqueeze(1), [128, 8, 64])
                ssj_lo = _bc(SS[:, j, 0:32].unsqueeze(1), [128, 8, 32])
                ssj_hi = _bc(SS[:, j, 32:64].unsqueeze(1), [128, 8, 32])
                for (src, t_src, aa, t_aa, bb, t_bb, dst, t_dst, xin) in (
                        (qs, t_qs, qa, t_qa, qb, t_qb, qt, t_qt, "xiq"), (ks, t_ks, ka, t_ka, kb, t_kb, kt, t_kt, "xik")):
                    s4 = src[b][:].rearrange("p (h two d) -> p h two d", two=2, d=32)
                    b4 = bb[b][:].rearrange("p (h two d) -> p h two d", two=2, d=32)
                    V(lambda e, b=b, src=src, aa=aa, ccj=ccj: e.tensor_tensor(out=aa[b][:].rearrange("p (h d) -> p h d", d=64),
                                                                              in0=src[b][:].rearrange("p (h d) -> p h d", d=64), in1=ccj, op=ALU.mult),
                      [t_src[b], t_CC], [t_aa[b]])
                    V(lambda e, s4=s4, b4=b4, ssj_lo=ssj_lo: e.tensor_tensor(out=b4[:, :, 0, :], in0=s4[:, :, 1, :], in1=ssj_lo, op=ALU.mult),
                      [t_src[b], t_SS], [t_bb[b]])
                    V(lambda e, s4=s4, b4=b4, ssj_hi=ssj_hi: e.tensor_tensor(out=b4[:, :, 1, :], in0=s4[:, :, 0, :], in1=ssj_hi, op=ALU.mult),
                      [t_src[b], t_SS], [t_bb[b]])
                    V(lambda e, b=b, aa=aa, bb=bb: e.tensor_tensor(out=aa[b][:], in0=aa[b][:], in1=bb[b][:], op=ALU.add), [t_aa[b], t_bb[b]], [t_aa[b]])
                    V(lambda e, b=b, aa=aa, dst=dst, xin=xin: e.tensor_tensor(out=dst[b][:].rearrange("p (h d) -> p h d", d=64),
                                                                           in0=aa[b][:].rearrange("p (h d) -> p h d", d=64),
                                                                           in1=_bc(C(xin).unsqueeze(2), [128, 8, 64]), op=ALU.mult),
                      [t_aa[b], t_cst], [t_dst[b]])
                bqk = nxt()
                pb2 = psb[bqk][:].bitcast(BF16)
                for i in range(4):
                    M(lambda e, b=b, i=i, pb2=pb2: e.transpose(pb2[:, i * 128:(i + 1) * 128], qt[b][:, i * 128:(i + 1) * 128], identb[:]),
                      [t_qt[b], t_identb], [t_ps[bqk]])
                for i in range(4):
                    M(lambda e, b=b, i=i, pb2=pb2: e.transpose(pb2[:, 512 + i * 128:512 + (i + 1) * 128], kt[b][:, i * 128:(i + 1) * 128], identb[:]),
                      [t_kt[b], t_identb], [t_ps[bqk]])
                A(lambda e, b=b, pb2=pb2: e.copy(qkT[b][:, 0:512], pb2[:, 0:512]), [t_ps[bqk]], [t_qkT[b]])
                A(lambda e, b=b, pb2=pb2: e.copy(qkT[b][0:64, 512:1024], pb2[0:64, 512:1024]), [t_ps[bqk]], [t_qkT[b]])
                A(lambda e, b=b, pb2=pb2: e.copy(qkT[b][64:128, 1024:1536], pb2[64:128, 512:1024]), [t_ps[bqk]], [t_qkT[b]])
                bs = [nxt(), nxt()]
                for h in range(H):
                    hb = (h % 2) * 64
                    hh = h // 2
                    bank = bs[h // 4]
                    M(lambda e, b=b, h=h, hb=hb, hh=hh, bank=bank: e.matmul(
                        psb[bank][:, (h % 4) * 128:(h % 4 + 1) * 128],
                        lhsT=qkT[b][:, 512 + (h % 2) * 512 + hh * 128:512 + (h % 2) * 512 + (hh + 1) * 128],
                        rhs=qkT[b][:, hh * 128:(hh + 1) * 128], start=True, stop=True),
                      [t_qkT[b]], [t_ps[bank]])
                mask4 = _bc(C("mask").unsqueeze(1), [128, 4, 128])
                for half in range(2):
                    V(lambda e, b=b, half=half, bs=bs: e.tensor_tensor(out=PT[b][:, half * 512:(half + 1) * 512].rearrange("p (h c) -> p h c", c=128),
                                                                in0=psb[bs[half]][:].rearrange("p (h c) -> p h c", c=128), in1=mask4, op=ALU.mult),
                      [t_ps[bs[half]], t_cst], [t_PT[b]])
                by, bkv = nxt(), nxt()
                for h in range(H):
                    hb = (h % 2) * 64
                    hh = h // 2
                    M(lambda e, b=b, h=h, by=by: e.matmul(psb[by][:, h * 64:(h + 1) * 64], lhsT=PT[b][:, h * 128:(h + 1) * 128],
                                                   rhs=vb[b][:, h * 64:(h + 1) * 64], start=True, stop=False),
                      [t_PT[b], t_vb[b]], [t_ps[by]])
                    M(lambda e, b=b, h=h, hb=hb, hh=hh, by=by: e.matmul(psb[by][:, h * 64:(h + 1) * 64], lhsT=qkT[b][:, hh * 128:(hh + 1) * 128],
                                                                 rhs=stateb[:, h * 64:(h + 1) * 64], start=False, stop=True),
                      [t_qkT[b], t_stateb], [t_ps[by]])
                for hh in range(4):
                    M(lambda e, b=b, hh=hh, bkv=bkv: e.matmul(psb[bkv][:, hh * 128:(hh + 1) * 128], lhsT=kt[b][:, hh * 128:(hh + 1) * 128],
                                                     rhs=vb[b][:, hh * 128:(hh + 1) * 128], start=True, stop=True),
                      [t_kt[b], t_vb[b]], [t_ps[bkv]])
                V(lambda e, bkv=bkv: e.tensor_tensor(out=state[:], in0=state[:], in1=psb[bkv][:], op=ALU.add), [t_state, t_ps[bkv]], [t_state])
                V(lambda e: e.tensor_tensor(out=state[:], in0=state[:], in1=C("gc"), op=ALU.mult), [t_state, t_cst], [t_state])
                V(lambda e: e.tensor_copy(stateb[:], state[:]), [t_state], [t_stateb])
                A(lambda e, b=b, by=by: e.copy(ysb[b][:], psb[by][:]), [t_ps[by]], [t_ysb[b]])
                A(lambda e, b=b, by=by: e.activation(out=ysq[b][:], in_=psb[by][:], func=AF.Square), [t_ps[by]], [t_ysq[b]])
                G(lambda e, b=b: e.tensor_tensor(out=gsg[b][:], in0=sg[b][:], in1=R("gnw"), op=ALU.mult), [t_sg[b], t_rows], [t_gsg[b]])
                g8 = gst8[b]
                V(lambda e, b=b, g8=g8: e.tensor_reduce(out=g8[:, 0, :], in_=ysb[b][:].rearrange("p (h d) -> p h d", d=64), axis=AX.X, op=ALU.add),
                  [t_ysb[b]], [t_gst8[b]])
                V(lambda e, b=b, g8=g8: e.tensor_reduce(out=g8[:, 1, :], in_=ysq[b][:].rearrange("p (h d) -> p h d", d=64), axis=AX.X, op=ALU.add),
                  [t_ysq[b]], [t_gst8[b]])
                V(lambda e, g8=g8: e.tensor_scalar(out=g8[:, 2, :], in0=g8[:, 0, :], scalar1=1.0 / DH, scalar2=None, op0=ALU.mult), [t_gst8[b]], [t_gst8[b]])
                V(lambda e, g8=g8: e.tensor_tensor(out=g8[:, 3, :], in0=g8[:, 2, :], in1=g8[:, 2, :], op=ALU.mult), [t_gst8[b]], [t_gst8[b]])
                V(lambda e, g8=g8: e.scalar_tensor_tensor(out=g8[:, 4, :], in0=g8[:, 1, :], scalar=1.0 / DH, in1=g8[:, 3, :], op0=ALU.mult, op1=ALU.subtract),
                  [t_gst8[b]], [t_gst8[b]])
                V(lambda e, g8=g8: e.tensor_scalar(out=g8[:, 4, :], in0=g8[:, 4, :], scalar1=EPS, scalar2=None, op0=ALU.add), [t_gst8[b]], [t_gst8[b]])
                G(lambda e, g8=g8: e.tensor_tensor(out=g8[:, 5, :], in0=g8[:, 4, :], in1=nhalf[:, 0:8], op=ALU.pow), [t_gst8[b], t_nhalf], [t_gst8[b]])
                y3 = ysb[b][:].rearrange("p (h d) -> p h d", d=64)
                V(lambda e, y3=y3, g8=g8: e.tensor_tensor(out=y3, in0=y3, in1=_bc(g8[:, 2, :].unsqueeze(2), [128, 8, 64]), op=ALU.subtract),
                  [t_ysb[b], t_gst8[b]], [t_ysb[b]])
                V(lambda e, y3=y3, g8=g8: e.tensor_tensor(out=y3, in0=y3, in1=_bc(g8[:, 5, :].unsqueeze(2), [128, 8, 64]), op=ALU.mult),
                  [t_ysb[b], t_gst8[b]], [t_ysb[b]])
                V(lambda e, b=b: e.tensor_tensor(out=ret[b][:], in0=ysb[b][:], in1=gsg[b][:], op=ALU.mult), [t_ysb[b], t_gsg[b]], [t_ret[b]])
                brt = nxt()
                pb1 = psb[brt][:].bitcast(BF16)
                for i in range(4):
                    M(lambda e, b=b, i=i, pb1=pb1: e.transpose(pb1[:, i * 128:(i + 1) * 128], ret[b][:, i * 128:(i + 1) * 128], identb[:]),
                      [t_ret[b], t_identb], [t_ps[brt]])
                A(lambda e, b=b, pb1=pb1: e.copy(mixT[b][:, 0:4, :], pb1[:, 0:512].rearrange("p (c t) -> p c t", c=4)), [t_ps[brt]], [t_mixT[b]])
                bo, _ = _off["bands"]
                bpl, bmx = nxt(), nxt()
                for g in range(4):
                    kind = 1 if j == 0 else 0
                    M(lambda e, g=g, ub=ub, kind=kind, bo=bo, j=j, bpl=bpl: e.matmul(psb[bpl][:, g * 128:(g + 1) * 128], lhsT=us[ub][:, g * 128:(g + 1) * 128],
                                                                       rhs=cst[:, bo + (3 * g + kind) * 128:bo + (3 * g + kind + 1) * 128],
                                                                       start=True, stop=(j == 0)),
                      [t_us[ub], t_cst], [t_ps[bpl]])
                    if j > 0:
                        M(lambda e, g=g, ub=ub, bo=bo, bpl=bpl: e.matmul(psb[bpl][:, g * 128:(g + 1) * 128], lhsT=us[1 - ub][:, g * 128:(g + 1) * 128],
                                                                rhs=cst[:, bo + (3 * g + 2) * 128:bo + (3 * g + 3) * 128], start=False, stop=True),
                          [t_us[1 - ub], t_cst], [t_ps[bpl]])
                V(lambda e, b=b, bpl=bpl: e.tensor_copy(pooledT[b][:], psb[bpl][:].rearrange("p (g t) -> p g t", g=4)), [t_ps[bpl]], [t_pooledT[b]])
                for g in range(4):
                    M(lambda e, b=b, g=g, bmx=bmx: e.matmul(psb[bmx][:, g * 128:(g + 1) * 128], lhsT=poolw[:, g, :], rhs=pooledT[b][:, g, :], start=True, stop=True),
                      [t_poolw, t_pooledT[b]], [t_ps[bmx]])
                po, _ = _off["pscale"]
                V(lambda e, b=b, po=po, bmx=bmx: e.tensor_tensor(out=mixT[b][:, 4:8, :], in0=psb[bmx][:].rearrange("p (g t) -> p g t", g=4),
                                                        in1=_bc(cst[:, po:po + 4].unsqueeze(2), [128, 4, 128]), op=ALU.mult),
                  [t_ps[bmx], t_cst], [t_mixT[b]])
                bh = [nxt(), nxt()]
                for nb in range(2):
                    for c in range(8):
                        M(lambda e, b=b, nb=nb, c=c, bh=bh: e.matmul(psb[bh[nb]][:], lhsT=mixT[b][:, c, :], rhs=w_out[:, c, nb * 512:(nb + 1) * 512],
                                                             start=(c == 0), stop=(c == 7)),
                          [t_mixT[b], t_wout], [t_ps[bh[nb]]], n=530)
                for nb in range(2):
                    V(lambda e, b=b, nb=nb, bh=bh: e.tensor_tensor(out=ht[b][:, nb * 512:(nb + 1) * 512], in0=psb[bh[nb]][:], in1=xt[b][:, nb * 512:(nb + 1) * 512], op=ALU.add),
                      [t_ps[bh[nb]], t_xt[b]], [t_ht[b]])
                P.dma("sp", lambda e, j=j, b=b: e.dma_start(out=h_d[j * 128:(j + 1) * 128, :], in_=ht[b][:]), t_ht[b], reads=[t_ht[b]], writes=[t_hd[j]])
                if debug:
                    P.dma("sp", lambda e, j=j, b=b: e.dma_start(out=dbg["h1"][j * 128:(j + 1) * 128, :], in_=ht[b][:]), t_dbgs[j % 8], reads=[t_ht[b]])
                if phases < 2:
                    continue

                A(lambda e, b=b: e.activation(out=junk[0][:], in_=ht[b][:], func=AF.Square, accum_out=ssq[b][:, 4:5]),
                  [t_ht[b]], [t_junk[0], t_ssq[b]])
                V(lambda e, b=b: e.tensor_scalar(out=ssq[b][:, 5:6], in0=ssq[b][:, 4:5], scalar1=1.0 / D, scalar2=EPS, op0=ALU.mult, op1=ALU.add),
                  [t_ssq[b]], [t_ssq[b]])
                G(lambda e, b=b: e.tensor_tensor(out=ssq[b][:, 6:7], in0=ssq[b][:, 5:6], in1=nhalf[:, 0:1], op=ALU.pow),
                  [t_ssq[b], t_nhalf], [t_ssq[b]])
                V(lambda e, b=b: e.scalar_tensor_tensor(out=xn2[b][:], in0=ht[b][:], scalar=ssq[b][:, 6:7], in1=R("nmoe"), op0=ALU.mult, op1=ALU.mult),
                  [t_ht[b], t_ssq[b], t_rows], [t_xn2[b]])
                bx = j % NBX
                A(lambda e, b=b, bx=bx: e.copy(xn2b[bx][:], xn2[b][:]), [t_xn2[b]], [t_xn2b[bx]])
                bn = [nxt(), nxt()]
                blg = nxt()
                for c in range(8):
                    bank = bn[c // 4]
                    M(lambda e, b=b, c=c, bank=bank: e.transpose(psb[bank][:, (c % 4) * 128:(c % 4 + 1) * 128], xn2[b][:, c * 128:(c + 1) * 128], ident),
                      [t_xn2[b], t_cst], [t_ps[bank]])
                V(lambda e, b=b, bn=bn: e.tensor_copy(xn2T[b][:, 0:4, :], psb[bn[0]][:].rearrange("p (c t) -> p c t", c=4)), [t_ps[bn[0]]], [t_xn2T[b]])
                A(lambda e, b=b, bn=bn: e.copy(xn2T[b][:, 4:8, :], psb[bn[1]][:].rearrange("p (c t) -> p c t", c=4)), [t_ps[bn[1]]], [t_xn2T[b]])
                for c in range(8):
                    M(lambda e, b=b, c=c, blg=blg: e.matmul(psb[blg][:, 0:NE], lhsT=xn2T[b][:, c, :], rhs=rw[:, c, :], start=(c == 0), stop=(c == 7)),
                      [t_xn2T[b], t_rw], [t_ps[blg]])
                r_ = rt[b]
                t8 = top8[b]
                V(lambda e, r_=r_, blg=blg: e.tensor_tensor(out=r_[:, 0, :], in0=psb[blg][:, 0:NE], in1=R("rb"), op=ALU.add), [t_ps[blg], t_rows], [t_rt[b]])
                V(lambda e, r_=r_, t8=t8: e.max(out=t8[:, 0:8], in_=r_[:, 0, :]), [t_rt[b]], [t_top8[b]])
                V(lambda e, t8=t8: e.tensor_scalar(out=t8[:, 8:9], in0=t8[:, 0:1], scalar1=-1.0, scalar2=None, op0=ALU.mult), [t_top8[b]], [t_top8[b]])
                A(lambda e, t8=t8: e.activation(out=t8[:, 10:14], in_=t8[:, 0:4], func=AF.Exp, bias=t8[:, 8:9], accum_out=t8[:, 9:10]),
                  [t_top8[b]], [t_top8[b]])
                V(lambda e, t8=t8: e.reciprocal(t8[:, 14:15], t8[:, 9:10]), [t_top8[b]], [t_top8[b]])
                V(lambda e, r_=r_, t8=t8: e.tensor_scalar(out=r_[:, 1, :], in0=r_[:, 0, :], scalar1=t8[:, 3:4], scalar2=None, op0=ALU.is_ge),
                  [t_rt[b], t_top8[b]], [t_rt[b]])
                M(lambda e, r_=r_, blg=blg: e.matmul(psb[blg][:, 32:64], lhsT=C("ustrict"), rhs=r_[:, 1, :], start=True, stop=False), [t_rt[b], t_cst], [t_ps[blg]])
                M(lambda e, blg=blg: e.matmul(psb[blg][:, 32:64], lhsT=C("ones"), rhs=macc[:], start=False, stop=True), [t_macc, t_cst], [t_ps[blg]])
                V(lambda e, r_=r_: e.tensor_tensor(out=macc[:], in0=macc[:], in1=r_[:, 1, :], op=ALU.add), [t_macc, t_rt[b]], [t_macc])
                V(lambda e, r_=r_, blg=blg: e.tensor_copy(r_[:, 2, :], psb[blg][:, 32:64]), [t_ps[blg]], [t_rt[b]])
                V(lambda e, r_=r_: e.tensor_scalar(out=r_[:, 3, :], in0=r_[:, 2, :], scalar1=float(CAP) - 0.5, scalar2=BIGPOS, op0=ALU.is_ge, op1=ALU.mult),
                  [t_rt[b]], [t_rt[b]])
                V(lambda e, r_=r_: e.tensor_tensor(out=r_[:, 4, :], in0=r_[:, 2, :], in1=C("ecap"), op=ALU.add), [t_rt[b], t_cst], [t_rt[b]])
                V(lambda e, r_=r_: e.tensor_tensor(out=r_[:, 4, :], in0=r_[:, 4, :], in1=r_[:, 3, :], op=ALU.add), [t_rt[b]], [t_rt[b]])
                for k in range(TOPK):
                    V(lambda e, r_=r_, t8=t8, b=b, k=k: e.scalar_tensor_tensor(out=r_[:, 6, :], in0=r_[:, 0, :], scalar=t8[:, k:k + 1], in1=r_[:, 4, :],
                                                                             op0=ALU.is_equal, op1=ALU.mult, accum_out=posk[b][:, k:k + 1]),
                      [t_rt[b], t_top8[b]], [t_rt[b], t_posk[b]])
                V(lambda e, r_=r_, b=b: e.tensor_scalar(out=r_[:, 7, 0:4], in0=posk[b][:, 0:4], scalar1=BIGPOS * 0.5, scalar2=None, op0=ALU.is_lt),
                  [t_posk[b]], [t_rt[b]])
                V(lambda e, r_=r_, t8=t8, j=j: e.scalar_tensor_tensor(out=gates[:, j, :], in0=t8[:, 10:14], scalar=t8[:, 14:15], in1=r_[:, 7, 0:4],
                                                                      op0=ALU.mult, op1=ALU.mult),
                  [t_top8[b], t_rt[b]], [t_gates[j]])
                V(lambda e, b=b, j=j: e.tensor_copy(posi[:, j, :], posk[b][:, 0:4]), [t_posk[b]], [t_posi[j]])
                def scatter(j=j, bx=bx):
                    for k in range(TOPK):
                        P.dma("pool", lambda e, bx=bx, j=j, k=k: e.indirect_dma_start(
                            out=xs_d, out_offset=bass.IndirectOffsetOnAxis(ap=posi[:, j, k:k + 1], axis=0), in_=xn2b[bx][:], in_offset=None,
                            bounds_check=_bcreg(e, nc), oob_is_err=False), t_xn2b[bx], reads=[t_xn2b[bx], t_posi[j], t_zfd], writes=[t_xsd],
                            nbytes=256 * 1024, delay=_OPT["sdel"])
                pend.append(scatter)
                if len(pend) >= NBX - 1:
                    pend.pop(0)()
            for f_ in pend:
                f_()

            P.barrier(alltoks)

        if phases >= 3:
            build_phase3(locals())
        if phases >= 4:
            build_phase4(locals())

        if debug and phases >= 2:
            P.dma("sp", lambda e: e.dma_start(out=dbg["gate"], in_=gates[:].rearrange("p j k -> p (j k)")), T("dbg_g"), reads=t_gates)
            P.dma("sp", lambda e: e.dma_start(out=dbg["pos"], in_=posi[:].rearrange("p j k -> p (j k)")), T("dbg_p"), reads=t_posi)
        P.barrier(alltoks)
        stats = P.emit()
    return nc, stats


def make_in_maps(inp, ncores=8):
    cst, rows = host_consts(inp)
    wgu = inp["expert_w_gate_up"][0]
    w_g = np.ascontiguousarray(wgu[:, :, 0::2])
    w_u = np.ascontiguousarray(wgu[:, :, 1::2])
    shared = {
        "cst": cst, "rows": rows,
        "w_in": np.ascontiguousarray(inp["w_in"][0]), "w_out": np.ascontiguousarray(inp["w_out"][0]),
        "pool_w": np.ascontiguousarray(inp["pool_w"][0]), "router_w": np.ascontiguousarray(inp["router_w"][0]),
        "w_g": w_g, "w_u": w_u, "w_d": np.ascontiguousarray(inp["expert_w_down"][0]),
        "b_d": np.ascontiguousarray(inp["expert_b_down"][0]),
        "ple_gate_w": np.ascontiguousarray(inp["ple_gate_w"][0]), "ple_proj_w": np.ascontiguousarray(inp["ple_proj_w"][0]),
    }
    maps = []
    for b in range(ncores):
        m = dict(shared)
        m["x"] = np.ascontiguousarray(inp["x"][b])
        m["p"] = np.ascontiguousarray(inp["p"][0, b])
        m["pos"] = np.ascontiguousarray(inp["positions"][b].reshape(NT, 128).T.astype(np.int32))
        maps.append(m)
    return maps


def build_phase3(L):
    nc, P, T, sbuf, cst = L["nc"], L["P"], L["T"], L["sbuf"], L["cst"]
    V, A, G, M = L["V"], L["A"], L["G"], L["M"]
    psb, t_ps, t_cst, identb, t_identb = L["psb"], L["t_ps"], L["t_cst"], L["identb"], L["t_identb"]
    xs_d, ys_d, wg_d, wu_d, wd_d, bd_d = L["xs_d"], L["ys_d"], L["wg_d"], L["wu_d"], L["wd_d"], L["bd_d"]
    t_xsd, t_ysd, alltoks = L["t_xsd"], L["t_ysd"], L["alltoks"]
    n_exp = L.get("n_exp", NE)
    NSL = (CAP + 127) // 128
    HALF = CAP // 2

    def rows(i):
        return 128

    def tstart(i):
        return min(i * 128, CAP - 128)
    with ExitStack() as st:
        wgu = [sbuf(st, f"wgu{i}", [128, 8, 2048], BF16) for i in range(2)]
        wdn = [sbuf(st, f"wdn{i}", [128, 8, 1024], BF16) for i in range(2)]
        t_wgu = [T(f"wgu{i}") for i in range(2)]
        t_wdn = [T(f"wdn{i}") for i in range(2)]
        xtok = sbuf(st, "xtok", [128, NSL, D], BF16)
        t_xtok = T("xtok")
        XT = [sbuf(st, f"XT{i}", [128, 8, CAP], BF16) for i in range(2)]
        t_XT = [T(f"XT{i}") for i in range(2)]
        actT = [sbuf(st, f"actT{i}", [128, 8, CAP], BF16) for i in range(2)]
        t_actT = [T(f"actT{i}") for i in range(2)]
        NTMP = 2
        tmp = [sbuf(st, f"etmp{i}", [128, 4, HALF], F32) for i in range(NTMP)]
        t_tmp = [[T(f"etmp{i}_{k}") for k in range(4)] for i in range(NTMP)]
        NY = 4
        ysb = [sbuf(st, f"ysb3_{i}", [128, D], F32) for i in range(NY)]
        t_ysb = [T(f"ysb3_{i}") for i in range(NY)]
        bdb = [sbuf(st, f"bdb{i}", [128, D], F32) for i in range(2)]
        t_bdb = [T(f"bdb{i}") for i in range(2)]
        abg = sbuf(st, "abg", [128, 256], F32)
        t_abg = T("abg")
        ob, _ = _off["bgate"]
        ou, _ = _off["bup"]
        V(lambda e: e.tensor_scalar(out=abg[:], in0=cst[:, ob:ob + 256], scalar1=ALPHA, scalar2=None, op0=ALU.mult), [t_cst], [t_abg])
        bu1 = sbuf(st, "bu1", [128, 256], F32)
        V(lambda e: e.tensor_scalar(out=bu1[:], in0=cst[:, ou:ou + 256], scalar1=1.0, scalar2=None, op0=ALU.add), [t_cst], [t_abg])
        SIG7 = float(1.0 / (1.0 + np.exp(-ALPHA * LIMIT)))

        def load_wgu(e_idx):
            sl = e_idx % 2
            wg_v = wg_d[e_idx].rearrange("(c p) n -> p c n", p=128)
            wu_v = wu_d[e_idx].rearrange("(c p) n -> p c n", p=128)
            for hc in range(2):
                cs = slice(hc * 4, hc * 4 + 4)
                P.dma("pool", lambda e, cs=cs, sl=sl, wg_v=wg_v: e.dma_start(out=wgu[sl][:, cs, 0:1024], in_=wg_v[:, cs, :]), t_wgu[sl], writes=[t_wgu[sl]])
                P.dma("pool", lambda e, cs=cs, sl=sl, wu_v=wu_v: e.dma_start(out=wgu[sl][:, cs, 1024:2048], in_=wu_v[:, cs, :]), t_wgu[sl], writes=[t_wgu[sl]])

        def load_wd(e_idx):
            sl = e_idx % 2
            wd_v = wd_d[e_idx].rearrange("(c p) n -> p c n", p=128)
            for hc in range(2):
                cs = slice(hc * 4, hc * 4 + 4)
                P.dma("pool", lambda e, cs=cs, sl=sl, wd_v=wd_v: e.dma_start(out=wdn[sl][:, cs, :], in_=wd_v[:, cs, :]), t_wdn[sl], writes=[t_wdn[sl]])
            P.dma("sp", lambda e, sl=sl, e_idx=e_idx: e.dma_start(out=bdb[sl][:], in_=bd_d[e_idx].partition_broadcast(128)), t_bdb[sl], writes=[t_bdb[sl]])

        evac_rr = [0]

        def stage_T(e_idx):
            sl = e_idx % 2
            nfull = CAP // 128
            P.dma("sp", lambda e, e_idx=e_idx, nfull=nfull: e.dma_start(out=xtok[:, 0:nfull, :],
                                                                        in_=xs_d[e_idx * CAP:e_idx * CAP + nfull * 128, :].rearrange("(i p) d -> p i d", p=128)),
                  t_xtok, reads=[t_xsd], writes=[t_xtok], nbytes=nfull * 256 * 1024)
            if CAP % 128:
                P.dma("sp", lambda e, e_idx=e_idx, nfull=nfull: e.dma_start(out=xtok[:, nfull, :], in_=xs_d[(e_idx + 1) * CAP - 128:(e_idx + 1) * CAP, :]),
                      t_xtok, reads=[t_xsd], writes=[t_xtok], nbytes=256 * 1024)
            for i in range(NSL):
                bank = 4
                pbv = psb[bank][:].bitcast(BF16)
                ri = rows(i)
                for c in range(8):
                    M(lambda e, i=i, c=c, pbv=pbv, ri=ri: e.transpose(pbv[:, c * 128:c * 128 + ri], xtok[0:ri, i, c * 128:(c + 1) * 128], identb[0:ri, 0:ri]),
                      [t_xtok, t_identb], [t_ps[bank]])
                eng = A if evac_rr[0] % 2 == 0 else V
                evac_rr[0] += 1
                if eng is A:
                    A(lambda e, i=i, sl=sl, pbv=pbv, ri=ri: e.copy(XT[sl][:, :, tstart(i):tstart(i) + ri], pbv.rearrange("p (c s) -> p c s", c=8)[:, :, 0:ri]),
                      [t_ps[bank]], [t_XT[sl]])
                else:
                    V(lambda e, i=i, sl=sl, pbv=pbv, ri=ri: e.tensor_copy(XT[sl][:, :, tstart(i):tstart(i) + ri], pbv.rearrange("p (c s) -> p c s", c=8)[:, :, 0:ri]),
                      [t_ps[bank]], [t_XT[sl]])

        cnt = [0]

        def stage_GU(e_idx):
            sl = e_idx % 2
            for jc in range(8):
                for hf in range(2):
                    pp = cnt[0] % 2
                    tb = cnt[0] % NTMP
                    cnt[0] += 1
                    bg, bu = psb[2 * pp], psb[2 * pp + 1]
                    ssl = slice(hf * HALF, (hf + 1) * HALF)
                    for c in range(8):
                        M(lambda e, sl=sl, jc=jc, c=c, bg=bg, ssl=ssl: e.matmul(bg[:, 0:HALF], lhsT=wgu[sl][:, c, jc * 128:(jc + 1) * 128], rhs=XT[sl][:, c, ssl],
                                                                             start=(c == 0), stop=(c == 7)),
                          [t_wgu[sl], t_XT[sl]], [t_ps[2 * pp]], n=HALF)
                    for c in range(8):
                        M(lambda e, sl=sl, jc=jc, c=c, bu=bu, ssl=ssl: e.matmul(bu[:, 0:HALF], lhsT=wgu[sl][:, c, 1024 + jc * 128:1024 + (jc + 1) * 128], rhs=XT[sl][:, c, ssl],
                                                                             start=(c == 0), stop=(c == 7)),
                          [t_wgu[sl], t_XT[sl]], [t_ps[2 * pp + 1]], n=HALF)
                    col = e_idx * 8 + jc
                    tm, tt = tmp[tb], t_tmp[tb]
                    A(lambda e, tm=tm, bg=bg, col=col: e.activation(out=tm[:, 0, :], in_=bg[:, 0:HALF], func=AF.Sigmoid, bias=abg[:, col:col + 1], scale=ALPHA),
                      [t_ps[2 * pp], t_abg], [tt[0]])
                    A(lambda e, tm=tm, bg=bg, col=col: e.activation(out=tm[:, 1, :], in_=bg[:, 0:HALF], func=AF.Identity, bias=cst[:, ob + col:ob + col + 1], scale=1.0),
                      [t_ps[2 * pp], t_cst], [tt[1]])
                    A(lambda e, tm=tm, bu=bu, col=col: e.activation(out=tm[:, 2, :], in_=bu[:, 0:HALF], func=AF.Identity, bias=bu1[:, col:col + 1], scale=1.0),
                      [t_ps[2 * pp + 1], t_abg], [tt[2]])
                    V(lambda e, tm=tm: e.tensor_scalar(out=tm[:, 2, :], in0=tm[:, 2, :], scalar1=LIMIT + 1.0, scalar2=-LIMIT + 1.0, op0=ALU.min, op1=ALU.max), [tt[2]], [tt[2]])
                    V(lambda e, tm=tm: e.scalar_tensor_tensor(out=tm[:, 3, :], in0=tm[:, 1, :], scalar=LIMIT, in1=tm[:, 2, :], op0=ALU.min, op1=ALU.mult),
                      [tt[1], tt[2]], [tt[3]])
                    V(lambda e, tm=tm, sl=sl, jc=jc, ssl=ssl: e.scalar_tensor_tensor(out=actT[sl][:, jc, ssl], in0=tm[:, 0, :], scalar=SIG7, in1=tm[:, 3, :],
                                                                                  op0=ALU.min, op1=ALU.mult), [tt[0], tt[3]], [t_actT[sl]])

        ycnt = [0]
        ybank = [0]

        def stage_down(e_idx):
            sl = e_idx % 2
            for i in range(NSL):
                yb = ycnt[0] % NY
                ycnt[0] += 1
                ri = rows(i)
                for nb in range(2):
                    bk = 5 + ybank[0] % 3
                    ybank[0] += 1
                    for jc in range(8):
                        M(lambda e, sl=sl, i=i, nb=nb, jc=jc, bk=bk, ri=ri: e.matmul(psb[bk][0:ri, :], lhsT=actT[sl][:, jc, tstart(i):tstart(i) + ri], rhs=wdn[sl][:, jc, nb * 512:(nb + 1) * 512],
                                                                              start=(jc == 0), stop=(jc == 7)),
                          [t_actT[sl], t_wdn[sl]], [t_ps[bk]], n=512)
                    V(lambda e, yb=yb, nb=nb, sl=sl, bk=bk, ri=ri: e.tensor_tensor(out=ysb[yb][0:ri, nb * 512:(nb + 1) * 512], in0=psb[bk][0:ri, :], in1=bdb[sl][0:ri, nb * 512:(nb + 1) * 512], op=ALU.add),
                      [t_ps[bk], t_bdb[sl]], [t_ysb[yb]], n=512)
                ov = i * 128 - tstart(i)
                r0 = e_idx * CAP + i * 128
                P.dma("sp", lambda e, yb=yb, r0=r0, ov=ov: e.dma_start(out=ys_d[r0:r0 + 128 - ov, :], in_=ysb[yb][ov:128, :]), t_ysb[yb], reads=[t_ysb[yb]], writes=[t_ysd],
                      nbytes=(128 - ov) * 4096)

        load_wgu(0)
        load_wd(0)
        load_wgu(1)
        stage_T(0)
        if n_exp > 1:
            stage_T(1)
        stage_GU(0)
        for s_ in range(n_exp):
            if s_ + 2 < n_exp:
                load_wgu(s_ + 2)
            if s_ + 1 < n_exp:
                load_wd(s_ + 1)
            if s_ + 2 < n_exp:
                stage_T(s_ + 2)
            if s_ + 1 < n_exp:
                stage_GU(s_ + 1)
            stage_down(s_)
        P.barrier(alltoks)


def build_phase4(L):
    nc, P, T, sbuf, cst = L["nc"], L["P"], L["T"], L["sbuf"], L["cst"]
    V, A, G, M = L["V"], L["A"], L["G"], L["M"]
    psb, t_ps, t_cst, identb, t_identb = L["psb"], L["t_ps"], L["t_cst"], L["identb"], L["t_identb"]
    ys_d, h_d, p_d, out_d, pgw_d, ppw_d, rows_d = L["ys_d"], L["h_d"], L["p_d"], L["out_d"], L["pgw_d"], L["ppw_d"], L["rows_d"]
    t_ysd, t_hd, alltoks = L["t_ysd"], L["t_hd"], L["alltoks"]
    gates, posi, t_gates, t_posi, nhalf, t_nhalf = L["gates"], L["posi"], L["t_gates"], L["t_posi"], L["nhalf"], L["t_nhalf"]
    nt, dbg, debug = L["nt"], L["dbg"], L["debug"]
    ident = cst[:, _off["ident"][0]:_off["ident"][0] + 128]
    with ExitStack() as st:
        pgw = sbuf(st, "pgw", [128, 8, D], BF16)
        t_pgw = T("pgw")
        ppw = sbuf(st, "ppw", [128, 2, D], BF16)
        t_ppw = T("ppw")
        fnw = sbuf(st, "fnw", [128, D], F32)
        t_fnw = T("fnw")
        stg = [sbuf(st, f"stg4_{i}", [128, D], F32) for i in range(2)]
        t_stg = [T(f"stg4_{i}") for i in range(2)]
        pgw_v = pgw_d.rearrange("(c p) n -> p c n", p=128)
        on, _ = _off["nple"]
        for c in range(8):
            s_ = c % 2
            P.dma("sp", lambda e, c=c, s_=s_: e.dma_start(out=stg[s_][:], in_=pgw_v[:, c, :]), t_stg[s_], writes=[t_stg[s_]])
            if c % 2 == 0:
                V(lambda e, c=c, s_=s_: e.tensor_scalar(out=pgw[:, c, :], in0=stg[s_][:], scalar1=cst[:, on + c:on + c + 1], scalar2=None, op0=ALU.mult),
                  [t_stg[s_], t_cst], [t_pgw], n=1024)
            else:
                A(lambda e, c=c, s_=s_: e.activation(out=pgw[:, c, :], in_=stg[s_][:], func=AF.Copy, scale=cst[:, on + c:on + c + 1]),
                  [t_stg[s_], t_cst], [t_pgw], n=1024)
        P.dma("pool", lambda e: e.dma_start(out=ppw[:], in_=ppw_d.rearrange("(c p) n -> p c n", p=128)), t_ppw, writes=[t_ppw])
        fo, fw = _roff["fnw"]
        P.dma("sp", lambda e: e.dma_start(out=fnw[:], in_=rows_d[0, fo:fo + fw].partition_broadcast(128)), t_fnw, writes=[t_fnw])

        NB = 4
        bctr = [0]

        def nxt():
            v = bctr[0] % 8
            bctr[0] += 1
            return v

        def mk(name, shape, dtype, n=NB):
            return [sbuf(st, f"{name}{i}", shape, dtype) for i in range(n)], [T(f"{name}{i}") for i in range(n)]
        hb_, t_hb = mk("h4", [128, D], F32)
        pt_, t_pt = mk("p4", [128, PLE], F32)
        yk, t_yk = mk("yk", [128, 4, D], F32)
        junk, t_junk = mk("junk4", [128, D], BF16, 1)
        sq, t_sq = mk("sq4", [128, 8], F32)
        xn3, t_xn3 = mk("xn3", [128, D], BF16)
        xn3T, t_xn3T = mk("xn3T", [128, 8, 128], BF16)
        pT, t_pT = mk("pT", [128, 2, 128], BF16)
        sgm, t_sgm = mk("sgm", [128, D], F32)
        ot, t_ot = mk("ot", [128, D], F32)
        h3, _unused = mk("h3_", [128, D], F32)
        t_sgmh = [[T(f"sgmh{i}_{k}") for k in range(2)] for i in range(NB)]
        t_h3h = [[T(f"h3h{i}_{k}") for k in range(2)] for i in range(NB)]
        for i_ in range(NB):
            G(lambda e, i_=i_: e.memset(yk[i_][:], 0.0), (), [t_yk[i_]])

        for j in range(nt):
            b = j % NB
            P.dma("sp", lambda e, j=j, b=b: e.dma_start(out=hb_[b][:], in_=h_d[j * 128:(j + 1) * 128, :]), t_hb[b], reads=[t_hd[j]], writes=[t_hb[b]])
            P.dma("sp", lambda e, j=j, b=b: e.dma_start(out=pt_[b][:], in_=p_d[j * 128:(j + 1) * 128, :]), t_pt[b], writes=[t_pt[b]])
            for k in range(TOPK):
                P.dma("pool", lambda e, j=j, b=b, k=k: e.indirect_dma_start(
                    out=yk[b][:, k, :], out_offset=None, in_=ys_d, in_offset=bass.IndirectOffsetOnAxis(ap=posi[:, j, k:k + 1], axis=0),
                    bounds_check=_bcreg(e, nc), oob_is_err=False), t_yk[b], reads=[t_ysd, t_posi[j]], writes=[t_yk[b]])
            for k in range(TOPK):
                V(lambda e, j=j, b=b, k=k: e.scalar_tensor_tensor(out=hb_[b][:], in0=yk[b][:, k, :], scalar=gates[:, j, k:k + 1], in1=hb_[b][:],
                                                                 op0=ALU.mult, op1=ALU.add), [t_yk[b], t_gates[j], t_hb[b]], [t_hb[b]])
            if debug:
                P.dma("sp", lambda e, j=j, b=b: e.dma_start(out=dbg["h2"][j * 128:(j + 1) * 128, :], in_=hb_[b][:]), L["t_dbgs"][8 + j % 8], reads=[t_hb[b]])
            A(lambda e, b=b: e.activation(out=junk[0][:], in_=hb_[b][:], func=AF.Square, accum_out=sq[b][:, 0:1]), [t_hb[b]], [t_junk[0], t_sq[b]])
            V(lambda e, b=b: e.tensor_scalar(out=sq[b][:, 1:2], in0=sq[b][:, 0:1], scalar1=1.0 / D, scalar2=EPS, op0=ALU.mult, op1=ALU.add), [t_sq[b]], [t_sq[b]])
            G(lambda e, b=b: e.tensor_tensor(out=sq[b][:, 2:3], in0=sq[b][:, 1:2], in1=nhalf[:, 0:1], op=ALU.pow), [t_sq[b], t_nhalf], [t_sq[b]])
            A(lambda e, b=b: e.activation(out=xn3[b][:], in_=hb_[b][:], func=AF.Copy, scale=sq[b][:, 2:3]), [t_hb[b], t_sq[b]], [t_xn3[b]])
            b0_, b1_ = nxt(), nxt()
            bg_ = [nxt(), nxt()]
            bp_ = [nxt(), nxt()]
            pb0 = psb[b0_][:].bitcast(BF16)
            for c in range(8):
                M(lambda e, b=b, c=c, pb0=pb0: e.transpose(pb0[:, c * 128:(c + 1) * 128], xn3[b][:, c * 128:(c + 1) * 128], identb[:]),
                  [t_xn3[b], t_identb], [t_ps[b0_]])
            V(lambda e, b=b, pb0=pb0: e.tensor_copy(xn3T[b][:], pb0.rearrange("p (c t) -> p c t", c=8)), [t_ps[b0_]], [t_xn3T[b]], n=1024)
            for c in range(2):
                M(lambda e, b=b, c=c, b1_=b1_: e.transpose(psb[b1_][:, c * 128:(c + 1) * 128], pt_[b][:, c * 128:(c + 1) * 128], ident), [t_pt[b], t_cst], [t_ps[b1_]], n=400)
            A(lambda e, b=b, b1_=b1_: e.copy(pT[b][:], psb[b1_][:, 0:256].rearrange("p (c t) -> p c t", c=2)), [t_ps[b1_]], [t_pT[b]], n=256)
            for nb in range(2):
                for c in range(8):
                    M(lambda e, b=b, nb=nb, c=c, bg_=bg_: e.matmul(psb[bg_[nb]][:], lhsT=xn3T[b][:, c, :], rhs=pgw[:, c, nb * 512:(nb + 1) * 512], start=(c == 0), stop=(c == 7)),
                      [t_xn3T[b], t_pgw], [t_ps[bg_[nb]]], n=600)
            for nb in range(2):
                for c in range(2):
                    M(lambda e, b=b, nb=nb, c=c, bp_=bp_: e.matmul(psb[bp_[nb]][:], lhsT=pT[b][:, c, :], rhs=ppw[:, c, nb * 512:(nb + 1) * 512], start=(c == 0), stop=(c == 1)),
                      [t_pT[b], t_ppw], [t_ps[bp_[nb]]], n=600)
            for nb in range(2):
                hs = slice(nb * 512, (nb + 1) * 512)
                A(lambda e, b=b, nb=nb, bg_=bg_, hs=hs: e.activation(out=sgm[b][:, hs], in_=psb[bg_[nb]][:], func=AF.Sigmoid), [t_ps[bg_[nb]]], [t_sgmh[b][nb]], n=512)
                V(lambda e, b=b, nb=nb, bp_=bp_, hs=hs: e.tensor_tensor(out=sgm[b][:, hs], in0=sgm[b][:, hs], in1=psb[bp_[nb]][:], op=ALU.mult),
                  [t_sgmh[b][nb], t_ps[bp_[nb]]], [t_sgmh[b][nb]], n=512)
                V(lambda e, b=b, hs=hs: e.tensor_tensor(out=h3[b][:, hs], in0=hb_[b][:, hs], in1=sgm[b][:, hs], op=ALU.add), [t_hb[b], t_sgmh[b][nb]], [t_h3h[b][nb]], n=512)
            A(lambda e, b=b: e.activation(out=junk[0][:], in_=h3[b][:], func=AF.Square, accum_out=sq[b][:, 4:5]), t_h3h[b], [t_junk[0], t_sq[b]])
            V(lambda e, b=b: e.tensor_scalar(out=sq[b][:, 5:6], in0=sq[b][:, 4:5], scalar1=1.0 / D, scalar2=EPS, op0=ALU.mult, op1=ALU.add), [t_sq[b]], [t_sq[b]])
            G(lambda e, b=b: e.tensor_tensor(out=sq[b][:, 6:7], in0=sq[b][:, 5:6], in1=nhalf[:, 0:1], op=ALU.pow), [t_sq[b], t_nhalf], [t_sq[b]])
            V(lambda e, b=b: e.scalar_tensor_tensor(out=ot[b][:], in0=h3[b][:], scalar=sq[b][:, 6:7], in1=fnw[:], op0=ALU.mult, op1=ALU.mult),
              t_h3h[b] + [t_sq[b], t_fnw], [t_ot[b]])
            P.dma("sp", lambda e, j=j, b=b: e.dma_start(out=out_d[j * 128:(j + 1) * 128, :], in_=ot[b][:]), t_ot[b], reads=[t_ot[b]])
        P.barrier(alltoks)


_CACHE = {}


def kernel(**inputs):
    inp = {k: np.asarray(v) for k, v in inputs.items()}
    if "nc" not in _CACHE:
        _CACHE["nc"] = build()[0]
    nc = _CACHE["nc"]
    maps = make_in_maps(inp, ncores=8)
    res = run_bass_kernel_spmd(nc, maps, core_ids=list(range(8)))
    out = np.stack([np.asarray(r["out"], dtype=np.float32) for r in res.results], axis=0)
    return out.reshape(8, SEQ, D)
```

```python
import numpy as np
from contextlib import ExitStack
import concourse.bass as bass
import concourse.mybir as mybir
from concourse.bass_utils import run_bass_kernel_spmd

F32 = mybir.dt.float32
BF16 = mybir.dt.bfloat16
I32 = mybir.dt.int32
U32 = mybir.dt.uint32
AF = mybir.ActivationFunctionType
ALU = mybir.AluOpType
AX = mybir.AxisListType

ENGS = ("pe", "dve", "act", "pool", "sp")


class Tok:
    __slots__ = ("name", "w", "r", "sem", "cnt", "multi")

    def __init__(self, name):
        self.name = name
        self.multi = False
        self.w = None
        self.r = {}
        self.sem = None
        self.cnt = 0


class Op:
    __slots__ = ("eng", "fn", "deps", "signal", "sigval", "dma", "idx", "dur", "xfer", "start", "fin", "prev_dma", "delay")

    def __init__(self, eng, fn, deps):
        self.eng = eng
        self.fn = fn
        self.deps = deps
        self.signal = False
        self.sigval = None
        self.dma = None
        self.dur = 0.5
        self.xfer = 0.0
        self.start = 0.0
        self.fin = 0.0
        self.prev_dma = None
        self.delay = 0.0


class DmaDep:
    __slots__ = ("tok", "val", "op")

    def __init__(self, tok, val, op=None):
        self.tok = tok
        self.val = val
        self.op = op


import os as _os
_OPT = {"est": int(_os.environ.get("KEST", "1")), "slack": float(_os.environ.get("KSLACK", "1.2")), "sdel": float(_os.environ.get("KSDEL", "25")), "crit": int(_os.environ.get("KCRIT", "1")), "run": int(_os.environ.get("KRUN", "2500"))}


class _Probe:
    def __getattr__(self, name):
        def f(*a, **k):
            self.__dict__["call"] = (name, a, k)
            return self
        return f


def _estimate(eng, fn):
    pr = _Probe()
    try:
        fn(pr)
        name, a, k = pr.call
    except Exception:
        return None

    def free(ap):
        n = 1
        for d in list(ap.shape)[1:]:
            n *= int(d)
        return n
    try:
        if eng == "pe":
            if name == "matmul":
                rhs = k.get("rhs", a[2] if len(a) > 2 else None)
                cols = free(rhs)
                mult = 4.0 if rhs.dtype == F32 else 1.0
                return 0.05 + 1.15 * mult * max(cols, 64) / 2400.0
            if name == "transpose":
                src = k.get("in_", a[1] if len(a) > 1 else None)
                mult = 3.0 if src.dtype == F32 else 1.0
                return 0.05 + mult * 128 / 2400.0 * 1.2
            return 0.1
        out = k.get("out", a[0] if a else None)
        n = free(out)
        if eng == "dve":
            return 0.12 + n / 960.0
        if eng == "act":
            return 0.22 + n / 1150.0
        if eng == "pool":
            if name == "tensor_scalar":
                return 0.5 + n / 70.0
            return 0.3 + n / 560.0
    except Exception:
        return None
    return None


class Prog:
    def __init__(self, nc, stack):
        self.nc = nc
        self.stack = stack
        self.ops = {e: [] for e in ENGS}
        self.nsem = 0
        self.limit = None
        self.count = 0
        self.segs = [[]]
        self.last_dma = {}
        self.sched = True
        self.slack = _OPT["slack"]

    def tok(self, name):
        return Tok(name)

    def toks(self, name, n):
        return [Tok(f"{name}{i}") for i in range(n)]

    def _collect(self, eng, reads, writes, dma_semtok=None):
        deps = []
        for t in reads:
            if t.multi:
                deps.extend(t.w.values())
            elif t.w is not None:
                deps.append(t.w)
        for t in writes:
            if t.multi:
                deps.extend(t.r.values())
                continue
            if t.w is not None:
                w = t.w
                skip = False
                if isinstance(w, DmaDep) and dma_semtok is not None and w.tok is dma_semtok:
                    skip = True
                if not skip:
                    deps.append(w)
            deps.extend(t.r.values())
        return deps

    def _commit(self, dep, key, reads, writes):
        for t in reads:
            if isinstance(dep, Op):
                t.r[id(dep)] = dep
            else:
                t.r[key] = dep
        for t in writes:
            if t.multi:
                t.w[key] = dep
                continue
            t.w = dep
            t.r = {}

    def op(self, eng, fn, reads=(), writes=(), n=None):
        self.count += 1
        if self.limit is not None and self.count > self.limit:
            return None
        deps = self._collect(eng, reads, writes)
        o = Op(eng, fn, deps)
        if n is not None:
            if eng == "pe":
                o.dur = 0.03 + n / 2400.0
            elif eng == "dve":
                o.dur = 0.12 + n / 960.0
            elif eng == "act":
                o.dur = 0.22 + n / 1200.0
            elif eng == "pool":
                o.dur = 0.25 + n / 450.0
        else:
            est = _estimate(eng, fn) if _OPT["est"] else None
            o.dur = est if est is not None else {"pe": 0.12, "dve": 0.5, "act": 0.6, "pool": 1.15, "sp": 0.1}[eng]
        self.ops[eng].append(o)
        self.segs[-1].append(o)
        self._commit(o, eng, reads, writes)
        return o

    def dma(self, eng, fn, semtok, reads=(), writes=(), nbytes=None, delay=0.0):
        self.count += 1
        if self.limit is not None and self.count > self.limit:
            return None
        deps = self._collect(eng, reads, writes, dma_semtok=semtok)
        o = Op(eng, fn, deps)
        if semtok.sem is None:
            semtok.sem = self.stack.enter_context(self.nc.semaphore(f"d{self.nsem}_{semtok.name}"))
            self.nsem += 1
        semtok.cnt += 16
        o.dma = DmaDep(semtok, semtok.cnt, o)
        o.dur = 1.2 if eng == "pool" else 0.15
        o.xfer = 2.0 + (nbytes or 0) / 150e3
        o.delay = delay
        o.prev_dma = self.last_dma.get(id(semtok))
        self.last_dma[id(semtok)] = o
        self.ops[eng].append(o)
        self.segs[-1].append(o)
        self._commit(o.dma, ("dma", id(semtok)), reads, writes)
        return o

    def wait_all(self, eng, toks):
        deps = self._collect(eng, (), toks)
        o = Op(eng, None, deps)
        o.dur = 0.05
        self.ops[eng].append(o)
        self.segs[-1].append(o)
        return o

    def barrier(self, all_toks):
        for e in ENGS:
            self.wait_all(e, all_toks)
        self.segs.append([])

    def schedule(self, runahead=None):
        runahead = runahead or _OPT["run"]
        new_ops = {e: [] for e in ENGS}
        t_base = 0.0
        for seg in self.segs:
            if not seg:
                continue
            n = len(seg)
            pos = {id(o): i for i, o in enumerate(seg)}
            succ = [[] for _ in range(n)]
            ndep = [0] * n
            preds = [None] * n
            for i, o in enumerate(seg):
                ps_ = []
                for d in o.deps:
                    p = d if isinstance(d, Op) else d.op
                    k = pos.get(id(p))
                    if k is None:
                        continue
                    ps_.append((k, isinstance(d, Op)))
                if o.prev_dma is not None:
                    k = pos.get(id(o.prev_dma))
                    if k is not None:
                        ps_.append((k, None))
                preds[i] = ps_
                ks = set(k for k, _ in ps_)
                ndep[i] = len(ks)
                for k in ks:
                    succ[k].append(i)
            bott = [0.0] * n
            for i in range(n - 1, -1, -1):
                m = 0.0
                for k in succ[i]:
                    if bott[k] > m:
                        m = bott[k]
                bott[i] = seg[i].dur + seg[i].xfer + m
            ready = {e: [] for e in ENGS}
            free = {e: t_base for e in ENGS}
            scheduled = [False] * n
            low = 0

            def make_ready(i):
                o = seg[i]
                t = t_base
                for k, kind in preds[i]:
                    p = seg[k]
                    if kind is None:
                        ft = p.start
                    elif kind:
                        ft = p.fin
                    else:
                        ft = p.fin + p.xfer
                    if kind is not None and p.eng != o.eng:
                        ft += 0.06
                    if ft > t:
                        t = ft
                ready[o.eng].append((t + o.delay, i))

            for i in range(n):
                if ndep[i] == 0:
                    make_ready(i)
            nleft = n
            while nleft > 0:
                while low < n and scheduled[low]:
                    low += 1
                lim = low + runahead
                best = None
                cands = []
                tmin = None
                for e in ENGS:
                    fr = free[e]
                    for (rt, i) in ready[e]:
                        if i >= lim:
                            continue
                        st_ = rt if rt > fr else fr
                        cands.append((st_, i, e, rt))
                        if tmin is None or st_ < tmin:
                            tmin = st_
                if cands:
                    slack = self.slack
                    if _OPT["crit"]:
                        best = max((c for c in cands if c[0] <= tmin + slack), key=lambda c: (bott[c[1]], -c[1]))
                    else:
                        best = min((c for c in cands if c[0] <= tmin + slack), key=lambda c: c[1])
                if best is None:
                    for e in ENGS:
                        for (rt, i) in ready[e]:
                            st_ = max(rt, free[e])
                            if best is None or i < best[1]:
                                best = (st_, i, e, rt)
                assert best is not None, "scheduler stuck"
                st_, i, e, rt = best
                ready[e].remove((rt, i))
                o = seg[i]
                o.start = st_
                o.fin = st_ + o.dur
                free[e] = o.fin
                scheduled[i] = True
                new_ops[e].append(o)
                nleft -= 1
                for k in succ[i]:
                    ndep[k] -= 1
                    if ndep[k] == 0:
                        make_ready(k)
            t_base = max(max(free.values()), max((o.fin + o.xfer) for o in seg))
        self.ops = new_ops
        self.est_total = t_base

    def emit(self):
        nc = self.nc
        if self.sched:
            self.schedule()
        for e in ENGS:
            for o in self.ops[e]:
                for d in o.deps:
                    if isinstance(d, Op) and not (d.eng == "pe" and e == "pe"):
                        d.signal = True
        esem = {}
        for e in ENGS:
            n = 0
            for o in self.ops[e]:
                if o.signal:
                    n += 1
                    o.sigval = n
            esem[e] = self.stack.enter_context(nc.semaphore(f"eng_{e}"))
        self.esem = esem
        stats = {}
        with nc.Block() as block:
            def run(ename, engine):
                waited = {}
                nw = 0
                for o in self.ops[ename]:
                    for d in o.deps:
                        if isinstance(d, Op):
                            if d.eng == "pe" and ename == "pe":
                                continue
                            sem, val, key = esem[d.eng], d.sigval, d.eng
                        else:
                            sem, val, key = d.tok.sem, d.val, id(d.tok)
                        if waited.get(key, 0) >= val:
                            continue
                        waited[key] = val
                        engine.wait_ge(sem, val)
                        nw += 1
                    if o.fn is None:
                        continue
                    inst = o.fn(engine)
                    if o.dma is not None:
                        inst.then_inc(o.dma.tok.sem, 16)
                    elif o.signal:
                        inst.then_inc(esem[ename], 1)
                stats[ename] = (len(self.ops[ename]), nw)

            @block.tensor
            def _(e):
                run("pe", e)

            @block.vector
            def _(e):
                run("dve", e)

            @block.scalar
            def _(e):
                run("act", e)

            @block.gpsimd
            def _(e):
                run("pool", e)

            @block.sync
            def _(e):
                run("sp", e)
        self.stats = stats
        return stats


D = 1024
SEQ = 4096
NT = SEQ // 128
H = 8
DH = 64
NE = 32
TOPK = 4
CAP = 640
NSLOT = NE * CAP
PLE = 256
EPS = 1e-5
LIMIT = 7.0
ALPHA = 1.702
BIGPOS = 1.0e6
WINS = (2, 4, 8, 16)

_off = {}
_n = 0
for _name, _w in (("ident", 128), ("mask", 128), ("ustrict", 128), ("ones", 128), ("invf", 64),
                  ("xiq", 8), ("xik", 8), ("gc", 512), ("bands", 12 * 128), ("ecap", 32),
                  ("nmix", 8), ("nple", 8), ("pscale", 4), ("bgate", 256), ("bup", 256)):
    _off[_name] = (_n, _w)
    _n += _w
CST_N = _n
_roff = {}
_n = 0
for _name, _w in (("gnw", 512), ("nmoe", 1024), ("rb", 32), ("fnw", 1024)):
    _roff[_name] = (_n, _w)
    _n += _w
ROW_N = _n


def host_consts(inp):
    c = np.zeros((128, CST_N), np.float32)

    def put(name, arr):
        o, w = _off[name]
        c[:, o:o + w] = np.asarray(arr, np.float32).reshape(128, w)

    idx = np.arange(128)
    put("ident", np.eye(128))
    put("mask", (idx[None, :] >= idx[:, None]).astype(np.float32))
    put("ustrict", (idx[:, None] < idx[None, :]).astype(np.float32))
    put("ones", np.ones((128, 128)))
    half = DH // 2
    invf = (10000.0 ** (-(np.arange(half, dtype=np.float32)) / np.float32(half))).astype(np.float32)
    put("invf", np.tile(np.concatenate([invf, invf])[None, :], (128, 1)))
    lg = np.log1p(-np.power(2.0, -5.0 - np.arange(H, dtype=np.float64)))
    cpos = (idx + 1.0)[:, None]
    put("xiq", np.exp(cpos * lg[None, :]))
    put("xik", np.exp(-cpos * lg[None, :]) * (DH ** -0.5))
    gc = np.zeros((128, 512))
    for p in range(128):
        for h in range(H):
            if p // 64 == h % 2:
                gc[p, h * 64:(h + 1) * 64] = np.exp(128.0 * lg[h])
    put("gc", gc)
    bands = np.zeros((128, 12, 128))
    for g, w in enumerate(WINS):
        tp = idx[:, None]
        t = idx[None, :]
        inwin = (tp <= t) & (tp > t - w)
        bands[:, 3 * g + 0, :] = inwin / float(w) - (tp == t)
        cnt0 = np.minimum(t + 1.0, float(w))
        bands[:, 3 * g + 1, :] = inwin / cnt0 - (tp == t)
        bands[:, 3 * g + 2, :] = (tp >= 129 + t - w) / float(w)
    put("bands", bands)
    put("ecap", np.tile((np.arange(NE) * CAP)[None, :], (128, 1)))
    put("nmix", inp["norm_mix_w"][0].reshape(8, 128).T)
    put("nple", inp["norm_ple_w"][0].reshape(8, 128).T)
    put("pscale", inp["pool_scale"][0].reshape(4, 128).T)
    bgu = inp["expert_b_gate_up"][0]
    put("bgate", bgu[:, 0::2].reshape(NE, 8, 128).transpose(2, 0, 1))
    put("bup", bgu[:, 1::2].reshape(NE, 8, 128).transpose(2, 0, 1))
    r = np.zeros((1, ROW_N), np.float32)
    for name, v in (("gnw", inp["ret_gn_w"][0]), ("nmoe", inp["norm_moe_w"][0]),
                    ("rb", inp["router_b"][0]), ("fnw", inp["final_norm_w"])):
        o, w = _roff[name]
        r[0, o:o + w] = v
    return c, r


def _bc(ap, shape):
    return ap.broadcast_to(shape)


_BCREG = {}


def _bcreg(e, nc):
    if _BCREG.get("nc") is not nc:
        r = e.alloc_register("slot_bound")
        e.reg_mov(r, NSLOT - 1)
        _BCREG["nc"] = nc
        _BCREG["r"] = r
    return _BCREG["r"]


class Ctx:
    pass


def build(nt=NT, phases=4, debug=False, limit=None, n_exp=NE, sched=True):
    nc = bass.Bass("TRN2", target_bir_lowering=False)
    dt = lambda name, shape, dtype, kind: nc.dram_tensor(name, shape, dtype, kind=kind).ap()
    x_d = dt("x", [SEQ, D], F32, "ExternalInput")
    p_d = dt("p", [SEQ, PLE], F32, "ExternalInput")
    pos_d = dt("pos", [128, NT], I32, "ExternalInput")
    cst_d = dt("cst", [128, CST_N], F32, "ExternalInput")
    rows_d = dt("rows", [1, ROW_N], F32, "ExternalInput")
    win_d = dt("w_in", [D, 2560], F32, "ExternalInput")
    wout_d = dt("w_out", [D, D], F32, "ExternalInput")
    poolw_d = dt("pool_w", [4, 128, 128], F32, "ExternalInput")
    rw_d = dt("router_w", [D, NE], F32, "ExternalInput")
    wg_d = dt("w_g", [NE, D, D], F32, "ExternalInput")
    wu_d = dt("w_u", [NE, D, D], F32, "ExternalInput")
    wd_d = dt("w_d", [NE, D, D], F32, "ExternalInput")
    bd_d = dt("b_d", [NE, D], F32, "ExternalInput")
    pgw_d = dt("ple_gate_w", [D, D], F32, "ExternalInput")
    ppw_d = dt("ple_proj_w", [PLE, D], F32, "ExternalInput")
    out_d = dt("out", [SEQ, D], F32, "ExternalOutput")
    xs_d = nc.dram_tensor("xs_scr", [NSLOT, D], BF16).ap()
    ys_d = nc.dram_tensor("ys_scr", [NSLOT, D], F32).ap()
    h_d = nc.dram_tensor("h_scr", [SEQ, D], F32).ap()
    dbg = {}
    if debug:
        dbg["h1"] = dt("dbg_h1", [SEQ, D], F32, "ExternalOutput")
        dbg["gate"] = dt("dbg_gate", [128, NT * 4], F32, "ExternalOutput")
        dbg["pos"] = dt("dbg_pos", [128, NT * 4], I32, "ExternalOutput")
        dbg["h2"] = dt("dbg_h2", [SEQ, D], F32, "ExternalOutput")

    with ExitStack() as gst:
        P = Prog(nc, gst)
        P.limit = limit
        P.sched = sched
        alltoks = []

        def T(name):
            t = P.tok(name)
            alltoks.append(t)
            return t

        def sbuf(st, name, shape, dtype):
            return st.enter_context(nc.sbuf_tensor("s_" + name, shape, dtype))

        V = lambda fn, r=(), w=(), n=None: P.op("dve", fn, r, w, n)
        A = lambda fn, r=(), w=(), n=None: P.op("act", fn, r, w, n)
        G = lambda fn, r=(), w=(), n=None: P.op("pool", fn, r, w, n)
        M = lambda fn, r=(), w=(), n=None: P.op("pe", fn, r, w, n)

        cst = sbuf(gst, "cst", [128, CST_N], F32)
        t_cst = T("cst")
        identb = sbuf(gst, "identb", [128, 128], BF16)
        t_identb = T("identb")
        gates = sbuf(gst, "gates", [128, NT, 4], F32)
        posi = sbuf(gst, "posi", [128, NT, 4], I32)
        t_gates = [T(f"gates{j}") for j in range(NT)]
        t_posi = [T(f"posi{j}") for j in range(NT)]
        nhalf = sbuf(gst, "nhalf", [128, 8], F32)
        t_nhalf = T("nhalf")
        wgu_buf, wd_buf, t_wgu, t_wd = [], [], [], []
        psb = [gst.enter_context(nc.psum_tensor(f"ps{i}", [128, 512], F32)) for i in range(8)]
        t_ps = [T(f"ps{i}") for i in range(8)]

        def C(name):
            o, w = _off[name]
            return cst[:, o:o + w]

        t_hd = [T(f"hd{j}") for j in range(NT)]
        t_dbgs = [T(f"dbgs{i}") for i in range(16)] if debug else []
        t_xsd = T("xsd")
        t_xsd.multi = True
        t_xsd.w = {}
        t_zfd = T("zfd")
        t_ysd = T("ysd")
        t_ysd.multi = True
        t_ysd.w = {}

        P.dma("sp", lambda e: e.dma_start(out=cst[:], in_=cst_d), t_cst, writes=[t_cst])
        V(lambda e: e.tensor_copy(identb[:], C("ident")), [t_cst], [t_identb])
        G(lambda e: e.memset(nhalf[:], -0.5), (), [t_nhalf])
        if debug:
            G(lambda e: e.memset(gates[:], 0.0), (), t_gates)
            G(lambda e: e.memset(posi[:], 0), (), t_posi)
        ident = C("ident")

        def load_expert(e_idx, slot):
            wg_v = wg_d[e_idx].rearrange("(c p) n -> p c n", p=128)
            wu_v = wu_d[e_idx].rearrange("(c p) n -> p c n", p=128)
            wd_v = wd_d[e_idx].rearrange("(c p) n -> p c n", p=128)
            for hc in range(2):
                cs = slice(hc * 4, hc * 4 + 4)
                P.dma("pool", lambda e, cs=cs: e.dma_start(out=wgu_buf[slot][:, cs, 0:1024], in_=wg_v[:, cs, :]),
                      t_wgu[slot], writes=[t_wgu[slot]])
                P.dma("pool", lambda e, cs=cs: e.dma_start(out=wgu_buf[slot][:, cs, 1024:2048], in_=wu_v[:, cs, :]),
                      t_wgu[slot], writes=[t_wgu[slot]])
                P.dma("pool", lambda e, cs=cs: e.dma_start(out=wd_buf[slot][:, cs, :], in_=wd_v[:, cs, :]),
                      t_wd[slot], writes=[t_wd[slot]])

        with ExitStack() as st:
            RN1 = _roff["fnw"][0]
            rows = sbuf(st, "rows", [128, RN1], F32)
            t_rows = T("rows")
            P.dma("sp", lambda e: e.dma_start(out=rows[:], in_=rows_d[0, 0:RN1].partition_broadcast(128)), t_rows, writes=[t_rows])

            def R(name):
                o, w = _roff[name]
                return rows[:, o:o + w]

            w_in = sbuf(st, "w_in", [128, 8, 2560], BF16)
            t_win = T("w_in")
            w_out = sbuf(st, "w_out", [128, 8, 1024], BF16)
            t_wout = T("w_out")
            poolw = sbuf(st, "poolw", [128, 4, 128], BF16)
            t_poolw = T("poolw")
            rw = sbuf(st, "rw", [128, 8, NE], F32)
            t_rw = T("rw")
            posi_t = sbuf(st, "pos_i", [128, NT], I32)
            posf = sbuf(st, "pos_f", [128, NT], F32)
            CC = sbuf(st, "CC", [128, NT, 64], F32)
            SS = sbuf(st, "SS", [128, NT, 64], F32)
            st_setup = ExitStack()
            ang = sbuf(st_setup, "ang", [128, NT, 64], F32)
            tmpa = sbuf(st_setup, "tmpa", [128, NT, 64], F32)
            stage = [sbuf(st_setup, f"stage{i}", [128, 1280], F32) for i in range(2)]
            t_stage = [T(f"stage{i}") for i in range(2)]
            win_v = win_d.rearrange("(c p) n -> p c n", p=128)
            for c2 in range(16):
                s = c2 % 2
                c, hf = c2 // 2, c2 % 2
                P.dma("sp", lambda e, c=c, s=s, hf=hf: e.dma_start(out=stage[s][:], in_=win_v[:, c, hf * 1280:(hf + 1) * 1280]), t_stage[s], writes=[t_stage[s]])
                o, _ = _off["nmix"]
                if c2 % 2 == 0:
                    V(lambda e, c=c, s=s, o=o, hf=hf: e.tensor_scalar(out=w_in[:, c, hf * 1280:(hf + 1) * 1280], in0=stage[s][:], scalar1=cst[:, o + c:o + c + 1],
                                                                    scalar2=None, op0=ALU.mult), [t_stage[s], t_cst], [t_win], n=1280)
                else:
                    A(lambda e, c=c, s=s, o=o, hf=hf: e.activation(out=w_in[:, c, hf * 1280:(hf + 1) * 1280], in_=stage[s][:], func=AF.Copy, scale=cst[:, o + c:o + c + 1]),
                      [t_stage[s], t_cst], [t_win], n=1280)
            P.dma("pool", lambda e: e.dma_start(out=w_out[:], in_=wout_d.rearrange("(c p) n -> p c n", p=128)), t_wout, writes=[t_wout])
            P.dma("pool", lambda e: e.dma_start(out=poolw[:], in_=poolw_d.rearrange("g c d -> c g d")), t_poolw, writes=[t_poolw])
            P.dma("sp", lambda e: e.dma_start(out=rw[:], in_=rw_d.rearrange("(c p) n -> p c n", p=128)), t_rw, writes=[t_rw])

            t_pos, t_ang, t_tmpa, t_CC, t_SS = T("pos"), T("ang"), T("tmpa"), T("CC"), T("SS")
            P.dma("sp", lambda e: e.dma_start(out=posi_t[:], in_=pos_d), t_pos, writes=[t_pos])
            V(lambda e: e.tensor_copy(posf[:], posi_t[:]), [t_pos], [t_pos])
            V(lambda e: e.tensor_tensor(out=ang[:], in0=_bc(posf[:].unsqueeze(2), [128, NT, 64]),
                                        in1=_bc(C("invf").unsqueeze(1), [128, NT, 64]), op=ALU.mult), [t_pos, t_cst], [t_ang])
            TWO_PI = float(2.0 * np.pi)

            def sin_table(dst, t_dst, shift):
                V(lambda e: e.tensor_scalar(out=tmpa[:], in0=ang[:], scalar1=shift, scalar2=None, op0=ALU.add), [t_ang], [t_tmpa])
                V(lambda e: e.tensor_scalar(out=dst[:].bitcast(I32), in0=tmpa[:], scalar1=1.0 / TWO_PI, scalar2=None, op0=ALU.mult), [t_tmpa], [t_dst])
                V(lambda e: e.tensor_copy(dst[:], dst[:].bitcast(I32)), [t_dst], [t_dst])
                V(lambda e: e.scalar_tensor_tensor(out=tmpa[:], in0=dst[:], scalar=-TWO_PI, in1=tmpa[:], op0=ALU.mult, op1=ALU.add),
                  [t_dst, t_tmpa], [t_tmpa])
                V(lambda e: e.tensor_scalar(out=dst[:], in0=tmpa[:], scalar1=float(np.pi), scalar2=-TWO_PI, op0=ALU.is_gt, op1=ALU.mult),
                  [t_tmpa], [t_dst])
                V(lambda e: e.tensor_tensor(out=tmpa[:], in0=tmpa[:], in1=dst[:], op=ALU.add), [t_tmpa, t_dst], [t_tmpa])
                V(lambda e: e.tensor_scalar(out=dst[:], in0=tmpa[:], scalar1=-float(np.pi), scalar2=TWO_PI, op0=ALU.is_lt, op1=ALU.mult),
                  [t_tmpa], [t_dst])
                V(lambda e: e.tensor_tensor(out=tmpa[:], in0=tmpa[:], in1=dst[:], op=ALU.add), [t_tmpa, t_dst], [t_tmpa])
                V(lambda e: e.tensor_scalar(out=tmpa[:], in0=tmpa[:], scalar1=-3.1415925, scalar2=3.1415925, op0=ALU.max, op1=ALU.min),
                  [t_tmpa], [t_tmpa])
                A(lambda e: e.activation(out=dst[:], in_=tmpa[:], func=AF.Sin), [t_tmpa], [t_dst])

            sin_table(CC, t_CC, float(np.pi / 2))
            sin_table(SS, t_SS, 0.0)
            V(lambda e: e.tensor_scalar(out=SS[:, :, 0:32], in0=SS[:, :, 0:32], scalar1=-1.0, scalar2=None, op0=ALU.mult), [t_SS], [t_SS])

            P.barrier(alltoks)
            st_setup.close()
            state = sbuf(st, "state", [128, 512], F32)
            stateb = sbuf(st, "stateb", [128, 512], BF16)
            t_state, t_stateb = T("state"), T("stateb")
            V(lambda e: e.memset(state[:], 0.0), (), [t_state])
            V(lambda e: e.memset(stateb[:], 0.0), (), [t_stateb])
            macc = sbuf(st, "macc", [128, NE], F32)
            t_macc = T("macc")
            V(lambda e: e.memset(macc[:], 0.0), (), [t_macc])
            us = [sbuf(st, f"us{i}", [128, 512], F32) for i in range(2)]
            t_us = [T(f"us{i}") for i in range(2)]

            NB = 2
            def mk(name, shape, dtype, n=NB):
                return [sbuf(st, f"{name}{i}", shape, dtype) for i in range(n)], [T(f"{name}{i}") for i in range(n)]
            xt, t_xt = mk("xt", [128, D], F32)
            junk, t_junk = mk("junk", [128, D], BF16, 1)
            ssq, t_ssq = mk("ssq", [128, 8], F32)
            xT, t_xT = mk("xT", [128, 8, 128], BF16)
            qs, t_qs = mk("qs", [128, 512], F32)
            ks, t_ks = mk("ks", [128, 512], F32)
            vb, t_vb = mk("vb", [128, 512], BF16)
            sg, t_sg = mk("sg", [128, 512], F32)
            qa, t_qa = mk("qa", [128, 512], F32)
            qb, t_qb = mk("qb", [128, 512], F32)
            ka, t_ka, kb, t_kb = qa, t_qa, qb, t_qb
            qt, t_qt = mk("qt", [128, 512], BF16)
            kt, t_kt = mk("kt", [128, 512], BF16)
            qkT, t_qkT = mk("qkT", [128, 1536], BF16)
            for i_ in range(NB):
                V(lambda e, i_=i_: e.memset(qkT[i_][:], 0.0), (), [t_qkT[i_]])
            PT, t_PT = mk("PT", [128, 1024], BF16)
            ysb, t_ysb = mk("ysb", [128, 512], F32)
            ysq, t_ysq = mk("ysq", [128, 512], F32, 1)
            ysq, t_ysq = ysq * 2, t_ysq * 2
            gst8, t_gst8 = mk("gst8", [128, 6, 8], F32)
            gsg, t_gsg = mk("gsg", [128, 512], F32)
            ret, t_ret = mk("ret", [128, 512], BF16)
            mixT, t_mixT = mk("mixT", [128, 8, 128], BF16)
            pooledT, t_pooledT = mk("pooledT", [128, 4, 128], BF16)
            ht, t_ht = mk("ht", [128, D], F32)
            xn2, t_xn2 = mk("xn2", [128, D], F32, 1)
            xn2, t_xn2 = xn2 * 2, t_xn2 * 2
            NBX = 5
            xn2b, t_xn2b = mk("xn2b", [128, D], BF16, NBX)
            pend = []
            zt = sbuf(st, "zt", [128, 2048], BF16)
            t_zt, t_zf = T("zt"), T("zf")
            G(lambda e: e.memset(zt[:], 0.0), (), [t_zt])
            assert NSLOT % 256 == 0
            for kz in range(NSLOT // 256):
                P.dma("act" if kz % 2 else "sp", lambda e, kz=kz: e.dma_start(out=xs_d[kz * 256:(kz + 1) * 256, :].rearrange("(p r) d -> p (r d)", p=128), in_=zt[:]),
                      t_zf, reads=[t_zt], writes=[t_zfd], nbytes=512 * 1024)
            xn2T, t_xn2T = mk("xn2T", [128, 8, 128], F32, 1)
            xn2T, t_xn2T = xn2T * 2, t_xn2T * 2
            rt, t_rt = mk("rt", [128, 8, 32], F32)
            top8, t_top8 = mk("top8", [128, 16], F32)
            posk, t_posk = mk("posk", [128, 4], F32)

            bank_ctr = [0]

            def nxt():
                v = bank_ctr[0] % 8
                bank_ctr[0] += 1
                return v

            for j in range(nt):
                b = j % NB
                P.dma("sp", lambda e, j=j, b=b: e.dma_start(out=xt[b][:], in_=x_d[j * 128:(j + 1) * 128, :]), t_xt[b], writes=[t_xt[b]])
                A(lambda e, b=b: e.activation(out=junk[0][:], in_=xt[b][:], func=AF.Square, accum_out=ssq[b][:, 0:1]),
                  [t_xt[b]], [t_junk[0], t_ssq[b]])
                V(lambda e, b=b: e.tensor_scalar(out=ssq[b][:, 1:2], in0=ssq[b][:, 0:1], scalar1=1.0 / D, scalar2=EPS, op0=ALU.mult, op1=ALU.add),
                  [t_ssq[b]], [t_ssq[b]])
                G(lambda e, b=b: e.tensor_tensor(out=ssq[b][:, 2:3], in0=ssq[b][:, 1:2], in1=nhalf[:, 0:1], op=ALU.pow),
                  [t_ssq[b], t_nhalf], [t_ssq[b]])
                rstd = ssq[b][:, 2:3]
                bx0, bx1 = nxt(), nxt()
                for c in range(8):
                    bank = (bx0, bx1)[c // 4]
                    M(lambda e, b=b, c=c, bank=bank: e.transpose(psb[bank][:, (c % 4) * 128:(c % 4 + 1) * 128], xt[b][:, c * 128:(c + 1) * 128], ident),
                      [t_xt[b], t_cst], [t_ps[bank]])
                V(lambda e, b=b, bx0=bx0: e.tensor_copy(xT[b][:, 0:4, :], psb[bx0][:].rearrange("p (c t) -> p c t", c=4)), [t_ps[bx0]], [t_xT[b]])
                A(lambda e, b=b, bx1=bx1: e.copy(xT[b][:, 4:8, :], psb[bx1][:].rearrange("p (c t) -> p c t", c=4)), [t_ps[bx1]], [t_xT[b]])
                bp = [nxt() for _ in range(5)]
                for nb in range(5):
                    for c in range(8):
                        M(lambda e, b=b, nb=nb, c=c, bk=bp[nb]: e.matmul(psb[bk][:], lhsT=xT[b][:, c, :], rhs=w_in[:, c, nb * 512:(nb + 1) * 512],
                                                             start=(c == 0), stop=(c == 7)),
                          [t_xT[b], t_win], [t_ps[bp[nb]]], n=530)
                ub = j % 2
                A(lambda e, bp=bp, b=b: e.activation(out=qs[b][:], in_=psb[bp[0]][:], func=AF.Copy, scale=ssq[b][:, 2:3]), [t_ps[bp[0]], t_ssq[b]], [t_qs[b]])
                A(lambda e, bp=bp, b=b: e.activation(out=ks[b][:], in_=psb[bp[1]][:], func=AF.Copy, scale=ssq[b][:, 2:3]), [t_ps[bp[1]], t_ssq[b]], [t_ks[b]])
                A(lambda e, bp=bp, b=b: e.activation(out=vb[b][:], in_=psb[bp[2]][:], func=AF.Copy, scale=ssq[b][:, 2:3]), [t_ps[bp[2]], t_ssq[b]], [t_vb[b]])
                A(lambda e, bp=bp, b=b: e.activation(out=sg[b][:], in_=psb[bp[3]][:], func=AF.Silu, scale=ssq[b][:, 2:3]), [t_ps[bp[3]], t_ssq[b]], [t_sg[b]])
                A(lambda e, bp=bp, b=b, ub=ub: e.activation(out=us[ub][:], in_=psb[bp[4]][:], func=AF.Copy, scale=ssq[b][:, 2:3]), [t_ps[bp[4]], t_ssq[b]], [t_us[ub]])
                ccj = _bc(CC[:, j, :].unsqueeze(1), [128, 8, 64])
                ssj_lo = _bc(SS[:, j, 0:32].unsqueeze(1), [128, 8, 32])
                ssj_hi = _bc(SS[:, j, 32:64].unsqueeze(1), [128, 8, 32])
                for (src, t_src, aa, t_aa, bb, t_bb, dst, t_dst, xin) in (
                        (qs, t_qs, qa, t_qa, qb, t_qb, qt, t_qt, "xiq"), (ks, t_ks, ka, t_ka, kb, t_kb, kt, t_kt, "xik")):
                    s4 = src[b][:].rearrange("p (h two d) -> p h two d", two=2, d=32)
                    b4 = bb[b][:].rearrange("p (h two d) -> p h two d", two=2, d=32)
                    V(lambda e, b=b, src=src, aa=aa, ccj=ccj: e.tensor_tensor(out=aa[b][:].rearrange("p (h d) -> p h d", d=64),
                                                                              in0=src[b][:].rearrange("p (h d) -> p h d", d=64), in1=ccj, op=ALU.mult),
                      [t_src[b], t_CC], [t_aa[b]])
                    V(lambda e, s4=s4, b4=b4, ssj_lo=ssj_lo: e.tensor_tensor(out=b4[:, :, 0, :], in0=s4[:, :, 1, :], in1=ssj_lo, op=ALU.mult),
                      [t_src[b], t_SS], [t_bb[b]])
                    V(lambda e, s4=s4, b4=b4, ssj_hi=ssj_hi: e.tensor_tensor(out=b4[:, :, 1, :], in0=s4[:, :, 0, :], in1=ssj_hi, op=ALU.mult),
                      [t_src[b], t_SS], [t_bb[b]])
                    V(lambda e, b=b, aa=aa, bb=bb: e.tensor_tensor(out=aa[b][:], in0=aa[b][:], in1=bb[b][:], op=ALU.add), [t_aa[b], t_bb[b]], [t_aa[b]])
                    V(lambda e, b=b, aa=aa, dst=dst, xin=xin: e.tensor_tensor(out=dst[b][:].rearrange("p (h d) -> p h d", d=64),
                                                                           in0=aa[b][:].rearrange("p (h d) -> p h d", d=64),
                                                                           in1=_bc(C(xin).unsqueeze(2), [128, 8, 64]), op=ALU.mult),
                      [t_aa[b], t_cst], [t_dst[b]])
                bqk = nxt()
                pb2 = psb[bqk][:].bitcast(BF16)
                for i in range(4):
                    M(lambda e, b=b, i=i, pb2=pb2: e.transpose(pb2[:, i * 128:(i + 1) * 128], qt[b][:, i * 128:(i + 1) * 128], identb[:]),
                      [t_qt[b], t_identb], [t_ps[bqk]])
                for i in range(4):
                    M(lambda e, b=b, i=i, pb2=pb2: e.transpose(pb2[:, 512 + i * 128:512 + (i + 1) * 128], kt[b][:, i * 128:(i + 1) * 128], identb[:]),
                      [t_kt[b], t_identb], [t_ps[bqk]])
                A(lambda e, b=b, pb2=pb2: e.copy(qkT[b][:, 0:512], pb2[:, 0:512]), [t_ps[bqk]], [t_qkT[b]])
                A(lambda e, b=b, pb2=pb2: e.copy(qkT[b][0:64, 512:1024], pb2[0:64, 512:1024]), [t_ps[bqk]], [t_qkT[b]])
                A(lambda e, b=b, pb2=pb2: e.copy(qkT[b][64:128, 1024:1536], pb2[64:128, 512:1024]), [t_ps[bqk]], [t_qkT[b]])
                bs = [nxt(), nxt()]
                for h in range(H):
                    hb = (h % 2) * 64
                    hh = h // 2
                    bank = bs[h // 4]
                    M(lambda e, b=b, h=h, hb=hb, hh=hh, bank=bank: e.matmul(
                        psb[bank][:, (h % 4) * 128:(h % 4 + 1) * 128],
                        lhsT=qkT[b][:, 512 + (h % 2) * 512 + hh * 128:512 + (h % 2) * 512 + (hh + 1) * 128],
                        rhs=qkT[b][:, hh * 128:(hh + 1) * 128], start=True, stop=True),
                      [t_qkT[b]], [t_ps[bank]])
                mask4 = _bc(C("mask").unsqueeze(1), [128, 4, 128])
                for half in range(2):
                    V(lambda e, b=b, half=half, bs=bs: e.tensor_tensor(out=PT[b][:, half * 512:(half + 1) * 512].rearrange("p (h c) -> p h c", c=128),
                                                                in0=psb[bs[half]][:].rearrange("p (h c) -> p h c", c=128), in1=mask4, op=ALU.mult),
                      [t_ps[bs[half]], t_cst], [t_PT[b]])
                by, bkv = nxt(), nxt()
                for h in range(H):
                    hb = (h % 2) * 64
                    hh = h // 2
                    M(lambda e, b=b, h=h, by=by: e.matmul(psb[by][:, h * 64:(h + 1) * 64], lhsT=PT[b][:, h * 128:(h + 1) * 128],
                                                   rhs=vb[b][:, h * 64:(h + 1) * 64], start=True, stop=False),
                      [t_PT[b], t_vb[b]], [t_ps[by]])
                    M(lambda e, b=b, h=h, hb=hb, hh=hh, by=by: e.matmul(psb[by][:, h * 64:(h + 1) * 64], lhsT=qkT[b][:, hh * 128:(hh + 1) * 128],
                                                                 rhs=stateb[:, h * 64:(h + 1) * 64], start=False, stop=True),
                      [t_qkT[b], t_stateb], [t_ps[by]])
                for hh in range(4):
                    M(lambda e, b=b, hh=hh, bkv=bkv: e.matmul(psb[bkv][:, hh * 128:(hh + 1) * 128], lhsT=kt[b][:, hh * 128:(hh + 1) * 128],
                                                     rhs=vb[b][:, hh * 128:(hh + 1) * 128], start=True, stop=True),
                      [t_kt[b], t_vb[b]], [t_ps[bkv]])
                V(lambda e, bkv=bkv: e.tensor_tensor(out=state[:], in0=state[:], in1=psb[bkv][:], op=ALU.add), [t_state, t_ps[bkv]], [t_state])
                V(lambda e: e.tensor_tensor(out=state[:], in0=state[:], in1=C("gc"), op=ALU.mult), [t_state, t_cst], [t_state])
                V(lambda e: e.tensor_copy(stateb[:], state[:]), [t_state], [t_stateb])
                A(lambda e, b=b, by=by: e.copy(ysb[b][:], psb[by][:]), [t_ps[by]], [t_ysb[b]])
                A(lambda e, b=b, by=by: e.activation(out=ysq[b][:], in_=psb[by][:], func=AF.Square), [t_ps[by]], [t_ysq[b]])
                G(lambda e, b=b: e.tensor_tensor(out=gsg[b][:], in0=sg[b][:], in1=R("gnw"), op=ALU.mult), [t_sg[b], t_rows], [t_gsg[b]])
                g8 = gst8[b]
                V(lambda e, b=b, g8=g8: e.tensor_reduce(out=g8[:, 0, :], in_=ysb[b][:].rearrange("p (h d) -> p h d", d=64), axis=AX.X, op=ALU.add),
                  [t_ysb[b]], [t_gst8[b]])
                V(lambda e, b=b, g8=g8: e.tensor_reduce(out=g8[:, 1, :], in_=ysq[b][:].rearrange("p (h d) -> p h d", d=64), axis=AX.X, op=ALU.add),
                  [t_ysq[b]], [t_gst8[b]])
                V(lambda e, g8=g8: e.tensor_scalar(out=g8[:, 2, :], in0=g8[:, 0, :], scalar1=1.0 / DH, scalar2=None, op0=ALU.mult), [t_gst8[b]], [t_gst8[b]])
                V(lambda e, g8=g8: e.tensor_tensor(out=g8[:, 3, :], in0=g8[:, 2, :], in1=g8[:, 2, :], op=ALU.mult), [t_gst8[b]], [t_gst8[b]])
                V(lambda e, g8=g8: e.scalar_tensor_tensor(out=g8[:, 4, :], in0=g8[:, 1, :], scalar=1.0 / DH, in1=g8[:, 3, :], op0=ALU.mult, op1=ALU.subtract),
                  [t_gst8[b]], [t_gst8[b]])
                V(lambda e, g8=g8: e.tensor_scalar(out=g8[:, 4, :], in0=g8[:, 4, :], scalar1=EPS, scalar2=None, op0=ALU.add), [t_gst8[b]], [t_gst8[b]])
                G(lambda e, g8=g8: e.tensor_tensor(out=g8[:, 5, :], in0=g8[:, 4, :], in1=nhalf[:, 0:8], op=ALU.pow), [t_gst8[b], t_nhalf], [t_gst8[b]])
                y3 = ysb[b][:].rearrange("p (h d) -> p h d", d=64)
                V(lambda e, y3=y3, g8=g8: e.tensor_tensor(out=y3, in0=y3, in1=_bc(g8[:, 2, :].unsqueeze(2), [128, 8, 64]), op=ALU.subtract),
                  [t_ysb[b], t_gst8[b]], [t_ysb[b]])
                V(lambda e, y3=y3, g8=g8: e.tensor_tensor(out=y3, in0=y3, in1=_bc(g8[:, 5, :].unsqueeze(2), [128, 8, 64]), op=ALU.mult),
                  [t_ysb[b], t_gst8[b]], [t_ysb[b]])
                V(lambda e, b=b: e.tensor_tensor(out=ret[b][:], in0=ysb[b][:], in1=gsg[b][:], op=ALU.mult), [t_ysb[b], t_gsg[b]], [t_ret[b]])
                brt = nxt()
                pb1 = psb[brt][:].bitcast(BF16)
                for i in range(4):
                    M(lambda e, b=b, i=i, pb1=pb1: e.transpose(pb1[:, i * 128:(i + 1) * 128], ret[b][:, i * 128:(i + 1) * 128], identb[:]),
                      [t_ret[b], t_identb], [t_ps[brt]])
                A(lambda e, b=b, pb1=pb1: e.copy(mixT[b][:, 0:4, :], pb1[:, 0:512].rearrange("p (c t) -> p c t", c=4)), [t_ps[brt]], [t_mixT[b]])
                bo, _ = _off["bands"]
                bpl, bmx = nxt(), nxt()
                for g in range(4):
                    kind = 1 if j == 0 else 0
                    M(lambda e, g=g, ub=ub, kind=kind, bo=bo, j=j, bpl=bpl: e.matmul(psb[bpl][:, g * 128:(g + 1) * 128], lhsT=us[ub][:, g * 128:(g + 1) * 128],
                                                                       rhs=cst[:, bo + (3 * g + kind) * 128:bo + (3 * g + kind + 1) * 128],
                                                                       start=True, stop=(j == 0)),
                      [t_us[ub], t_cst], [t_ps[bpl]])
                    if j > 0:
                        M(lambda e, g=g, ub=ub, bo=bo, bpl=bpl: e.matmul(psb[bpl][:, g * 128:(g + 1) * 128], lhsT=us[1 - ub][:, g * 128:(g + 1) * 128],
                                                                rhs=cst[:, bo + (3 * g + 2) * 128:bo + (3 * g + 3) * 128], start=False, stop=True),
                          [t_us[1 - ub], t_cst], [t_ps[bpl]])
                V(lambda e, b=b, bpl=bpl: e.tensor_copy(pooledT[b][:], psb[bpl][:].rearrange("p (g t) -> p g t", g=4)), [t_ps[bpl]], [t_pooledT[b]])
                for g in range(4):
                    M(lambda e, b=b, g=g, bmx=bmx: e.matmul(psb[bmx][:, g * 128:(g + 1) * 128], lhsT=poolw[:, g, :], rhs=pooledT[b][:, g, :], start=True, stop=True),
                      [t_poolw, t_pooledT[b]], [t_ps[bmx]])
                po, _ = _off["pscale"]
                V(lambda e, b=b, po=po, bmx=bmx: e.tensor_tensor(out=mixT[b][:, 4:8, :], in0=psb[bmx][:].rearrange("p (g t) -> p g t", g=4),
                                                        in1=_bc(cst[:, po:po + 4].unsqueeze(2), [128, 4, 128]), op=ALU.mult),
                  [t_ps[bmx], t_cst], [t_mixT[b]])
                bh = [nxt(), nxt()]
                for nb in range(2):
                    for c in range(8):
                        M(lambda e, b=b, nb=nb, c=c, bh=bh: e.matmul(psb[bh[nb]][:], lhsT=mixT[b][:, c, :], rhs=w_out[:, c, nb * 512:(nb + 1) * 512],
                                                             start=(c == 0), stop=(c == 7)),
                          [t_mixT[b], t_wout], [t_ps[bh[nb]]], n=530)
                for nb in range(2):
                    V(lambda e, b=b, nb=nb, bh=bh: e.tensor_tensor(out=ht[b][:, nb * 512:(nb + 1) * 512], in0=psb[bh[nb]][:], in1=xt[b][:, nb * 512:(nb + 1) * 512], op=ALU.add),
                      [t_ps[bh[nb]], t_xt[b]], [t_ht[b]])
                P.dma("sp", lambda e, j=j, b=b: e.dma_start(out=h_d[j * 128:(j + 1) * 128, :], in_=ht[b][:]), t_ht[b], reads=[t_ht[b]], writes=[t_hd[j]])
                if debug:
                    P.dma("sp", lambda e, j=j, b=b: e.dma_start(out=dbg["h1"][j * 128:(j + 1) * 128, :], in_=ht[b][:]), t_dbgs[j % 8], reads=[t_ht[b]])
                if phases < 2:
                    continue

                A(lambda e, b=b: e.activation(out=junk[0][:], in_=ht[b][:], func=AF.Square, accum_out=ssq[b][:, 4:5]),
                  [t_ht[b]], [t_junk[0], t_ssq[b]])
                V(lambda e, b=b: e.tensor_scalar(out=ssq[b][:, 5:6], in0=ssq[b][:, 4:5], scalar1=1.0 / D, scalar2=EPS, op0=ALU.mult, op1=ALU.add),
                  [t_ssq[b]], [t_ssq[b]])
                G(lambda e, b=b: e.tensor_tensor(out=ssq[b][:, 6:7], in0=ssq[b][:, 5:6], in1=nhalf[:, 0:1], op=ALU.pow),
                  [t_ssq[b], t_nhalf], [t_ssq[b]])
                V(lambda e, b=b: e.scalar_tensor_tensor(out=xn2[b][:], in0=ht[b][:], scalar=ssq[b][:, 6:7], in1=R("nmoe"), op0=ALU.mult, op1=ALU.mult),
                  [t_ht[b], t_ssq[b], t_rows], [t_xn2[b]])
                bx = j % NBX
                A(lambda e, b=b, bx=bx: e.copy(xn2b[bx][:], xn2[b][:]), [t_xn2[b]], [t_xn2b[bx]])
                bn = [nxt(), nxt()]
                blg = nxt()
                for c in range(8):
                    bank = bn[c // 4]
                    M(lambda e, b=b, c=c, bank=bank: e.transpose(psb[bank][:, (c % 4) * 128:(c % 4 + 1) * 128], xn2[b][:, c * 128:(c + 1) * 128], ident),
                      [t_xn2[b], t_cst], [t_ps[bank]])
                V(lambda e, b=b, bn=bn: e.tensor_copy(xn2T[b][:, 0:4, :], psb[bn[0]][:].rearrange("p (c t) -> p c t", c=4)), [t_ps[bn[0]]], [t_xn2T[b]])
                A(lambda e, b=b, bn=bn: e.copy(xn2T[b][:, 4:8, :], psb[bn[1]][:].rearrange("p (c t) -> p c t", c=4)), [t_ps[bn[1]]], [t_xn2T[b]])
                for c in range(8):
                    M(lambda e, b=b, c=c, blg=blg: e.matmul(psb[blg][:, 0:NE], lhsT=xn2T[b][:, c, :], rhs=rw[:, c, :], start=(c == 0), stop=(c == 7)),
                      [t_xn2T[b], t_rw], [t_ps[blg]])
                r_ = rt[b]
                t8 = top8[b]
                V(lambda e, r_=r_, blg=blg: e.tensor_tensor(out=r_[:, 0, :], in0=psb[blg][:, 0:NE], in1=R("rb"), op=ALU.add), [t_ps[blg], t_rows], [t_rt[b]])
                V(lambda e, r_=r_, t8=t8: e.max(out=t8[:, 0:8], in_=r_[:, 0, :]), [t_rt[b]], [t_top8[b]])
                V(lambda e, t8=t8: e.tensor_scalar(out=t8[:, 8:9], in0=t8[:, 0:1], scalar1=-1.0, scalar2=None, op0=ALU.mult), [t_top8[b]], [t_top8[b]])
                A(lambda e, t8=t8: e.activation(out=t8[:, 10:14], in_=t8[:, 0:4], func=AF.Exp, bias=t8[:, 8:9], accum_out=t8[:, 9:10]),
                  [t_top8[b]], [t_top8[b]])
                V(lambda e, t8=t8: e.reciprocal(t8[:, 14:15], t8[:, 9:10]), [t_top8[b]], [t_top8[b]])
                V(lambda e, r_=r_, t8=t8: e.tensor_scalar(out=r_[:, 1, :], in0=r_[:, 0, :], scalar1=t8[:, 3:4], scalar2=None, op0=ALU.is_ge),
                  [t_rt[b], t_top8[b]], [t_rt[b]])
                M(lambda e, r_=r_, blg=blg: e.matmul(psb[blg][:, 32:64], lhsT=C("ustrict"), rhs=r_[:, 1, :], start=True, stop=False), [t_rt[b], t_cst], [t_ps[blg]])
                M(lambda e, blg=blg: e.matmul(psb[blg][:, 32:64], lhsT=C("ones"), rhs=macc[:], start=False, stop=True), [t_macc, t_cst], [t_ps[blg]])
                V(lambda e, r_=r_: e.tensor_tensor(out=macc[:], in0=macc[:], in1=r_[:, 1, :], op=ALU.add), [t_macc, t_rt[b]], [t_macc])
                V(lambda e, r_=r_, blg=blg: e.tensor_copy(r_[:, 2, :], psb[blg][:, 32:64]), [t_ps[blg]], [t_rt[b]])
                V(lambda e, r_=r_: e.tensor_scalar(out=r_[:, 3, :], in0=r_[:, 2, :], scalar1=float(CAP) - 0.5, scalar2=BIGPOS, op0=ALU.is_ge, op1=ALU.mult),
                  [t_rt[b]], [t_rt[b]])
                V(lambda e, r_=r_: e.tensor_tensor(out=r_[:, 4, :], in0=r_[:, 2, :], in1=C("ecap"), op=ALU.add), [t_rt[b], t_cst], [t_rt[b]])
                V(lambda e, r_=r_: e.tensor_tensor(out=r_[:, 4, :], in0=r_[:, 4, :], in1=r_[:, 3, :], op=ALU.add), [t_rt[b]], [t_rt[b]])
                for k in range(TOPK):
                    V(lambda e, r_=r_, t8=t8, b=b, k=k: e.scalar_tensor_tensor(out=r_[:, 6, :], in0=r_[:, 0, :], scalar=t8[:, k:k + 1], in1=r_[:, 4, :],
                                                                             op0=ALU.is_equal, op1=ALU.mult, accum_out=posk[b][:, k:k + 1]),
                      [t_rt[b], t_top8[b]], [t_rt[b], t_posk[b]])
                V(lambda e, r_=r_, b=b: e.tensor_scalar(out=r_[:, 7, 0:4], in0=posk[b][:, 0:4], scalar1=BIGPOS * 0.5, scalar2=None, op0=ALU.is_lt),
                  [t_posk[b]], [t_rt[b]])
                V(lambda e, r_=r_, t8=t8, j=j: e.scalar_tensor_tensor(out=gates[:, j, :], in0=t8[:, 10:14], scalar=t8[:, 14:15], in1=r_[:, 7, 0:4],
                                                                      op0=ALU.mult, op1=ALU.mult),
                  [t_top8[b], t_rt[b]], [t_gates[j]])
                V(lambda e, b=b, j=j: e.tensor_copy(posi[:, j, :], posk[b][:, 0:4]), [t_posk[b]], [t_posi[j]])
                def scatter(j=j, bx=bx):
                    for k in range(TOPK):
                        P.dma("pool", lambda e, bx=bx, j=j, k=k: e.indirect_dma_start(
                            out=xs_d, out_offset=bass.IndirectOffsetOnAxis(ap=posi[:, j, k:k + 1], axis=0), in_=xn2b[bx][:], in_offset=None,
                            bounds_check=_bcreg(e, nc), oob_is_err=False), t_xn2b[bx], reads=[t_xn2b[bx], t_posi[j], t_zfd], writes=[t_xsd],
                            nbytes=256 * 1024, delay=_OPT["sdel"])
                pend.append(scatter)
                if len(pend) >= NBX - 1:
                    pend.pop(0)()
            for f_ in pend:
                f_()

            P.barrier(alltoks)

        if phases >= 3:
            build_phase3(locals())
        if phases >= 4:
            build_phase4(locals())

        if debug and phases >= 2:
            P.dma("sp", lambda e: e.dma_start(out=dbg["gate"], in_=gates[:].rearrange("p j k -> p (j k)")), T("dbg_g"), reads=t_gates)
            P.dma("sp", lambda e: e.dma_start(out=dbg["pos"], in_=posi[:].rearrange("p j k -> p (j k)")), T("dbg_p"), reads=t_posi)
        P.barrier(alltoks)
        stats = P.emit()
    return nc, stats


def make_in_maps(inp, ncores=8):
    cst, rows = host_consts(inp)
    wgu = inp["expert_w_gate_up"][0]
    w_g = np.ascontiguousarray(wgu[:, :, 0::2])
    w_u = np.ascontiguousarray(wgu[:, :, 1::2])
    shared = {
        "cst": cst, "rows": rows,
        "w_in": np.ascontiguousarray(inp["w_in"][0]), "w_out": np.ascontiguousarray(inp["w_out"][0]),
        "pool_w": np.ascontiguousarray(inp["pool_w"][0]), "router_w": np.ascontiguousarray(inp["router_w"][0]),
        "w_g": w_g, "w_u": w_u, "w_d": np.ascontiguousarray(inp["expert_w_down"][0]),
        "b_d": np.ascontiguousarray(inp["expert_b_down"][0]),
        "ple_gate_w": np.ascontiguousarray(inp["ple_gate_w"][0]), "ple_proj_w": np.ascontiguousarray(inp["ple_proj_w"][0]),
    }
    maps = []
    for b in range(ncores):
        m = dict(shared)
        m["x"] = np.ascontiguousarray(inp["x"][b])
        m["p"] = np.ascontiguousarray(inp["p"][0, b])
        m["pos"] = np.ascontiguousarray(inp["positions"][b].reshape(NT, 128).T.astype(np.int32))
        maps.append(m)
    return maps


def build_phase3(L):
    nc, P, T, sbuf, cst = L["nc"], L["P"], L["T"], L["sbuf"], L["cst"]
    V, A, G, M = L["V"], L["A"], L["G"], L["M"]
    psb, t_ps, t_cst, identb, t_identb = L["psb"], L["t_ps"], L["t_cst"], L["identb"], L["t_identb"]
    xs_d, ys_d, wg_d, wu_d, wd_d, bd_d = L["xs_d"], L["ys_d"], L["wg_d"], L["wu_d"], L["wd_d"], L["bd_d"]
    t_xsd, t_ysd, alltoks = L["t_xsd"], L["t_ysd"], L["alltoks"]
    n_exp = L.get("n_exp", NE)
    NSL = (CAP + 127) // 128
    HALF = CAP // 2

    def rows(i):
        return 128

    def tstart(i):
        return min(i * 128, CAP - 128)
    with ExitStack() as st:
        wgu = [sbuf(st, f"wgu{i}", [128, 8, 2048], BF16) for i in range(2)]
        wdn = [sbuf(st, f"wdn{i}", [128, 8, 1024], BF16) for i in range(2)]
        t_wgu = [T(f"wgu{i}") for i in range(2)]
        t_wdn = [T(f"wdn{i}") for i in range(2)]
        xtok = sbuf(st, "xtok", [128, NSL, D], BF16)
        t_xtok = T("xtok")
        XT = [sbuf(st, f"XT{i}", [128, 8, CAP], BF16) for i in range(2)]
        t_XT = [T(f"XT{i}") for i in range(2)]
        actT = [sbuf(st, f"actT{i}", [128, 8, CAP], BF16) for i in range(2)]
        t_actT = [T(f"actT{i}") for i in range(2)]
        NTMP = 2
        tmp = [sbuf(st, f"etmp{i}", [128, 4, HALF], F32) for i in range(NTMP)]
        t_tmp = [[T(f"etmp{i}_{k}") for k in range(4)] for i in range(NTMP)]
        NY = 4
        ysb = [sbuf(st, f"ysb3_{i}", [128, D], F32) for i in range(NY)]
        t_ysb = [T(f"ysb3_{i}") for i in range(NY)]
        bdb = [sbuf(st, f"bdb{i}", [128, D], F32) for i in range(2)]
        t_bdb = [T(f"bdb{i}") for i in range(2)]
        abg = sbuf(st, "abg", [128, 256], F32)
        t_abg = T("abg")
        ob, _ = _off["bgate"]
        ou, _ = _off["bup"]
        V(lambda e: e.tensor_scalar(out=abg[:], in0=cst[:, ob:ob + 256], scalar1=ALPHA, scalar2=None, op0=ALU.mult), [t_cst], [t_abg])
        bu1 = sbuf(st, "bu1", [128, 256], F32)
        V(lambda e: e.tensor_scalar(out=bu1[:], in0=cst[:, ou:ou + 256], scalar1=1.0, scalar2=None, op0=ALU.add), [t_cst], [t_abg])
        SIG7 = float(1.0 / (1.0 + np.exp(-ALPHA * LIMIT)))

        def load_wgu(e_idx):
            sl = e_idx % 2
            wg_v = wg_d[e_idx].rearrange("(c p) n -> p c n", p=128)
            wu_v = wu_d[e_idx].rearrange("(c p) n -> p c n", p=128)
            for hc in range(2):
                cs = slice(hc * 4, hc * 4 + 4)
                P.dma("pool", lambda e, cs=cs, sl=sl, wg_v=wg_v: e.dma_start(out=wgu[sl][:, cs, 0:1024], in_=wg_v[:, cs, :]), t_wgu[sl], writes=[t_wgu[sl]])
                P.dma("pool", lambda e, cs=cs, sl=sl, wu_v=wu_v: e.dma_start(out=wgu[sl][:, cs, 1024:2048], in_=wu_v[:, cs, :]), t_wgu[sl], writes=[t_wgu[sl]])

        def load_wd(e_idx):
            sl = e_idx % 2
            wd_v = wd_d[e_idx].rearrange("(c p) n -> p c n", p=128)
            for hc in range(2):
                cs = slice(hc * 4, hc * 4 + 4)
                P.dma("pool", lambda e, cs=cs, sl=sl, wd_v=wd_v: e.dma_start(out=wdn[sl][:, cs, :], in_=wd_v[:, cs, :]), t_wdn[sl], writes=[t_wdn[sl]])
            P.dma("sp", lambda e, sl=sl, e_idx=e_idx: e.dma_start(out=bdb[sl][:], in_=bd_d[e_idx].partition_broadcast(128)), t_bdb[sl], writes=[t_bdb[sl]])

        evac_rr = [0]

        def stage_T(e_idx):
            sl = e_idx % 2
            nfull = CAP // 128
            P.dma("sp", lambda e, e_idx=e_idx, nfull=nfull: e.dma_start(out=xtok[:, 0:nfull, :],
                                                                        in_=xs_d[e_idx * CAP:e_idx * CAP + nfull * 128, :].rearrange("(i p) d -> p i d", p=128)),
                  t_xtok, reads=[t_xsd], writes=[t_xtok])
            if CAP % 128:
                P.dma("sp", lambda e, e_idx=e_idx, nfull=nfull: e.dma_start(out=xtok[:, nfull, :], in_=xs_d[(e_idx + 1) * CAP - 128:(e_idx + 1) * CAP, :]),
                      t_xtok, reads=[t_xsd], writes=[t_xtok], nbytes=256 * 1024)
            for i in range(NSL):
                bank = 4
                pbv = psb[bank][:].bitcast(BF16)
                ri = rows(i)
                for c in range(8):
                    M(lambda e, i=i, c=c, pbv=pbv, ri=ri: e.transpose(pbv[:, c * 128:c * 128 + ri], xtok[0:ri, i, c * 128:(c + 1) * 128], identb[0:ri, 0:ri]),
                      [t_xtok, t_identb], [t_ps[bank]])
                eng = A if evac_rr[0] % 2 == 0 else V
                evac_rr[0] += 1
                if eng is A:
                    A(lambda e, i=i, sl=sl, pbv=pbv, ri=ri: e.copy(XT[sl][:, :, tstart(i):tstart(i) + ri], pbv.rearrange("p (c s) -> p c s", c=8)[:, :, 0:ri]),
                      [t_ps[bank]], [t_XT[sl]])
                else:
                    V(lambda e, i=i, sl=sl, pbv=pbv, ri=ri: e.tensor_copy(XT[sl][:, :, tstart(i):tstart(i) + ri], pbv.rearrange("p (c s) -> p c s", c=8)[:, :, 0:ri]),
                      [t_ps[bank]], [t_XT[sl]])

        cnt = [0]

        def stage_GU(e_idx):
            sl = e_idx % 2
            for jc in range(8):
                for hf in range(2):
                    pp = cnt[0] % 2
                    tb = cnt[0] % NTMP
                    cnt[0] += 1
                    bg, bu = psb[2 * pp], psb[2 * pp + 1]
                    ssl = slice(hf * HALF, (hf + 1) * HALF)
                    for c in range(8):
                        M(lambda e, sl=sl, jc=jc, c=c, bg=bg, ssl=ssl: e.matmul(bg[:, 0:HALF], lhsT=wgu[sl][:, c, jc * 128:(jc + 1) * 128], rhs=XT[sl][:, c, ssl],
                                                                             start=(c == 0), stop=(c == 7)),
                          [t_wgu[sl], t_XT[sl]], [t_ps[2 * pp]], n=HALF)
                    for c in range(8):
                        M(lambda e, sl=sl, jc=jc, c=c, bu=bu, ssl=ssl: e.matmul(bu[:, 0:HALF], lhsT=wgu[sl][:, c, 1024 + jc * 128:1024 + (jc + 1) * 128], rhs=XT[sl][:, c, ssl],
                                                                             start=(c == 0), stop=(c == 7)),
                          [t_wgu[sl], t_XT[sl]], [t_ps[2 * pp + 1]], n=HALF)
                    col = e_idx * 8 + jc
                    tm, tt = tmp[tb], t_tmp[tb]
                    A(lambda e, tm=tm, bg=bg, col=col: e.activation(out=tm[:, 0, :], in_=bg[:, 0:HALF], func=AF.Sigmoid, bias=abg[:, col:col + 1], scale=ALPHA),
                      [t_ps[2 * pp], t_abg], [tt[0]])
                    A(lambda e, tm=tm, bg=bg, col=col: e.activation(out=tm[:, 1, :], in_=bg[:, 0:HALF], func=AF.Identity, bias=cst[:, ob + col:ob + col + 1], scale=1.0),
                      [t_ps[2 * pp], t_cst], [tt[1]])
                    A(lambda e, tm=tm, bu=bu, col=col: e.activation(out=tm[:, 2, :], in_=bu[:, 0:HALF], func=AF.Identity, bias=bu1[:, col:col + 1], scale=1.0),
                      [t_ps[2 * pp + 1], t_abg], [tt[2]])
                    V(lambda e, tm=tm: e.tensor_scalar(out=tm[:, 2, :], in0=tm[:, 2, :], scalar1=LIMIT + 1.0, scalar2=-LIMIT + 1.0, op0=ALU.min, op1=ALU.max), [tt[2]], [tt[2]])
                    V(lambda e, tm=tm: e.scalar_tensor_tensor(out=tm[:, 3, :], in0=tm[:, 1, :], scalar=LIMIT, in1=tm[:, 2, :], op0=ALU.min, op1=ALU.mult),
                      [tt[1], tt[2]], [tt[3]])
                    V(lambda e, tm=tm, sl=sl, jc=jc, ssl=ssl: e.scalar_tensor_tensor(out=actT[sl][:, jc, ssl], in0=tm[:, 0, :], scalar=SIG7, in1=tm[:, 3, :],
                                                                                  op0=ALU.min, op1=ALU.mult), [tt[0], tt[3]], [t_actT[sl]])

        ycnt = [0]
        ybank = [0]

        def stage_down(e_idx):
            sl = e_idx % 2
            for i in range(NSL):
                yb = ycnt[0] % NY
                ycnt[0] += 1
                ri = rows(i)
                for nb in range(2):
                    bk = 5 + ybank[0] % 3
                    ybank[0] += 1
                    for jc in range(8):
                        M(lambda e, sl=sl, i=i, nb=nb, jc=jc, bk=bk, ri=ri: e.matmul(psb[bk][0:ri, :], lhsT=actT[sl][:, jc, tstart(i):tstart(i) + ri], rhs=wdn[sl][:, jc, nb * 512:(nb + 1) * 512],
                                                                              start=(jc == 0), stop=(jc == 7)),
                          [t_actT[sl], t_wdn[sl]], [t_ps[bk]], n=512)
                    V(lambda e, yb=yb, nb=nb, sl=sl, bk=bk, ri=ri: e.tensor_tensor(out=ysb[yb][0:ri, nb * 512:(nb + 1) * 512], in0=psb[bk][0:ri, :], in1=bdb[sl][0:ri, nb * 512:(nb + 1) * 512], op=ALU.add),
                      [t_ps[bk], t_bdb[sl]], [t_ysb[yb]], n=512)
                ov = i * 128 - tstart(i)
                r0 = e_idx * CAP + i * 128
                P.dma("sp", lambda e, yb=yb, r0=r0, ov=ov: e.dma_start(out=ys_d[r0:r0 + 128 - ov, :], in_=ysb[yb][ov:128, :]), t_ysb[yb], reads=[t_ysb[yb]], writes=[t_ysd],
                      nbytes=(128 - ov) * 4096)

        load_wgu(0)
        load_wd(0)
        load_wgu(1)
        stage_T(0)
        if n_exp > 1:
            stage_T(1)
        stage_GU(0)
        for s_ in range(n_exp):
            if s_ + 2 < n_exp:
                load_wgu(s_ + 2)
            if s_ + 1 < n_exp:
                load_wd(s_ + 1)
            if s_ + 2 < n_exp:
                stage_T(s_ + 2)
            if s_ + 1 < n_exp:
                stage_GU(s_ + 1)
            stage_down(s_)
        P.barrier(alltoks)


def build_phase4(L):
    nc, P, T, sbuf, cst = L["nc"], L["P"], L["T"], L["sbuf"], L["cst"]
    V, A, G, M = L["V"], L["A"], L["G"], L["M"]
    psb, t_ps, t_cst, identb, t_identb = L["psb"], L["t_ps"], L["t_cst"], L["identb"], L["t_identb"]
    ys_d, h_d, p_d, out_d, pgw_d, ppw_d, rows_d = L["ys_d"], L["h_d"], L["p_d"], L["out_d"], L["pgw_d"], L["ppw_d"], L["rows_d"]
    t_ysd, t_hd, alltoks = L["t_ysd"], L["t_hd"], L["alltoks"]
    gates, posi, t_gates, t_posi, nhalf, t_nhalf = L["gates"], L["posi"], L["t_gates"], L["t_posi"], L["nhalf"], L["t_nhalf"]
    nt, dbg, debug = L["nt"], L["dbg"], L["debug"]
    ident = cst[:, _off["ident"][0]:_off["ident"][0] + 128]
    with ExitStack() as st:
        pgw = sbuf(st, "pgw", [128, 8, D], BF16)
        t_pgw = T("pgw")
        ppw = sbuf(st, "ppw", [128, 2, D], BF16)
        t_ppw = T("ppw")
        fnw = sbuf(st, "fnw", [128, D], F32)
        t_fnw = T("fnw")
        stg = [sbuf(st, f"stg4_{i}", [128, D], F32) for i in range(2)]
        t_stg = [T(f"stg4_{i}") for i in range(2)]
        pgw_v = pgw_d.rearrange("(c p) n -> p c n", p=128)
        on, _ = _off["nple"]
        for c in range(8):
            s_ = c % 2
            P.dma("sp", lambda e, c=c, s_=s_: e.dma_start(out=stg[s_][:], in_=pgw_v[:, c, :]), t_stg[s_], writes=[t_stg[s_]])
            if c % 2 == 0:
                V(lambda e, c=c, s_=s_: e.tensor_scalar(out=pgw[:, c, :], in0=stg[s_][:], scalar1=cst[:, on + c:on + c + 1], scalar2=None, op0=ALU.mult),
                  [t_stg[s_], t_cst], [t_pgw], n=1024)
            else:
                A(lambda e, c=c, s_=s_: e.activation(out=pgw[:, c, :], in_=stg[s_][:], func=AF.Copy, scale=cst[:, on + c:on + c + 1]),
                  [t_stg[s_], t_cst], [t_pgw], n=1024)
        P.dma("pool", lambda e: e.dma_start(out=ppw[:], in_=ppw_d.rearrange("(c p) n -> p c n", p=128)), t_ppw, writes=[t_ppw])
        fo, fw = _roff["fnw"]
        P.dma("sp", lambda e: e.dma_start(out=fnw[:], in_=rows_d[0, fo:fo + fw].partition_broadcast(128)), t_fnw, writes=[t_fnw])

        NB = 4
        bctr = [0]

        def nxt():
            v = bctr[0] % 8
            bctr[0] += 1
            return v

        def mk(name, shape, dtype, n=NB):
            return [sbuf(st, f"{name}{i}", shape, dtype) for i in range(n)], [T(f"{name}{i}") for i in range(n)]
        hb_, t_hb = mk("h4", [128, D], F32)
        pt_, t_pt = mk("p4", [128, PLE], F32)
        yk, t_yk = mk("yk", [128, 4, D], F32)
        junk, t_junk = mk("junk4", [128, D], BF16, 1)
        sq, t_sq = mk("sq4", [128, 8], F32)
        xn3, t_xn3 = mk("xn3", [128, D], BF16)
        xn3T, t_xn3T = mk("xn3T", [128, 8, 128], BF16)
        pT, t_pT = mk("pT", [128, 2, 128], BF16)
        sgm, t_sgm = mk("sgm", [128, D], F32)
        ot, t_ot = mk("ot", [128, D], F32)
        h3, _unused = mk("h3_", [128, D], F32)
        t_sgmh = [[T(f"sgmh{i}_{k}") for k in range(2)] for i in range(NB)]
        t_h3h = [[T(f"h3h{i}_{k}") for k in range(2)] for i in range(NB)]
        for i_ in range(NB):
            G(lambda e, i_=i_: e.memset(yk[i_][:], 0.0), (), [t_yk[i_]])

        for j in range(nt):
            b = j % NB
            P.dma("sp", lambda e, j=j, b=b: e.dma_start(out=hb_[b][:], in_=h_d[j * 128:(j + 1) * 128, :]), t_hb[b], reads=[t_hd[j]], writes=[t_hb[b]])
            P.dma("sp", lambda e, j=j, b=b: e.dma_start(out=pt_[b][:], in_=p_d[j * 128:(j + 1) * 128, :]), t_pt[b], writes=[t_pt[b]])
            for k in range(TOPK):
                P.dma("pool", lambda e, j=j, b=b, k=k: e.indirect_dma_start(
                    out=yk[b][:, k, :], out_offset=None, in_=ys_d, in_offset=bass.IndirectOffsetOnAxis(ap=posi[:, j, k:k + 1], axis=0),
                    bounds_check=_bcreg(e, nc), oob_is_err=False), t_yk[b], reads=[t_ysd, t_posi[j]], writes=[t_yk[b]])
            for k in range(TOPK):
                V(lambda e, j=j, b=b, k=k: e.scalar_tensor_tensor(out=hb_[b][:], in0=yk[b][:, k, :], scalar=gates[:, j, k:k + 1], in1=hb_[b][:],
                                                                 op0=ALU.mult, op1=ALU.add), [t_yk[b], t_gates[j], t_hb[b]], [t_hb[b]])
            if debug:
                P.dma("sp", lambda e, j=j, b=b: e.dma_start(out=dbg["h2"][j * 128:(j + 1) * 128, :], in_=hb_[b][:]), L["t_dbgs"][8 + j % 8], reads=[t_hb[b]])
            A(lambda e, b=b: e.activation(out=junk[0][:], in_=hb_[b][:], func=AF.Square, accum_out=sq[b][:, 0:1]), [t_hb[b]], [t_junk[0], t_sq[b]])
            V(lambda e, b=b: e.tensor_scalar(out=sq[b][:, 1:2], in0=sq[b][:, 0:1], scalar1=1.0 / D, scalar2=EPS, op0=ALU.mult, op1=ALU.add), [t_sq[b]], [t_sq[b]])
            G(lambda e, b=b: e.tensor_tensor(out=sq[b][:, 2:3], in0=sq[b][:, 1:2], in1=nhalf[:, 0:1], op=ALU.pow), [t_sq[b], t_nhalf], [t_sq[b]])
            A(lambda e, b=b: e.activation(out=xn3[b][:], in_=hb_[b][:], func=AF.Copy, scale=sq[b][:, 2:3]), [t_hb[b], t_sq[b]], [t_xn3[b]])
            b0_, b1_ = nxt(), nxt()
            bg_ = [nxt(), nxt()]
            bp_ = [nxt(), nxt()]
            pb0 = psb[b0_][:].bitcast(BF16)
            for c in range(8):
                M(lambda e, b=b, c=c, pb0=pb0: e.transpose(pb0[:, c * 128:(c + 1) * 128], xn3[b][:, c * 128:(c + 1) * 128], identb[:]),
                  [t_xn3[b], t_identb], [t_ps[b0_]])
            V(lambda e, b=b, pb0=pb0: e.tensor_copy(xn3T[b][:], pb0.rearrange("p (c t) -> p c t", c=8)), [t_ps[b0_]], [t_xn3T[b]], n=1024)
            for c in range(2):
                M(lambda e, b=b, c=c, b1_=b1_: e.transpose(psb[b1_][:, c * 128:(c + 1) * 128], pt_[b][:, c * 128:(c + 1) * 128], ident), [t_pt[b], t_cst], [t_ps[b1_]], n=400)
            A(lambda e, b=b, b1_=b1_: e.copy(pT[b][:], psb[b1_][:, 0:256].rearrange("p (c t) -> p c t", c=2)), [t_ps[b1_]], [t_pT[b]], n=256)
            for nb in range(2):
                for c in range(8):
                    M(lambda e, b=b, nb=nb, c=c, bg_=bg_: e.matmul(psb[bg_[nb]][:], lhsT=xn3T[b][:, c, :], rhs=pgw[:, c, nb * 512:(nb + 1) * 512], start=(c == 0), stop=(c == 7)),
                      [t_xn3T[b], t_pgw], [t_ps[bg_[nb]]], n=600)
            for nb in range(2):
                for c in range(2):
                    M(lambda e, b=b, nb=nb, c=c, bp_=bp_: e.matmul(psb[bp_[nb]][:], lhsT=pT[b][:, c, :], rhs=ppw[:, c, nb * 512:(nb + 1) * 512], start=(c == 0), stop=(c == 1)),
                      [t_pT[b], t_ppw], [t_ps[bp_[nb]]], n=600)
            for nb in range(2):
                hs = slice(nb * 512, (nb + 1) * 512)
                A(lambda e, b=b, nb=nb, bg_=bg_, hs=hs: e.activation(out=sgm[b][:, hs], in_=psb[bg_[nb]][:], func=AF.Sigmoid), [t_ps[bg_[nb]]], [t_sgmh[b][nb]], n=512)
                V(lambda e, b=b, nb=nb, bp_=bp_, hs=hs: e.tensor_tensor(out=sgm[b][:, hs], in0=sgm[b][:, hs], in1=psb[bp_[nb]][:], op=ALU.mult),
                  [t_sgmh[b][nb], t_ps[bp_[nb]]], [t_sgmh[b][nb]], n=512)
                V(lambda e, b=b, hs=hs: e.tensor_tensor(out=h3[b][:, hs], in0=hb_[b][:, hs], in1=sgm[b][:, hs], op=ALU.add), [t_hb[b], t_sgmh[b][nb]], [t_h3h[b][nb]], n=512)
            A(lambda e, b=b: e.activation(out=junk[0][:], in_=h3[b][:], func=AF.Square, accum_out=sq[b][:, 4:5]), t_h3h[b], [t_junk[0], t_sq[b]])
            V(lambda e, b=b: e.tensor_scalar(out=sq[b][:, 5:6], in0=sq[b][:, 4:5], scalar1=1.0 / D, scalar2=EPS, op0=ALU.mult, op1=ALU.add), [t_sq[b]], [t_sq[b]])
            G(lambda e, b=b: e.tensor_tensor(out=sq[b][:, 6:7], in0=sq[b][:, 5:6], in1=nhalf[:, 0:1], op=ALU.pow), [t_sq[b], t_nhalf], [t_sq[b]])
            V(lambda e, b=b: e.scalar_tensor_tensor(out=ot[b][:], in0=h3[b][:], scalar=sq[b][:, 6:7], in1=fnw[:], op0=ALU.mult, op1=ALU.mult),
              t_h3h[b] + [t_sq[b], t_fnw], [t_ot[b]])
            P.dma("sp", lambda e, j=j, b=b: e.dma_start(out=out_d[j * 128:(j + 1) * 128, :], in_=ot[b][:]), t_ot[b], reads=[t_ot[b]])
        P.barrier(alltoks)


_CACHE = {}


def kernel(**inputs):
    inp = {k: np.asarray(v) for k, v in inputs.items()}
    if "nc" not in _CACHE:
        _CACHE["nc"] = build()[0]
    nc = _CACHE["nc"]
    maps = make_in_maps(inp, ncores=8)
    res = run_bass_kernel_spmd(nc, maps, core_ids=list(range(8)))
    out = np.stack([np.asarray(r["out"], dtype=np.float32) for r in res.results], axis=0)
    return out.reshape(8, SEQ, D)
```

```python
import numpy as np
from contextlib import ExitStack
import concourse.bass as bass
import concourse.mybir as mybir
from concourse.bass_utils import run_bass_kernel_spmd

F32 = mybir.dt.float32
BF16 = mybir.dt.bfloat16
I32 = mybir.dt.int32
U32 = mybir.dt.uint32
AF = mybir.ActivationFunctionType
ALU = mybir.AluOpType
AX = mybir.AxisListType

ENGS = ("pe", "dve", "act", "pool", "sp")


class Tok:
    __slots__ = ("name", "w", "r", "sem", "cnt", "multi")

    def __init__(self, name):
        self.name = name
        self.multi = False
        self.w = None
        self.r = {}
        self.sem = None
        self.cnt = 0


class Op:
    __slots__ = ("eng", "fn", "deps", "signal", "sigval", "dma", "idx", "dur", "xfer", "start", "fin", "prev_dma", "delay")

    def __init__(self, eng, fn, deps):
        self.eng = eng
        self.fn = fn
        self.deps = deps
        self.signal = False
        self.sigval = None
        self.dma = None
        self.dur = 0.5
        self.xfer = 0.0
        self.start = 0.0
        self.fin = 0.0
        self.prev_dma = None
        self.delay = 0.0


class DmaDep:
    __slots__ = ("tok", "val", "op")

    def __init__(self, tok, val, op=None):
        self.tok = tok
        self.val = val
        self.op = op


import os as _os
_OPT = {"est": int(_os.environ.get("KEST", "1")), "slack": float(_os.environ.get("KSLACK", "0.6")), "sdel": float(_os.environ.get("KSDEL", "25")), "crit": int(_os.environ.get("KCRIT", "1")), "gx": float(_os.environ.get("KGX", "3.5")), "run": int(_os.environ.get("KRUN", "2500"))}


class _Probe:
    def __getattr__(self, name):
        def f(*a, **k):
            self.__dict__["call"] = (name, a, k)
            return self
        return f


def _estimate(eng, fn):
    pr = _Probe()
    try:
        fn(pr)
        name, a, k = pr.call
    except Exception:
        return None

    def free(ap):
        n = 1
        for d in list(ap.shape)[1:]:
            n *= int(d)
        return n
    try:
        if eng == "pe":
            if name == "matmul":
                rhs = k.get("rhs", a[2] if len(a) > 2 else None)
                cols = free(rhs)
                mult = 4.0 if rhs.dtype == F32 else 1.0
                return 0.05 + 1.15 * mult * max(cols, 64) / 2400.0
            if name == "transpose":
                src = k.get("in_", a[1] if len(a) > 1 else None)
                mult = 3.0 if src.dtype == F32 else 1.0
                return 0.05 + mult * 128 / 2400.0 * 1.2
            return 0.1
        out = k.get("out", a[0] if a else None)
        n = free(out)
        if eng == "dve":
            return 0.12 + n / 960.0
        if eng == "act":
            return 0.22 + n / 1150.0
        if eng == "pool":
            if name == "tensor_scalar":
                return 0.5 + n / 70.0
            return 0.3 + n / 560.0
    except Exception:
        return None
    return None


class Prog:
    def __init__(self, nc, stack):
        self.nc = nc
        self.stack = stack
        self.ops = {e: [] for e in ENGS}
        self.nsem = 0
        self.limit = None
        self.count = 0
        self.segs = [[]]
        self.last_dma = {}
        self.sched = True
        self.slack = _OPT["slack"]

    def tok(self, name):
        return Tok(name)

    def toks(self, name, n):
        return [Tok(f"{name}{i}") for i in range(n)]

    def _collect(self, eng, reads, writes, dma_semtok=None):
        deps = []
        for t in reads:
            if t.multi:
                deps.extend(t.w.values())
            elif t.w is not None:
                deps.append(t.w)
        for t in writes:
            if t.multi:
                deps.extend(t.r.values())
                continue
            if t.w is not None:
                w = t.w
                skip = False
                if isinstance(w, DmaDep) and dma_semtok is not None and w.tok is dma_semtok:
                    skip = True
                if not skip:
                    deps.append(w)
            deps.extend(t.r.values())
        return deps

    def _commit(self, dep, key, reads, writes):
        for t in reads:
            if isinstance(dep, Op):
                t.r[id(dep)] = dep
            else:
                t.r[key] = dep
        for t in writes:
            if t.multi:
                t.w[key] = dep
                continue
            t.w = dep
            t.r = {}

    def op(self, eng, fn, reads=(), writes=(), n=None):
        self.count += 1
        if self.limit is not None and self.count > self.limit:
            return None
        deps = self._collect(eng, reads, writes)
        o = Op(eng, fn, deps)
        if n is not None:
            if eng == "pe":
                o.dur = 0.03 + n / 2400.0
            elif eng == "dve":
                o.dur = 0.12 + n / 960.0
            elif eng == "act":
                o.dur = 0.22 + n / 1200.0
            elif eng == "pool":
                o.dur = 0.25 + n / 450.0
        else:
            est = _estimate(eng, fn) if _OPT["est"] else None
            o.dur = est if est is not None else {"pe": 0.12, "dve": 0.5, "act": 0.6, "pool": 1.15, "sp": 0.1}[eng]
        self.ops[eng].append(o)
        self.segs[-1].append(o)
        self._commit(o, eng, reads, writes)
        return o

    def dma(self, eng, fn, semtok, reads=(), writes=(), nbytes=None, delay=0.0):
        self.count += 1
        if self.limit is not None and self.count > self.limit:
            return None
        deps = self._collect(eng, reads, writes, dma_semtok=semtok)
        o = Op(eng, fn, deps)
        if semtok.sem is None:
            semtok.sem = self.stack.enter_context(self.nc.semaphore(f"d{self.nsem}_{semtok.name}"))
            self.nsem += 1
        semtok.cnt += 16
        o.dma = DmaDep(semtok, semtok.cnt, o)
        o.dur = 1.2 if eng == "pool" else 0.15
        o.xfer = 2.0 + (nbytes or 0) / 150e3
        o.delay = delay
        o.prev_dma = self.last_dma.get(id(semtok))
        self.last_dma[id(semtok)] = o
        self.ops[eng].append(o)
        self.segs[-1].append(o)
        self._commit(o.dma, ("dma", id(semtok)), reads, writes)
        return o

    def wait_all(self, eng, toks):
        deps = self._collect(eng, (), toks)
        o = Op(eng, None, deps)
        o.dur = 0.05
        self.ops[eng].append(o)
        self.segs[-1].append(o)
        return o

    def barrier(self, all_toks):
        for e in ENGS:
            self.wait_all(e, all_toks)
        self.segs.append([])

    def schedule(self, runahead=None):
        runahead = runahead or _OPT["run"]
        new_ops = {e: [] for e in ENGS}
        t_base = 0.0
        for seg in self.segs:
            if not seg:
                continue
            n = len(seg)
            pos = {id(o): i for i, o in enumerate(seg)}
            succ = [[] for _ in range(n)]
            ndep = [0] * n
            preds = [None] * n
            for i, o in enumerate(seg):
                ps_ = []
                for d in o.deps:
                    p = d if isinstance(d, Op) else d.op
                    k = pos.get(id(p))
                    if k is None:
                        continue
                    ps_.append((k, isinstance(d, Op)))
                if o.prev_dma is not None:
                    k = pos.get(id(o.prev_dma))
                    if k is not None:
                        ps_.append((k, None))
                preds[i] = ps_
                ks = set(k for k, _ in ps_)
                ndep[i] = len(ks)
                for k in ks:
                    succ[k].append(i)
            bott = [0.0] * n
            for i in range(n - 1, -1, -1):
                m = 0.0
                for k in succ[i]:
                    if bott[k] > m:
                        m = bott[k]
                bott[i] = seg[i].dur + seg[i].xfer + m
            ready = {e: [] for e in ENGS}
            free = {e: t_base for e in ENGS}
            scheduled = [False] * n
            low = 0

            def make_ready(i):
                o = seg[i]
                t = t_base
                for k, kind in preds[i]:
                    p = seg[k]
                    if kind is None:
                        ft = p.start
                    elif kind:
                        ft = p.fin
                    else:
                        ft = p.fin + p.xfer
                    if kind is not None and p.eng != o.eng:
                        ft += 0.06
                    if ft > t:
                        t = ft
                ready[o.eng].append((t + o.delay, i))

            for i in range(n):
                if ndep[i] == 0:
                    make_ready(i)
            nleft = n
            while nleft > 0:
                while low < n and scheduled[low]:
                    low += 1
                lim = low + runahead
                best = None
                cands = []
                tmin = None
                for e in ENGS:
                    fr = free[e]
                    for (rt, i) in ready[e]:
                        if i >= lim:
                            continue
                        st_ = rt if rt > fr else fr
                        cands.append((st_, i, e, rt))
                        if tmin is None or st_ < tmin:
                            tmin = st_
                if cands:
                    slack = self.slack
                    if _OPT["crit"]:
                        best = max((c for c in cands if c[0] <= tmin + slack), key=lambda c: (bott[c[1]], -c[1]))
                    else:
                        best = min((c for c in cands if c[0] <= tmin + slack), key=lambda c: c[1])
                if best is None:
                    for e in ENGS:
                        for (rt, i) in ready[e]:
                            st_ = max(rt, free[e])
                            if best is None or i < best[1]:
                                best = (st_, i, e, rt)
                assert best is not None, "scheduler stuck"
                st_, i, e, rt = best
                ready[e].remove((rt, i))
                o = seg[i]
                o.start = st_
                o.fin = st_ + o.dur
                free[e] = o.fin
                scheduled[i] = True
                new_ops[e].append(o)
                nleft -= 1
                for k in succ[i]:
                    ndep[k] -= 1
                    if ndep[k] == 0:
                        make_ready(k)
            t_base = max(max(free.values()), max((o.fin + o.xfer) for o in seg))
        self.ops = new_ops
        self.est_total = t_base

    def emit(self):
        nc = self.nc
        if self.sched:
            self.schedule()
        for e in ENGS:
            for o in self.ops[e]:
                for d in o.deps:
                    if isinstance(d, Op) and not (d.eng == "pe" and e == "pe"):
                        d.signal = True
        esem = {}
        for e in ENGS:
            n = 0
            for o in self.ops[e]:
                if o.signal:
                    n += 1
                    o.sigval = n
            esem[e] = self.stack.enter_context(nc.semaphore(f"eng_{e}"))
        self.esem = esem
        stats = {}
        with nc.Block() as block:
            def run(ename, engine):
                waited = {}
                nw = 0
                for o in self.ops[ename]:
                    for d in o.deps:
                        if isinstance(d, Op):
                            if d.eng == "pe" and ename == "pe":
                                continue
                            sem, val, key = esem[d.eng], d.sigval, d.eng
                        else:
                            sem, val, key = d.tok.sem, d.val, id(d.tok)
                        if waited.get(key, 0) >= val:
                            continue
                        waited[key] = val
                        engine.wait_ge(sem, val)
                        nw += 1
                    if o.fn is None:
                        continue
                    inst = o.fn(engine)
                    if o.dma is not None:
                        inst.then_inc(o.dma.tok.sem, 16)
                    elif o.signal:
                        inst.then_inc(esem[ename], 1)
                stats[ename] = (len(self.ops[ename]), nw)

            @block.tensor
            def _(e):
                run("pe", e)

            @block.vector
            def _(e):
                run("dve", e)

            @block.scalar
            def _(e):
                run("act", e)

            @block.gpsimd
            def _(e):
                run("pool", e)

            @block.sync
            def _(e):
                run("sp", e)
        self.stats = stats
        return stats


D = 1024
SEQ = 4096
NT = SEQ // 128
H = 8
DH = 64
NE = 32
TOPK = 4
CAP = 640
NSLOT = NE * CAP
PLE = 256
EPS = 1e-5
LIMIT = 7.0
ALPHA = 1.702
BIGPOS = 1.0e6
WINS = (2, 4, 8, 16)

_off = {}
_n = 0
for _name, _w in (("ident", 128), ("mask", 128), ("ustrict", 128), ("ones", 128), ("invf", 64),
                  ("xiq", 8), ("xik", 8), ("gc", 512), ("bands", 12 * 128), ("ecap", 32),
                  ("nmix", 8), ("nple", 8), ("pscale", 4), ("bgate", 256), ("bup", 256)):
    _off[_name] = (_n, _w)
    _n += _w
CST_N = _n
_roff = {}
_n = 0
for _name, _w in (("gnw", 512), ("nmoe", 1024), ("rb", 32), ("fnw", 1024)):
    _roff[_name] = (_n, _w)
    _n += _w
ROW_N = _n


def host_consts(inp):
    c = np.zeros((128, CST_N), np.float32)

    def put(name, arr):
        o, w = _off[name]
        c[:, o:o + w] = np.asarray(arr, np.float32).reshape(128, w)

    idx = np.arange(128)
    put("ident", np.eye(128))
    put("mask", (idx[None, :] >= idx[:, None]).astype(np.float32))
    put("ustrict", (idx[:, None] < idx[None, :]).astype(np.float32))
    put("ones", np.ones((128, 128)))
    half = DH // 2
    invf = (10000.0 ** (-(np.arange(half, dtype=np.float32)) / np.float32(half))).astype(np.float32)
    put("invf", np.tile(np.concatenate([invf, invf])[None, :], (128, 1)))
    lg = np.log1p(-np.power(2.0, -5.0 - np.arange(H, dtype=np.float64)))
    cpos = (idx + 1.0)[:, None]
    put("xiq", np.exp(cpos * lg[None, :]))
    put("xik", np.exp(-cpos * lg[None, :]) * (DH ** -0.5))
    gc = np.zeros((128, 512))
    for p in range(128):
        for h in range(H):
            if p // 64 == h % 2:
                gc[p, h * 64:(h + 1) * 64] = np.exp(128.0 * lg[h])
    put("gc", gc)
    bands = np.zeros((128, 12, 128))
    for g, w in enumerate(WINS):
        tp = idx[:, None]
        t = idx[None, :]
        inwin = (tp <= t) & (tp > t - w)
        bands[:, 3 * g + 0, :] = inwin / float(w) - (tp == t)
        cnt0 = np.minimum(t + 1.0, float(w))
        bands[:, 3 * g + 1, :] = inwin / cnt0 - (tp == t)
        bands[:, 3 * g + 2, :] = (tp >= 129 + t - w) / float(w)
    put("bands", bands)
    put("ecap", np.tile((np.arange(NE) * CAP)[None, :], (128, 1)))
    put("nmix", inp["norm_mix_w"][0].reshape(8, 128).T)
    put("nple", inp["norm_ple_w"][0].reshape(8, 128).T)
    put("pscale", inp["pool_scale"][0].reshape(4, 128).T)
    bgu = inp["expert_b_gate_up"][0]
    put("bgate", bgu[:, 0::2].reshape(NE, 8, 128).transpose(2, 0, 1))
    put("bup", bgu[:, 1::2].reshape(NE, 8, 128).transpose(2, 0, 1))
    r = np.zeros((1, ROW_N), np.float32)
    for name, v in (("gnw", inp["ret_gn_w"][0]), ("nmoe", inp["norm_moe_w"][0]),
                    ("rb", inp["router_b"][0]), ("fnw", inp["final_norm_w"])):
        o, w = _roff[name]
        r[0, o:o + w] = v
    return c, r


def _bc(ap, shape):
    return ap.broadcast_to(shape)


_BCREG = {}


def _bcreg(e, nc):
    if _BCREG.get("nc") is not nc:
        r = e.alloc_register("slot_bound")
        e.reg_mov(r, NSLOT - 1)
        _BCREG["nc"] = nc
        _BCREG["r"] = r
    return _BCREG["r"]


class Ctx:
    pass


def build(nt=NT, phases=4, debug=False, limit=None, n_exp=NE, sched=True):
    nc = bass.Bass("TRN2", target_bir_lowering=False)
    dt = lambda name, shape, dtype, kind: nc.dram_tensor(name, shape, dtype, kind=kind).ap()
    x_d = dt("x", [SEQ, D], F32, "ExternalInput")
    p_d = dt("p", [SEQ, PLE], F32, "ExternalInput")
    pos_d = dt("pos", [128, NT], I32, "ExternalInput")
    cst_d = dt("cst", [128, CST_N], F32, "ExternalInput")
    rows_d = dt("rows", [1, ROW_N], F32, "ExternalInput")
    win_d = dt("w_in", [D, 2560], F32, "ExternalInput")
    wout_d = dt("w_out", [D, D], F32, "ExternalInput")
    poolw_d = dt("pool_w", [4, 128, 128], F32, "ExternalInput")
    rw_d = dt("router_w", [D, NE], F32, "ExternalInput")
    wg_d = dt("w_g", [NE, D, D], F32, "ExternalInput")
    wu_d = dt("w_u", [NE, D, D], F32, "ExternalInput")
    wd_d = dt("w_d", [NE, D, D], F32, "ExternalInput")
    bd_d = dt("b_d", [NE, D], F32, "ExternalInput")
    pgw_d = dt("ple_gate_w", [D, D], F32, "ExternalInput")
    ppw_d = dt("ple_proj_w", [PLE, D], F32, "ExternalInput")
    out_d = dt("out", [SEQ, D], F32, "ExternalOutput")
    xs_d = nc.dram_tensor("xs_scr", [NSLOT, D], BF16).ap()
    ys_d = nc.dram_tensor("ys_scr", [NSLOT, D], F32).ap()
    h_d = nc.dram_tensor("h_scr", [SEQ, D], F32).ap()
    dbg = {}
    if debug:
        dbg["h1"] = dt("dbg_h1", [SEQ, D], F32, "ExternalOutput")
        dbg["gate"] = dt("dbg_gate", [128, NT * 4], F32, "ExternalOutput")
        dbg["pos"] = dt("dbg_pos", [128, NT * 4], I32, "ExternalOutput")
        dbg["h2"] = dt("dbg_h2", [SEQ, D], F32, "ExternalOutput")

    with ExitStack() as gst:
        P = Prog(nc, gst)
        P.limit = limit
        P.sched = sched
        alltoks = []

        def T(name):
            t = P.tok(name)
            alltoks.append(t)
            return t

        def sbuf(st, name, shape, dtype):
            return st.enter_context(nc.sbuf_tensor("s_" + name, shape, dtype))

        V = lambda fn, r=(), w=(), n=None: P.op("dve", fn, r, w, n)
        A = lambda fn, r=(), w=(), n=None: P.op("act", fn, r, w, n)
        G = lambda fn, r=(), w=(), n=None: P.op("pool", fn, r, w, n)
        M = lambda fn, r=(), w=(), n=None: P.op("pe", fn, r, w, n)

        cst = sbuf(gst, "cst", [128, CST_N], F32)
        t_cst = T("cst")
        identb = sbuf(gst, "identb", [128, 128], BF16)
        t_identb = T("identb")
        gates = sbuf(gst, "gates", [128, NT, 4], F32)
        posi = sbuf(gst, "posi", [128, NT, 4], I32)
        t_gates = [T(f"gates{j}") for j in range(NT)]
        t_posi = [T(f"posi{j}") for j in range(NT)]
        nhalf = sbuf(gst, "nhalf", [128, 8], F32)
        t_nhalf = T("nhalf")
        wgu_buf, wd_buf, t_wgu, t_wd = [], [], [], []
        psb = [gst.enter_context(nc.psum_tensor(f"ps{i}", [128, 512], F32)) for i in range(8)]
        t_ps = [T(f"ps{i}") for i in range(8)]

        def C(name):
            o, w = _off[name]
            return cst[:, o:o + w]

        t_hd = [T(f"hd{j}") for j in range(NT)]
        t_dbgs = [T(f"dbgs{i}") for i in range(16)] if debug else []
        t_xsd = T("xsd")
        t_xsd.multi = True
        t_xsd.w = {}
        t_zfd = T("zfd")
        t_ysd = T("ysd")
        t_ysd.multi = True
        t_ysd.w = {}

        P.dma("sp", lambda e: e.dma_start(out=cst[:], in_=cst_d), t_cst, writes=[t_cst])
        V(lambda e: e.tensor_copy(identb[:], C("ident")), [t_cst], [t_identb])
        G(lambda e: e.memset(nhalf[:], -0.5), (), [t_nhalf])
        if debug:
            G(lambda e: e.memset(gates[:], 0.0), (), t_gates)
            G(lambda e: e.memset(posi[:], 0), (), t_posi)
        ident = C("ident")

        def load_expert(e_idx, slot):
            wg_v = wg_d[e_idx].rearrange("(c p) n -> p c n", p=128)
            wu_v = wu_d[e_idx].rearrange("(c p) n -> p c n", p=128)
            wd_v = wd_d[e_idx].rearrange("(c p) n -> p c n", p=128)
            for hc in range(2):
                cs = slice(hc * 4, hc * 4 + 4)
                P.dma("pool", lambda e, cs=cs: e.dma_start(out=wgu_buf[slot][:, cs, 0:1024], in_=wg_v[:, cs, :]),
                      t_wgu[slot], writes=[t_wgu[slot]])
                P.dma("pool", lambda e, cs=cs: e.dma_start(out=wgu_buf[slot][:, cs, 1024:2048], in_=wu_v[:, cs, :]),
                      t_wgu[slot], writes=[t_wgu[slot]])
                P.dma("pool", lambda e, cs=cs: e.dma_start(out=wd_buf[slot][:, cs, :], in_=wd_v[:, cs, :]),
                      t_wd[slot], writes=[t_wd[slot]])

        with ExitStack() as st:
            RN1 = _roff["fnw"][0]
            rows = sbuf(st, "rows", [128, RN1], F32)
            t_rows = T("rows")
            P.dma("sp", lambda e: e.dma_start(out=rows[:], in_=rows_d[0, 0:RN1].partition_broadcast(128)), t_rows, writes=[t_rows])

            def R(name):
                o, w = _roff[name]
                return rows[:, o:o + w]

            w_in = sbuf(st, "w_in", [128, 8, 2560], BF16)
            t_win = T("w_in")
            w_out = sbuf(st, "w_out", [128, 8, 1024], BF16)
            t_wout = T("w_out")
            poolw = sbuf(st, "poolw", [128, 4, 128], BF16)
            t_poolw = T("poolw")
            rw = sbuf(st, "rw", [128, 8, NE], F32)
            t_rw = T("rw")
            posi_t = sbuf(st, "pos_i", [128, NT], I32)
            posf = sbuf(st, "pos_f", [128, NT], F32)
            CC = sbuf(st, "CC", [128, NT, 64], F32)
            SS = sbuf(st, "SS", [128, NT, 64], F32)
            st_setup = ExitStack()
            ang = sbuf(st_setup, "ang", [128, NT, 64], F32)
            tmpa = sbuf(st_setup, "tmpa", [128, NT, 64], F32)
            stage = [sbuf(st_setup, f"stage{i}", [128, 1280], F32) for i in range(2)]
            t_stage = [T(f"stage{i}") for i in range(2)]
            win_v = win_d.rearrange("(c p) n -> p c n", p=128)
            for c2 in range(16):
                s = c2 % 2
                c, hf = c2 // 2, c2 % 2
                P.dma("sp", lambda e, c=c, s=s, hf=hf: e.dma_start(out=stage[s][:], in_=win_v[:, c, hf * 1280:(hf + 1) * 1280]), t_stage[s], writes=[t_stage[s]])
                o, _ = _off["nmix"]
                if c2 % 2 == 0:
                    V(lambda e, c=c, s=s, o=o, hf=hf: e.tensor_scalar(out=w_in[:, c, hf * 1280:(hf + 1) * 1280], in0=stage[s][:], scalar1=cst[:, o + c:o + c + 1],
                                                                    scalar2=None, op0=ALU.mult), [t_stage[s], t_cst], [t_win], n=1280)
                else:
                    A(lambda e, c=c, s=s, o=o, hf=hf: e.activation(out=w_in[:, c, hf * 1280:(hf + 1) * 1280], in_=stage[s][:], func=AF.Copy, scale=cst[:, o + c:o + c + 1]),
                      [t_stage[s], t_cst], [t_win], n=1280)
            P.dma("pool", lambda e: e.dma_start(out=w_out[:], in_=wout_d.rearrange("(c p) n -> p c n", p=128)), t_wout, writes=[t_wout])
            P.dma("pool", lambda e: e.dma_start(out=poolw[:], in_=poolw_d.rearrange("g c d -> c g d")), t_poolw, writes=[t_poolw])
            P.dma("sp", lambda e: e.dma_start(out=rw[:], in_=rw_d.rearrange("(c p) n -> p c n", p=128)), t_rw, writes=[t_rw])

            t_pos, t_ang, t_tmpa, t_CC, t_SS = T("pos"), T("ang"), T("tmpa"), T("CC"), T("SS")
            P.dma("sp", lambda e: e.dma_start(out=posi_t[:], in_=pos_d), t_pos, writes=[t_pos])
            V(lambda e: e.tensor_copy(posf[:], posi_t[:]), [t_pos], [t_pos])
            V(lambda e: e.tensor_tensor(out=ang[:], in0=_bc(posf[:].unsqueeze(2), [128, NT, 64]),
                                        in1=_bc(C("invf").unsqueeze(1), [128, NT, 64]), op=ALU.mult), [t_pos, t_cst], [t_ang])
            TWO_PI = float(2.0 * np.pi)

            def sin_table(dst, t_dst, shift):
                V(lambda e: e.tensor_scalar(out=tmpa[:], in0=ang[:], scalar1=shift, scalar2=None, op0=ALU.add), [t_ang], [t_tmpa])
                V(lambda e: e.tensor_scalar(out=dst[:].bitcast(I32), in0=tmpa[:], scalar1=1.0 / TWO_PI, scalar2=None, op0=ALU.mult), [t_tmpa], [t_dst])
                V(lambda e: e.tensor_copy(dst[:], dst[:].bitcast(I32)), [t_dst], [t_dst])
                V(lambda e: e.scalar_tensor_tensor(out=tmpa[:], in0=dst[:], scalar=-TWO_PI, in1=tmpa[:], op0=ALU.mult, op1=ALU.add),
                  [t_dst, t_tmpa], [t_tmpa])
                V(lambda e: e.tensor_scalar(out=dst[:], in0=tmpa[:], scalar1=float(np.pi), scalar2=-TWO_PI, op0=ALU.is_gt, op1=ALU.mult),
                  [t_tmpa], [t_dst])
                V(lambda e: e.tensor_tensor(out=tmpa[:], in0=tmpa[:], in1=dst[:], op=ALU.add), [t_tmpa, t_dst], [t_tmpa])
                V(lambda e: e.tensor_scalar(out=dst[:], in0=tmpa[:], scalar1=-float(np.pi), scalar2=TWO_PI, op0=ALU.is_lt, op1=ALU.mult),
                  [t_tmpa], [t_dst])
                V(lambda e: e.tensor_tensor(out=tmpa[:], in0=tmpa[:], in1=dst[:], op=ALU.add), [t_tmpa, t_dst], [t_tmpa])
                V(lambda e: e.tensor_scalar(out=tmpa[:], in0=tmpa[:], scalar1=-3.1415925, scalar2=3.1415925, op0=ALU.max, op1=ALU.min),
                  [t_tmpa], [t_tmpa])
                A(lambda e: e.activation(out=dst[:], in_=tmpa[:], func=AF.Sin), [t_tmpa], [t_dst])

            sin_table(CC, t_CC, float(np.pi / 2))
            sin_table(SS, t_SS, 0.0)
            V(lambda e: e.tensor_scalar(out=SS[:, :, 0:32], in0=SS[:, :, 0:32], scalar1=-1.0, scalar2=None, op0=ALU.mult), [t_SS], [t_SS])

            P.barrier(alltoks)
            st_setup.close()
            state = sbuf(st, "state", [128, 512], F32)
            stateb = sbuf(st, "stateb", [128, 512], BF16)
            t_state, t_stateb = T("state"), T("stateb")
            V(lambda e: e.memset(state[:], 0.0), (), [t_state])
            V(lambda e: e.memset(stateb[:], 0.0), (), [t_stateb])
            macc = sbuf(st, "macc", [128, NE], F32)
            t_macc = T("macc")
            V(lambda e: e.memset(macc[:], 0.0), (), [t_macc])
            us = [sbuf(st, f"us{i}", [128, 512], F32) for i in range(2)]
            t_us = [T(f"us{i}") for i in range(2)]

            NB = 2
            def mk(name, shape, dtype, n=NB):
                return [sbuf(st, f"{name}{i}", shape, dtype) for i in range(n)], [T(f"{name}{i}") for i in range(n)]
            xt, t_xt = mk("xt", [128, D], F32)
            junk, t_junk = mk("junk", [128, D], BF16, 1)
            ssq, t_ssq = mk("ssq", [128, 8], F32)
            xT, t_xT = mk("xT", [128, 8, 128], BF16)
            qs, t_qs = mk("qs", [128, 512], F32)
            ks, t_ks = mk("ks", [128, 512], F32)
            vb, t_vb = mk("vb", [128, 512], BF16)
            sg, t_sg = mk("sg", [128, 512], F32)
            qa, t_qa = mk("qa", [128, 512], F32)
            qb, t_qb = mk("qb", [128, 512], F32)
            ka, t_ka, kb, t_kb = qa, t_qa, qb, t_qb
            qt, t_qt = mk("qt", [128, 512], BF16)
            kt, t_kt = mk("kt", [128, 512], BF16)
            qkT, t_qkT = mk("qkT", [128, 1536], BF16)
            for i_ in range(NB):
                V(lambda e, i_=i_: e.memset(qkT[i_][:], 0.0), (), [t_qkT[i_]])
            PT, t_PT = mk("PT", [128, 1024], BF16)
            ysb, t_ysb = mk("ysb", [128, 512], F32)
            ysq, t_ysq = mk("ysq", [128, 512], F32, 1)
            ysq, t_ysq = ysq * 2, t_ysq * 2
            gst8, t_gst8 = mk("gst8", [128, 6, 8], F32)
            gsg, t_gsg = mk("gsg", [128, 512], F32)
            ret, t_ret = mk("ret", [128, 512], BF16)
            mixT, t_mixT = mk("mixT", [128, 8, 128], BF16)
            pooledT, t_pooledT = mk("pooledT", [128, 4, 128], BF16)
            ht, t_ht = mk("ht", [128, D], F32)
            xn2, t_xn2 = mk("xn2", [128, D], F32, 1)
            xn2, t_xn2 = xn2 * 2, t_xn2 * 2
            NBX = 5
            xn2b, t_xn2b = mk("xn2b", [128, D], BF16, NBX)
            pend = []
            zt = sbuf(st, "zt", [128, 2048], BF16)
            t_zt, t_zf = T("zt"), T("zf")
            G(lambda e: e.memset(zt[:], 0.0), (), [t_zt])
            assert NSLOT % 256 == 0
            for kz in range(NSLOT // 256):
                P.dma("act" if kz % 2 else "sp", lambda e, kz=kz: e.dma_start(out=xs_d[kz * 256:(kz + 1) * 256, :].rearrange("(p r) d -> p (r d)", p=128), in_=zt[:]),
                      t_zf, reads=[t_zt], writes=[t_zfd], nbytes=512 * 1024)
            xn2T, t_xn2T = mk("xn2T", [128, 8, 128], F32, 1)
            xn2T, t_xn2T = xn2T * 2, t_xn2T * 2
            rt, t_rt = mk("rt", [128, 8, 32], F32)
            top8, t_top8 = mk("top8", [128, 16], F32)
            posk, t_posk = mk("posk", [128, 4], F32)

            bank_ctr = [0]

            def nxt():
                v = bank_ctr[0] % 8
                bank_ctr[0] += 1
                return v

            for j in range(nt):
                b = j % NB
                P.dma("sp", lambda e, j=j, b=b: e.dma_start(out=xt[b][:], in_=x_d[j * 128:(j + 1) * 128, :]), t_xt[b], writes=[t_xt[b]])
                A(lambda e, b=b: e.activation(out=junk[0][:], in_=xt[b][:], func=AF.Square, accum_out=ssq[b][:, 0:1]),
                  [t_xt[b]], [t_junk[0], t_ssq[b]])
                V(lambda e, b=b: e.tensor_scalar(out=ssq[b][:, 1:2], in0=ssq[b][:, 0:1], scalar1=1.0 / D, scalar2=EPS, op0=ALU.mult, op1=ALU.add),
                  [t_ssq[b]], [t_ssq[b]])
                G(lambda e, b=b: e.tensor_tensor(out=ssq[b][:, 2:3], in0=ssq[b][:, 1:2], in1=nhalf[:, 0:1], op=ALU.pow),
                  [t_ssq[b], t_nhalf], [t_ssq[b]])
                rstd = ssq[b][:, 2:3]
                bx0, bx1 = nxt(), nxt()
                for c in range(8):
                    bank = (bx0, bx1)[c // 4]
                    M(lambda e, b=b, c=c, bank=bank: e.transpose(psb[bank][:, (c % 4) * 128:(c % 4 + 1) * 128], xt[b][:, c * 128:(c + 1) * 128], ident),
                      [t_xt[b], t_cst], [t_ps[bank]])
                V(lambda e, b=b, bx0=bx0: e.tensor_copy(xT[b][:, 0:4, :], psb[bx0][:].rearrange("p (c t) -> p c t", c=4)), [t_ps[bx0]], [t_xT[b]])
                A(lambda e, b=b, bx1=bx1: e.copy(xT[b][:, 4:8, :], psb[bx1][:].rearrange("p (c t) -> p c t", c=4)), [t_ps[bx1]], [t_xT[b]])
                bp = [nxt() for _ in range(5)]
                for nb in range(5):
                    for c in range(8):
                        M(lambda e, b=b, nb=nb, c=c, bk=bp[nb]: e.matmul(psb[bk][:], lhsT=xT[b][:, c, :], rhs=w_in[:, c, nb * 512:(nb + 1) * 512],
                                                             start=(c == 0), stop=(c == 7)),
                          [t_xT[b], t_win], [t_ps[bp[nb]]], n=530)
                ub = j % 2
                A(lambda e, bp=bp, b=b: e.activation(out=qs[b][:], in_=psb[bp[0]][:], func=AF.Copy, scale=ssq[b][:, 2:3]), [t_ps[bp[0]], t_ssq[b]], [t_qs[b]])
                A(lambda e, bp=bp, b=b: e.activation(out=ks[b][:], in_=psb[bp[1]][:], func=AF.Copy, scale=ssq[b][:, 2:3]), [t_ps[bp[1]], t_ssq[b]], [t_ks[b]])
                A(lambda e, bp=bp, b=b: e.activation(out=vb[b][:], in_=psb[bp[2]][:], func=AF.Copy, scale=ssq[b][:, 2:3]), [t_ps[bp[2]], t_ssq[b]], [t_vb[b]])
                A(lambda e, bp=bp, b=b: e.activation(out=sg[b][:], in_=psb[bp[3]][:], func=AF.Silu, scale=ssq[b][:, 2:3]), [t_ps[bp[3]], t_ssq[b]], [t_sg[b]])
                A(lambda e, bp=bp, b=b, ub=ub: e.activation(out=us[ub][:], in_=psb[bp[4]][:], func=AF.Copy, scale=ssq[b][:, 2:3]), [t_ps[bp[4]], t_ssq[b]], [t_us[ub]])
                ccj = _bc(CC[:, j, :].unsqueeze(1), [128, 8, 64])
                ssj_lo = _bc(SS[:, j, 0:32].unsqueeze(1), [128, 8, 32])
                ssj_hi = _bc(SS[:, j, 32:64].unsqueeze(1), [128, 8, 32])
                for (src, t_src, aa, t_aa, bb, t_bb, dst, t_dst, xin) in (
                        (qs, t_qs, qa, t_qa, qb, t_qb, qt, t_qt, "xiq"), (ks, t_ks, ka, t_ka, kb, t_kb, kt, t_kt, "xik")):
                    s4 = src[b][:].rearrange("p (h two d) -> p h two d", two=2, d=32)
                    b4 = bb[b][:].rearrange("p (h two d) -> p h two d", two=2, d=32)
                    V(lambda e, b=b, src=src, aa=aa, ccj=ccj: e.tensor_tensor(out=aa[b][:].rearrange("p (h d) -> p h d", d=64),
                                                                              in0=src[b][:].rearrange("p (h d) -> p h d", d=64), in1=ccj, op=ALU.mult),
                      [t_src[b], t_CC], [t_aa[b]])
                    V(lambda e, s4=s4, b4=b4, ssj_lo=ssj_lo: e.tensor_tensor(out=b4[:, :, 0, :], in0=s4[:, :, 1, :], in1=ssj_lo, op=ALU.mult),
                      [t_src[b], t_SS], [t_bb[b]])
                    V(lambda e, s4=s4, b4=b4, ssj_hi=ssj_hi: e.tensor_tensor(out=b4[:, :, 1, :], in0=s4[:, :, 0, :], in1=ssj_hi, op=ALU.mult),
                      [t_src[b], t_SS], [t_bb[b]])
                    V(lambda e, b=b, aa=aa, bb=bb: e.tensor_tensor(out=aa[b][:], in0=aa[b][:], in1=bb[b][:], op=ALU.add), [t_aa[b], t_bb[b]], [t_aa[b]])
                    V(lambda e, b=b, aa=aa, dst=dst, xin=xin: e.tensor_tensor(out=dst[b][:].rearrange("p (h d) -> p h d", d=64),
                                                                           in0=aa[b][:].rearrange("p (h d) -> p h d", d=64),
                                                                           in1=_bc(C(xin).unsqueeze(2), [128, 8, 64]), op=ALU.mult),
                      [t_aa[b], t_cst], [t_dst[b]])
                bqk = nxt()
                pb2 = psb[bqk][:].bitcast(BF16)
                for i in range(4):
                    M(lambda e, b=b, i=i, pb2=pb2: e.transpose(pb2[:, i * 128:(i + 1) * 128], qt[b][:, i * 128:(i + 1) * 128], identb[:]),
                      [t_qt[b], t_identb], [t_ps[bqk]])
                for i in range(4):
                    M(lambda e, b=b, i=i, pb2=pb2: e.transpose(pb2[:, 512 + i * 128:512 + (i + 1) * 128], kt[b][:, i * 128:(i + 1) * 128], identb[:]),
                      [t_kt[b], t_identb], [t_ps[bqk]])
                A(lambda e, b=b, pb2=pb2: e.copy(qkT[b][:, 0:512], pb2[:, 0:512]), [t_ps[bqk]], [t_qkT[b]])
                A(lambda e, b=b, pb2=pb2: e.copy(qkT[b][0:64, 512:1024], pb2[0:64, 512:1024]), [t_ps[bqk]], [t_qkT[b]])
                A(lambda e, b=b, pb2=pb2: e.copy(qkT[b][64:128, 1024:1536], pb2[64:128, 512:1024]), [t_ps[bqk]], [t_qkT[b]])
                bs = [nxt(), nxt()]
                for h in range(H):
                    hb = (h % 2) * 64
                    hh = h // 2
                    bank = bs[h // 4]
                    M(lambda e, b=b, h=h, hb=hb, hh=hh, bank=bank: e.matmul(
                        psb[bank][:, (h % 4) * 128:(h % 4 + 1) * 128],
                        lhsT=qkT[b][:, 512 + (h % 2) * 512 + hh * 128:512 + (h % 2) * 512 + (hh + 1) * 128],
                        rhs=qkT[b][:, hh * 128:(hh + 1) * 128], start=True, stop=True),
                      [t_qkT[b]], [t_ps[bank]])
                mask4 = _bc(C("mask").unsqueeze(1), [128, 4, 128])
                for half in range(2):
                    V(lambda e, b=b, half=half, bs=bs: e.tensor_tensor(out=PT[b][:, half * 512:(half + 1) * 512].rearrange("p (h c) -> p h c", c=128),
                                                                in0=psb[bs[half]][:].rearrange("p (h c) -> p h c", c=128), in1=mask4, op=ALU.mult),
                      [t_ps[bs[half]], t_cst], [t_PT[b]])
                by, bkv = nxt(), nxt()
                for h in range(H):
                    hb = (h % 2) * 64
                    hh = h // 2
                    M(lambda e, b=b, h=h, by=by: e.matmul(psb[by][:, h * 64:(h + 1) * 64], lhsT=PT[b][:, h * 128:(h + 1) * 128],
                                                   rhs=vb[b][:, h * 64:(h + 1) * 64], start=True, stop=False),
                      [t_PT[b], t_vb[b]], [t_ps[by]])
                    M(lambda e, b=b, h=h, hb=hb, hh=hh, by=by: e.matmul(psb[by][:, h * 64:(h + 1) * 64], lhsT=qkT[b][:, hh * 128:(hh + 1) * 128],
                                                                 rhs=stateb[:, h * 64:(h + 1) * 64], start=False, stop=True),
                      [t_qkT[b], t_stateb], [t_ps[by]])
                for hh in range(4):
                    M(lambda e, b=b, hh=hh, bkv=bkv: e.matmul(psb[bkv][:, hh * 128:(hh + 1) * 128], lhsT=kt[b][:, hh * 128:(hh + 1) * 128],
                                                     rhs=vb[b][:, hh * 128:(hh + 1) * 128], start=True, stop=True),
                      [t_kt[b], t_vb[b]], [t_ps[bkv]])
                V(lambda e, bkv=bkv: e.tensor_tensor(out=state[:], in0=state[:], in1=psb[bkv][:], op=ALU.add), [t_state, t_ps[bkv]], [t_state])
                V(lambda e: e.tensor_tensor(out=state[:], in0=state[:], in1=C("gc"), op=ALU.mult), [t_state, t_cst], [t_state])
                V(lambda e: e.tensor_copy(stateb[:], state[:]), [t_state], [t_stateb])
                A(lambda e, b=b, by=by: e.copy(ysb[b][:], psb[by][:]), [t_ps[by]], [t_ysb[b]])
                A(lambda e, b=b, by=by: e.activation(out=ysq[b][:], in_=psb[by][:], func=AF.Square), [t_ps[by]], [t_ysq[b]])
                G(lambda e, b=b: e.tensor_tensor(out=gsg[b][:], in0=sg[b][:], in1=R("gnw"), op=ALU.mult), [t_sg[b], t_rows], [t_gsg[b]])
                g8 = gst8[b]
                V(lambda e, b=b, g8=g8: e.tensor_reduce(out=g8[:, 0, :], in_=ysb[b][:].rearrange("p (h d) -> p h d", d=64), axis=AX.X, op=ALU.add),
                  [t_ysb[b]], [t_gst8[b]])
                V(lambda e, b=b, g8=g8: e.tensor_reduce(out=g8[:, 1, :], in_=ysq[b][:].rearrange("p (h d) -> p h d", d=64), axis=AX.X, op=ALU.add),
                  [t_ysq[b]], [t_gst8[b]])
                V(lambda e, g8=g8: e.tensor_scalar(out=g8[:, 2, :], in0=g8[:, 0, :], scalar1=1.0 / DH, scalar2=None, op0=ALU.mult), [t_gst8[b]], [t_gst8[b]])
                V(lambda e, g8=g8: e.tensor_tensor(out=g8[:, 3, :], in0=g8[:, 2, :], in1=g8[:, 2, :], op=ALU.mult), [t_gst8[b]], [t_gst8[b]])
                V(lambda e, g8=g8: e.scalar_tensor_tensor(out=g8[:, 4, :], in0=g8[:, 1, :], scalar=1.0 / DH, in1=g8[:, 3, :], op0=ALU.mult, op1=ALU.subtract),
                  [t_gst8[b]], [t_gst8[b]])
                V(lambda e, g8=g8: e.tensor_scalar(out=g8[:, 4, :], in0=g8[:, 4, :], scalar1=EPS, scalar2=None, op0=ALU.add), [t_gst8[b]], [t_gst8[b]])
                G(lambda e, g8=g8: e.tensor_tensor(out=g8[:, 5, :], in0=g8[:, 4, :], in1=nhalf[:, 0:8], op=ALU.pow), [t_gst8[b], t_nhalf], [t_gst8[b]])
                y3 = ysb[b][:].rearrange("p (h d) -> p h d", d=64)
                V(lambda e, y3=y3, g8=g8: e.tensor_tensor(out=y3, in0=y3, in1=_bc(g8[:, 2, :].unsqueeze(2), [128, 8, 64]), op=ALU.subtract),
                  [t_ysb[b], t_gst8[b]], [t_ysb[b]])
                V(lambda e, y3=y3, g8=g8: e.tensor_tensor(out=y3, in0=y3, in1=_bc(g8[:, 5, :].unsqueeze(2), [128, 8, 64]), op=ALU.mult),
                  [t_ysb[b], t_gst8[b]], [t_ysb[b]])
                V(lambda e, b=b: e.tensor_tensor(out=ret[b][:], in0=ysb[b][:], in1=gsg[b][:], op=ALU.mult), [t_ysb[b], t_gsg[b]], [t_ret[b]])
                brt = nxt()
                pb1 = psb[brt][:].bitcast(BF16)
                for i in range(4):
                    M(lambda e, b=b, i=i, pb1=pb1: e.transpose(pb1[:, i * 128:(i + 1) * 128], ret[b][:, i * 128:(i + 1) * 128], identb[:]),
                      [t_ret[b], t_identb], [t_ps[brt]])
                A(lambda e, b=b, pb1=pb1: e.copy(mixT[b][:, 0:4, :], pb1[:, 0:512].rearrange("p (c t) -> p c t", c=4)), [t_ps[brt]], [t_mixT[b]])
                bo, _ = _off["bands"]
                bpl, bmx = nxt(), nxt()
                for g in range(4):
                    kind = 1 if j == 0 else 0
                    M(lambda e, g=g, ub=ub, kind=kind, bo=bo, j=j, bpl=bpl: e.matmul(psb[bpl][:, g * 128:(g + 1) * 128], lhsT=us[ub][:, g * 128:(g + 1) * 128],
                                                                       rhs=cst[:, bo + (3 * g + kind) * 128:bo + (3 * g + kind + 1) * 128],
                                                                       start=True, stop=(j == 0)),
                      [t_us[ub], t_cst], [t_ps[bpl]])
                    if j > 0:
                        M(lambda e, g=g, ub=ub, bo=bo, bpl=bpl: e.matmul(psb[bpl][:, g * 128:(g + 1) * 128], lhsT=us[1 - ub][:, g * 128:(g + 1) * 128],
                                                                rhs=cst[:, bo + (3 * g + 2) * 128:bo + (3 * g + 3) * 128], start=False, stop=True),
                          [t_us[1 - ub], t_cst], [t_ps[bpl]])
                V(lambda e, b=b, bpl=bpl: e.tensor_copy(pooledT[b][:], psb[bpl][:].rearrange("p (g t) -> p g t", g=4)), [t_ps[bpl]], [t_pooledT[b]])
                for g in range(4):
                    M(lambda e, b=b, g=g, bmx=bmx: e.matmul(psb[bmx][:, g * 128:(g + 1) * 128], lhsT=poolw[:, g, :], rhs=pooledT[b][:, g, :], start=True, stop=True),
                      [t_poolw, t_pooledT[b]], [t_ps[bmx]])
                po, _ = _off["pscale"]
                V(lambda e, b=b, po=po, bmx=bmx: e.tensor_tensor(out=mixT[b][:, 4:8, :], in0=psb[bmx][:].rearrange("p (g t) -> p g t", g=4),
                                                        in1=_bc(cst[:, po:po + 4].unsqueeze(2), [128, 4, 128]), op=ALU.mult),
                  [t_ps[bmx], t_cst], [t_mixT[b]])
                bh = [nxt(), nxt()]
                for nb in range(2):
                    for c in range(8):
                        M(lambda e, b=b, nb=nb, c=c, bh=bh: e.matmul(psb[bh[nb]][:], lhsT=mixT[b][:, c, :], rhs=w_out[:, c, nb * 512:(nb + 1) * 512],
                                                             start=(c == 0), stop=(c == 7)),
                          [t_mixT[b], t_wout], [t_ps[bh[nb]]], n=530)
                for nb in range(2):
                    V(lambda e, b=b, nb=nb, bh=bh: e.tensor_tensor(out=ht[b][:, nb * 512:(nb + 1) * 512], in0=psb[bh[nb]][:], in1=xt[b][:, nb * 512:(nb + 1) * 512], op=ALU.add),
                      [t_ps[bh[nb]], t_xt[b]], [t_ht[b]])
                P.dma("sp", lambda e, j=j, b=b: e.dma_start(out=h_d[j * 128:(j + 1) * 128, :], in_=ht[b][:]), t_ht[b], reads=[t_ht[b]], writes=[t_hd[j]])
                if debug:
                    P.dma("sp", lambda e, j=j, b=b: e.dma_start(out=dbg["h1"][j * 128:(j + 1) * 128, :], in_=ht[b][:]), t_dbgs[j % 8], reads=[t_ht[b]])
                if phases < 2:
                    continue

                A(lambda e, b=b: e.activation(out=junk[0][:], in_=ht[b][:], func=AF.Square, accum_out=ssq[b][:, 4:5]),
                  [t_ht[b]], [t_junk[0], t_ssq[b]])
                V(lambda e, b=b: e.tensor_scalar(out=ssq[b][:, 5:6], in0=ssq[b][:, 4:5], scalar1=1.0 / D, scalar2=EPS, op0=ALU.mult, op1=ALU.add),
                  [t_ssq[b]], [t_ssq[b]])
                G(lambda e, b=b: e.tensor_tensor(out=ssq[b][:, 6:7], in0=ssq[b][:, 5:6], in1=nhalf[:, 0:1], op=ALU.pow),
                  [t_ssq[b], t_nhalf], [t_ssq[b]])
                V(lambda e, b=b: e.scalar_tensor_tensor(out=xn2[b][:], in0=ht[b][:], scalar=ssq[b][:, 6:7], in1=R("nmoe"), op0=ALU.mult, op1=ALU.mult),
                  [t_ht[b], t_ssq[b], t_rows], [t_xn2[b]])
                bx = j % NBX
                A(lambda e, b=b, bx=bx: e.copy(xn2b[bx][:], xn2[b][:]), [t_xn2[b]], [t_xn2b[bx]])
                bn = [nxt(), nxt()]
                blg = nxt()
                for c in range(8):
                    bank = bn[c // 4]
                    M(lambda e, b=b, c=c, bank=bank: e.transpose(psb[bank][:, (c % 4) * 128:(c % 4 + 1) * 128], xn2[b][:, c * 128:(c + 1) * 128], ident),
                      [t_xn2[b], t_cst], [t_ps[bank]])
                V(lambda e, b=b, bn=bn: e.tensor_copy(xn2T[b][:, 0:4, :], psb[bn[0]][:].rearrange("p (c t) -> p c t", c=4)), [t_ps[bn[0]]], [t_xn2T[b]])
                A(lambda e, b=b, bn=bn: e.copy(xn2T[b][:, 4:8, :], psb[bn[1]][:].rearrange("p (c t) -> p c t", c=4)), [t_ps[bn[1]]], [t_xn2T[b]])
                for c in range(8):
                    M(lambda e, b=b, c=c, blg=blg: e.matmul(psb[blg][:, 0:NE], lhsT=xn2T[b][:, c, :], rhs=rw[:, c, :], start=(c == 0), stop=(c == 7)),
                      [t_xn2T[b], t_rw], [t_ps[blg]])
                r_ = rt[b]
                t8 = top8[b]
                V(lambda e, r_=r_, blg=blg: e.tensor_tensor(out=r_[:, 0, :], in0=psb[blg][:, 0:NE], in1=R("rb"), op=ALU.add), [t_ps[blg], t_rows], [t_rt[b]])
                V(lambda e, r_=r_, t8=t8: e.max(out=t8[:, 0:8], in_=r_[:, 0, :]), [t_rt[b]], [t_top8[b]])
                V(lambda e, t8=t8: e.tensor_scalar(out=t8[:, 8:9], in0=t8[:, 0:1], scalar1=-1.0, scalar2=None, op0=ALU.mult), [t_top8[b]], [t_top8[b]])
                A(lambda e, t8=t8: e.activation(out=t8[:, 10:14], in_=t8[:, 0:4], func=AF.Exp, bias=t8[:, 8:9], accum_out=t8[:, 9:10]),
                  [t_top8[b]], [t_top8[b]])
                V(lambda e, t8=t8: e.reciprocal(t8[:, 14:15], t8[:, 9:10]), [t_top8[b]], [t_top8[b]])
                V(lambda e, r_=r_, t8=t8: e.tensor_scalar(out=r_[:, 1, :], in0=r_[:, 0, :], scalar1=t8[:, 3:4], scalar2=None, op0=ALU.is_ge),
                  [t_rt[b], t_top8[b]], [t_rt[b]])
                M(lambda e, r_=r_, blg=blg: e.matmul(psb[blg][:, 32:64], lhsT=C("ustrict"), rhs=r_[:, 1, :], start=True, stop=False), [t_rt[b], t_cst], [t_ps[blg]])
                M(lambda e, blg=blg: e.matmul(psb[blg][:, 32:64], lhsT=C("ones"), rhs=macc[:], start=False, stop=True), [t_macc, t_cst], [t_ps[blg]])
                V(lambda e, r_=r_: e.tensor_tensor(out=macc[:], in0=macc[:], in1=r_[:, 1, :], op=ALU.add), [t_macc, t_rt[b]], [t_macc])
                V(lambda e, r_=r_, blg=blg: e.tensor_copy(r_[:, 2, :], psb[blg][:, 32:64]), [t_ps[blg]], [t_rt[b]])
                V(lambda e, r_=r_: e.tensor_scalar(out=r_[:, 3, :], in0=r_[:, 2, :], scalar1=float(CAP) - 0.5, scalar2=BIGPOS, op0=ALU.is_ge, op1=ALU.mult),
                  [t_rt[b]], [t_rt[b]])
                V(lambda e, r_=r_: e.tensor_tensor(out=r_[:, 4, :], in0=r_[:, 2, :], in1=C("ecap"), op=ALU.add), [t_rt[b], t_cst], [t_rt[b]])
                V(lambda e, r_=r_: e.tensor_tensor(out=r_[:, 4, :], in0=r_[:, 4, :], in1=r_[:, 3, :], op=ALU.add), [t_rt[b]], [t_rt[b]])
                for k in range(TOPK):
                    V(lambda e, r_=r_, t8=t8, b=b, k=k: e.scalar_tensor_tensor(out=r_[:, 6, :], in0=r_[:, 0, :], scalar=t8[:, k:k + 1], in1=r_[:, 4, :],
                                                                             op0=ALU.is_equal, op1=ALU.mult, accum_out=posk[b][:, k:k + 1]),
                      [t_rt[b], t_top8[b]], [t_rt[b], t_posk[b]])
                V(lambda e, r_=r_, b=b: e.tensor_scalar(out=r_[:, 7, 0:4], in0=posk[b][:, 0:4], scalar1=BIGPOS * 0.5, scalar2=None, op0=ALU.is_lt),
                  [t_posk[b]], [t_rt[b]])
                V(lambda e, r_=r_, t8=t8, j=j: e.scalar_tensor_tensor(out=gates[:, j, :], in0=t8[:, 10:14], scalar=t8[:, 14:15], in1=r_[:, 7, 0:4],
                                                                      op0=ALU.mult, op1=ALU.mult),
                  [t_top8[b], t_rt[b]], [t_gates[j]])
                V(lambda e, b=b, j=j: e.tensor_copy(posi[:, j, :], posk[b][:, 0:4]), [t_posk[b]], [t_posi[j]])
                def scatter(j=j, bx=bx):
                    for k in range(TOPK):
                        P.dma("pool", lambda e, bx=bx, j=j, k=k: e.indirect_dma_start(
                            out=xs_d, out_offset=bass.IndirectOffsetOnAxis(ap=posi[:, j, k:k + 1], axis=0), in_=xn2b[bx][:], in_offset=None,
                            bounds_check=_bcreg(e, nc), oob_is_err=False), t_xn2b[bx], reads=[t_xn2b[bx], t_posi[j], t_zfd], writes=[t_xsd],
                            nbytes=256 * 1024, delay=_OPT["sdel"])
                pend.append(scatter)
                if len(pend) >= NBX - 1:
                    pend.pop(0)()
            for f_ in pend:
                f_()

            P.barrier(alltoks)

        if phases >= 3:
            build_phase3(locals())
        if phases >= 4:
            build_phase4(locals())

        if debug and phases >= 2:
            P.dma("sp", lambda e: e.dma_start(out=dbg["gate"], in_=gates[:].rearrange("p j k -> p (j k)")), T("dbg_g"), reads=t_gates)
            P.dma("sp", lambda e: e.dma_start(out=dbg["pos"], in_=posi[:].rearrange("p j k -> p (j k)")), T("dbg_p"), reads=t_posi)
        P.barrier(alltoks)
        stats = P.emit()
    return nc, stats


def make_in_maps(inp, ncores=8):
    cst, rows = host_consts(inp)
    wgu = inp["expert_w_gate_up"][0]
    w_g = np.ascontiguousarray(wgu[:, :, 0::2])
    w_u = np.ascontiguousarray(wgu[:, :, 1::2])
    shared = {
        "cst": cst, "rows": rows,
        "w_in": np.ascontiguousarray(inp["w_in"][0]), "w_out": np.ascontiguousarray(inp["w_out"][0]),
        "pool_w": np.ascontiguousarray(inp["pool_w"][0]), "router_w": np.ascontiguousarray(inp["router_w"][0]),
        "w_g": w_g, "w_u": w_u, "w_d": np.ascontiguousarray(inp["expert_w_down"][0]),
        "b_d": np.ascontiguousarray(inp["expert_b_down"][0]),
        "ple_gate_w": np.ascontiguousarray(inp["ple_gate_w"][0]), "ple_proj_w": np.ascontiguousarray(inp["ple_proj_w"][0]),
    }
    maps = []
    for b in range(ncores):
        m = dict(shared)
        m["x"] = np.ascontiguousarray(inp["x"][b])
        m["p"] = np.ascontiguousarray(inp["p"][0, b])
        m["pos"] = np.ascontiguousarray(inp["positions"][b].reshape(NT, 128).T.astype(np.int32))
        maps.append(m)
    return maps


def build_phase3(L):
    nc, P, T, sbuf, cst = L["nc"], L["P"], L["T"], L["sbuf"], L["cst"]
    V, A, G, M = L["V"], L["A"], L["G"], L["M"]
    psb, t_ps, t_cst, identb, t_identb = L["psb"], L["t_ps"], L["t_cst"], L["identb"], L["t_identb"]
    xs_d, ys_d, wg_d, wu_d, wd_d, bd_d = L["xs_d"], L["ys_d"], L["wg_d"], L["wu_d"], L["wd_d"], L["bd_d"]
    t_xsd, t_ysd, alltoks = L["t_xsd"], L["t_ysd"], L["alltoks"]
    n_exp = L.get("n_exp", NE)
    NSL = (CAP + 127) // 128
    HALF = CAP // 2

    def rows(i):
        return 128

    def tstart(i):
        return min(i * 128, CAP - 128)
    with ExitStack() as st:
        wgu = [sbuf(st, f"wgu{i}", [128, 8, 2048], BF16) for i in range(2)]
        wdn = [sbuf(st, f"wdn{i}", [128, 8, 1024], BF16) for i in range(2)]
        t_wgu = [T(f"wgu{i}") for i in range(2)]
        t_wdn = [T(f"wdn{i}") for i in range(2)]
        xtok = sbuf(st, "xtok", [128, NSL, D], BF16)
        t_xtok = T("xtok")
        XT = [sbuf(st, f"XT{i}", [128, 8, CAP], BF16) for i in range(2)]
        t_XT = [T(f"XT{i}") for i in range(2)]
        actT = [sbuf(st, f"actT{i}", [128, 8, CAP], BF16) for i in range(2)]
        t_actT = [T(f"actT{i}") for i in range(2)]
        NTMP = 2
        tmp = [sbuf(st, f"etmp{i}", [128, 4, HALF], F32) for i in range(NTMP)]
        t_tmp = [[T(f"etmp{i}_{k}") for k in range(4)] for i in range(NTMP)]
        NY = 4
        ysb = [sbuf(st, f"ysb3_{i}", [128, D], F32) for i in range(NY)]
        t_ysb = [T(f"ysb3_{i}") for i in range(NY)]
        bdb = [sbuf(st, f"bdb{i}", [128, D], F32) for i in range(2)]
        t_bdb = [T(f"bdb{i}") for i in range(2)]
        abg = sbuf(st, "abg", [128, 256], F32)
        t_abg = T("abg")
        ob, _ = _off["bgate"]
        ou, _ = _off["bup"]
        V(lambda e: e.tensor_scalar(out=abg[:], in0=cst[:, ob:ob + 256], scalar1=ALPHA, scalar2=None, op0=ALU.mult), [t_cst], [t_abg])
        bu1 = sbuf(st, "bu1", [128, 256], F32)
        V(lambda e: e.tensor_scalar(out=bu1[:], in0=cst[:, ou:ou + 256], scalar1=1.0, scalar2=None, op0=ALU.add), [t_cst], [t_abg])
        SIG7 = float(1.0 / (1.0 + np.exp(-ALPHA * LIMIT)))

        def load_wgu(e_idx):
            sl = e_idx % 2
            wg_v = wg_d[e_idx].rearrange("(c p) n -> p c n", p=128)
            wu_v = wu_d[e_idx].rearrange("(c p) n -> p c n", p=128)
            for hc in range(2):
                cs = slice(hc * 4, hc * 4 + 4)
                P.dma("pool", lambda e, cs=cs, sl=sl, wg_v=wg_v: e.dma_start(out=wgu[sl][:, cs, 0:1024], in_=wg_v[:, cs, :]), t_wgu[sl], writes=[t_wgu[sl]])
                P.dma("pool", lambda e, cs=cs, sl=sl, wu_v=wu_v: e.dma_start(out=wgu[sl][:, cs, 1024:2048], in_=wu_v[:, cs, :]), t_wgu[sl], writes=[t_wgu[sl]])

        def load_wd(e_idx):
            sl = e_idx % 2
            wd_v = wd_d[e_idx].rearrange("(c p) n -> p c n", p=128)
            for hc in range(2):
                cs = slice(hc * 4, hc * 4 + 4)
                P.dma("pool", lambda e, cs=cs, sl=sl, wd_v=wd_v: e.dma_start(out=wdn[sl][:, cs, :], in_=wd_v[:, cs, :]), t_wdn[sl], writes=[t_wdn[sl]])
            P.dma("sp", lambda e, sl=sl, e_idx=e_idx: e.dma_start(out=bdb[sl][:], in_=bd_d[e_idx].partition_broadcast(128)), t_bdb[sl], writes=[t_bdb[sl]])

        evac_rr = [0]

        def stage_T(e_idx):
            sl = e_idx % 2
            nfull = CAP // 128
            P.dma("sp", lambda e, e_idx=e_idx, nfull=nfull: e.dma_start(out=xtok[:, 0:nfull, :],
                                                                        in_=xs_d[e_idx * CAP:e_idx * CAP + nfull * 128, :].rearrange("(i p) d -> p i d", p=128)),
                  t_xtok, reads=[t_xsd], writes=[t_xtok])
            if CAP % 128:
                P.dma("sp", lambda e, e_idx=e_idx, nfull=nfull: e.dma_start(out=xtok[:, nfull, :], in_=xs_d[(e_idx + 1) * CAP - 128:(e_idx + 1) * CAP, :]),
                      t_xtok, reads=[t_xsd], writes=[t_xtok], nbytes=256 * 1024)
            for i in range(NSL):
                bank = 4
                pbv = psb[bank][:].bitcast(BF16)
                ri = rows(i)
                for c in range(8):
                    M(lambda e, i=i, c=c, pbv=pbv, ri=ri: e.transpose(pbv[:, c * 128:c * 128 + ri], xtok[0:ri, i, c * 128:(c + 1) * 128], identb[0:ri, 0:ri]),
                      [t_xtok, t_identb], [t_ps[bank]])
                eng = A if evac_rr[0] % 2 == 0 else V
                evac_rr[0] += 1
                if eng is A:
                    A(lambda e, i=i, sl=sl, pbv=pbv, ri=ri: e.copy(XT[sl][:, :, tstart(i):tstart(i) + ri], pbv.rearrange("p (c s) -> p c s", c=8)[:, :, 0:ri]),
                      [t_ps[bank]], [t_XT[sl]])
                else:
                    V(lambda e, i=i, sl=sl, pbv=pbv, ri=ri: e.tensor_copy(XT[sl][:, :, tstart(i):tstart(i) + ri], pbv.rearrange("p (c s) -> p c s", c=8)[:, :, 0:ri]),
                      [t_ps[bank]], [t_XT[sl]])

        cnt = [0]

        def stage_GU(e_idx):
            sl = e_idx % 2
            for jc in range(8):
                for hf in range(2):
                    pp = cnt[0] % 2
                    tb = cnt[0] % NTMP
                    cnt[0] += 1
                    bg, bu = psb[2 * pp], psb[2 * pp + 1]
                    ssl = slice(hf * HALF, (hf + 1) * HALF)
                    for c in range(8):
                        M(lambda e, sl=sl, jc=jc, c=c, bg=bg, ssl=ssl: e.matmul(bg[:, 0:HALF], lhsT=wgu[sl][:, c, jc * 128:(jc + 1) * 128], rhs=XT[sl][:, c, ssl],
                                                                             start=(c == 0), stop=(c == 7)),
                          [t_wgu[sl], t_XT[sl]], [t_ps[2 * pp]], n=HALF)
                    for c in range(8):
                        M(lambda e, sl=sl, jc=jc, c=c, bu=bu, ssl=ssl: e.matmul(bu[:, 0:HALF], lhsT=wgu[sl][:, c, 1024 + jc * 128:1024 + (jc + 1) * 128], rhs=XT[sl][:, c, ssl],
                                                                             start=(c == 0), stop=(c == 7)),
                          [t_wgu[sl], t_XT[sl]], [t_ps[2 * pp + 1]], n=HALF)
                    col = e_idx * 8 + jc
                    tm, tt = tmp[tb], t_tmp[tb]
                    A(lambda e, tm=tm, bg=bg, col=col: e.activation(out=tm[:, 0, :], in_=bg[:, 0:HALF], func=AF.Sigmoid, bias=abg[:, col:col + 1], scale=ALPHA),
                      [t_ps[2 * pp], t_abg], [tt[0]])
                    A(lambda e, tm=tm, bg=bg, col=col: e.activation(out=tm[:, 1, :], in_=bg[:, 0:HALF], func=AF.Identity, bias=cst[:, ob + col:ob + col + 1], scale=1.0),
                      [t_ps[2 * pp], t_cst], [tt[1]])
                    A(lambda e, tm=tm, bu=bu, col=col: e.activation(out=tm[:, 2, :], in_=bu[:, 0:HALF], func=AF.Identity, bias=bu1[:, col:col + 1], scale=1.0),
                      [t_ps[2 * pp + 1], t_abg], [tt[2]])
                    V(lambda e, tm=tm: e.tensor_scalar(out=tm[:, 2, :], in0=tm[:, 2, :], scalar1=LIMIT + 1.0, scalar2=-LIMIT + 1.0, op0=ALU.min, op1=ALU.max), [tt[2]], [tt[2]])
                    V(lambda e, tm=tm: e.scalar_tensor_tensor(out=tm[:, 3, :], in0=tm[:, 1, :], scalar=LIMIT, in1=tm[:, 2, :], op0=ALU.min, op1=ALU.mult),
                      [tt[1], tt[2]], [tt[3]])
                    V(lambda e, tm=tm, sl=sl, jc=jc, ssl=ssl: e.scalar_tensor_tensor(out=actT[sl][:, jc, ssl], in0=tm[:, 0, :], scalar=SIG7, in1=tm[:, 3, :],
                                                                                  op0=ALU.min, op1=ALU.mult), [tt[0], tt[3]], [t_actT[sl]])

        ycnt = [0]
        ybank = [0]

        def stage_down(e_idx):
            sl = e_idx % 2
            for i in range(NSL):
                yb = ycnt[0] % NY
                ycnt[0] += 1
                ri = rows(i)
                for nb in range(2):
                    bk = 5 + ybank[0] % 3
                    ybank[0] += 1
                    for jc in range(8):
                        M(lambda e, sl=sl, i=i, nb=nb, jc=jc, bk=bk, ri=ri: e.matmul(psb[bk][0:ri, :], lhsT=actT[sl][:, jc, tstart(i):tstart(i) + ri], rhs=wdn[sl][:, jc, nb * 512:(nb + 1) * 512],
                                                                              start=(jc == 0), stop=(jc == 7)),
                          [t_actT[sl], t_wdn[sl]], [t_ps[bk]], n=512)
                    V(lambda e, yb=yb, nb=nb, sl=sl, bk=bk, ri=ri: e.tensor_tensor(out=ysb[yb][0:ri, nb * 512:(nb + 1) * 512], in0=psb[bk][0:ri, :], in1=bdb[sl][0:ri, nb * 512:(nb + 1) * 512], op=ALU.add),
                      [t_ps[bk], t_bdb[sl]], [t_ysb[yb]], n=512)
                ov = i * 128 - tstart(i)
                r0 = e_idx * CAP + i * 128
                P.dma("sp", lambda e, yb=yb, r0=r0, ov=ov: e.dma_start(out=ys_d[r0:r0 + 128 - ov, :], in_=ysb[yb][ov:128, :]), t_ysb[yb], reads=[t_ysb[yb]], writes=[t_ysd],
                      nbytes=(128 - ov) * 4096)

        load_wgu(0)
        load_wd(0)
        load_wgu(1)
        stage_T(0)
        if n_exp > 1:
            stage_T(1)
        stage_GU(0)
        for s_ in range(n_exp):
            if s_ + 2 < n_exp:
                load_wgu(s_ + 2)
            if s_ + 1 < n_exp:
                load_wd(s_ + 1)
            if s_ + 2 < n_exp:
                stage_T(s_ + 2)
            if s_ + 1 < n_exp:
                stage_GU(s_ + 1)
            stage_down(s_)
        P.barrier(alltoks)


def build_phase4(L):
    nc, P, T, sbuf, cst = L["nc"], L["P"], L["T"], L["sbuf"], L["cst"]
    V, A, G, M = L["V"], L["A"], L["G"], L["M"]
    psb, t_ps, t_cst, identb, t_identb = L["psb"], L["t_ps"], L["t_cst"], L["identb"], L["t_identb"]
    ys_d, h_d, p_d, out_d, pgw_d, ppw_d, rows_d = L["ys_d"], L["h_d"], L["p_d"], L["out_d"], L["pgw_d"], L["ppw_d"], L["rows_d"]
    t_ysd, t_hd, alltoks = L["t_ysd"], L["t_hd"], L["alltoks"]
    gates, posi, t_gates, t_posi, nhalf, t_nhalf = L["gates"], L["posi"], L["t_gates"], L["t_posi"], L["nhalf"], L["t_nhalf"]
    nt, dbg, debug = L["nt"], L["dbg"], L["debug"]
    ident = cst[:, _off["ident"][0]:_off["ident"][0] + 128]
    with ExitStack() as st:
        pgw = sbuf(st, "pgw", [128, 8, D], BF16)
        t_pgw = T("pgw")
        ppw = sbuf(st, "ppw", [128, 2, D], BF16)
        t_ppw = T("ppw")
        fnw = sbuf(st, "fnw", [128, D], F32)
        t_fnw = T("fnw")
        stg = [sbuf(st, f"stg4_{i}", [128, D], F32) for i in range(2)]
        t_stg = [T(f"stg4_{i}") for i in range(2)]
        pgw_v = pgw_d.rearrange("(c p) n -> p c n", p=128)
        on, _ = _off["nple"]
        for c in range(8):
            s_ = c % 2
            P.dma("sp", lambda e, c=c, s_=s_: e.dma_start(out=stg[s_][:], in_=pgw_v[:, c, :]), t_stg[s_], writes=[t_stg[s_]])
            if c % 2 == 0:
                V(lambda e, c=c, s_=s_: e.tensor_scalar(out=pgw[:, c, :], in0=stg[s_][:], scalar1=cst[:, on + c:on + c + 1], scalar2=None, op0=ALU.mult),
                  [t_stg[s_], t_cst], [t_pgw], n=1024)
            else:
                A(lambda e, c=c, s_=s_: e.activation(out=pgw[:, c, :], in_=stg[s_][:], func=AF.Copy, scale=cst[:, on + c:on + c + 1]),
                  [t_stg[s_], t_cst], [t_pgw], n=1024)
        P.dma("pool", lambda e: e.dma_start(out=ppw[:], in_=ppw_d.rearrange("(c p) n -> p c n", p=128)), t_ppw, writes=[t_ppw])
        fo, fw = _roff["fnw"]
        P.dma("sp", lambda e: e.dma_start(out=fnw[:], in_=rows_d[0, fo:fo + fw].partition_broadcast(128)), t_fnw, writes=[t_fnw])

        NB = 4
        bctr = [0]

        def nxt():
            v = bctr[0] % 8
            bctr[0] += 1
            return v

        def mk(name, shape, dtype, n=NB):
            return [sbuf(st, f"{name}{i}", shape, dtype) for i in range(n)], [T(f"{name}{i}") for i in range(n)]
        hb_, t_hb = mk("h4", [128, D], F32)
        pt_, t_pt = mk("p4", [128, PLE], F32)
        yk, t_yk = mk("yk", [128, 4, D], F32)
        junk, t_junk = mk("junk4", [128, D], BF16, 1)
        sq, t_sq = mk("sq4", [128, 8], F32)
        xn3, t_xn3 = mk("xn3", [128, D], BF16)
        xn3T, t_xn3T = mk("xn3T", [128, 8, 128], BF16)
        pT, t_pT = mk("pT", [128, 2, 128], BF16)
        sgm, t_sgm = mk("sgm", [128, D], F32)
        ot, t_ot = mk("ot", [128, D], F32)
        h3, _unused = mk("h3_", [128, D], F32)
        t_sgmh = [[T(f"sgmh{i}_{k}") for k in range(2)] for i in range(NB)]
        t_h3h = [[T(f"h3h{i}_{k}") for k in range(2)] for i in range(NB)]
        for i_ in range(NB):
            G(lambda e, i_=i_: e.memset(yk[i_][:], 0.0), (), [t_yk[i_]])

        for j in range(nt):
            b = j % NB
            P.dma("sp", lambda e, j=j, b=b: e.dma_start(out=hb_[b][:], in_=h_d[j * 128:(j + 1) * 128, :]), t_hb[b], reads=[t_hd[j]], writes=[t_hb[b]])
            P.dma("sp", lambda e, j=j, b=b: e.dma_start(out=pt_[b][:], in_=p_d[j * 128:(j + 1) * 128, :]), t_pt[b], writes=[t_pt[b]])
            for k in range(TOPK):
                P.dma("pool", lambda e, j=j, b=b, k=k: e.indirect_dma_start(
                    out=yk[b][:, k, :], out_offset=None, in_=ys_d, in_offset=bass.IndirectOffsetOnAxis(ap=posi[:, j, k:k + 1], axis=0),
                    bounds_check=_bcreg(e, nc), oob_is_err=False), t_yk[b], reads=[t_ysd, t_posi[j]], writes=[t_yk[b]], nbytes=int(_OPT["gx"] * 150e3))
            for k in range(TOPK):
                V(lambda e, j=j, b=b, k=k: e.scalar_tensor_tensor(out=hb_[b][:], in0=yk[b][:, k, :], scalar=gates[:, j, k:k + 1], in1=hb_[b][:],
                                                                 op0=ALU.mult, op1=ALU.add), [t_yk[b], t_gates[j], t_hb[b]], [t_hb[b]])
            if debug:
                P.dma("sp", lambda e, j=j, b=b: e.dma_start(out=dbg["h2"][j * 128:(j + 1) * 128, :], in_=hb_[b][:]), L["t_dbgs"][8 + j % 8], reads=[t_hb[b]])
            A(lambda e, b=b: e.activation(out=junk[0][:], in_=hb_[b][:], func=AF.Square, accum_out=sq[b][:, 0:1]), [t_hb[b]], [t_junk[0], t_sq[b]])
            V(lambda e, b=b: e.tensor_scalar(out=sq[b][:, 1:2], in0=sq[b][:, 0:1], scalar1=1.0 / D, scalar2=EPS, op0=ALU.mult, op1=ALU.add), [t_sq[b]], [t_sq[b]])
            G(lambda e, b=b: e.tensor_tensor(out=sq[b][:, 2:3], in0=sq[b][:, 1:2], in1=nhalf[:, 0:1], op=ALU.pow), [t_sq[b], t_nhalf], [t_sq[b]])
            A(lambda e, b=b: e.activation(out=xn3[b][:], in_=hb_[b][:], func=AF.Copy, scale=sq[b][:, 2:3]), [t_hb[b], t_sq[b]], [t_xn3[b]])
            b0_, b1_ = nxt(), nxt()
            bg_ = [nxt(), nxt()]
            bp_ = [nxt(), nxt()]
            pb0 = psb[b0_][:].bitcast(BF16)
            for c in range(8):
                M(lambda e, b=b, c=c, pb0=pb0: e.transpose(pb0[:, c * 128:(c + 1) * 128], xn3[b][:, c * 128:(c + 1) * 128], identb[:]),
                  [t_xn3[b], t_identb], [t_ps[b0_]])
            A(lambda e, b=b, pb0=pb0: e.copy(xn3T[b][:], pb0.rearrange("p (c t) -> p c t", c=8)), [t_ps[b0_]], [t_xn3T[b]], n=1024)
            for c in range(2):
                M(lambda e, b=b, c=c, b1_=b1_: e.transpose(psb[b1_][:, c * 128:(c + 1) * 128], pt_[b][:, c * 128:(c + 1) * 128], ident), [t_pt[b], t_cst], [t_ps[b1_]], n=400)
            A(lambda e, b=b, b1_=b1_: e.copy(pT[b][:], psb[b1_][:, 0:256].rearrange("p (c t) -> p c t", c=2)), [t_ps[b1_]], [t_pT[b]], n=256)
            for nb in range(2):
                for c in range(8):
                    M(lambda e, b=b, nb=nb, c=c, bg_=bg_: e.matmul(psb[bg_[nb]][:], lhsT=xn3T[b][:, c, :], rhs=pgw[:, c, nb * 512:(nb + 1) * 512], start=(c == 0), stop=(c == 7)),
                      [t_xn3T[b], t_pgw], [t_ps[bg_[nb]]], n=600)
            for nb in range(2):
                for c in range(2):
                    M(lambda e, b=b, nb=nb, c=c, bp_=bp_: e.matmul(psb[bp_[nb]][:], lhsT=pT[b][:, c, :], rhs=ppw[:, c, nb * 512:(nb + 1) * 512], start=(c == 0), stop=(c == 1)),
                      [t_pT[b], t_ppw], [t_ps[bp_[nb]]], n=600)
            for nb in range(2):
                hs = slice(nb * 512, (nb + 1) * 512)
                A(lambda e, b=b, nb=nb, bg_=bg_, hs=hs: e.activation(out=sgm[b][:, hs], in_=psb[bg_[nb]][:], func=AF.Sigmoid), [t_ps[bg_[nb]]], [t_sgmh[b][nb]], n=512)
                V(lambda e, b=b, nb=nb, bp_=bp_, hs=hs: e.tensor_tensor(out=sgm[b][:, hs], in0=sgm[b][:, hs], in1=psb[bp_[nb]][:], op=ALU.mult),
                  [t_sgmh[b][nb], t_ps[bp_[nb]]], [t_sgmh[b][nb]], n=512)
                V(lambda e, b=b, hs=hs: e.tensor_tensor(out=h3[b][:, hs], in0=hb_[b][:, hs], in1=sgm[b][:, hs], op=ALU.add), [t_hb[b], t_sgmh[b][nb]], [t_h3h[b][nb]], n=512)
            A(lambda e, b=b: e.activation(out=junk[0][:], in_=h3[b][:], func=AF.Square, accum_out=sq[b][:, 4:5]), t_h3h[b], [t_junk[0], t_sq[b]])
            V(lambda e, b=b: e.tensor_scalar(out=sq[b][:, 5:6], in0=sq[b][:, 4:5], scalar1=1.0 / D, scalar2=EPS, op0=ALU.mult, op1=ALU.add), [t_sq[b]], [t_sq[b]])
            G(lambda e, b=b: e.tensor_tensor(out=sq[b][:, 6:7], in0=sq[b][:, 5:6], in1=nhalf[:, 0:1], op=ALU.pow), [t_sq[b], t_nhalf], [t_sq[b]])
            A(lambda e, b=b: e.activation(out=ot[b][:], in_=h3[b][:], func=AF.Copy, scale=sq[b][:, 6:7]), t_h3h[b] + [t_sq[b]], [t_ot[b]], n=1024)
            G(lambda e, b=b: e.tensor_tensor(out=ot[b][:], in0=ot[b][:], in1=fnw[:], op=ALU.mult), [t_ot[b], t_fnw], [t_ot[b]], n=1024)
            P.dma("sp", lambda e, j=j, b=b: e.dma_start(out=out_d[j * 128:(j + 1) * 128, :], in_=ot[b][:]), t_ot[b], reads=[t_ot[b]])
        P.barrier(alltoks)


_CACHE = {}


def kernel(**inputs):
    inp = {k: np.asarray(v) for k, v in inputs.items()}
    if "nc" not in _CACHE:
        _CACHE["nc"] = build()[0]
    nc = _CACHE["nc"]
    maps = make_in_maps(inp, ncores=8)
    res = run_bass_kernel_spmd(nc, maps, core_ids=list(range(8)))
    out = np.stack([np.asarray(r["out"], dtype=np.float32) for r in res.results], axis=0)
    return out.reshape(8, SEQ, D)
```

```python
import numpy as np
from contextlib import ExitStack
import concourse.bass as bass
import concourse.mybir as mybir
from concourse.bass_utils import run_bass_kernel_spmd

F32 = mybir.dt.float32
BF16 = mybir.dt.bfloat16
I32 = mybir.dt.int32
U32 = mybir.dt.uint32
AF = mybir.ActivationFunctionType
ALU = mybir.AluOpType
AX = mybir.AxisListType

ENGS = ("pe", "dve", "act", "pool", "sp")


class Tok:
    __slots__ = ("name", "w", "r", "sem", "cnt", "multi")

    def __init__(self, name):
        self.name = name
        self.multi = False
        self.w = None
        self.r = {}
        self.sem = None
        self.cnt = 0


class Op:
    __slots__ = ("eng", "fn", "deps", "signal", "sigval", "dma", "idx", "dur", "xfer", "start", "fin", "prev_dma", "delay")

    def __init__(self, eng, fn, deps):
        self.eng = eng
        self.fn = fn
        self.deps = deps
        self.signal = False
        self.sigval = None
        self.dma = None
        self.dur = 0.5
        self.xfer = 0.0
        self.start = 0.0
        self.fin = 0.0
        self.prev_dma = None
        self.delay = 0.0


class DmaDep:
    __slots__ = ("tok", "val", "op")

    def __init__(self, tok, val, op=None):
        self.tok = tok
        self.val = val
        self.op = op


import os as _os
_OPT = {"est": int(_os.environ.get("KEST", "1")), "slack": float(_os.environ.get("KSLACK", "0.45")), "sdel": float(_os.environ.get("KSDEL", "25")), "crit": int(_os.environ.get("KCRIT", "1")), "gx": float(_os.environ.get("KGX", "3.5")), "run": int(_os.environ.get("KRUN", "2500"))}


class _Probe:
    def __getattr__(self, name):
        def f(*a, **k):
            self.__dict__["call"] = (name, a, k)
            return self
        return f


def _estimate(eng, fn):
    pr = _Probe()
    try:
        fn(pr)
        name, a, k = pr.call
    except Exception:
        return None

    def free(ap):
        n = 1
        for d in list(ap.shape)[1:]:
            n *= int(d)
        return n
    try:
        if eng == "pe":
            if name == "matmul":
                rhs = k.get("rhs", a[2] if len(a) > 2 else None)
                cols = free(rhs)
                mult = 4.0 if rhs.dtype == F32 else 1.0
                return 0.05 + 1.15 * mult * max(cols, 64) / 2400.0
            if name == "transpose":
                src = k.get("in_", a[1] if len(a) > 1 else None)
                mult = 3.0 if src.dtype == F32 else 1.0
                return 0.05 + mult * 128 / 2400.0 * 1.2
            return 0.1
        out = k.get("out", a[0] if a else None)
        n = free(out)
        if eng == "dve":
            return 0.12 + n / 960.0
        if eng == "act":
            return 0.22 + n / 1150.0
        if eng == "pool":
            if name == "tensor_scalar":
                return 0.5 + n / 70.0
            return 0.3 + n / 560.0
    except Exception:
        return None
    return None


class Prog:
    def __init__(self, nc, stack):
        self.nc = nc
        self.stack = stack
        self.ops = {e: [] for e in ENGS}
        self.nsem = 0
        self.limit = None
        self.count = 0
        self.segs = [[]]
        self.last_dma = {}
        self.sched = True
        self.slack = _OPT["slack"]

    def tok(self, name):
        return Tok(name)

    def toks(self, name, n):
        return [Tok(f"{name}{i}") for i in range(n)]

    def _collect(self, eng, reads, writes, dma_semtok=None):
        deps = []
        for t in reads:
            if t.multi:
                deps.extend(t.w.values())
            elif t.w is not None:
                deps.append(t.w)
        for t in writes:
            if t.multi:
                deps.extend(t.r.values())
                continue
            if t.w is not None:
                w = t.w
                skip = False
                if isinstance(w, DmaDep) and dma_semtok is not None and w.tok is dma_semtok:
                    skip = True
                if not skip:
                    deps.append(w)
            deps.extend(t.r.values())
        return deps

    def _commit(self, dep, key, reads, writes):
        for t in reads:
            if isinstance(dep, Op):
                t.r[id(dep)] = dep
            else:
                t.r[key] = dep
        for t in writes:
            if t.multi:
                t.w[key] = dep
                continue
            t.w = dep
            t.r = {}

    def op(self, eng, fn, reads=(), writes=(), n=None):
        self.count += 1
        if self.limit is not None and self.count > self.limit:
            return None
        deps = self._collect(eng, reads, writes)
        o = Op(eng, fn, deps)
        if n is not None:
            if eng == "pe":
                o.dur = 0.03 + n / 2400.0
            elif eng == "dve":
                o.dur = 0.12 + n / 960.0
            elif eng == "act":
                o.dur = 0.22 + n / 1200.0
            elif eng == "pool":
                o.dur = 0.25 + n / 450.0
        else:
            est = _estimate(eng, fn) if _OPT["est"] else None
            o.dur = est if est is not None else {"pe": 0.12, "dve": 0.5, "act": 0.6, "pool": 1.15, "sp": 0.1}[eng]
        self.ops[eng].append(o)
        self.segs[-1].append(o)
        self._commit(o, eng, reads, writes)
        return o

    def dma(self, eng, fn, semtok, reads=(), writes=(), nbytes=None, delay=0.0):
        self.count += 1
        if self.limit is not None and self.count > self.limit:
            return None
        deps = self._collect(eng, reads, writes, dma_semtok=semtok)
        o = Op(eng, fn, deps)
        if semtok.sem is None:
            semtok.sem = self.stack.enter_context(self.nc.semaphore(f"d{self.nsem}_{semtok.name}"))
            self.nsem += 1
        semtok.cnt += 16
        o.dma = DmaDep(semtok, semtok.cnt, o)
        o.dur = 1.2 if eng == "pool" else 0.15
        o.xfer = 2.0 + (nbytes or 0) / 150e3
        o.delay = delay
        o.prev_dma = self.last_dma.get(id(semtok))
        self.last_dma[id(semtok)] = o
        self.ops[eng].append(o)
        self.segs[-1].append(o)
        self._commit(o.dma, ("dma", id(semtok)), reads, writes)
        return o

    def wait_all(self, eng, toks):
        deps = self._collect(eng, (), toks)
        o = Op(eng, None, deps)
        o.dur = 0.05
        self.ops[eng].append(o)
        self.segs[-1].append(o)
        return o

    def barrier(self, all_toks):
        for e in ENGS:
            self.wait_all(e, all_toks)
        self.segs.append([])

    def schedule(self, runahead=None):
        runahead = runahead or _OPT["run"]
        new_ops = {e: [] for e in ENGS}
        t_base = 0.0
        for seg in self.segs:
            if not seg:
                continue
            n = len(seg)
            pos = {id(o): i for i, o in enumerate(seg)}
            succ = [[] for _ in range(n)]
            ndep = [0] * n
            preds = [None] * n
            for i, o in enumerate(seg):
                ps_ = []
                for d in o.deps:
                    p = d if isinstance(d, Op) else d.op
                    k = pos.get(id(p))
                    if k is None:
                        continue
                    ps_.append((k, isinstance(d, Op)))
                if o.prev_dma is not None:
                    k = pos.get(id(o.prev_dma))
                    if k is not None:
                        ps_.append((k, None))
                preds[i] = ps_
                ks = set(k for k, _ in ps_)
                ndep[i] = len(ks)
                for k in ks:
                    succ[k].append(i)
            bott = [0.0] * n
            for i in range(n - 1, -1, -1):
                m = 0.0
                for k in succ[i]:
                    if bott[k] > m:
                        m = bott[k]
                bott[i] = seg[i].dur + seg[i].xfer + m
            ready = {e: [] for e in ENGS}
            free = {e: t_base for e in ENGS}
            scheduled = [False] * n
            low = 0

            def make_ready(i):
                o = seg[i]
                t = t_base
                for k, kind in preds[i]:
                    p = seg[k]
                    if kind is None:
                        ft = p.start
                    elif kind:
                        ft = p.fin
                    else:
                        ft = p.fin + p.xfer
                    if kind is not None and p.eng != o.eng:
                        ft += 0.06
                    if ft > t:
                        t = ft
                ready[o.eng].append((t + o.delay, i))

            for i in range(n):
                if ndep[i] == 0:
                    make_ready(i)
            nleft = n
            while nleft > 0:
                while low < n and scheduled[low]:
                    low += 1
                lim = low + runahead
                best = None
                cands = []
                tmin = None
                for e in ENGS:
                    fr = free[e]
                    for (rt, i) in ready[e]:
                        if i >= lim:
                            continue
                        st_ = rt if rt > fr else fr
                        cands.append((st_, i, e, rt))
                        if tmin is None or st_ < tmin:
                            tmin = st_
                if cands:
                    slack = self.slack
                    if _OPT["crit"]:
                        best = max((c for c in cands if c[0] <= tmin + slack), key=lambda c: (bott[c[1]], -c[1]))
                    else:
                        best = min((c for c in cands if c[0] <= tmin + slack), key=lambda c: c[1])
                if best is None:
                    for e in ENGS:
                        for (rt, i) in ready[e]:
                            st_ = max(rt, free[e])
                            if best is None or i < best[1]:
                                best = (st_, i, e, rt)
                assert best is not None, "scheduler stuck"
                st_, i, e, rt = best
                ready[e].remove((rt, i))
                o = seg[i]
                o.start = st_
                o.fin = st_ + o.dur
                free[e] = o.fin
                scheduled[i] = True
                new_ops[e].append(o)
                nleft -= 1
                for k in succ[i]:
                    ndep[k] -= 1
                    if ndep[k] == 0:
                        make_ready(k)
            t_base = max(max(free.values()), max((o.fin + o.xfer) for o in seg))
        self.ops = new_ops
        self.est_total = t_base

    def emit(self):
        nc = self.nc
        if self.sched:
            self.schedule()
        for e in ENGS:
            for o in self.ops[e]:
                for d in o.deps:
                    if isinstance(d, Op) and not (d.eng == "pe" and e == "pe"):
                        d.signal = True
        esem = {}
        for e in ENGS:
            n = 0
            for o in self.ops[e]:
                if o.signal:
                    n += 1
                    o.sigval = n
            esem[e] = self.stack.enter_context(nc.semaphore(f"eng_{e}"))
        self.esem = esem
        stats = {}
        with nc.Block() as block:
            def run(ename, engine):
                waited = {}
                nw = 0
                for o in self.ops[ename]:
                    for d in o.deps:
                        if isinstance(d, Op):
                            if d.eng == "pe" and ename == "pe":
                                continue
                            sem, val, key = esem[d.eng], d.sigval, d.eng
                        else:
                            sem, val, key = d.tok.sem, d.val, id(d.tok)
                        if waited.get(key, 0) >= val:
                            continue
                        waited[key] = val
                        engine.wait_ge(sem, val)
                        nw += 1
                    if o.fn is None:
                        continue
                    inst = o.fn(engine)
                    if o.dma is not None:
                        inst.then_inc(o.dma.tok.sem, 16)
                    elif o.signal:
                        inst.then_inc(esem[ename], 1)
                stats[ename] = (len(self.ops[ename]), nw)

            @block.tensor
            def _(e):
                run("pe", e)

            @block.vector
            def _(e):
                run("dve", e)

            @block.scalar
            def _(e):
                run("act", e)

            @block.gpsimd
            def _(e):
                run("pool", e)

            @block.sync
            def _(e):
                run("sp", e)
        self.stats = stats
        return stats


D = 1024
SEQ = 4096
NT = SEQ // 128
H = 8
DH = 64
NE = 32
TOPK = 4
CAP = 640
NSLOT = NE * CAP
PLE = 256
EPS = 1e-5
LIMIT = 7.0
ALPHA = 1.702
BIGPOS = 1.0e6
WINS = (2, 4, 8, 16)

_off = {}
_n = 0
for _name, _w in (("ident", 128), ("mask", 128), ("ustrict", 128), ("ones", 128), ("invf", 64),
                  ("xiq", 8), ("xik", 8), ("gc", 512), ("bands", 12 * 128), ("ecap", 32),
                  ("nmix", 8), ("nple", 8), ("pscale", 4), ("bgate", 256), ("bup", 256)):
    _off[_name] = (_n, _w)
    _n += _w
CST_N = _n
_roff = {}
_n = 0
for _name, _w in (("gnw", 512), ("nmoe", 1024), ("rb", 32), ("fnw", 1024)):
    _roff[_name] = (_n, _w)
    _n += _w
ROW_N = _n


def host_consts(inp):
    c = np.zeros((128, CST_N), np.float32)

    def put(name, arr):
        o, w = _off[name]
        c[:, o:o + w] = np.asarray(arr, np.float32).reshape(128, w)

    idx = np.arange(128)
    put("ident", np.eye(128))
    put("mask", (idx[None, :] >= idx[:, None]).astype(np.float32))
    put("ustrict", (idx[:, None] < idx[None, :]).astype(np.float32))
    put("ones", np.ones((128, 128)))
    half = DH // 2
    invf = (10000.0 ** (-(np.arange(half, dtype=np.float32)) / np.float32(half))).astype(np.float32)
    put("invf", np.tile(np.concatenate([invf, invf])[None, :], (128, 1)))
    lg = np.log1p(-np.power(2.0, -5.0 - np.arange(H, dtype=np.float64)))
    cpos = (idx + 1.0)[:, None]
    put("xiq", np.exp(cpos * lg[None, :]))
    put("xik", np.exp(-cpos * lg[None, :]) * (DH ** -0.5))
    gc = np.zeros((128, 512))
    for p in range(128):
        for h in range(H):
            if p // 64 == h % 2:
                gc[p, h * 64:(h + 1) * 64] = np.exp(128.0 * lg[h])
    put("gc", gc)
    bands = np.zeros((128, 12, 128))
    for g, w in enumerate(WINS):
        tp = idx[:, None]
        t = idx[None, :]
        inwin = (tp <= t) & (tp > t - w)
        bands[:, 3 * g + 0, :] = inwin / float(w) - (tp == t)
        cnt0 = np.minimum(t + 1.0, float(w))
        bands[:, 3 * g + 1, :] = inwin / cnt0 - (tp == t)
        bands[:, 3 * g + 2, :] = (tp >= 129 + t - w) / float(w)
    put("bands", bands)
    put("ecap", np.tile((np.arange(NE) * CAP)[None, :], (128, 1)))
    put("nmix", inp["norm_mix_w"][0].reshape(8, 128).T)
    put("nple", inp["norm_ple_w"][0].reshape(8, 128).T)
    put("pscale", inp["pool_scale"][0].reshape(4, 128).T)
    bgu = inp["expert_b_gate_up"][0]
    put("bgate", bgu[:, 0::2].reshape(NE, 8, 128).transpose(2, 0, 1))
    put("bup", bgu[:, 1::2].reshape(NE, 8, 128).transpose(2, 0, 1))
    r = np.zeros((1, ROW_N), np.float32)
    for name, v in (("gnw", inp["ret_gn_w"][0]), ("nmoe", inp["norm_moe_w"][0]),
                    ("rb", inp["router_b"][0]), ("fnw", inp["final_norm_w"])):
        o, w = _roff[name]
        r[0, o:o + w] = v
    return c, r


def _bc(ap, shape):
    return ap.broadcast_to(shape)


_BCREG = {}


def _bcreg(e, nc):
    if _BCREG.get("nc") is not nc:
        r = e.alloc_register("slot_bound")
        e.reg_mov(r, NSLOT - 1)
        _BCREG["nc"] = nc
        _BCREG["r"] = r
    return _BCREG["r"]


class Ctx:
    pass


def build(nt=NT, phases=4, debug=False, limit=None, n_exp=NE, sched=True):
    nc = bass.Bass("TRN2", target_bir_lowering=False)
    dt = lambda name, shape, dtype, kind: nc.dram_tensor(name, shape, dtype, kind=kind).ap()
    x_d = dt("x", [SEQ, D], F32, "ExternalInput")
    p_d = dt("p", [SEQ, PLE], F32, "ExternalInput")
    pos_d = dt("pos", [128, NT], I32, "ExternalInput")
    cst_d = dt("cst", [128, CST_N], F32, "ExternalInput")
    rows_d = dt("rows", [1, ROW_N], F32, "ExternalInput")
    win_d = dt("w_in", [D, 2560], F32, "ExternalInput")
    wout_d = dt("w_out", [D, D], F32, "ExternalInput")
    poolw_d = dt("pool_w", [4, 128, 128], F32, "ExternalInput")
    rw_d = dt("router_w", [D, NE], F32, "ExternalInput")
    wg_d = dt("w_g", [NE, D, D], F32, "ExternalInput")
    wu_d = dt("w_u", [NE, D, D], F32, "ExternalInput")
    wd_d = dt("w_d", [NE, D, D], F32, "ExternalInput")
    bd_d = dt("b_d", [NE, D], F32, "ExternalInput")
    pgw_d = dt("ple_gate_w", [D, D], F32, "ExternalInput")
    ppw_d = dt("ple_proj_w", [PLE, D], F32, "ExternalInput")
    out_d = dt("out", [SEQ, D], F32, "ExternalOutput")
    xs_d = nc.dram_tensor("xs_scr", [NSLOT, D], BF16).ap()
    ys_d = nc.dram_tensor("ys_scr", [NSLOT, D], F32).ap()
    h_d = nc.dram_tensor("h_scr", [SEQ, D], F32).ap()
    dbg = {}
    if debug:
        dbg["h1"] = dt("dbg_h1", [SEQ, D], F32, "ExternalOutput")
        dbg["gate"] = dt("dbg_gate", [128, NT * 4], F32, "ExternalOutput")
        dbg["pos"] = dt("dbg_pos", [128, NT * 4], I32, "ExternalOutput")
        dbg["h2"] = dt("dbg_h2", [SEQ, D], F32, "ExternalOutput")

    with ExitStack() as gst:
        P = Prog(nc, gst)
        P.limit = limit
        P.sched = sched
        alltoks = []

        def T(name):
            t = P.tok(name)
            alltoks.append(t)
            return t

        def sbuf(st, name, shape, dtype):
            return st.enter_context(nc.sbuf_tensor("s_" + name, shape, dtype))

        V = lambda fn, r=(), w=(), n=None: P.op("dve", fn, r, w, n)
        A = lambda fn, r=(), w=(), n=None: P.op("act", fn, r, w, n)
        G = lambda fn, r=(), w=(), n=None: P.op("pool", fn, r, w, n)
        M = lambda fn, r=(), w=(), n=None: P.op("pe", fn, r, w, n)

        cst = sbuf(gst, "cst", [128, CST_N], F32)
        t_cst = T("cst")
        identb = sbuf(gst, "identb", [128, 128], BF16)
        t_identb = T("identb")
        gates = sbuf(gst, "gates", [128, NT, 4], F32)
        posi = sbuf(gst, "posi", [128, NT, 4], I32)
        t_gates = [T(f"gates{j}") for j in range(NT)]
        t_posi = [T(f"posi{j}") for j in range(NT)]
        nhalf = sbuf(gst, "nhalf", [128, 8], F32)
        t_nhalf = T("nhalf")
        wgu_buf, wd_buf, t_wgu, t_wd = [], [], [], []
        psb = [gst.enter_context(nc.psum_tensor(f"ps{i}", [128, 512], F32)) for i in range(8)]
        t_ps = [T(f"ps{i}") for i in range(8)]

        def C(name):
            o, w = _off[name]
            return cst[:, o:o + w]

        t_hd = [T(f"hd{j}") for j in range(NT)]
        t_dbgs = [T(f"dbgs{i}") for i in range(16)] if debug else []
        t_xsd = T("xsd")
        t_xsd.multi = True
        t_xsd.w = {}
        t_zfd = T("zfd")
        t_ysd = T("ysd")
        t_ysd.multi = True
        t_ysd.w = {}

        P.dma("sp", lambda e: e.dma_start(out=cst[:], in_=cst_d), t_cst, writes=[t_cst])
        V(lambda e: e.tensor_copy(identb[:], C("ident")), [t_cst], [t_identb])
        G(lambda e: e.memset(nhalf[:], -0.5), (), [t_nhalf])
        if debug:
            G(lambda e: e.memset(gates[:], 0.0), (), t_gates)
            G(lambda e: e.memset(posi[:], 0), (), t_posi)
        ident = C("ident")

        def load_expert(e_idx, slot):
            wg_v = wg_d[e_idx].rearrange("(c p) n -> p c n", p=128)
            wu_v = wu_d[e_idx].rearrange("(c p) n -> p c n", p=128)
            wd_v = wd_d[e_idx].rearrange("(c p) n -> p c n", p=128)
            for hc in range(2):
                cs = slice(hc * 4, hc * 4 + 4)
                P.dma("pool", lambda e, cs=cs: e.dma_start(out=wgu_buf[slot][:, cs, 0:1024], in_=wg_v[:, cs, :]),
                      t_wgu[slot], writes=[t_wgu[slot]])
                P.dma("pool", lambda e, cs=cs: e.dma_start(out=wgu_buf[slot][:, cs, 1024:2048], in_=wu_v[:, cs, :]),
                      t_wgu[slot], writes=[t_wgu[slot]])
                P.dma("pool", lambda e, cs=cs: e.dma_start(out=wd_buf[slot][:, cs, :], in_=wd_v[:, cs, :]),
                      t_wd[slot], writes=[t_wd[slot]])

        with ExitStack() as st:
            RN1 = _roff["fnw"][0]
            rows = sbuf(st, "rows", [128, RN1], F32)
            t_rows = T("rows")
            P.dma("sp", lambda e: e.dma_start(out=rows[:], in_=rows_d[0, 0:RN1].partition_broadcast(128)), t_rows, writes=[t_rows])

            def R(name):
                o, w = _roff[name]
                return rows[:, o:o + w]

            w_in = sbuf(st, "w_in", [128, 8, 2560], BF16)
            t_win = T("w_in")
            w_out = sbuf(st, "w_out", [128, 8, 1024], BF16)
            t_wout = T("w_out")
            poolw = sbuf(st, "poolw", [128, 4, 128], BF16)
            t_poolw = T("poolw")
            rw = sbuf(st, "rw", [128, 8, NE], F32)
            t_rw = T("rw")
            posi_t = sbuf(st, "pos_i", [128, NT], I32)
            posf = sbuf(st, "pos_f", [128, NT], F32)
            CC = sbuf(st, "CC", [128, NT, 64], F32)
            SS = sbuf(st, "SS", [128, NT, 64], F32)
            st_setup = ExitStack()
            ang = sbuf(st_setup, "ang", [128, NT, 64], F32)
            tmpa = sbuf(st_setup, "tmpa", [128, NT, 64], F32)
            stage = [sbuf(st_setup, f"stage{i}", [128, 1280], F32) for i in range(2)]
            t_stage = [T(f"stage{i}") for i in range(2)]
            win_v = win_d.rearrange("(c p) n -> p c n", p=128)
            for c2 in range(16):
                s = c2 % 2
                c, hf = c2 // 2, c2 % 2
                P.dma("sp", lambda e, c=c, s=s, hf=hf: e.dma_start(out=stage[s][:], in_=win_v[:, c, hf * 1280:(hf + 1) * 1280]), t_stage[s], writes=[t_stage[s]])
                o, _ = _off["nmix"]
                if c2 % 2 == 0:
                    V(lambda e, c=c, s=s, o=o, hf=hf: e.tensor_scalar(out=w_in[:, c, hf * 1280:(hf + 1) * 1280], in0=stage[s][:], scalar1=cst[:, o + c:o + c + 1],
                                                                    scalar2=None, op0=ALU.mult), [t_stage[s], t_cst], [t_win], n=1280)
                else:
                    A(lambda e, c=c, s=s, o=o, hf=hf: e.activation(out=w_in[:, c, hf * 1280:(hf + 1) * 1280], in_=stage[s][:], func=AF.Copy, scale=cst[:, o + c:o + c + 1]),
                      [t_stage[s], t_cst], [t_win], n=1280)
            P.dma("pool", lambda e: e.dma_start(out=w_out[:], in_=wout_d.rearrange("(c p) n -> p c n", p=128)), t_wout, writes=[t_wout])
            P.dma("pool", lambda e: e.dma_start(out=poolw[:], in_=poolw_d.rearrange("g c d -> c g d")), t_poolw, writes=[t_poolw])
            P.dma("sp", lambda e: e.dma_start(out=rw[:], in_=rw_d.rearrange("(c p) n -> p c n", p=128)), t_rw, writes=[t_rw])

            t_pos, t_ang, t_tmpa, t_CC, t_SS = T("pos"), T("ang"), T("tmpa"), T("CC"), T("SS")
            P.dma("sp", lambda e: e.dma_start(out=posi_t[:], in_=pos_d), t_pos, writes=[t_pos])
            V(lambda e: e.tensor_copy(posf[:], posi_t[:]), [t_pos], [t_pos])
            V(lambda e: e.tensor_tensor(out=ang[:], in0=_bc(posf[:].unsqueeze(2), [128, NT, 64]),
                                        in1=_bc(C("invf").unsqueeze(1), [128, NT, 64]), op=ALU.mult), [t_pos, t_cst], [t_ang])
            TWO_PI = float(2.0 * np.pi)

            def sin_table(dst, t_dst, shift):
                V(lambda e: e.tensor_scalar(out=tmpa[:], in0=ang[:], scalar1=shift, scalar2=None, op0=ALU.add), [t_ang], [t_tmpa])
                V(lambda e: e.tensor_scalar(out=dst[:].bitcast(I32), in0=tmpa[:], scalar1=1.0 / TWO_PI, scalar2=None, op0=ALU.mult), [t_tmpa], [t_dst])
                V(lambda e: e.tensor_copy(dst[:], dst[:].bitcast(I32)), [t_dst], [t_dst])
                V(lambda e: e.scalar_tensor_tensor(out=tmpa[:], in0=dst[:], scalar=-TWO_PI, in1=tmpa[:], op0=ALU.mult, op1=ALU.add),
                  [t_dst, t_tmpa], [t_tmpa])
                V(lambda e: e.tensor_scalar(out=dst[:], in0=tmpa[:], scalar1=float(np.pi), scalar2=-TWO_PI, op0=ALU.is_gt, op1=ALU.mult),
                  [t_tmpa], [t_dst])
                V(lambda e: e.tensor_tensor(out=tmpa[:], in0=tmpa[:], in1=dst[:], op=ALU.add), [t_tmpa, t_dst], [t_tmpa])
                V(lambda e: e.tensor_scalar(out=dst[:], in0=tmpa[:], scalar1=-float(np.pi), scalar2=TWO_PI, op0=ALU.is_lt, op1=ALU.mult),
                  [t_tmpa], [t_dst])
                V(lambda e: e.tensor_tensor(out=tmpa[:], in0=tmpa[:], in1=dst[:], op=ALU.add), [t_tmpa, t_dst], [t_tmpa])
                V(lambda e: e.tensor_scalar(out=tmpa[:], in0=tmpa[:], scalar1=-3.1415925, scalar2=3.1415925, op0=ALU.max, op1=ALU.min),
                  [t_tmpa], [t_tmpa])
                A(lambda e: e.activation(out=dst[:], in_=tmpa[:], func=AF.Sin), [t_tmpa], [t_dst])

            sin_table(CC, t_CC, float(np.pi / 2))
            sin_table(SS, t_SS, 0.0)
            V(lambda e: e.tensor_scalar(out=SS[:, :, 0:32], in0=SS[:, :, 0:32], scalar1=-1.0, scalar2=None, op0=ALU.mult), [t_SS], [t_SS])

            P.barrier(alltoks)
            st_setup.close()
            state = sbuf(st, "state", [128, 512], F32)
            stateb = sbuf(st, "stateb", [128, 512], BF16)
            t_state, t_stateb = T("state"), T("stateb")
            V(lambda e: e.memset(state[:], 0.0), (), [t_state])
            V(lambda e: e.memset(stateb[:], 0.0), (), [t_stateb])
            macc = sbuf(st, "macc", [128, NE], F32)
            t_macc = T("macc")
            V(lambda e: e.memset(macc[:], 0.0), (), [t_macc])
            us = [sbuf(st, f"us{i}", [128, 512], F32) for i in range(2)]
            t_us = [T(f"us{i}") for i in range(2)]

            NB = 2
            def mk(name, shape, dtype, n=NB):
                return [sbuf(st, f"{name}{i}", shape, dtype) for i in range(n)], [T(f"{name}{i}") for i in range(n)]
            xt, t_xt = mk("xt", [128, D], F32)
            junk, t_junk = mk("junk", [128, D], BF16, 1)
            ssq, t_ssq = mk("ssq", [128, 8], F32)
            xT, t_xT = mk("xT", [128, 8, 128], BF16)
            qs, t_qs = mk("qs", [128, 512], F32)
            ks, t_ks = mk("ks", [128, 512], F32)
            vb, t_vb = mk("vb", [128, 512], BF16)
            sg, t_sg = mk("sg", [128, 512], F32)
            qa, t_qa = mk("qa", [128, 512], F32)
            qb, t_qb = mk("qb", [128, 512], F32)
            ka, t_ka, kb, t_kb = qa, t_qa, qb, t_qb
            qt, t_qt = mk("qt", [128, 512], BF16)
            kt, t_kt = mk("kt", [128, 512], BF16)
            qkT, t_qkT = mk("qkT", [128, 1536], BF16)
            for i_ in range(NB):
                V(lambda e, i_=i_: e.memset(qkT[i_][:], 0.0), (), [t_qkT[i_]])
            PT, t_PT = mk("PT", [128, 1024], BF16)
            ysb, t_ysb = mk("ysb", [128, 512], F32)
            ysq, t_ysq = mk("ysq", [128, 512], F32, 1)
            ysq, t_ysq = ysq * 2, t_ysq * 2
            gst8, t_gst8 = mk("gst8", [128, 6, 8], F32)
            gsg, t_gsg = mk("gsg", [128, 512], F32)
            ret, t_ret = mk("ret", [128, 512], BF16)
            mixT, t_mixT = mk("mixT", [128, 8, 128], BF16)
            pooledT, t_pooledT = mk("pooledT", [128, 4, 128], BF16)
            ht, t_ht = mk("ht", [128, D], F32)
            xn2, t_xn2 = mk("xn2", [128, D], F32, 1)
            xn2, t_xn2 = xn2 * 2, t_xn2 * 2
            NBX = 5
            xn2b, t_xn2b = mk("xn2b", [128, D], BF16, NBX)
            pend = []
            zt = sbuf(st, "zt", [128, 2048], BF16)
            t_zt, t_zf = T("zt"), T("zf")
            G(lambda e: e.memset(zt[:], 0.0), (), [t_zt])
            def emit_zero_fill(extra_reads):
                if nt < NT:
                    starts = list(range(0, NSLOT, 256))
                else:
                    starts = [kz * CAP + CAP - 256 for kz in range(NE)]
                for r0 in starts:
                    P.dma("act", lambda e, r0=r0: e.dma_start(out=xs_d[r0:r0 + 256, :].rearrange("(p r) d -> p (r d)", p=128), in_=zt[:]),
                          t_zf, reads=[t_zt] + extra_reads, writes=[t_zfd], nbytes=512 * 1024)
            xn2T, t_xn2T = mk("xn2T", [128, 8, 128], F32, 1)
            xn2T, t_xn2T = xn2T * 2, t_xn2T * 2
            rt, t_rt = mk("rt", [128, 8, 32], F32)
            top8, t_top8 = mk("top8", [128, 16], F32)
            posk, t_posk = mk("posk", [128, 4], F32)

            bank_ctr = [0]

            def nxt():
                v = bank_ctr[0] % 8
                bank_ctr[0] += 1
                return v

            for j in range(nt):
                b = j % NB
                P.dma("sp", lambda e, j=j, b=b: e.dma_start(out=xt[b][:], in_=x_d[j * 128:(j + 1) * 128, :]), t_xt[b], writes=[t_xt[b]])
                if j == min(1, nt - 1):
                    emit_zero_fill([t_xt[b]])
                A(lambda e, b=b: e.activation(out=junk[0][:], in_=xt[b][:], func=AF.Square, accum_out=ssq[b][:, 0:1]),
                  [t_xt[b]], [t_junk[0], t_ssq[b]])
                V(lambda e, b=b: e.tensor_scalar(out=ssq[b][:, 1:2], in0=ssq[b][:, 0:1], scalar1=1.0 / D, scalar2=EPS, op0=ALU.mult, op1=ALU.add),
                  [t_ssq[b]], [t_ssq[b]])
                G(lambda e, b=b: e.tensor_tensor(out=ssq[b][:, 2:3], in0=ssq[b][:, 1:2], in1=nhalf[:, 0:1], op=ALU.pow),
                  [t_ssq[b], t_nhalf], [t_ssq[b]])
                rstd = ssq[b][:, 2:3]
                bx0, bx1 = nxt(), nxt()
                for c in range(8):
                    bank = (bx0, bx1)[c // 4]
                    M(lambda e, b=b, c=c, bank=bank: e.transpose(psb[bank][:, (c % 4) * 128:(c % 4 + 1) * 128], xt[b][:, c * 128:(c + 1) * 128], ident),
                      [t_xt[b], t_cst], [t_ps[bank]])
                V(lambda e, b=b, bx0=bx0: e.tensor_copy(xT[b][:, 0:4, :], psb[bx0][:].rearrange("p (c t) -> p c t", c=4)), [t_ps[bx0]], [t_xT[b]])
                A(lambda e, b=b, bx1=bx1: e.copy(xT[b][:, 4:8, :], psb[bx1][:].rearrange("p (c t) -> p c t", c=4)), [t_ps[bx1]], [t_xT[b]])
                bp = [nxt() for _ in range(5)]
                for nb in range(5):
                    for c in range(8):
                        M(lambda e, b=b, nb=nb, c=c, bk=bp[nb]: e.matmul(psb[bk][:], lhsT=xT[b][:, c, :], rhs=w_in[:, c, nb * 512:(nb + 1) * 512],
                                                             start=(c == 0), stop=(c == 7)),
                          [t_xT[b], t_win], [t_ps[bp[nb]]], n=530)
                ub = j % 2
                A(lambda e, bp=bp, b=b: e.activation(out=qs[b][:], in_=psb[bp[0]][:], func=AF.Copy, scale=ssq[b][:, 2:3]), [t_ps[bp[0]], t_ssq[b]], [t_qs[b]])
                A(lambda e, bp=bp, b=b: e.activation(out=ks[b][:], in_=psb[bp[1]][:], func=AF.Copy, scale=ssq[b][:, 2:3]), [t_ps[bp[1]], t_ssq[b]], [t_ks[b]])
                A(lambda e, bp=bp, b=b: e.activation(out=vb[b][:], in_=psb[bp[2]][:], func=AF.Copy, scale=ssq[b][:, 2:3]), [t_ps[bp[2]], t_ssq[b]], [t_vb[b]])
                A(lambda e, bp=bp, b=b: e.activation(out=sg[b][:], in_=psb[bp[3]][:], func=AF.Silu, scale=ssq[b][:, 2:3]), [t_ps[bp[3]], t_ssq[b]], [t_sg[b]])
                A(lambda e, bp=bp, b=b, ub=ub: e.activation(out=us[ub][:], in_=psb[bp[4]][:], func=AF.Copy, scale=ssq[b][:, 2:3]), [t_ps[bp[4]], t_ssq[b]], [t_us[ub]])
                ccj = _bc(CC[:, j, :].unsqueeze(1), [128, 8, 64])
                ssj_lo = _bc(SS[:, j, 0:32].unsqueeze(1), [128, 8, 32])
                ssj_hi = _bc(SS[:, j, 32:64].unsqueeze(1), [128, 8, 32])
                for (src, t_src, aa, t_aa, bb, t_bb, dst, t_dst, xin) in (
                        (qs, t_qs, qa, t_qa, qb, t_qb, qt, t_qt, "xiq"), (ks, t_ks, ka, t_ka, kb, t_kb, kt, t_kt, "xik")):
                    s4 = src[b][:].rearrange("p (h two d) -> p h two d", two=2, d=32)
                    b4 = bb[b][:].rearrange("p (h two d) -> p h two d", two=2, d=32)
                    V(lambda e, b=b, src=src, aa=aa, ccj=ccj: e.tensor_tensor(out=aa[b][:].rearrange("p (h d) -> p h d", d=64),
                                                                              in0=src[b][:].rearrange("p (h d) -> p h d", d=64), in1=ccj, op=ALU.mult),
                      [t_src[b], t_CC], [t_aa[b]])
                    V(lambda e, s4=s4, b4=b4, ssj_lo=ssj_lo: e.tensor_tensor(out=b4[:, :, 0, :], in0=s4[:, :, 1, :], in1=ssj_lo, op=ALU.mult),
                      [t_src[b], t_SS], [t_bb[b]])
                    V(lambda e, s4=s4, b4=b4, ssj_hi=ssj_hi: e.tensor_tensor(out=b4[:, :, 1, :], in0=s4[:, :, 0, :], in1=ssj_hi, op=ALU.mult),
                      [t_src[b], t_SS], [t_bb[b]])
                    V(lambda e, b=b, aa=aa, bb=bb: e.tensor_tensor(out=aa[b][:], in0=aa[b][:], in1=bb[b][:], op=ALU.add), [t_aa[b], t_bb[b]], [t_aa[b]])
                    V(lambda e, b=b, aa=aa, dst=dst, xin=xin: e.tensor_tensor(out=dst[b][:].rearrange("p (h d) -> p h d", d=64),
                                                                           in0=aa[b][:].rearrange("p (h d) -> p h d", d=64),
                                                                           in1=_bc(C(xin).unsqueeze(2), [128, 8, 64]), op=ALU.mult),
                      [t_aa[b], t_cst], [t_dst[b]])
                bqk = nxt()
                pb2 = psb[bqk][:].bitcast(BF16)
                for i in range(4):
                    M(lambda e, b=b, i=i, pb2=pb2: e.transpose(pb2[:, i * 128:(i + 1) * 128], qt[b][:, i * 128:(i + 1) * 128], identb[:]),
                      [t_qt[b], t_identb], [t_ps[bqk]])
                for i in range(4):
                    M(lambda e, b=b, i=i, pb2=pb2: e.transpose(pb2[:, 512 + i * 128:512 + (i + 1) * 128], kt[b][:, i * 128:(i + 1) * 128], identb[:]),
                      [t_kt[b], t_identb], [t_ps[bqk]])
                A(lambda e, b=b, pb2=pb2: e.copy(qkT[b][:, 0:512], pb2[:, 0:512]), [t_ps[bqk]], [t_qkT[b]])
                A(lambda e, b=b, pb2=pb2: e.copy(qkT[b][0:64, 512:1024], pb2[0:64, 512:1024]), [t_ps[bqk]], [t_qkT[b]])
                A(lambda e, b=b, pb2=pb2: e.copy(qkT[b][64:128, 1024:1536], pb2[64:128, 512:1024]), [t_ps[bqk]], [t_qkT[b]])
                bs = [nxt(), nxt()]
                for h in range(H):
                    hb = (h % 2) * 64
                    hh = h // 2
                    bank = bs[h // 4]
                    M(lambda e, b=b, h=h, hb=hb, hh=hh, bank=bank: e.matmul(
                        psb[bank][:, (h % 4) * 128:(h % 4 + 1) * 128],
                        lhsT=qkT[b][:, 512 + (h % 2) * 512 + hh * 128:512 + (h % 2) * 512 + (hh + 1) * 128],
                        rhs=qkT[b][:, hh * 128:(hh + 1) * 128], start=True, stop=True),
                      [t_qkT[b]], [t_ps[bank]])
                mask4 = _bc(C("mask").unsqueeze(1), [128, 4, 128])
                for half in range(2):
                    V(lambda e, b=b, half=half, bs=bs: e.tensor_tensor(out=PT[b][:, half * 512:(half + 1) * 512].rearrange("p (h c) -> p h c", c=128),
                                                                in0=psb[bs[half]][:].rearrange("p (h c) -> p h c", c=128), in1=mask4, op=ALU.mult),
                      [t_ps[bs[half]], t_cst], [t_PT[b]])
                by, bkv = nxt(), nxt()
                for h in range(H):
                    hb = (h % 2) * 64
                    hh = h // 2
                    M(lambda e, b=b, h=h, by=by: e.matmul(psb[by][:, h * 64:(h + 1) * 64], lhsT=PT[b][:, h * 128:(h + 1) * 128],
                                                   rhs=vb[b][:, h * 64:(h + 1) * 64], start=True, stop=False),
                      [t_PT[b], t_vb[b]], [t_ps[by]])
                    M(lambda e, b=b, h=h, hb=hb, hh=hh, by=by: e.matmul(psb[by][:, h * 64:(h + 1) * 64], lhsT=qkT[b][:, hh * 128:(hh + 1) * 128],
                                                                 rhs=stateb[:, h * 64:(h + 1) * 64], start=False, stop=True),
                      [t_qkT[b], t_stateb], [t_ps[by]])
                for hh in range(4):
                    M(lambda e, b=b, hh=hh, bkv=bkv: e.matmul(psb[bkv][:, hh * 128:(hh + 1) * 128], lhsT=kt[b][:, hh * 128:(hh + 1) * 128],
                                                     rhs=vb[b][:, hh * 128:(hh + 1) * 128], start=True, stop=True),
                      [t_kt[b], t_vb[b]], [t_ps[bkv]])
                V(lambda e, bkv=bkv: e.tensor_tensor(out=state[:], in0=state[:], in1=psb[bkv][:], op=ALU.add), [t_state, t_ps[bkv]], [t_state])
                V(lambda e: e.tensor_tensor(out=state[:], in0=state[:], in1=C("gc"), op=ALU.mult), [t_state, t_cst], [t_state])
                V(lambda e: e.tensor_copy(stateb[:], state[:]), [t_state], [t_stateb])
                A(lambda e, b=b, by=by: e.copy(ysb[b][:], psb[by][:]), [t_ps[by]], [t_ysb[b]])
                A(lambda e, b=b, by=by: e.activation(out=ysq[b][:], in_=psb[by][:], func=AF.Square), [t_ps[by]], [t_ysq[b]])
                G(lambda e, b=b: e.tensor_tensor(out=gsg[b][:], in0=sg[b][:], in1=R("gnw"), op=ALU.mult), [t_sg[b], t_rows], [t_gsg[b]])
                g8 = gst8[b]
                V(lambda e, b=b, g8=g8: e.tensor_reduce(out=g8[:, 0, :], in_=ysb[b][:].rearrange("p (h d) -> p h d", d=64), axis=AX.X, op=ALU.add),
                  [t_ysb[b]], [t_gst8[b]])
                V(lambda e, b=b, g8=g8: e.tensor_reduce(out=g8[:, 1, :], in_=ysq[b][:].rearrange("p (h d) -> p h d", d=64), axis=AX.X, op=ALU.add),
                  [t_ysq[b]], [t_gst8[b]])
                V(lambda e, g8=g8: e.tensor_scalar(out=g8[:, 2, :], in0=g8[:, 0, :], scalar1=1.0 / DH, scalar2=None, op0=ALU.mult), [t_gst8[b]], [t_gst8[b]])
                V(lambda e, g8=g8: e.tensor_tensor(out=g8[:, 3, :], in0=g8[:, 2, :], in1=g8[:, 2, :], op=ALU.mult), [t_gst8[b]], [t_gst8[b]])
                V(lambda e, g8=g8: e.scalar_tensor_tensor(out=g8[:, 4, :], in0=g8[:, 1, :], scalar=1.0 / DH, in1=g8[:, 3, :], op0=ALU.mult, op1=ALU.subtract),
                  [t_gst8[b]], [t_gst8[b]])
                V(lambda e, g8=g8: e.tensor_scalar(out=g8[:, 4, :], in0=g8[:, 4, :], scalar1=EPS, scalar2=None, op0=ALU.add), [t_gst8[b]], [t_gst8[b]])
                G(lambda e, g8=g8: e.tensor_tensor(out=g8[:, 5, :], in0=g8[:, 4, :], in1=nhalf[:, 0:8], op=ALU.pow), [t_gst8[b], t_nhalf], [t_gst8[b]])
                y3 = ysb[b][:].rearrange("p (h d) -> p h d", d=64)
                V(lambda e, y3=y3, g8=g8: e.tensor_tensor(out=y3, in0=y3, in1=_bc(g8[:, 2, :].unsqueeze(2), [128, 8, 64]), op=ALU.subtract),
                  [t_ysb[b], t_gst8[b]], [t_ysb[b]])
                V(lambda e, y3=y3, g8=g8: e.tensor_tensor(out=y3, in0=y3, in1=_bc(g8[:, 5, :].unsqueeze(2), [128, 8, 64]), op=ALU.mult),
                  [t_ysb[b], t_gst8[b]], [t_ysb[b]])
                V(lambda e, b=b: e.tensor_tensor(out=ret[b][:], in0=ysb[b][:], in1=gsg[b][:], op=ALU.mult), [t_ysb[b], t_gsg[b]], [t_ret[b]])
                brt = nxt()
                pb1 = psb[brt][:].bitcast(BF16)
                for i in range(4):
                    M(lambda e, b=b, i=i, pb1=pb1: e.transpose(pb1[:, i * 128:(i + 1) * 128], ret[b][:, i * 128:(i + 1) * 128], identb[:]),
                      [t_ret[b], t_identb], [t_ps[brt]])
                A(lambda e, b=b, pb1=pb1: e.copy(mixT[b][:, 0:4, :], pb1[:, 0:512].rearrange("p (c t) -> p c t", c=4)), [t_ps[brt]], [t_mixT[b]])
                bo, _ = _off["bands"]
                bpl, bmx = nxt(), nxt()
                for g in range(4):
                    kind = 1 if j == 0 else 0
                    M(lambda e, g=g, ub=ub, kind=kind, bo=bo, j=j, bpl=bpl: e.matmul(psb[bpl][:, g * 128:(g + 1) * 128], lhsT=us[ub][:, g * 128:(g + 1) * 128],
                                                                       rhs=cst[:, bo + (3 * g + kind) * 128:bo + (3 * g + kind + 1) * 128],
                                                                       start=True, stop=(j == 0)),
                      [t_us[ub], t_cst], [t_ps[bpl]])
                    if j > 0:
                        M(lambda e, g=g, ub=ub, bo=bo, bpl=bpl: e.matmul(psb[bpl][:, g * 128:(g + 1) * 128], lhsT=us[1 - ub][:, g * 128:(g + 1) * 128],
                                                                rhs=cst[:, bo + (3 * g + 2) * 128:bo + (3 * g + 3) * 128], start=False, stop=True),
                          [t_us[1 - ub], t_cst], [t_ps[bpl]])
                V(lambda e, b=b, bpl=bpl: e.tensor_copy(pooledT[b][:], psb[bpl][:].rearrange("p (g t) -> p g t", g=4)), [t_ps[bpl]], [t_pooledT[b]])
                for g in range(4):
                    M(lambda e, b=b, g=g, bmx=bmx: e.matmul(psb[bmx][:, g * 128:(g + 1) * 128], lhsT=poolw[:, g, :], rhs=pooledT[b][:, g, :], start=True, stop=True),
                      [t_poolw, t_pooledT[b]], [t_ps[bmx]])
                po, _ = _off["pscale"]
                V(lambda e, b=b, po=po, bmx=bmx: e.tensor_tensor(out=mixT[b][:, 4:8, :], in0=psb[bmx][:].rearrange("p (g t) -> p g t", g=4),
                                                        in1=_bc(cst[:, po:po + 4].unsqueeze(2), [128, 4, 128]), op=ALU.mult),
                  [t_ps[bmx], t_cst], [t_mixT[b]])
                bh = [nxt(), nxt()]
                for nb in range(2):
                    for c in range(8):
                        M(lambda e, b=b, nb=nb, c=c, bh=bh: e.matmul(psb[bh[nb]][:], lhsT=mixT[b][:, c, :], rhs=w_out[:, c, nb * 512:(nb + 1) * 512],
                                                             start=(c == 0), stop=(c == 7)),
                          [t_mixT[b], t_wout], [t_ps[bh[nb]]], n=530)
                for nb in range(2):
                    V(lambda e, b=b, nb=nb, bh=bh: e.tensor_tensor(out=ht[b][:, nb * 512:(nb + 1) * 512], in0=psb[bh[nb]][:], in1=xt[b][:, nb * 512:(nb + 1) * 512], op=ALU.add),
                      [t_ps[bh[nb]], t_xt[b]], [t_ht[b]])
                P.dma("sp", lambda e, j=j, b=b: e.dma_start(out=h_d[j * 128:(j + 1) * 128, :], in_=ht[b][:]), t_ht[b], reads=[t_ht[b]], writes=[t_hd[j]])
                if debug:
                    P.dma("sp", lambda e, j=j, b=b: e.dma_start(out=dbg["h1"][j * 128:(j + 1) * 128, :], in_=ht[b][:]), t_dbgs[j % 8], reads=[t_ht[b]])
                if phases < 2:
                    continue

                A(lambda e, b=b: e.activation(out=junk[0][:], in_=ht[b][:], func=AF.Square, accum_out=ssq[b][:, 4:5]),
                  [t_ht[b]], [t_junk[0], t_ssq[b]])
                V(lambda e, b=b: e.tensor_scalar(out=ssq[b][:, 5:6], in0=ssq[b][:, 4:5], scalar1=1.0 / D, scalar2=EPS, op0=ALU.mult, op1=ALU.add),
                  [t_ssq[b]], [t_ssq[b]])
                G(lambda e, b=b: e.tensor_tensor(out=ssq[b][:, 6:7], in0=ssq[b][:, 5:6], in1=nhalf[:, 0:1], op=ALU.pow),
                  [t_ssq[b], t_nhalf], [t_ssq[b]])
                V(lambda e, b=b: e.scalar_tensor_tensor(out=xn2[b][:], in0=ht[b][:], scalar=ssq[b][:, 6:7], in1=R("nmoe"), op0=ALU.mult, op1=ALU.mult),
                  [t_ht[b], t_ssq[b], t_rows], [t_xn2[b]])
                bx = j % NBX
                A(lambda e, b=b, bx=bx: e.copy(xn2b[bx][:], xn2[b][:]), [t_xn2[b]], [t_xn2b[bx]])
                bn = [nxt(), nxt()]
                blg = nxt()
                for c in range(8):
                    bank = bn[c // 4]
                    M(lambda e, b=b, c=c, bank=bank: e.transpose(psb[bank][:, (c % 4) * 128:(c % 4 + 1) * 128], xn2[b][:, c * 128:(c + 1) * 128], ident),
                      [t_xn2[b], t_cst], [t_ps[bank]])
                V(lambda e, b=b, bn=bn: e.tensor_copy(xn2T[b][:, 0:4, :], psb[bn[0]][:].rearrange("p (c t) -> p c t", c=4)), [t_ps[bn[0]]], [t_xn2T[b]])
                A(lambda e, b=b, bn=bn: e.copy(xn2T[b][:, 4:8, :], psb[bn[1]][:].rearrange("p (c t) -> p c t", c=4)), [t_ps[bn[1]]], [t_xn2T[b]])
                for c in range(8):
                    M(lambda e, b=b, c=c, blg=blg: e.matmul(psb[blg][:, 0:NE], lhsT=xn2T[b][:, c, :], rhs=rw[:, c, :], start=(c == 0), stop=(c == 7)),
                      [t_xn2T[b], t_rw], [t_ps[blg]])
                r_ = rt[b]
                t8 = top8[b]
                V(lambda e, r_=r_, blg=blg: e.tensor_tensor(out=r_[:, 0, :], in0=psb[blg][:, 0:NE], in1=R("rb"), op=ALU.add), [t_ps[blg], t_rows], [t_rt[b]])
                V(lambda e, r_=r_, t8=t8: e.max(out=t8[:, 0:8], in_=r_[:, 0, :]), [t_rt[b]], [t_top8[b]])
                V(lambda e, t8=t8: e.tensor_scalar(out=t8[:, 8:9], in0=t8[:, 0:1], scalar1=-1.0, scalar2=None, op0=ALU.mult), [t_top8[b]], [t_top8[b]])
                A(lambda e, t8=t8: e.activation(out=t8[:, 10:14], in_=t8[:, 0:4], func=AF.Exp, bias=t8[:, 8:9], accum_out=t8[:, 9:10]),
                  [t_top8[b]], [t_top8[b]])
                V(lambda e, t8=t8: e.reciprocal(t8[:, 14:15], t8[:, 9:10]), [t_top8[b]], [t_top8[b]])
                V(lambda e, r_=r_, t8=t8: e.tensor_scalar(out=r_[:, 1, :], in0=r_[:, 0, :], scalar1=t8[:, 3:4], scalar2=None, op0=ALU.is_ge),
                  [t_rt[b], t_top8[b]], [t_rt[b]])
                M(lambda e, r_=r_, blg=blg: e.matmul(psb[blg][:, 32:64], lhsT=C("ustrict"), rhs=r_[:, 1, :], start=True, stop=False), [t_rt[b], t_cst], [t_ps[blg]])
                M(lambda e, blg=blg: e.matmul(psb[blg][:, 32:64], lhsT=C("ones"), rhs=macc[:], start=False, stop=True), [t_macc, t_cst], [t_ps[blg]])
                V(lambda e, r_=r_: e.tensor_tensor(out=macc[:], in0=macc[:], in1=r_[:, 1, :], op=ALU.add), [t_macc, t_rt[b]], [t_macc])
                V(lambda e, r_=r_, blg=blg: e.tensor_copy(r_[:, 2, :], psb[blg][:, 32:64]), [t_ps[blg]], [t_rt[b]])
                V(lambda e, r_=r_: e.tensor_scalar(out=r_[:, 3, :], in0=r_[:, 2, :], scalar1=float(CAP) - 0.5, scalar2=BIGPOS, op0=ALU.is_ge, op1=ALU.mult),
                  [t_rt[b]], [t_rt[b]])
                V(lambda e, r_=r_: e.tensor_tensor(out=r_[:, 4, :], in0=r_[:, 2, :], in1=C("ecap"), op=ALU.add), [t_rt[b], t_cst], [t_rt[b]])
                V(lambda e, r_=r_: e.tensor_tensor(out=r_[:, 4, :], in0=r_[:, 4, :], in1=r_[:, 3, :], op=ALU.add), [t_rt[b]], [t_rt[b]])
                for k in range(TOPK):
                    V(lambda e, r_=r_, t8=t8, b=b, k=k: e.scalar_tensor_tensor(out=r_[:, 6, :], in0=r_[:, 0, :], scalar=t8[:, k:k + 1], in1=r_[:, 4, :],
                                                                             op0=ALU.is_equal, op1=ALU.mult, accum_out=posk[b][:, k:k + 1]),
                      [t_rt[b], t_top8[b]], [t_rt[b], t_posk[b]])
                V(lambda e, r_=r_, b=b: e.tensor_scalar(out=r_[:, 7, 0:4], in0=posk[b][:, 0:4], scalar1=BIGPOS * 0.5, scalar2=None, op0=ALU.is_lt),
                  [t_posk[b]], [t_rt[b]])
                V(lambda e, r_=r_, t8=t8, j=j: e.scalar_tensor_tensor(out=gates[:, j, :], in0=t8[:, 10:14], scalar=t8[:, 14:15], in1=r_[:, 7, 0:4],
                                                                      op0=ALU.mult, op1=ALU.mult),
                  [t_top8[b], t_rt[b]], [t_gates[j]])
                V(lambda e, b=b, j=j: e.tensor_copy(posi[:, j, :], posk[b][:, 0:4]), [t_posk[b]], [t_posi[j]])
                def scatter(j=j, bx=bx):
                    for k in range(TOPK):
                        P.dma("pool", lambda e, bx=bx, j=j, k=k: e.indirect_dma_start(
                            out=xs_d, out_offset=bass.IndirectOffsetOnAxis(ap=posi[:, j, k:k + 1], axis=0), in_=xn2b[bx][:], in_offset=None,
                            bounds_check=_bcreg(e, nc), oob_is_err=False), t_xn2b[bx], reads=[t_xn2b[bx], t_posi[j], t_zfd], writes=[t_xsd],
                            nbytes=256 * 1024, delay=_OPT["sdel"])
                pend.append(scatter)
                if len(pend) >= NBX - 1:
                    pend.pop(0)()
            for f_ in pend:
                f_()

            P.barrier(alltoks)

        if phases >= 3:
            build_phase3(locals())
        if phases >= 4:
            build_phase4(locals())

        if debug and phases >= 2:
            P.dma("sp", lambda e: e.dma_start(out=dbg["gate"], in_=gates[:].rearrange("p j k -> p (j k)")), T("dbg_g"), reads=t_gates)
            P.dma("sp", lambda e: e.dma_start(out=dbg["pos"], in_=posi[:].rearrange("p j k -> p (j k)")), T("dbg_p"), reads=t_posi)
        P.barrier(alltoks)
        stats = P.emit()
    return nc, stats


def make_in_maps(inp, ncores=8):
    cst, rows = host_consts(inp)
    wgu = inp["expert_w_gate_up"][0]
    w_g = np.ascontiguousarray(wgu[:, :, 0::2])
    w_u = np.ascontiguousarray(wgu[:, :, 1::2])
    shared = {
        "cst": cst, "rows": rows,
        "w_in": np.ascontiguousarray(inp["w_in"][0]), "w_out": np.ascontiguousarray(inp["w_out"][0]),
        "pool_w": np.ascontiguousarray(inp["pool_w"][0]), "router_w": np.ascontiguousarray(inp["router_w"][0]),
        "w_g": w_g, "w_u": w_u, "w_d": np.ascontiguousarray(inp["expert_w_down"][0]),
        "b_d": np.ascontiguousarray(inp["expert_b_down"][0]),
        "ple_gate_w": np.ascontiguousarray(inp["ple_gate_w"][0]), "ple_proj_w": np.ascontiguousarray(inp["ple_proj_w"][0]),
    }
    maps = []
    for b in range(ncores):
        m = dict(shared)
        m["x"] = np.ascontiguousarray(inp["x"][b])
        m["p"] = np.ascontiguousarray(inp["p"][0, b])
        m["pos"] = np.ascontiguousarray(inp["positions"][b].reshape(NT, 128).T.astype(np.int32))
        maps.append(m)
    return maps


def build_phase3(L):
    nc, P, T, sbuf, cst = L["nc"], L["P"], L["T"], L["sbuf"], L["cst"]
    V, A, G, M = L["V"], L["A"], L["G"], L["M"]
    psb, t_ps, t_cst, identb, t_identb = L["psb"], L["t_ps"], L["t_cst"], L["identb"], L["t_identb"]
    xs_d, ys_d, wg_d, wu_d, wd_d, bd_d = L["xs_d"], L["ys_d"], L["wg_d"], L["wu_d"], L["wd_d"], L["bd_d"]
    t_xsd, t_ysd, alltoks = L["t_xsd"], L["t_ysd"], L["alltoks"]
    n_exp = L.get("n_exp", NE)
    NSL = (CAP + 127) // 128
    HALF = CAP // 2

    def rows(i):
        return 128

    def tstart(i):
        return min(i * 128, CAP - 128)
    with ExitStack() as st:
        wgu = [sbuf(st, f"wgu{i}", [128, 8, 2048], BF16) for i in range(2)]
        wdn = [sbuf(st, f"wdn{i}", [128, 8, 1024], BF16) for i in range(2)]
        t_wgu = [T(f"wgu{i}") for i in range(2)]
        t_wdn = [T(f"wdn{i}") for i in range(2)]
        xtok = sbuf(st, "xtok", [128, NSL, D], BF16)
        t_xtok = T("xtok")
        XT = [sbuf(st, f"XT{i}", [128, 8, CAP], BF16) for i in range(2)]
        t_XT = [T(f"XT{i}") for i in range(2)]
        actT = [sbuf(st, f"actT{i}", [128, 8, CAP], BF16) for i in range(2)]
        t_actT = [T(f"actT{i}") for i in range(2)]
        NTMP = 2
        tmp = [sbuf(st, f"etmp{i}", [128, 4, HALF], F32) for i in range(NTMP)]
        t_tmp = [[T(f"etmp{i}_{k}") for k in range(4)] for i in range(NTMP)]
        NY = 4
        ysb = [sbuf(st, f"ysb3_{i}", [128, D], F32) for i in range(NY)]
        t_ysb = [T(f"ysb3_{i}") for i in range(NY)]
        bdb = [sbuf(st, f"bdb{i}", [128, D], F32) for i in range(2)]
        t_bdb = [T(f"bdb{i}") for i in range(2)]
        abg = sbuf(st, "abg", [128, 256], F32)
        t_abg = T("abg")
        ob, _ = _off["bgate"]
        ou, _ = _off["bup"]
        V(lambda e: e.tensor_scalar(out=abg[:], in0=cst[:, ob:ob + 256], scalar1=ALPHA, scalar2=None, op0=ALU.mult), [t_cst], [t_abg])
        bu1 = sbuf(st, "bu1", [128, 256], F32)
        V(lambda e: e.tensor_scalar(out=bu1[:], in0=cst[:, ou:ou + 256], scalar1=1.0, scalar2=None, op0=ALU.add), [t_cst], [t_abg])
        SIG7 = float(1.0 / (1.0 + np.exp(-ALPHA * LIMIT)))

        def load_wgu(e_idx):
            sl = e_idx % 2
            wg_v = wg_d[e_idx].rearrange("(c p) n -> p c n", p=128)
            wu_v = wu_d[e_idx].rearrange("(c p) n -> p c n", p=128)
            for hc in range(2):
                cs = slice(hc * 4, hc * 4 + 4)
                P.dma("pool", lambda e, cs=cs, sl=sl, wg_v=wg_v: e.dma_start(out=wgu[sl][:, cs, 0:1024], in_=wg_v[:, cs, :]), t_wgu[sl], writes=[t_wgu[sl]])
                P.dma("pool", lambda e, cs=cs, sl=sl, wu_v=wu_v: e.dma_start(out=wgu[sl][:, cs, 1024:2048], in_=wu_v[:, cs, :]), t_wgu[sl], writes=[t_wgu[sl]])

        def load_wd(e_idx):
            sl = e_idx % 2
            wd_v = wd_d[e_idx].rearrange("(c p) n -> p c n", p=128)
            for hc in range(2):
                cs = slice(hc * 4, hc * 4 + 4)
                P.dma("pool", lambda e, cs=cs, sl=sl, wd_v=wd_v: e.dma_start(out=wdn[sl][:, cs, :], in_=wd_v[:, cs, :]), t_wdn[sl], writes=[t_wdn[sl]])
            P.dma("sp", lambda e, sl=sl, e_idx=e_idx: e.dma_start(out=bdb[sl][:], in_=bd_d[e_idx].partition_broadcast(128)), t_bdb[sl], writes=[t_bdb[sl]])

        evac_rr = [0]

        def stage_T(e_idx):
            sl = e_idx % 2
            nfull = CAP // 128
            P.dma("sp", lambda e, e_idx=e_idx, nfull=nfull: e.dma_start(out=xtok[:, 0:nfull, :],
                                                                        in_=xs_d[e_idx * CAP:e_idx * CAP + nfull * 128, :].rearrange("(i p) d -> p i d", p=128)),
                  t_xtok, reads=[t_xsd], writes=[t_xtok])
            if CAP % 128:
                P.dma("sp", lambda e, e_idx=e_idx, nfull=nfull: e.dma_start(out=xtok[:, nfull, :], in_=xs_d[(e_idx + 1) * CAP - 128:(e_idx + 1) * CAP, :]),
                      t_xtok, reads=[t_xsd], writes=[t_xtok], nbytes=256 * 1024)
            for i in range(NSL):
                bank = 4
                pbv = psb[bank][:].bitcast(BF16)
                ri = rows(i)
                for c in range(8):
                    M(lambda e, i=i, c=c, pbv=pbv, ri=ri: e.transpose(pbv[:, c * 128:c * 128 + ri], xtok[0:ri, i, c * 128:(c + 1) * 128], identb[0:ri, 0:ri]),
                      [t_xtok, t_identb], [t_ps[bank]])
                eng = A if evac_rr[0] % 2 == 0 else V
                evac_rr[0] += 1
                if eng is A:
                    A(lambda e, i=i, sl=sl, pbv=pbv, ri=ri: e.copy(XT[sl][:, :, tstart(i):tstart(i) + ri], pbv.rearrange("p (c s) -> p c s", c=8)[:, :, 0:ri]),
                      [t_ps[bank]], [t_XT[sl]])
                else:
                    V(lambda e, i=i, sl=sl, pbv=pbv, ri=ri: e.tensor_copy(XT[sl][:, :, tstart(i):tstart(i) + ri], pbv.rearrange("p (c s) -> p c s", c=8)[:, :, 0:ri]),
                      [t_ps[bank]], [t_XT[sl]])

        cnt = [0]

        def stage_GU(e_idx):
            sl = e_idx % 2
            for jc in range(8):
                for hf in range(2):
                    pp = cnt[0] % 2
                    tb = cnt[0] % NTMP
                    cnt[0] += 1
                    bg, bu = psb[2 * pp], psb[2 * pp + 1]
                    ssl = slice(hf * HALF, (hf + 1) * HALF)
                    for c in range(8):
                        M(lambda e, sl=sl, jc=jc, c=c, bg=bg, ssl=ssl: e.matmul(bg[:, 0:HALF], lhsT=wgu[sl][:, c, jc * 128:(jc + 1) * 128], rhs=XT[sl][:, c, ssl],
                                                                             start=(c == 0), stop=(c == 7)),
                          [t_wgu[sl], t_XT[sl]], [t_ps[2 * pp]], n=HALF)
                    for c in range(8):
                        M(lambda e, sl=sl, jc=jc, c=c, bu=bu, ssl=ssl: e.matmul(bu[:, 0:HALF], lhsT=wgu[sl][:, c, 1024 + jc * 128:1024 + (jc + 1) * 128], rhs=XT[sl][:, c, ssl],
                                                                             start=(c == 0), stop=(c == 7)),
                          [t_wgu[sl], t_XT[sl]], [t_ps[2 * pp + 1]], n=HALF)
                    col = e_idx * 8 + jc
                    tm, tt = tmp[tb], t_tmp[tb]
                    A(lambda e, tm=tm, bg=bg, col=col: e.activation(out=tm[:, 0, :], in_=bg[:, 0:HALF], func=AF.Sigmoid, bias=abg[:, col:col + 1], scale=ALPHA),
                      [t_ps[2 * pp], t_abg], [tt[0]])
                    A(lambda e, tm=tm, bg=bg, col=col: e.activation(out=tm[:, 1, :], in_=bg[:, 0:HALF], func=AF.Identity, bias=cst[:, ob + col:ob + col + 1], scale=1.0),
                      [t_ps[2 * pp], t_cst], [tt[1]])
                    A(lambda e, tm=tm, bu=bu, col=col: e.activation(out=tm[:, 2, :], in_=bu[:, 0:HALF], func=AF.Identity, bias=bu1[:, col:col + 1], scale=1.0),
                      [t_ps[2 * pp + 1], t_abg], [tt[2]])
                    V(lambda e, tm=tm: e.tensor_scalar(out=tm[:, 2, :], in0=tm[:, 2, :], scalar1=LIMIT + 1.0, scalar2=-LIMIT + 1.0, op0=ALU.min, op1=ALU.max), [tt[2]], [tt[2]])
                    V(lambda e, tm=tm: e.scalar_tensor_tensor(out=tm[:, 3, :], in0=tm[:, 1, :], scalar=LIMIT, in1=tm[:, 2, :], op0=ALU.min, op1=ALU.mult),
                      [tt[1], tt[2]], [tt[3]])
                    V(lambda e, tm=tm, sl=sl, jc=jc, ssl=ssl: e.scalar_tensor_tensor(out=actT[sl][:, jc, ssl], in0=tm[:, 0, :], scalar=SIG7, in1=tm[:, 3, :],
                                                                                  op0=ALU.min, op1=ALU.mult), [tt[0], tt[3]], [t_actT[sl]])

        ycnt = [0]
        ybank = [0]

        def stage_down(e_idx):
            sl = e_idx % 2
            for i in range(NSL):
                yb = ycnt[0] % NY
                ycnt[0] += 1
                ri = rows(i)
                for nb in range(2):
                    bk = 5 + ybank[0] % 3
                    ybank[0] += 1
                    for jc in range(8):
                        M(lambda e, sl=sl, i=i, nb=nb, jc=jc, bk=bk, ri=ri: e.matmul(psb[bk][0:ri, :], lhsT=actT[sl][:, jc, tstart(i):tstart(i) + ri], rhs=wdn[sl][:, jc, nb * 512:(nb + 1) * 512],
                                                                              start=(jc == 0), stop=(jc == 7)),
                          [t_actT[sl], t_wdn[sl]], [t_ps[bk]], n=512)
                    V(lambda e, yb=yb, nb=nb, sl=sl, bk=bk, ri=ri: e.tensor_tensor(out=ysb[yb][0:ri, nb * 512:(nb + 1) * 512], in0=psb[bk][0:ri, :], in1=bdb[sl][0:ri, nb * 512:(nb + 1) * 512], op=ALU.add),
                      [t_ps[bk], t_bdb[sl]], [t_ysb[yb]], n=512)
                ov = i * 128 - tstart(i)
                r0 = e_idx * CAP + i * 128
                P.dma("sp", lambda e, yb=yb, r0=r0, ov=ov: e.dma_start(out=ys_d[r0:r0 + 128 - ov, :], in_=ysb[yb][ov:128, :]), t_ysb[yb], reads=[t_ysb[yb]], writes=[t_ysd],
                      nbytes=(128 - ov) * 4096)

        load_wgu(0)
        load_wd(0)
        load_wgu(1)
        stage_T(0)
        if n_exp > 1:
            stage_T(1)
        stage_GU(0)
        for s_ in range(n_exp):
            if s_ + 2 < n_exp:
                load_wgu(s_ + 2)
            if s_ + 1 < n_exp:
                load_wd(s_ + 1)
            if s_ + 2 < n_exp:
                stage_T(s_ + 2)
            if s_ + 1 < n_exp:
                stage_GU(s_ + 1)
            stage_down(s_)
        P.barrier(alltoks)


def build_phase4(L):
    nc, P, T, sbuf, cst = L["nc"], L["P"], L["T"], L["sbuf"], L["cst"]
    V, A, G, M = L["V"], L["A"], L["G"], L["M"]
    psb, t_ps, t_cst, identb, t_identb = L["psb"], L["t_ps"], L["t_cst"], L["identb"], L["t_identb"]
    ys_d, h_d, p_d, out_d, pgw_d, ppw_d, rows_d = L["ys_d"], L["h_d"], L["p_d"], L["out_d"], L["pgw_d"], L["ppw_d"], L["rows_d"]
    t_ysd, t_hd, alltoks = L["t_ysd"], L["t_hd"], L["alltoks"]
    gates, posi, t_gates, t_posi, nhalf, t_nhalf = L["gates"], L["posi"], L["t_gates"], L["t_posi"], L["nhalf"], L["t_nhalf"]
    nt, dbg, debug = L["nt"], L["dbg"], L["debug"]
    ident = cst[:, _off["ident"][0]:_off["ident"][0] + 128]
    with ExitStack() as st:
        pgw = sbuf(st, "pgw", [128, 8, D], BF16)
        t_pgw = T("pgw")
        ppw = sbuf(st, "ppw", [128, 2, D], BF16)
        t_ppw = T("ppw")
        fnw = sbuf(st, "fnw", [128, D], F32)
        t_fnw = T("fnw")
        stg = [sbuf(st, f"stg4_{i}", [128, D], F32) for i in range(2)]
        t_stg = [T(f"stg4_{i}") for i in range(2)]
        pgw_v = pgw_d.rearrange("(c p) n -> p c n", p=128)
        on, _ = _off["nple"]
        for c in range(8):
            s_ = c % 2
            P.dma("sp", lambda e, c=c, s_=s_: e.dma_start(out=stg[s_][:], in_=pgw_v[:, c, :]), t_stg[s_], writes=[t_stg[s_]])
            if c % 2 == 0:
                V(lambda e, c=c, s_=s_: e.tensor_scalar(out=pgw[:, c, :], in0=stg[s_][:], scalar1=cst[:, on + c:on + c + 1], scalar2=None, op0=ALU.mult),
                  [t_stg[s_], t_cst], [t_pgw], n=1024)
            else:
                A(lambda e, c=c, s_=s_: e.activation(out=pgw[:, c, :], in_=stg[s_][:], func=AF.Copy, scale=cst[:, on + c:on + c + 1]),
                  [t_stg[s_], t_cst], [t_pgw], n=1024)
        P.dma("pool", lambda e: e.dma_start(out=ppw[:], in_=ppw_d.rearrange("(c p) n -> p c n", p=128)), t_ppw, writes=[t_ppw])
        fo, fw = _roff["fnw"]
        P.dma("sp", lambda e: e.dma_start(out=fnw[:], in_=rows_d[0, fo:fo + fw].partition_broadcast(128)), t_fnw, writes=[t_fnw])

        NB = 4
        bctr = [0]

        def nxt():
            v = bctr[0] % 8
            bctr[0] += 1
            return v

        def mk(name, shape, dtype, n=NB):
            return [sbuf(st, f"{name}{i}", shape, dtype) for i in range(n)], [T(f"{name}{i}") for i in range(n)]
        hb_, t_hb = mk("h4", [128, D], F32)
        pt_, t_pt = mk("p4", [128, PLE], F32)
        yk, t_yk = mk("yk", [128, 4, D], F32)
        junk, t_junk = mk("junk4", [128, D], BF16, 1)
        sq, t_sq = mk("sq4", [128, 8], F32)
        xn3, t_xn3 = mk("xn3", [128, D], BF16)
        xn3T, t_xn3T = mk("xn3T", [128, 8, 128], BF16)
        pT, t_pT = mk("pT", [128, 2, 128], BF16)
        sgm, t_sgm = mk("sgm", [128, D], F32)
        ot, t_ot = mk("ot", [128, D], F32)
        h3, _unused = mk("h3_", [128, D], F32)
        t_sgmh = [[T(f"sgmh{i}_{k}") for k in range(2)] for i in range(NB)]
        t_h3h = [[T(f"h3h{i}_{k}") for k in range(2)] for i in range(NB)]
        for i_ in range(NB):
            G(lambda e, i_=i_: e.memset(yk[i_][:], 0.0), (), [t_yk[i_]])

        for j in range(nt):
            b = j % NB
            P.dma("sp", lambda e, j=j, b=b: e.dma_start(out=hb_[b][:], in_=h_d[j * 128:(j + 1) * 128, :]), t_hb[b], reads=[t_hd[j]], writes=[t_hb[b]])
            P.dma("sp", lambda e, j=j, b=b: e.dma_start(out=pt_[b][:], in_=p_d[j * 128:(j + 1) * 128, :]), t_pt[b], writes=[t_pt[b]])
            for k in range(TOPK):
                P.dma("pool", lambda e, j=j, b=b, k=k: e.indirect_dma_start(
                    out=yk[b][:, k, :], out_offset=None, in_=ys_d, in_offset=bass.IndirectOffsetOnAxis(ap=posi[:, j, k:k + 1], axis=0),
                    bounds_check=_bcreg(e, nc), oob_is_err=False), t_yk[b], reads=[t_ysd, t_posi[j]], writes=[t_yk[b]], nbytes=int(_OPT["gx"] * 150e3))
            for k in range(TOPK):
                V(lambda e, j=j, b=b, k=k: e.scalar_tensor_tensor(out=hb_[b][:], in0=yk[b][:, k, :], scalar=gates[:, j, k:k + 1], in1=hb_[b][:],
                                                                 op0=ALU.mult, op1=ALU.add), [t_yk[b], t_gates[j], t_hb[b]], [t_hb[b]])
            if debug:
                P.dma("sp", lambda e, j=j, b=b: e.dma_start(out=dbg["h2"][j * 128:(j + 1) * 128, :], in_=hb_[b][:]), L["t_dbgs"][8 + j % 8], reads=[t_hb[b]])
            A(lambda e, b=b: e.activation(out=junk[0][:], in_=hb_[b][:], func=AF.Square, accum_out=sq[b][:, 0:1]), [t_hb[b]], [t_junk[0], t_sq[b]])
            V(lambda e, b=b: e.tensor_scalar(out=sq[b][:, 1:2], in0=sq[b][:, 0:1], scalar1=1.0 / D, scalar2=EPS, op0=ALU.mult, op1=ALU.add), [t_sq[b]], [t_sq[b]])
            G(lambda e, b=b: e.tensor_tensor(out=sq[b][:, 2:3], in0=sq[b][:, 1:2], in1=nhalf[:, 0:1], op=ALU.pow), [t_sq[b], t_nhalf], [t_sq[b]])
            A(lambda e, b=b: e.activation(out=xn3[b][:], in_=hb_[b][:], func=AF.Copy, scale=sq[b][:, 2:3]), [t_hb[b], t_sq[b]], [t_xn3[b]])
            b0_, b1_ = nxt(), nxt()
            bg_ = [nxt(), nxt()]
            bp_ = [nxt(), nxt()]
            pb0 = psb[b0_][:].bitcast(BF16)
            for c in range(8):
                M(lambda e, b=b, c=c, pb0=pb0: e.transpose(pb0[:, c * 128:(c + 1) * 128], xn3[b][:, c * 128:(c + 1) * 128], identb[:]),
                  [t_xn3[b], t_identb], [t_ps[b0_]])
            A(lambda e, b=b, pb0=pb0: e.copy(xn3T[b][:], pb0.rearrange("p (c t) -> p c t", c=8)), [t_ps[b0_]], [t_xn3T[b]], n=1024)
            for c in range(2):
                M(lambda e, b=b, c=c, b1_=b1_: e.transpose(psb[b1_][:, c * 128:(c + 1) * 128], pt_[b][:, c * 128:(c + 1) * 128], ident), [t_pt[b], t_cst], [t_ps[b1_]], n=400)
            A(lambda e, b=b, b1_=b1_: e.copy(pT[b][:], psb[b1_][:, 0:256].rearrange("p (c t) -> p c t", c=2)), [t_ps[b1_]], [t_pT[b]], n=256)
            for nb in range(2):
                for c in range(8):
                    M(lambda e, b=b, nb=nb, c=c, bg_=bg_: e.matmul(psb[bg_[nb]][:], lhsT=xn3T[b][:, c, :], rhs=pgw[:, c, nb * 512:(nb + 1) * 512], start=(c == 0), stop=(c == 7)),
                      [t_xn3T[b], t_pgw], [t_ps[bg_[nb]]], n=600)
            for nb in range(2):
                for c in range(2):
                    M(lambda e, b=b, nb=nb, c=c, bp_=bp_: e.matmul(psb[bp_[nb]][:], lhsT=pT[b][:, c, :], rhs=ppw[:, c, nb * 512:(nb + 1) * 512], start=(c == 0), stop=(c == 1)),
                      [t_pT[b], t_ppw], [t_ps[bp_[nb]]], n=600)
            for nb in range(2):
                hs = slice(nb * 512, (nb + 1) * 512)
                A(lambda e, b=b, nb=nb, bg_=bg_, hs=hs: e.activation(out=sgm[b][:, hs], in_=psb[bg_[nb]][:], func=AF.Sigmoid), [t_ps[bg_[nb]]], [t_sgmh[b][nb]], n=512)
                V(lambda e, b=b, nb=nb, bp_=bp_, hs=hs: e.tensor_tensor(out=sgm[b][:, hs], in0=sgm[b][:, hs], in1=psb[bp_[nb]][:], op=ALU.mult),
                  [t_sgmh[b][nb], t_ps[bp_[nb]]], [t_sgmh[b][nb]], n=512)
                V(lambda e, b=b, hs=hs: e.tensor_tensor(out=h3[b][:, hs], in0=hb_[b][:, hs], in1=sgm[b][:, hs], op=ALU.add), [t_hb[b], t_sgmh[b][nb]], [t_h3h[b][nb]], n=512)
            A(lambda e, b=b: e.activation(out=junk[0][:], in_=h3[b][:], func=AF.Square, accum_out=sq[b][:, 4:5]), t_h3h[b], [t_junk[0], t_sq[b]])
            V(lambda e, b=b: e.tensor_scalar(out=sq[b][:, 5:6], in0=sq[b][:, 4:5], scalar1=1.0 / D, scalar2=EPS, op0=ALU.mult, op1=ALU.add), [t_sq[b]], [t_sq[b]])
            G(lambda e, b=b: e.tensor_tensor(out=sq[b][:, 6:7], in0=sq[b][:, 5:6], in1=nhalf[:, 0:1], op=ALU.pow), [t_sq[b], t_nhalf], [t_sq[b]])
            A(lambda e, b=b: e.activation(out=ot[b][:], in_=h3[b][:], func=AF.Copy, scale=sq[b][:, 6:7]), t_h3h[b] + [t_sq[b]], [t_ot[b]], n=1024)
            G(lambda e, b=b: e.tensor_tensor(out=ot[b][:], in0=ot[b][:], in1=fnw[:], op=ALU.mult), [t_ot[b], t_fnw], [t_ot[b]], n=1024)
            P.dma("sp", lambda e, j=j, b=b: e.dma_start(out=out_d[j * 128:(j + 1) * 128, :], in_=ot[b][:]), t_ot[b], reads=[t_ot[b]])
        P.barrier(alltoks)


_CACHE = {}


def kernel(**inputs):
    inp = {k: np.asarray(v) for k, v in inputs.items()}
    if "nc" not in _CACHE:
        _CACHE["nc"] = build()[0]
    nc = _CACHE["nc"]
    maps = make_in_maps(inp, ncores=8)
    res = run_bass_kernel_spmd(nc, maps, core_ids=list(range(8)))
    out = np.stack([np.asarray(r["out"], dtype=np.float32) for r in res.results], axis=0)
    return out.reshape(8, SEQ, D)
```

```python
import numpy as np
from contextlib import ExitStack
import concourse.bass as bass
import concourse.mybir as mybir
from concourse.bass_utils import run_bass_kernel_spmd

F32 = mybir.dt.float32
BF16 = mybir.dt.bfloat16
I32 = mybir.dt.int32
U32 = mybir.dt.uint32
AF = mybir.ActivationFunctionType
ALU = mybir.AluOpType
AX = mybir.AxisListType

ENGS = ("pe", "dve", "act", "pool", "sp")


class Tok:
    __slots__ = ("name", "w", "r", "sem", "cnt", "multi")

    def __init__(self, name):
        self.name = name
        self.multi = False
        self.w = None
        self.r = {}
        self.sem = None
        self.cnt = 0


class Op:
    __slots__ = ("eng", "fn", "deps", "signal", "sigval", "dma", "idx", "dur", "xfer", "start", "fin", "prev_dma", "delay")

    def __init__(self, eng, fn, deps):
        self.eng = eng
        self.fn = fn
        self.deps = deps
        self.signal = False
        self.sigval = None
        self.dma = None
        self.dur = 0.5
        self.xfer = 0.0
        self.start = 0.0
        self.fin = 0.0
        self.prev_dma = None
        self.delay = 0.0


class DmaDep:
    __slots__ = ("tok", "val", "op")

    def __init__(self, tok, val, op=None):
        self.tok = tok
        self.val = val
        self.op = op


import os as _os
_OPT = {"est": int(_os.environ.get("KEST", "1")), "slack": float(_os.environ.get("KSLACK", "0.45")), "sdel": float(_os.environ.get("KSDEL", "25")), "crit": int(_os.environ.get("KCRIT", "1")), "gx": float(_os.environ.get("KGX", "3.5")), "run": int(_os.environ.get("KRUN", "2500"))}


class _Probe:
    def __getattr__(self, name):
        def f(*a, **k):
            self.__dict__["call"] = (name, a, k)
            return self
        return f


def _estimate(eng, fn):
    pr = _Probe()
    try:
        fn(pr)
        name, a, k = pr.call
    except Exception:
        return None

    def free(ap):
        n = 1
        for d in list(ap.shape)[1:]:
            n *= int(d)
        return n
    try:
        if eng == "pe":
            if name == "matmul":
                rhs = k.get("rhs", a[2] if len(a) > 2 else None)
                cols = free(rhs)
                mult = 4.0 if rhs.dtype == F32 else 1.0
                return 0.05 + 1.15 * mult * max(cols, 64) / 2400.0
            if name == "transpose":
                src = k.get("in_", a[1] if len(a) > 1 else None)
                mult = 3.0 if src.dtype == F32 else 1.0
                return 0.05 + mult * 128 / 2400.0 * 1.2
            return 0.1
        out = k.get("out", a[0] if a else None)
        n = free(out)
        if eng == "dve":
            return 0.12 + n / 960.0
        if eng == "act":
            return 0.22 + n / 1150.0
        if eng == "pool":
            if name == "tensor_scalar":
                return 0.5 + n / 70.0
            return 0.3 + n / 560.0
    except Exception:
        return None
    return None


class Prog:
    def __init__(self, nc, stack):
        self.nc = nc
        self.stack = stack
        self.ops = {e: [] for e in ENGS}
        self.nsem = 0
        self.limit = None
        self.count = 0
        self.segs = [[]]
        self.last_dma = {}
        self.sched = True
        self.slack = _OPT["slack"]

    def tok(self, name):
        return Tok(name)

    def toks(self, name, n):
        return [Tok(f"{name}{i}") for i in range(n)]

    def _collect(self, eng, reads, writes, dma_semtok=None):
        deps = []
        for t in reads:
            if t.multi:
                deps.extend(t.w.values())
            elif t.w is not None:
                deps.append(t.w)
        for t in writes:
            if t.multi:
                deps.extend(t.r.values())
                continue
            if t.w is not None:
                w = t.w
                skip = False
                if isinstance(w, DmaDep) and dma_semtok is not None and w.tok is dma_semtok:
                    skip = True
                if not skip:
                    deps.append(w)
            deps.extend(t.r.values())
        return deps

    def _commit(self, dep, key, reads, writes):
        for t in reads:
            if isinstance(dep, Op):
                t.r[id(dep)] = dep
            else:
                t.r[key] = dep
        for t in writes:
            if t.multi:
                t.w[key] = dep
                continue
            t.w = dep
            t.r = {}

    def op(self, eng, fn, reads=(), writes=(), n=None):
        self.count += 1
        if self.limit is not None and self.count > self.limit:
            return None
        deps = self._collect(eng, reads, writes)
        o = Op(eng, fn, deps)
        if n is not None:
            if eng == "pe":
                o.dur = 0.03 + n / 2400.0
            elif eng == "dve":
                o.dur = 0.12 + n / 960.0
            elif eng == "act":
                o.dur = 0.22 + n / 1200.0
            elif eng == "pool":
                o.dur = 0.25 + n / 450.0
        else:
            est = _estimate(eng, fn) if _OPT["est"] else None
            o.dur = est if est is not None else {"pe": 0.12, "dve": 0.5, "act": 0.6, "pool": 1.15, "sp": 0.1}[eng]
        self.ops[eng].append(o)
        self.segs[-1].append(o)
        self._commit(o, eng, reads, writes)
        return o

    def dma(self, eng, fn, semtok, reads=(), writes=(), nbytes=None, delay=0.0):
        self.count += 1
        if self.limit is not None and self.count > self.limit:
            return None
        deps = self._collect(eng, reads, writes, dma_semtok=semtok)
        o = Op(eng, fn, deps)
        if semtok.sem is None:
            semtok.sem = self.stack.enter_context(self.nc.semaphore(f"d{self.nsem}_{semtok.name}"))
            self.nsem += 1
        semtok.cnt += 16
        o.dma = DmaDep(semtok, semtok.cnt, o)
        o.dur = 1.2 if eng == "pool" else 0.15
        o.xfer = 2.0 + (nbytes or 0) / 150e3
        o.delay = delay
        o.prev_dma = self.last_dma.get(id(semtok))
        self.last_dma[id(semtok)] = o
        self.ops[eng].append(o)
        self.segs[-1].append(o)
        self._commit(o.dma, ("dma", id(semtok)), reads, writes)
        return o

    def wait_all(self, eng, toks):
        deps = self._collect(eng, (), toks)
        o = Op(eng, None, deps)
        o.dur = 0.05
        self.ops[eng].append(o)
        self.segs[-1].append(o)
        return o

    def barrier(self, all_toks):
        for e in ENGS:
            self.wait_all(e, all_toks)
        self.segs.append([])

    def schedule(self, runahead=None):
        runahead = runahead or _OPT["run"]
        new_ops = {e: [] for e in ENGS}
        t_base = 0.0
        for seg in self.segs:
            if not seg:
                continue
            n = len(seg)
            pos = {id(o): i for i, o in enumerate(seg)}
            succ = [[] for _ in range(n)]
            ndep = [0] * n
            preds = [None] * n
            for i, o in enumerate(seg):
                ps_ = []
                for d in o.deps:
                    p = d if isinstance(d, Op) else d.op
                    k = pos.get(id(p))
                    if k is None:
                        continue
                    ps_.append((k, isinstance(d, Op)))
                if o.prev_dma is not None:
                    k = pos.get(id(o.prev_dma))
                    if k is not None:
                        ps_.append((k, None))
                preds[i] = ps_
                ks = set(k for k, _ in ps_)
                ndep[i] = len(ks)
                for k in ks:
                    succ[k].append(i)
            bott = [0.0] * n
            for i in range(n - 1, -1, -1):
                m = 0.0
                for k in succ[i]:
                    if bott[k] > m:
                        m = bott[k]
                bott[i] = seg[i].dur + seg[i].xfer + m
            ready = {e: [] for e in ENGS}
            free = {e: t_base for e in ENGS}
            scheduled = [False] * n
            low = 0

            def make_ready(i):
                o = seg[i]
                t = t_base
                for k, kind in preds[i]:
                    p = seg[k]
                    if kind is None:
                        ft = p.start
                    elif kind:
                        ft = p.fin
                    else:
                        ft = p.fin + p.xfer
                    if kind is not None and p.eng != o.eng:
                        ft += 0.06
                    if ft > t:
                        t = ft
                ready[o.eng].append((t + o.delay, i))

            for i in range(n):
                if ndep[i] == 0:
                    make_ready(i)
            nleft = n
            while nleft > 0:
                while low < n and scheduled[low]:
                    low += 1
                lim = low + runahead
                best = None
                cands = []
                tmin = None
                for e in ENGS:
                    fr = free[e]
                    for (rt, i) in ready[e]:
                        if i >= lim:
                            continue
                        st_ = rt if rt > fr else fr
                        cands.append((st_, i, e, rt))
                        if tmin is None or st_ < tmin:
                            tmin = st_
                if cands:
                    slack = self.slack
                    if _OPT["crit"]:
                        best = max((c for c in cands if c[0] <= tmin + slack), key=lambda c: (bott[c[1]], -c[1]))
                    else:
                        best = min((c for c in cands if c[0] <= tmin + slack), key=lambda c: c[1])
                if best is None:
                    for e in ENGS:
                        for (rt, i) in ready[e]:
                            st_ = max(rt, free[e])
                            if best is None or i < best[1]:
                                best = (st_, i, e, rt)
                assert best is not None, "scheduler stuck"
                st_, i, e, rt = best
                ready[e].remove((rt, i))
                o = seg[i]
                o.start = st_
                o.fin = st_ + o.dur
                free[e] = o.fin
                scheduled[i] = True
                new_ops[e].append(o)
                nleft -= 1
                for k in succ[i]:
                    ndep[k] -= 1
                    if ndep[k] == 0:
                        make_ready(k)
            t_base = max(max(free.values()), max((o.fin + o.xfer) for o in seg))
        self.ops = new_ops
        self.est_total = t_base

    def emit(self):
        nc = self.nc
        if self.sched:
            self.schedule()
        for e in ENGS:
            for o in self.ops[e]:
                for d in o.deps:
                    if isinstance(d, Op) and not (d.eng == "pe" and e == "pe"):
                        d.signal = True
        esem = {}
        for e in ENGS:
            n = 0
            for o in self.ops[e]:
                if o.signal:
                    n += 1
                    o.sigval = n
            esem[e] = self.stack.enter_context(nc.semaphore(f"eng_{e}"))
        self.esem = esem
        stats = {}
        with nc.Block() as block:
            def run(ename, engine):
                waited = {}
                nw = 0
                for o in self.ops[ename]:
                    for d in o.deps:
                        if isinstance(d, Op):
                            if d.eng == "pe" and ename == "pe":
                                continue
                            sem, val, key = esem[d.eng], d.sigval, d.eng
                        else:
                            sem, val, key = d.tok.sem, d.val, id(d.tok)
                        if waited.get(key, 0) >= val:
                            continue
                        waited[key] = val
                        engine.wait_ge(sem, val)
                        nw += 1
                    if o.fn is None:
                        continue
                    inst = o.fn(engine)
                    if o.dma is not None:
                        inst.then_inc(o.dma.tok.sem, 16)
                    elif o.signal:
                        inst.then_inc(esem[ename], 1)
                stats[ename] = (len(self.ops[ename]), nw)

            @block.tensor
            def _(e):
                run("pe", e)

            @block.vector
            def _(e):
                run("dve", e)

            @block.scalar
            def _(e):
                run("act", e)

            @block.gpsimd
            def _(e):
                run("pool", e)

            @block.sync
            def _(e):
                run("sp", e)
        self.stats = stats
        return stats


D = 1024
SEQ = 4096
NT = SEQ // 128
H = 8
DH = 64
NE = 32
TOPK = 4
CAP = 640
NSLOT = NE * CAP
PLE = 256
EPS = 1e-5
LIMIT = 7.0
ALPHA = 1.702
BIGPOS = 1.0e6
WINS = (2, 4, 8, 16)

_off = {}
_n = 0
for _name, _w in (("ident", 128), ("mask", 128), ("ustrict", 128), ("ones", 128), ("invf", 64),
                  ("xiq", 8), ("xik", 8), ("gc", 512), ("bands", 12 * 128), ("ecap", 32),
                  ("nmix", 8), ("nple", 8), ("pscale", 4), ("bgate", 256), ("bup", 256)):
    _off[_name] = (_n, _w)
    _n += _w
CST_N = _n
_roff = {}
_n = 0
for _name, _w in (("gnw", 512), ("nmoe", 1024), ("rb", 32), ("fnw", 1024)):
    _roff[_name] = (_n, _w)
    _n += _w
ROW_N = _n


def host_consts(inp):
    c = np.zeros((128, CST_N), np.float32)

    def put(name, arr):
        o, w = _off[name]
        c[:, o:o + w] = np.asarray(arr, np.float32).reshape(128, w)

    idx = np.arange(128)
    put("ident", np.eye(128))
    put("mask", (idx[None, :] >= idx[:, None]).astype(np.float32))
    put("ustrict", (idx[:, None] < idx[None, :]).astype(np.float32))
    put("ones", np.ones((128, 128)))
    half = DH // 2
    invf = (10000.0 ** (-(np.arange(half, dtype=np.float32)) / np.float32(half))).astype(np.float32)
    put("invf", np.tile(np.concatenate([invf, invf])[None, :], (128, 1)))
    lg = np.log1p(-np.power(2.0, -5.0 - np.arange(H, dtype=np.float64)))
    cpos = (idx + 1.0)[:, None]
    put("xiq", np.exp(cpos * lg[None, :]))
    put("xik", np.exp(-cpos * lg[None, :]) * (DH ** -0.5))
    gc = np.zeros((128, 512))
    for p in range(128):
        for h in range(H):
            if p // 64 == h % 2:
                gc[p, h * 64:(h + 1) * 64] = np.exp(128.0 * lg[h])
    put("gc", gc)
    bands = np.zeros((128, 12, 128))
    for g, w in enumerate(WINS):
        tp = idx[:, None]
        t = idx[None, :]
        inwin = (tp <= t) & (tp > t - w)
        bands[:, 3 * g + 0, :] = inwin / float(w) - (tp == t)
        cnt0 = np.minimum(t + 1.0, float(w))
        bands[:, 3 * g + 1, :] = inwin / cnt0 - (tp == t)
        bands[:, 3 * g + 2, :] = (tp >= 129 + t - w) / float(w)
    put("bands", bands)
    put("ecap", np.tile((np.arange(NE) * CAP)[None, :], (128, 1)))
    put("nmix", inp["norm_mix_w"][0].reshape(8, 128).T)
    put("nple", inp["norm_ple_w"][0].reshape(8, 128).T)
    put("pscale", inp["pool_scale"][0].reshape(4, 128).T)
    bgu = inp["expert_b_gate_up"][0]
    put("bgate", bgu[:, 0::2].reshape(NE, 8, 128).transpose(2, 0, 1))
    put("bup", bgu[:, 1::2].reshape(NE, 8, 128).transpose(2, 0, 1))
    r = np.zeros((1, ROW_N), np.float32)
    for name, v in (("gnw", inp["ret_gn_w"][0]), ("nmoe", inp["norm_moe_w"][0]),
                    ("rb", inp["router_b"][0]), ("fnw", inp["final_norm_w"])):
        o, w = _roff[name]
        r[0, o:o + w] = v
    return c, r


def _bc(ap, shape):
    return ap.broadcast_to(shape)


_BCREG = {}


def _bcreg(e, nc):
    if _BCREG.get("nc") is not nc:
        r = e.alloc_register("slot_bound")
        e.reg_mov(r, NSLOT - 1)
        _BCREG["nc"] = nc
        _BCREG["r"] = r
    return _BCREG["r"]


class Ctx:
    pass


def build(nt=NT, phases=4, debug=False, limit=None, n_exp=NE, sched=True):
    nc = bass.Bass("TRN2", target_bir_lowering=False)
    dt = lambda name, shape, dtype, kind: nc.dram_tensor(name, shape, dtype, kind=kind).ap()
    x_d = dt("x", [SEQ, D], F32, "ExternalInput")
    p_d = dt("p", [SEQ, PLE], F32, "ExternalInput")
    pos_d = dt("pos", [128, NT], I32, "ExternalInput")
    cst_d = dt("cst", [128, CST_N], F32, "ExternalInput")
    rows_d = dt("rows", [1, ROW_N], F32, "ExternalInput")
    win_d = dt("w_in", [D, 2560], F32, "ExternalInput")
    wout_d = dt("w_out", [D, D], F32, "ExternalInput")
    poolw_d = dt("pool_w", [4, 128, 128], F32, "ExternalInput")
    rw_d = dt("router_w", [D, NE], F32, "ExternalInput")
    wg_d = dt("w_g", [NE, D, D], F32, "ExternalInput")
    wu_d = dt("w_u", [NE, D, D], F32, "ExternalInput")
    wd_d = dt("w_d", [NE, D, D], F32, "ExternalInput")
    bd_d = dt("b_d", [NE, D], F32, "ExternalInput")
    pgw_d = dt("ple_gate_w", [D, D], F32, "ExternalInput")
    ppw_d = dt("ple_proj_w", [PLE, D], F32, "ExternalInput")
    out_d = dt("out", [SEQ, D], F32, "ExternalOutput")
    xs_d = nc.dram_tensor("xs_scr", [NSLOT, D], BF16).ap()
    ys_d = nc.dram_tensor("ys_scr", [NSLOT, D], F32).ap()
    h_d = nc.dram_tensor("h_scr", [SEQ, D], F32).ap()
    dbg = {}
    if debug:
        dbg["h1"] = dt("dbg_h1", [SEQ, D], F32, "ExternalOutput")
        dbg["gate"] = dt("dbg_gate", [128, NT * 4], F32, "ExternalOutput")
        dbg["pos"] = dt("dbg_pos", [128, NT * 4], I32, "ExternalOutput")
        dbg["h2"] = dt("dbg_h2", [SEQ, D], F32, "ExternalOutput")

    with ExitStack() as gst:
        P = Prog(nc, gst)
        P.limit = limit
        P.sched = sched
        alltoks = []

        def T(name):
            t = P.tok(name)
            alltoks.append(t)
            return t

        def sbuf(st, name, shape, dtype):
            return st.enter_context(nc.sbuf_tensor("s_" + name, shape, dtype))

        V = lambda fn, r=(), w=(), n=None: P.op("dve", fn, r, w, n)
        A = lambda fn, r=(), w=(), n=None: P.op("act", fn, r, w, n)
        G = lambda fn, r=(), w=(), n=None: P.op("pool", fn, r, w, n)
        M = lambda fn, r=(), w=(), n=None: P.op("pe", fn, r, w, n)

        cst = sbuf(gst, "cst", [128, CST_N], F32)
        t_cst = T("cst")
        identb = sbuf(gst, "identb", [128, 128], BF16)
        t_identb = T("identb")
        gates = sbuf(gst, "gates", [128, NT, 4], F32)
        posi = sbuf(gst, "posi", [128, NT, 4], I32)
        t_gates = [T(f"gates{j}") for j in range(NT)]
        t_posi = [T(f"posi{j}") for j in range(NT)]
        nhalf = sbuf(gst, "nhalf", [128, 8], F32)
        t_nhalf = T("nhalf")
        wgu_buf, wd_buf, t_wgu, t_wd = [], [], [], []
        psb = [gst.enter_context(nc.psum_tensor(f"ps{i}", [128, 512], F32)) for i in range(8)]
        t_ps = [T(f"ps{i}") for i in range(8)]

        def C(name):
            o, w = _off[name]
            return cst[:, o:o + w]

        t_hd = [T(f"hd{j}") for j in range(NT)]
        t_dbgs = [T(f"dbgs{i}") for i in range(16)] if debug else []
        t_xsd = T("xsd")
        t_xsd.multi = True
        t_xsd.w = {}
        t_zfd = T("zfd")
        t_ysd = T("ysd")
        t_ysd.multi = True
        t_ysd.w = {}

        P.dma("sp", lambda e: e.dma_start(out=cst[:], in_=cst_d), t_cst, writes=[t_cst])
        V(lambda e: e.tensor_copy(identb[:], C("ident")), [t_cst], [t_identb])
        G(lambda e: e.memset(nhalf[:], -0.5), (), [t_nhalf])
        if debug:
            G(lambda e: e.memset(gates[:], 0.0), (), t_gates)
            G(lambda e: e.memset(posi[:], 0), (), t_posi)
        ident = C("ident")

        def load_expert(e_idx, slot):
            wg_v = wg_d[e_idx].rearrange("(c p) n -> p c n", p=128)
            wu_v = wu_d[e_idx].rearrange("(c p) n -> p c n", p=128)
            wd_v = wd_d[e_idx].rearrange("(c p) n -> p c n", p=128)
            for hc in range(2):
                cs = slice(hc * 4, hc * 4 + 4)
                P.dma("pool", lambda e, cs=cs: e.dma_start(out=wgu_buf[slot][:, cs, 0:1024], in_=wg_v[:, cs, :]),
                      t_wgu[slot], writes=[t_wgu[slot]])
                P.dma("pool", lambda e, cs=cs: e.dma_start(out=wgu_buf[slot][:, cs, 1024:2048], in_=wu_v[:, cs, :]),
                      t_wgu[slot], writes=[t_wgu[slot]])
                P.dma("pool", lambda e, cs=cs: e.dma_start(out=wd_buf[slot][:, cs, :], in_=wd_v[:, cs, :]),
                      t_wd[slot], writes=[t_wd[slot]])

        with ExitStack() as st:
            RN1 = _roff["fnw"][0]
            rows = sbuf(st, "rows", [128, RN1], F32)
            t_rows = T("rows")
            P.dma("sp", lambda e: e.dma_start(out=rows[:], in_=rows_d[0, 0:RN1].partition_broadcast(128)), t_rows, writes=[t_rows])

            def R(name):
                o, w = _roff[name]
                return rows[:, o:o + w]

            w_in = sbuf(st, "w_in", [128, 8, 2560], BF16)
            t_win = T("w_in")
            w_out = sbuf(st, "w_out", [128, 8, 1024], BF16)
            t_wout = T("w_out")
            poolw = sbuf(st, "poolw", [128, 4, 128], BF16)
            t_poolw = T("poolw")
            rw = sbuf(st, "rw", [128, 8, NE], F32)
            t_rw = T("rw")
            posi_t = sbuf(st, "pos_i", [128, NT], I32)
            posf = sbuf(st, "pos_f", [128, NT], F32)
            CC = sbuf(st, "CC", [128, NT, 64], F32)
            SS = sbuf(st, "SS", [128, NT, 64], F32)
            st_setup = ExitStack()
            ang = sbuf(st_setup, "ang", [128, NT, 64], F32)
            tmpa = sbuf(st_setup, "tmpa", [128, NT, 64], F32)
            stage = [sbuf(st_setup, f"stage{i}", [128, 1280], F32) for i in range(2)]
            t_stage = [T(f"stage{i}") for i in range(2)]
            win_v = win_d.rearrange("(c p) n -> p c n", p=128)
            for c2 in range(16):
                s = c2 % 2
                c, hf = c2 // 2, c2 % 2
                P.dma("sp", lambda e, c=c, s=s, hf=hf: e.dma_start(out=stage[s][:], in_=win_v[:, c, hf * 1280:(hf + 1) * 1280]), t_stage[s], writes=[t_stage[s]])
                o, _ = _off["nmix"]
                if c2 % 2 == 0:
                    V(lambda e, c=c, s=s, o=o, hf=hf: e.tensor_scalar(out=w_in[:, c, hf * 1280:(hf + 1) * 1280], in0=stage[s][:], scalar1=cst[:, o + c:o + c + 1],
                                                                    scalar2=None, op0=ALU.mult), [t_stage[s], t_cst], [t_win], n=1280)
                else:
                    A(lambda e, c=c, s=s, o=o, hf=hf: e.activation(out=w_in[:, c, hf * 1280:(hf + 1) * 1280], in_=stage[s][:], func=AF.Copy, scale=cst[:, o + c:o + c + 1]),
                      [t_stage[s], t_cst], [t_win], n=1280)
            P.dma("pool", lambda e: e.dma_start(out=w_out[:], in_=wout_d.rearrange("(c p) n -> p c n", p=128)), t_wout, writes=[t_wout])
            P.dma("pool", lambda e: e.dma_start(out=poolw[:], in_=poolw_d.rearrange("g c d -> c g d")), t_poolw, writes=[t_poolw])
            P.dma("sp", lambda e: e.dma_start(out=rw[:], in_=rw_d.rearrange("(c p) n -> p c n", p=128)), t_rw, writes=[t_rw])

            t_pos, t_ang, t_tmpa, t_CC, t_SS = T("pos"), T("ang"), T("tmpa"), T("CC"), T("SS")
            P.dma("sp", lambda e: e.dma_start(out=posi_t[:], in_=pos_d), t_pos, writes=[t_pos])
            V(lambda e: e.tensor_copy(posf[:], posi_t[:]), [t_pos], [t_pos])
            V(lambda e: e.tensor_tensor(out=ang[:], in0=_bc(posf[:].unsqueeze(2), [128, NT, 64]),
                                        in1=_bc(C("invf").unsqueeze(1), [128, NT, 64]), op=ALU.mult), [t_pos, t_cst], [t_ang])
            TWO_PI = float(2.0 * np.pi)

            def sin_table(dst, t_dst, shift):
                V(lambda e: e.tensor_scalar(out=tmpa[:], in0=ang[:], scalar1=shift, scalar2=None, op0=ALU.add), [t_ang], [t_tmpa])
                V(lambda e: e.tensor_scalar(out=dst[:].bitcast(I32), in0=tmpa[:], scalar1=1.0 / TWO_PI, scalar2=None, op0=ALU.mult), [t_tmpa], [t_dst])
                V(lambda e: e.tensor_copy(dst[:], dst[:].bitcast(I32)), [t_dst], [t_dst])
                V(lambda e: e.scalar_tensor_tensor(out=tmpa[:], in0=dst[:], scalar=-TWO_PI, in1=tmpa[:], op0=ALU.mult, op1=ALU.add),
                  [t_dst, t_tmpa], [t_tmpa])
                V(lambda e: e.tensor_scalar(out=dst[:], in0=tmpa[:], scalar1=float(np.pi), scalar2=-TWO_PI, op0=ALU.is_gt, op1=ALU.mult),
                  [t_tmpa], [t_dst])
                V(lambda e: e.tensor_tensor(out=tmpa[:], in0=tmpa[:], in1=dst[:], op=ALU.add), [t_tmpa, t_dst], [t_tmpa])
                V(lambda e: e.tensor_scalar(out=dst[:], in0=tmpa[:], scalar1=-float(np.pi), scalar2=TWO_PI, op0=ALU.is_lt, op1=ALU.mult),
                  [t_tmpa], [t_dst])
                V(lambda e: e.tensor_tensor(out=tmpa[:], in0=tmpa[:], in1=dst[:], op=ALU.add), [t_tmpa, t_dst], [t_tmpa])
                V(lambda e: e.tensor_scalar(out=tmpa[:], in0=tmpa[:], scalar1=-3.1415925, scalar2=3.1415925, op0=ALU.max, op1=ALU.min),
                  [t_tmpa], [t_tmpa])
                A(lambda e: e.activation(out=dst[:], in_=tmpa[:], func=AF.Sin), [t_tmpa], [t_dst])

            sin_table(CC, t_CC, float(np.pi / 2))
            sin_table(SS, t_SS, 0.0)
            V(lambda e: e.tensor_scalar(out=SS[:, :, 0:32], in0=SS[:, :, 0:32], scalar1=-1.0, scalar2=None, op0=ALU.mult), [t_SS], [t_SS])

            P.barrier(alltoks)
            st_setup.close()
            state = sbuf(st, "state", [128, 512], F32)
            stateb = sbuf(st, "stateb", [128, 512], BF16)
            t_state, t_stateb = T("state"), T("stateb")
            V(lambda e: e.memset(state[:], 0.0), (), [t_state])
            V(lambda e: e.memset(stateb[:], 0.0), (), [t_stateb])
            macc = sbuf(st, "macc", [128, NE], F32)
            t_macc = T("macc")
            V(lambda e: e.memset(macc[:], 0.0), (), [t_macc])
            us = [sbuf(st, f"us{i}", [128, 512], F32) for i in range(2)]
            t_us = [T(f"us{i}") for i in range(2)]

            NB = 2
            def mk(name, shape, dtype, n=NB):
                return [sbuf(st, f"{name}{i}", shape, dtype) for i in range(n)], [T(f"{name}{i}") for i in range(n)]
            xt, t_xt = mk("xt", [128, D], F32)
            junk, t_junk = mk("junk", [128, D], BF16, 1)
            ssq, t_ssq = mk("ssq", [128, 8], F32)
            xT, t_xT = mk("xT", [128, 8, 128], BF16)
            qs, t_qs = mk("qs", [128, 512], F32)
            ks, t_ks = mk("ks", [128, 512], F32)
            vb, t_vb = mk("vb", [128, 512], BF16)
            sg, t_sg = mk("sg", [128, 512], F32)
            qa, t_qa = mk("qa", [128, 512], F32)
            qb, t_qb = mk("qb", [128, 512], F32)
            ka, t_ka, kb, t_kb = qa, t_qa, qb, t_qb
            qt, t_qt = mk("qt", [128, 512], BF16)
            kt, t_kt = mk("kt", [128, 512], BF16)
            qkT, t_qkT = mk("qkT", [128, 1536], BF16)
            for i_ in range(NB):
                V(lambda e, i_=i_: e.memset(qkT[i_][:], 0.0), (), [t_qkT[i_]])
            PT, t_PT = mk("PT", [128, 1024], BF16)
            ysb, t_ysb = mk("ysb", [128, 512], F32)
            ysq, t_ysq = mk("ysq", [128, 512], F32, 1)
            ysq, t_ysq = ysq * 2, t_ysq * 2
            gst8, t_gst8 = mk("gst8", [128, 6, 8], F32)
            gsg, t_gsg = mk("gsg", [128, 512], F32)
            ret, t_ret = mk("ret", [128, 512], BF16)
            mixT, t_mixT = mk("mixT", [128, 8, 128], BF16)
            pooledT, t_pooledT = mk("pooledT", [128, 4, 128], BF16)
            ht, t_ht = mk("ht", [128, D], F32)
            xn2, t_xn2 = mk("xn2", [128, D], F32, 1)
            xn2, t_xn2 = xn2 * 2, t_xn2 * 2
            NBX = 5
            xn2b, t_xn2b = mk("xn2b", [128, D], BF16, NBX)
            pend = []
            zt = sbuf(st, "zt", [128, 2048], BF16)
            t_zt, t_zf = T("zt"), T("zf")
            G(lambda e: e.memset(zt[:], 0.0), (), [t_zt])
            def emit_zero_fill(extra_reads):
                if nt < NT:
                    starts = list(range(0, NSLOT, 256))
                else:
                    starts = [kz * CAP + CAP - 256 for kz in range(NE)]
                for r0 in starts:
                    P.dma("act", lambda e, r0=r0: e.dma_start(out=xs_d[r0:r0 + 256, :].rearrange("(p r) d -> p (r d)", p=128), in_=zt[:]),
                          t_zf, reads=[t_zt] + extra_reads, writes=[t_zfd], nbytes=512 * 1024)
            xn2T, t_xn2T = mk("xn2T", [128, 8, 128], F32, 1)
            xn2T, t_xn2T = xn2T * 2, t_xn2T * 2
            rt, t_rt = mk("rt", [128, 8, 32], F32)
            top8, t_top8 = mk("top8", [128, 16], F32)
            posk, t_posk = mk("posk", [128, 4], F32)

            bank_ctr = [0]

            def nxt():
                v = bank_ctr[0] % 8
                bank_ctr[0] += 1
                return v

            for j in range(nt):
                b = j % NB
                P.dma("sp", lambda e, j=j, b=b: e.dma_start(out=xt[b][:], in_=x_d[j * 128:(j + 1) * 128, :]), t_xt[b], writes=[t_xt[b]])
                if j == min(1, nt - 1):
                    emit_zero_fill([t_xt[b]])
                A(lambda e, b=b: e.activation(out=ht[b][:], in_=xt[b][:], func=AF.Square, accum_out=ssq[b][:, 0:1]),
                  [t_xt[b]], [t_ht[b], t_ssq[b]])
                V(lambda e, b=b: e.tensor_scalar(out=ssq[b][:, 1:2], in0=ssq[b][:, 0:1], scalar1=1.0 / D, scalar2=EPS, op0=ALU.mult, op1=ALU.add),
                  [t_ssq[b]], [t_ssq[b]])
                G(lambda e, b=b: e.tensor_tensor(out=ssq[b][:, 2:3], in0=ssq[b][:, 1:2], in1=nhalf[:, 0:1], op=ALU.pow),
                  [t_ssq[b], t_nhalf], [t_ssq[b]])
                rstd = ssq[b][:, 2:3]
                bx0, bx1 = nxt(), nxt()
                for c in range(8):
                    bank = (bx0, bx1)[c // 4]
                    M(lambda e, b=b, c=c, bank=bank: e.transpose(psb[bank][:, (c % 4) * 128:(c % 4 + 1) * 128], xt[b][:, c * 128:(c + 1) * 128], ident),
                      [t_xt[b], t_cst], [t_ps[bank]])
                V(lambda e, b=b, bx0=bx0: e.tensor_copy(xT[b][:, 0:4, :], psb[bx0][:].rearrange("p (c t) -> p c t", c=4)), [t_ps[bx0]], [t_xT[b]])
                A(lambda e, b=b, bx1=bx1: e.copy(xT[b][:, 4:8, :], psb[bx1][:].rearrange("p (c t) -> p c t", c=4)), [t_ps[bx1]], [t_xT[b]])
                bp = [nxt() for _ in range(5)]
                for nb in range(5):
                    for c in range(8):
                        M(lambda e, b=b, nb=nb, c=c, bk=bp[nb]: e.matmul(psb[bk][:], lhsT=xT[b][:, c, :], rhs=w_in[:, c, nb * 512:(nb + 1) * 512],
                                                             start=(c == 0), stop=(c == 7)),
                          [t_xT[b], t_win], [t_ps[bp[nb]]], n=530)
                ub = j % 2
                A(lambda e, bp=bp, b=b: e.activation(out=qs[b][:], in_=psb[bp[0]][:], func=AF.Copy, scale=ssq[b][:, 2:3]), [t_ps[bp[0]], t_ssq[b]], [t_qs[b]])
                A(lambda e, bp=bp, b=b: e.activation(out=ks[b][:], in_=psb[bp[1]][:], func=AF.Copy, scale=ssq[b][:, 2:3]), [t_ps[bp[1]], t_ssq[b]], [t_ks[b]])
                A(lambda e, bp=bp, b=b: e.activation(out=vb[b][:], in_=psb[bp[2]][:], func=AF.Copy, scale=ssq[b][:, 2:3]), [t_ps[bp[2]], t_ssq[b]], [t_vb[b]])
                A(lambda e, bp=bp, b=b: e.activation(out=sg[b][:], in_=psb[bp[3]][:], func=AF.Silu, scale=ssq[b][:, 2:3]), [t_ps[bp[3]], t_ssq[b]], [t_sg[b]])
                A(lambda e, bp=bp, b=b, ub=ub: e.activation(out=us[ub][:], in_=psb[bp[4]][:], func=AF.Copy, scale=ssq[b][:, 2:3]), [t_ps[bp[4]], t_ssq[b]], [t_us[ub]])
                ccj = _bc(CC[:, j, :].unsqueeze(1), [128, 8, 64])
                ssj_lo = _bc(SS[:, j, 0:32].unsqueeze(1), [128, 8, 32])
                ssj_hi = _bc(SS[:, j, 32:64].unsqueeze(1), [128, 8, 32])
                for (src, t_src, aa, t_aa, bb, t_bb, dst, t_dst, xin) in (
                        (qs, t_qs, qa, t_qa, qb, t_qb, qt, t_qt, "xiq"), (ks, t_ks, ka, t_ka, kb, t_kb, kt, t_kt, "xik")):
                    s4 = src[b][:].rearrange("p (h two d) -> p h two d", two=2, d=32)
                    b4 = bb[b][:].rearrange("p (h two d) -> p h two d", two=2, d=32)
                    V(lambda e, b=b, src=src, aa=aa, ccj=ccj: e.tensor_tensor(out=aa[b][:].rearrange("p (h d) -> p h d", d=64),
                                                                              in0=src[b][:].rearrange("p (h d) -> p h d", d=64), in1=ccj, op=ALU.mult),
                      [t_src[b], t_CC], [t_aa[b]])
                    V(lambda e, s4=s4, b4=b4, ssj_lo=ssj_lo: e.tensor_tensor(out=b4[:, :, 0, :], in0=s4[:, :, 1, :], in1=ssj_lo, op=ALU.mult),
                      [t_src[b], t_SS], [t_bb[b]])
                    V(lambda e, s4=s4, b4=b4, ssj_hi=ssj_hi: e.tensor_tensor(out=b4[:, :, 1, :], in0=s4[:, :, 0, :], in1=ssj_hi, op=ALU.mult),
                      [t_src[b], t_SS], [t_bb[b]])
                    V(lambda e, b=b, aa=aa, bb=bb: e.tensor_tensor(out=aa[b][:], in0=aa[b][:], in1=bb[b][:], op=ALU.add), [t_aa[b], t_bb[b]], [t_aa[b]])
                    V(lambda e, b=b, aa=aa, dst=dst, xin=xin: e.tensor_tensor(out=dst[b][:].rearrange("p (h d) -> p h d", d=64),
                                                                           in0=aa[b][:].rearrange("p (h d) -> p h d", d=64),
                                                                           in1=_bc(C(xin).unsqueeze(2), [128, 8, 64]), op=ALU.mult),
                      [t_aa[b], t_cst], [t_dst[b]])
                bqk = nxt()
                pb2 = psb[bqk][:].bitcast(BF16)
                for i in range(4):
                    M(lambda e, b=b, i=i, pb2=pb2: e.transpose(pb2[:, i * 128:(i + 1) * 128], qt[b][:, i * 128:(i + 1) * 128], identb[:]),
                      [t_qt[b], t_identb], [t_ps[bqk]])
                for i in range(4):
                    M(lambda e, b=b, i=i, pb2=pb2: e.transpose(pb2[:, 512 + i * 128:512 + (i + 1) * 128], kt[b][:, i * 128:(i + 1) * 128], identb[:]),
                      [t_kt[b], t_identb], [t_ps[bqk]])
                A(lambda e, b=b, pb2=pb2: e.copy(qkT[b][:, 0:512], pb2[:, 0:512]), [t_ps[bqk]], [t_qkT[b]])
                A(lambda e, b=b, pb2=pb2: e.copy(qkT[b][0:64, 512:1024], pb2[0:64, 512:1024]), [t_ps[bqk]], [t_qkT[b]])
                A(lambda e, b=b, pb2=pb2: e.copy(qkT[b][64:128, 1024:1536], pb2[64:128, 512:1024]), [t_ps[bqk]], [t_qkT[b]])
                bs = [nxt(), nxt()]
                for h in range(H):
                    hb = (h % 2) * 64
                    hh = h // 2
                    bank = bs[h // 4]
                    M(lambda e, b=b, h=h, hb=hb, hh=hh, bank=bank: e.matmul(
                        psb[bank][:, (h % 4) * 128:(h % 4 + 1) * 128],
                        lhsT=qkT[b][:, 512 + (h % 2) * 512 + hh * 128:512 + (h % 2) * 512 + (hh + 1) * 128],
                        rhs=qkT[b][:, hh * 128:(hh + 1) * 128], start=True, stop=True),
                      [t_qkT[b]], [t_ps[bank]])
                mask4 = _bc(C("mask").unsqueeze(1), [128, 4, 128])
                for half in range(2):
                    V(lambda e, b=b, half=half, bs=bs: e.tensor_tensor(out=PT[b][:, half * 512:(half + 1) * 512].rearrange("p (h c) -> p h c", c=128),
                                                                in0=psb[bs[half]][:].rearrange("p (h c) -> p h c", c=128), in1=mask4, op=ALU.mult),
                      [t_ps[bs[half]], t_cst], [t_PT[b]])
                by, bkv = nxt(), nxt()
                for h in range(H):
                    hb = (h % 2) * 64
                    hh = h // 2
                    M(lambda e, b=b, h=h, by=by: e.matmul(psb[by][:, h * 64:(h + 1) * 64], lhsT=PT[b][:, h * 128:(h + 1) * 128],
                                                   rhs=vb[b][:, h * 64:(h + 1) * 64], start=True, stop=False),
                      [t_PT[b], t_vb[b]], [t_ps[by]])
                    M(lambda e, b=b, h=h, hb=hb, hh=hh, by=by: e.matmul(psb[by][:, h * 64:(h + 1) * 64], lhsT=qkT[b][:, hh * 128:(hh + 1) * 128],
                                                                 rhs=stateb[:, h * 64:(h + 1) * 64], start=False, stop=True),
                      [t_qkT[b], t_stateb], [t_ps[by]])
                for hh in range(4):
                    M(lambda e, b=b, hh=hh, bkv=bkv: e.matmul(psb[bkv][:, hh * 128:(hh + 1) * 128], lhsT=kt[b][:, hh * 128:(hh + 1) * 128],
                                                     rhs=vb[b][:, hh * 128:(hh + 1) * 128], start=True, stop=True),
                      [t_kt[b], t_vb[b]], [t_ps[bkv]])
                V(lambda e, bkv=bkv: e.tensor_tensor(out=state[:], in0=state[:], in1=psb[bkv][:], op=ALU.add), [t_state, t_ps[bkv]], [t_state])
                V(lambda e: e.tensor_tensor(out=state[:], in0=state[:], in1=C("gc"), op=ALU.mult), [t_state, t_cst], [t_state])
                V(lambda e: e.tensor_copy(stateb[:], state[:]), [t_state], [t_stateb])
                A(lambda e, b=b, by=by: e.copy(ysb[b][:], psb[by][:]), [t_ps[by]], [t_ysb[b]])
                A(lambda e, b=b, by=by: e.activation(out=ysq[b][:], in_=psb[by][:], func=AF.Square), [t_ps[by]], [t_ysq[b]])
                G(lambda e, b=b: e.tensor_tensor(out=gsg[b][:], in0=sg[b][:], in1=R("gnw"), op=ALU.mult), [t_sg[b], t_rows], [t_gsg[b]])
                g8 = gst8[b]
                V(lambda e, b=b, g8=g8: e.tensor_reduce(out=g8[:, 0, :], in_=ysb[b][:].rearrange("p (h d) -> p h d", d=64), axis=AX.X, op=ALU.add),
                  [t_ysb[b]], [t_gst8[b]])
                V(lambda e, b=b, g8=g8: e.tensor_reduce(out=g8[:, 1, :], in_=ysq[b][:].rearrange("p (h d) -> p h d", d=64), axis=AX.X, op=ALU.add),
                  [t_ysq[b]], [t_gst8[b]])
                V(lambda e, g8=g8: e.tensor_scalar(out=g8[:, 2, :], in0=g8[:, 0, :], scalar1=1.0 / DH, scalar2=None, op0=ALU.mult), [t_gst8[b]], [t_gst8[b]])
                V(lambda e, g8=g8: e.tensor_tensor(out=g8[:, 3, :], in0=g8[:, 2, :], in1=g8[:, 2, :], op=ALU.mult), [t_gst8[b]], [t_gst8[b]])
                V(lambda e, g8=g8: e.scalar_tensor_tensor(out=g8[:, 4, :], in0=g8[:, 1, :], scalar=1.0 / DH, in1=g8[:, 3, :], op0=ALU.mult, op1=ALU.subtract),
                  [t_gst8[b]], [t_gst8[b]])
                V(lambda e, g8=g8: e.tensor_scalar(out=g8[:, 4, :], in0=g8[:, 4, :], scalar1=EPS, scalar2=None, op0=ALU.add), [t_gst8[b]], [t_gst8[b]])
                G(lambda e, g8=g8: e.tensor_tensor(out=g8[:, 5, :], in0=g8[:, 4, :], in1=nhalf[:, 0:8], op=ALU.pow), [t_gst8[b], t_nhalf], [t_gst8[b]])
                y3 = ysb[b][:].rearrange("p (h d) -> p h d", d=64)
                V(lambda e, y3=y3, g8=g8: e.tensor_tensor(out=y3, in0=y3, in1=_bc(g8[:, 2, :].unsqueeze(2), [128, 8, 64]), op=ALU.subtract),
                  [t_ysb[b], t_gst8[b]], [t_ysb[b]])
                V(lambda e, y3=y3, g8=g8: e.tensor_tensor(out=y3, in0=y3, in1=_bc(g8[:, 5, :].unsqueeze(2), [128, 8, 64]), op=ALU.mult),
                  [t_ysb[b], t_gst8[b]], [t_ysb[b]])
                V(lambda e, b=b: e.tensor_tensor(out=ret[b][:], in0=ysb[b][:], in1=gsg[b][:], op=ALU.mult), [t_ysb[b], t_gsg[b]], [t_ret[b]])
                brt = nxt()
                pb1 = psb[brt][:].bitcast(BF16)
                for i in range(4):
                    M(lambda e, b=b, i=i, pb1=pb1: e.transpose(pb1[:, i * 128:(i + 1) * 128], ret[b][:, i * 128:(i + 1) * 128], identb[:]),
                      [t_ret[b], t_identb], [t_ps[brt]])
                A(lambda e, b=b, pb1=pb1: e.copy(mixT[b][:, 0:4, :], pb1[:, 0:512].rearrange("p (c t) -> p c t", c=4)), [t_ps[brt]], [t_mixT[b]])
                bo, _ = _off["bands"]
                bpl, bmx = nxt(), nxt()
                for g in range(4):
                    kind = 1 if j == 0 else 0
                    M(lambda e, g=g, ub=ub, kind=kind, bo=bo, j=j, bpl=bpl: e.matmul(psb[bpl][:, g * 128:(g + 1) * 128], lhsT=us[ub][:, g * 128:(g + 1) * 128],
                                                                       rhs=cst[:, bo + (3 * g + kind) * 128:bo + (3 * g + kind + 1) * 128],
                                                                       start=True, stop=(j == 0)),
                      [t_us[ub], t_cst], [t_ps[bpl]])
                    if j > 0:
                        M(lambda e, g=g, ub=ub, bo=bo, bpl=bpl: e.matmul(psb[bpl][:, g * 128:(g + 1) * 128], lhsT=us[1 - ub][:, g * 128:(g + 1) * 128],
                                                                rhs=cst[:, bo + (3 * g + 2) * 128:bo + (3 * g + 3) * 128], start=False, stop=True),
                          [t_us[1 - ub], t_cst], [t_ps[bpl]])
                V(lambda e, b=b, bpl=bpl: e.tensor_copy(pooledT[b][:], psb[bpl][:].rearrange("p (g t) -> p g t", g=4)), [t_ps[bpl]], [t_pooledT[b]])
                for g in range(4):
                    M(lambda e, b=b, g=g, bmx=bmx: e.matmul(psb[bmx][:, g * 128:(g + 1) * 128], lhsT=poolw[:, g, :], rhs=pooledT[b][:, g, :], start=True, stop=True),
                      [t_poolw, t_pooledT[b]], [t_ps[bmx]])
                po, _ = _off["pscale"]
                V(lambda e, b=b, po=po, bmx=bmx: e.tensor_tensor(out=mixT[b][:, 4:8, :], in0=psb[bmx][:].rearrange("p (g t) -> p g t", g=4),
                                                        in1=_bc(cst[:, po:po + 4].unsqueeze(2), [128, 4, 128]), op=ALU.mult),
                  [t_ps[bmx], t_cst], [t_mixT[b]])
                bh = [nxt(), nxt()]
                for nb in range(2):
                    for c in range(8):
                        M(lambda e, b=b, nb=nb, c=c, bh=bh: e.matmul(psb[bh[nb]][:], lhsT=mixT[b][:, c, :], rhs=w_out[:, c, nb * 512:(nb + 1) * 512],
                                                             start=(c == 0), stop=(c == 7)),
                          [t_mixT[b], t_wout], [t_ps[bh[nb]]], n=530)
                for nb in range(2):
                    V(lambda e, b=b, nb=nb, bh=bh: e.tensor_tensor(out=ht[b][:, nb * 512:(nb + 1) * 512], in0=psb[bh[nb]][:], in1=xt[b][:, nb * 512:(nb + 1) * 512], op=ALU.add),
                      [t_ps[bh[nb]], t_xt[b]], [t_ht[b]])
                P.dma("sp", lambda e, j=j, b=b: e.dma_start(out=h_d[j * 128:(j + 1) * 128, :], in_=ht[b][:]), t_ht[b], reads=[t_ht[b]], writes=[t_hd[j]])
                if debug:
                    P.dma("sp", lambda e, j=j, b=b: e.dma_start(out=dbg["h1"][j * 128:(j + 1) * 128, :], in_=ht[b][:]), t_dbgs[j % 8], reads=[t_ht[b]])
                if phases < 2:
                    continue

                A(lambda e, b=b: e.activation(out=xn2[b][:], in_=ht[b][:], func=AF.Square, accum_out=ssq[b][:, 4:5]),
                  [t_ht[b]], [t_xn2[b], t_ssq[b]])
                V(lambda e, b=b: e.tensor_scalar(out=ssq[b][:, 5:6], in0=ssq[b][:, 4:5], scalar1=1.0 / D, scalar2=EPS, op0=ALU.mult, op1=ALU.add),
                  [t_ssq[b]], [t_ssq[b]])
                G(lambda e, b=b: e.tensor_tensor(out=ssq[b][:, 6:7], in0=ssq[b][:, 5:6], in1=nhalf[:, 0:1], op=ALU.pow),
                  [t_ssq[b], t_nhalf], [t_ssq[b]])
                V(lambda e, b=b: e.scalar_tensor_tensor(out=xn2[b][:], in0=ht[b][:], scalar=ssq[b][:, 6:7], in1=R("nmoe"), op0=ALU.mult, op1=ALU.mult),
                  [t_ht[b], t_ssq[b], t_rows], [t_xn2[b]])
                bx = j % NBX
                A(lambda e, b=b, bx=bx: e.copy(xn2b[bx][:], xn2[b][:]), [t_xn2[b]], [t_xn2b[bx]])
                bn = [nxt(), nxt()]
                blg = nxt()
                for c in range(8):
                    bank = bn[c // 4]
                    M(lambda e, b=b, c=c, bank=bank: e.transpose(psb[bank][:, (c % 4) * 128:(c % 4 + 1) * 128], xn2[b][:, c * 128:(c + 1) * 128], ident),
                      [t_xn2[b], t_cst], [t_ps[bank]])
                V(lambda e, b=b, bn=bn: e.tensor_copy(xn2T[b][:, 0:4, :], psb[bn[0]][:].rearrange("p (c t) -> p c t", c=4)), [t_ps[bn[0]]], [t_xn2T[b]])
                A(lambda e, b=b, bn=bn: e.copy(xn2T[b][:, 4:8, :], psb[bn[1]][:].rearrange("p (c t) -> p c t", c=4)), [t_ps[bn[1]]], [t_xn2T[b]])
                for c in range(8):
                    M(lambda e, b=b, c=c, blg=blg: e.matmul(psb[blg][:, 0:NE], lhsT=xn2T[b][:, c, :], rhs=rw[:, c, :], start=(c == 0), stop=(c == 7)),
                      [t_xn2T[b], t_rw], [t_ps[blg]])
                r_ = rt[b]
                t8 = top8[b]
                V(lambda e, r_=r_, blg=blg: e.tensor_tensor(out=r_[:, 0, :], in0=psb[blg][:, 0:NE], in1=R("rb"), op=ALU.add), [t_ps[blg], t_rows], [t_rt[b]])
                V(lambda e, r_=r_, t8=t8: e.max(out=t8[:, 0:8], in_=r_[:, 0, :]), [t_rt[b]], [t_top8[b]])
                V(lambda e, t8=t8: e.tensor_scalar(out=t8[:, 8:9], in0=t8[:, 0:1], scalar1=-1.0, scalar2=None, op0=ALU.mult), [t_top8[b]], [t_top8[b]])
                A(lambda e, t8=t8: e.activation(out=t8[:, 10:14], in_=t8[:, 0:4], func=AF.Exp, bias=t8[:, 8:9], accum_out=t8[:, 9:10]),
                  [t_top8[b]], [t_top8[b]])
                V(lambda e, t8=t8: e.reciprocal(t8[:, 14:15], t8[:, 9:10]), [t_top8[b]], [t_top8[b]])
                V(lambda e, r_=r_, t8=t8: e.tensor_scalar(out=r_[:, 1, :], in0=r_[:, 0, :], scalar1=t8[:, 3:4], scalar2=None, op0=ALU.is_ge),
                  [t_rt[b], t_top8[b]], [t_rt[b]])
                M(lambda e, r_=r_, blg=blg: e.matmul(psb[blg][:, 32:64], lhsT=C("ustrict"), rhs=r_[:, 1, :], start=True, stop=False), [t_rt[b], t_cst], [t_ps[blg]])
                M(lambda e, blg=blg: e.matmul(psb[blg][:, 32:64], lhsT=C("ones"), rhs=macc[:], start=False, stop=True), [t_macc, t_cst], [t_ps[blg]])
                V(lambda e, r_=r_: e.tensor_tensor(out=macc[:], in0=macc[:], in1=r_[:, 1, :], op=ALU.add), [t_macc, t_rt[b]], [t_macc])
                V(lambda e, r_=r_, blg=blg: e.tensor_copy(r_[:, 2, :], psb[blg][:, 32:64]), [t_ps[blg]], [t_rt[b]])
                V(lambda e, r_=r_: e.tensor_scalar(out=r_[:, 3, :], in0=r_[:, 2, :], scalar1=float(CAP) - 0.5, scalar2=BIGPOS, op0=ALU.is_ge, op1=ALU.mult),
                  [t_rt[b]], [t_rt[b]])
                V(lambda e, r_=r_: e.tensor_tensor(out=r_[:, 4, :], in0=r_[:, 2, :], in1=C("ecap"), op=ALU.add), [t_rt[b], t_cst], [t_rt[b]])
                V(lambda e, r_=r_: e.tensor_tensor(out=r_[:, 4, :], in0=r_[:, 4, :], in1=r_[:, 3, :], op=ALU.add), [t_rt[b]], [t_rt[b]])
                for k in range(TOPK):
                    V(lambda e, r_=r_, t8=t8, b=b, k=k: e.scalar_tensor_tensor(out=r_[:, 6, :], in0=r_[:, 0, :], scalar=t8[:, k:k + 1], in1=r_[:, 4, :],
                                                                             op0=ALU.is_equal, op1=ALU.mult, accum_out=posk[b][:, k:k + 1]),
                      [t_rt[b], t_top8[b]], [t_rt[b], t_posk[b]])
                V(lambda e, r_=r_, b=b: e.tensor_scalar(out=r_[:, 7, 0:4], in0=posk[b][:, 0:4], scalar1=BIGPOS * 0.5, scalar2=None, op0=ALU.is_lt),
                  [t_posk[b]], [t_rt[b]])
                V(lambda e, r_=r_, t8=t8, j=j: e.scalar_tensor_tensor(out=gates[:, j, :], in0=t8[:, 10:14], scalar=t8[:, 14:15], in1=r_[:, 7, 0:4],
                                                                      op0=ALU.mult, op1=ALU.mult),
                  [t_top8[b], t_rt[b]], [t_gates[j]])
                V(lambda e, b=b, j=j: e.tensor_copy(posi[:, j, :], posk[b][:, 0:4]), [t_posk[b]], [t_posi[j]])
                def scatter(j=j, bx=bx):
                    for k in range(TOPK):
                        P.dma("pool", lambda e, bx=bx, j=j, k=k: e.indirect_dma_start(
                            out=xs_d, out_offset=bass.IndirectOffsetOnAxis(ap=posi[:, j, k:k + 1], axis=0), in_=xn2b[bx][:], in_offset=None,
                            bounds_check=_bcreg(e, nc), oob_is_err=False), t_xn2b[bx], reads=[t_xn2b[bx], t_posi[j], t_zfd], writes=[t_xsd],
                            nbytes=256 * 1024, delay=_OPT["sdel"])
                pend.append(scatter)
                if len(pend) >= NBX - 1:
                    pend.pop(0)()
            for f_ in pend:
                f_()

            P.barrier(alltoks)

        if phases >= 3:
            build_phase3(locals())
        if phases >= 4:
            build_phase4(locals())

        if debug and phases >= 2:
            P.dma("sp", lambda e: e.dma_start(out=dbg["gate"], in_=gates[:].rearrange("p j k -> p (j k)")), T("dbg_g"), reads=t_gates)
            P.dma("sp", lambda e: e.dma_start(out=dbg["pos"], in_=posi[:].rearrange("p j k -> p (j k)")), T("dbg_p"), reads=t_posi)
        P.barrier(alltoks)
        stats = P.emit()
    return nc, stats


def make_in_maps(inp, ncores=8):
    cst, rows = host_consts(inp)
    wgu = inp["expert_w_gate_up"][0]
    w_g = np.ascontiguousarray(wgu[:, :, 0::2])
    w_u = np.ascontiguousarray(wgu[:, :, 1::2])
    shared = {
        "cst": cst, "rows": rows,
        "w_in": np.ascontiguousarray(inp["w_in"][0]), "w_out": np.ascontiguousarray(inp["w_out"][0]),
        "pool_w": np.ascontiguousarray(inp["pool_w"][0]), "router_w": np.ascontiguousarray(inp["router_w"][0]),
        "w_g": w_g, "w_u": w_u, "w_d": np.ascontiguousarray(inp["expert_w_down"][0]),
        "b_d": np.ascontiguousarray(inp["expert_b_down"][0]),
        "ple_gate_w": np.ascontiguousarray(inp["ple_gate_w"][0]), "ple_proj_w": np.ascontiguousarray(inp["ple_proj_w"][0]),
    }
    maps = []
    for b in range(ncores):
        m = dict(shared)
        m["x"] = np.ascontiguousarray(inp["x"][b])
        m["p"] = np.ascontiguousarray(inp["p"][0, b])
        m["pos"] = np.ascontiguousarray(inp["positions"][b].reshape(NT, 128).T.astype(np.int32))
        maps.append(m)
    return maps


def build_phase3(L):
    nc, P, T, sbuf, cst = L["nc"], L["P"], L["T"], L["sbuf"], L["cst"]
    V, A, G, M = L["V"], L["A"], L["G"], L["M"]
    psb, t_ps, t_cst, identb, t_identb = L["psb"], L["t_ps"], L["t_cst"], L["identb"], L["t_identb"]
    xs_d, ys_d, wg_d, wu_d, wd_d, bd_d = L["xs_d"], L["ys_d"], L["wg_d"], L["wu_d"], L["wd_d"], L["bd_d"]
    t_xsd, t_ysd, alltoks = L["t_xsd"], L["t_ysd"], L["alltoks"]
    n_exp = L.get("n_exp", NE)
    NSL = (CAP + 127) // 128
    HALF = CAP // 2

    def rows(i):
        return 128

    def tstart(i):
        return min(i * 128, CAP - 128)
    with ExitStack() as st:
        wgu = [sbuf(st, f"wgu{i}", [128, 8, 2048], BF16) for i in range(2)]
        wdn = [sbuf(st, f"wdn{i}", [128, 8, 1024], BF16) for i in range(2)]
        t_wgu = [T(f"wgu{i}") for i in range(2)]
        t_wdn = [T(f"wdn{i}") for i in range(2)]
        xtok = sbuf(st, "xtok", [128, NSL, D], BF16)
        t_xtok = T("xtok")
        XT = [sbuf(st, f"XT{i}", [128, 8, CAP], BF16) for i in range(2)]
        t_XT = [T(f"XT{i}") for i in range(2)]
        actT = [sbuf(st, f"actT{i}", [128, 8, CAP], BF16) for i in range(2)]
        t_actT = [T(f"actT{i}") for i in range(2)]
        NTMP = 2
        tmp = [sbuf(st, f"etmp{i}", [128, 4, HALF], F32) for i in range(NTMP)]
        t_tmp = [[T(f"etmp{i}_{k}") for k in range(4)] for i in range(NTMP)]
        NY = 4
        ysb = [sbuf(st, f"ysb3_{i}", [128, D], F32) for i in range(NY)]
        t_ysb = [T(f"ysb3_{i}") for i in range(NY)]
        bdb = [sbuf(st, f"bdb{i}", [128, D], F32) for i in range(2)]
        t_bdb = [T(f"bdb{i}") for i in range(2)]
        abg = sbuf(st, "abg", [128, 256], F32)
        t_abg = T("abg")
        ob, _ = _off["bgate"]
        ou, _ = _off["bup"]
        V(lambda e: e.tensor_scalar(out=abg[:], in0=cst[:, ob:ob + 256], scalar1=ALPHA, scalar2=None, op0=ALU.mult), [t_cst], [t_abg])
        bu1 = sbuf(st, "bu1", [128, 256], F32)
        V(lambda e: e.tensor_scalar(out=bu1[:], in0=cst[:, ou:ou + 256], scalar1=1.0, scalar2=None, op0=ALU.add), [t_cst], [t_abg])
        SIG7 = float(1.0 / (1.0 + np.exp(-ALPHA * LIMIT)))

        def load_wgu(e_idx):
            sl = e_idx % 2
            wg_v = wg_d[e_idx].rearrange("(c p) n -> p c n", p=128)
            wu_v = wu_d[e_idx].rearrange("(c p) n -> p c n", p=128)
            for hc in range(2):
                cs = slice(hc * 4, hc * 4 + 4)
                P.dma("pool", lambda e, cs=cs, sl=sl, wg_v=wg_v: e.dma_start(out=wgu[sl][:, cs, 0:1024], in_=wg_v[:, cs, :]), t_wgu[sl], writes=[t_wgu[sl]])
                P.dma("pool", lambda e, cs=cs, sl=sl, wu_v=wu_v: e.dma_start(out=wgu[sl][:, cs, 1024:2048], in_=wu_v[:, cs, :]), t_wgu[sl], writes=[t_wgu[sl]])

        def load_wd(e_idx):
            sl = e_idx % 2
            wd_v = wd_d[e_idx].rearrange("(c p) n -> p c n", p=128)
            for hc in range(2):
                cs = slice(hc * 4, hc * 4 + 4)
                P.dma("pool", lambda e, cs=cs, sl=sl, wd_v=wd_v: e.dma_start(out=wdn[sl][:, cs, :], in_=wd_v[:, cs, :]), t_wdn[sl], writes=[t_wdn[sl]])
            P.dma("sp", lambda e, sl=sl, e_idx=e_idx: e.dma_start(out=bdb[sl][:], in_=bd_d[e_idx].partition_broadcast(128)), t_bdb[sl], writes=[t_bdb[sl]])

        evac_rr = [0]

        def stage_T(e_idx):
            sl = e_idx % 2
            nfull = CAP // 128
            P.dma("sp", lambda e, e_idx=e_idx, nfull=nfull: e.dma_start(out=xtok[:, 0:nfull, :],
                                                                        in_=xs_d[e_idx * CAP:e_idx * CAP + nfull * 128, :].rearrange("(i p) d -> p i d", p=128)),
                  t_xtok, reads=[t_xsd], writes=[t_xtok])
            if CAP % 128:
                P.dma("sp", lambda e, e_idx=e_idx, nfull=nfull: e.dma_start(out=xtok[:, nfull, :], in_=xs_d[(e_idx + 1) * CAP - 128:(e_idx + 1) * CAP, :]),
                      t_xtok, reads=[t_xsd], writes=[t_xtok], nbytes=256 * 1024)
            for i in range(NSL):
                bank = 4
                pbv = psb[bank][:].bitcast(BF16)
                ri = rows(i)
                for c in range(8):
                    M(lambda e, i=i, c=c, pbv=pbv, ri=ri: e.transpose(pbv[:, c * 128:c * 128 + ri], xtok[0:ri, i, c * 128:(c + 1) * 128], identb[0:ri, 0:ri]),
                      [t_xtok, t_identb], [t_ps[bank]])
                eng = A if evac_rr[0] % 2 == 0 else V
                evac_rr[0] += 1
                if eng is A:
                    A(lambda e, i=i, sl=sl, pbv=pbv, ri=ri: e.copy(XT[sl][:, :, tstart(i):tstart(i) + ri], pbv.rearrange("p (c s) -> p c s", c=8)[:, :, 0:ri]),
                      [t_ps[bank]], [t_XT[sl]])
                else:
                    V(lambda e, i=i, sl=sl, pbv=pbv, ri=ri: e.tensor_copy(XT[sl][:, :, tstart(i):tstart(i) + ri], pbv.rearrange("p (c s) -> p c s", c=8)[:, :, 0:ri]),
                      [t_ps[bank]], [t_XT[sl]])

        cnt = [0]

        def stage_GU(e_idx):
            sl = e_idx % 2
            for jc in range(8):
                for hf in range(2):
                    pp = cnt[0] % 2
                    tb = cnt[0] % NTMP
                    cnt[0] += 1
                    bg, bu = psb[2 * pp], psb[2 * pp + 1]
                    ssl = slice(hf * HALF, (hf + 1) * HALF)
                    for c in range(8):
                        M(lambda e, sl=sl, jc=jc, c=c, bg=bg, ssl=ssl: e.matmul(bg[:, 0:HALF], lhsT=wgu[sl][:, c, jc * 128:(jc + 1) * 128], rhs=XT[sl][:, c, ssl],
                                                                             start=(c == 0), stop=(c == 7)),
                          [t_wgu[sl], t_XT[sl]], [t_ps[2 * pp]], n=HALF)
                    for c in range(8):
                        M(lambda e, sl=sl, jc=jc, c=c, bu=bu, ssl=ssl: e.matmul(bu[:, 0:HALF], lhsT=wgu[sl][:, c, 1024 + jc * 128:1024 + (jc + 1) * 128], rhs=XT[sl][:, c, ssl],
                                                                             start=(c == 0), stop=(c == 7)),
                          [t_wgu[sl], t_XT[sl]], [t_ps[2 * pp + 1]], n=HALF)
                    col = e_idx * 8 + jc
                    tm, tt = tmp[tb], t_tmp[tb]
                    A(lambda e, tm=tm, bg=bg, col=col: e.activation(out=tm[:, 0, :], in_=bg[:, 0:HALF], func=AF.Sigmoid, bias=abg[:, col:col + 1], scale=ALPHA),
                      [t_ps[2 * pp], t_abg], [tt[0]])
                    A(lambda e, tm=tm, bg=bg, col=col: e.activation(out=tm[:, 1, :], in_=bg[:, 0:HALF], func=AF.Identity, bias=cst[:, ob + col:ob + col + 1], scale=1.0),
                      [t_ps[2 * pp], t_cst], [tt[1]])
                    A(lambda e, tm=tm, bu=bu, col=col: e.activation(out=tm[:, 2, :], in_=bu[:, 0:HALF], func=AF.Identity, bias=bu1[:, col:col + 1], scale=1.0),
                      [t_ps[2 * pp + 1], t_abg], [tt[2]])
                    V(lambda e, tm=tm: e.tensor_scalar(out=tm[:, 2, :], in0=tm[:, 2, :], scalar1=LIMIT + 1.0, scalar2=-LIMIT + 1.0, op0=ALU.min, op1=ALU.max), [tt[2]], [tt[2]])
                    V(lambda e, tm=tm: e.scalar_tensor_tensor(out=tm[:, 3, :], in0=tm[:, 1, :], scalar=LIMIT, in1=tm[:, 2, :], op0=ALU.min, op1=ALU.mult),
                      [tt[1], tt[2]], [tt[3]])
                    V(lambda e, tm=tm, sl=sl, jc=jc, ssl=ssl: e.scalar_tensor_tensor(out=actT[sl][:, jc, ssl], in0=tm[:, 0, :], scalar=SIG7, in1=tm[:, 3, :],
                                                                                  op0=ALU.min, op1=ALU.mult), [tt[0], tt[3]], [t_actT[sl]])

        ycnt = [0]
        ybank = [0]

        def stage_down(e_idx):
            sl = e_idx % 2
            for i in range(NSL):
                yb = ycnt[0] % NY
                ycnt[0] += 1
                ri = rows(i)
                for nb in range(2):
                    bk = 5 + ybank[0] % 3
                    ybank[0] += 1
                    for jc in range(8):
                        M(lambda e, sl=sl, i=i, nb=nb, jc=jc, bk=bk, ri=ri: e.matmul(psb[bk][0:ri, :], lhsT=actT[sl][:, jc, tstart(i):tstart(i) + ri], rhs=wdn[sl][:, jc, nb * 512:(nb + 1) * 512],
                                                                              start=(jc == 0), stop=(jc == 7)),
                          [t_actT[sl], t_wdn[sl]], [t_ps[bk]], n=512)
                    V(lambda e, yb=yb, nb=nb, sl=sl, bk=bk, ri=ri: e.tensor_tensor(out=ysb[yb][0:ri, nb * 512:(nb + 1) * 512], in0=psb[bk][0:ri, :], in1=bdb[sl][0:ri, nb * 512:(nb + 1) * 512], op=ALU.add),
                      [t_ps[bk], t_bdb[sl]], [t_ysb[yb]], n=512)
                ov = i * 128 - tstart(i)
                r0 = e_idx * CAP + i * 128
                P.dma("sp", lambda e, yb=yb, r0=r0, ov=ov: e.dma_start(out=ys_d[r0:r0 + 128 - ov, :], in_=ysb[yb][ov:128, :]), t_ysb[yb], reads=[t_ysb[yb]], writes=[t_ysd],
                      nbytes=(128 - ov) * 4096)

        load_wgu(0)
        load_wd(0)
        load_wgu(1)
        stage_T(0)
        if n_exp > 1:
            stage_T(1)
        stage_GU(0)
        for s_ in range(n_exp):
            if s_ + 2 < n_exp:
                load_wgu(s_ + 2)
            if s_ + 1 < n_exp:
                load_wd(s_ + 1)
            if s_ + 2 < n_exp:
                stage_T(s_ + 2)
            if s_ + 1 < n_exp:
                stage_GU(s_ + 1)
            stage_down(s_)
        P.barrier(alltoks)


def build_phase4(L):
    nc, P, T, sbuf, cst = L["nc"], L["P"], L["T"], L["sbuf"], L["cst"]
    V, A, G, M = L["V"], L["A"], L["G"], L["M"]
    psb, t_ps, t_cst, identb, t_identb = L["psb"], L["t_ps"], L["t_cst"], L["identb"], L["t_identb"]
    ys_d, h_d, p_d, out_d, pgw_d, ppw_d, rows_d = L["ys_d"], L["h_d"], L["p_d"], L["out_d"], L["pgw_d"], L["ppw_d"], L["rows_d"]
    t_ysd, t_hd, alltoks = L["t_ysd"], L["t_hd"], L["alltoks"]
    gates, posi, t_gates, t_posi, nhalf, t_nhalf = L["gates"], L["posi"], L["t_gates"], L["t_posi"], L["nhalf"], L["t_nhalf"]
    nt, dbg, debug = L["nt"], L["dbg"], L["debug"]
    ident = cst[:, _off["ident"][0]:_off["ident"][0] + 128]
    with ExitStack() as st:
        pgw = sbuf(st, "pgw", [128, 8, D], BF16)
        t_pgw = T("pgw")
        ppw = sbuf(st, "ppw", [128, 2, D], BF16)
        t_ppw = T("ppw")
        fnw = sbuf(st, "fnw", [128, D], F32)
        t_fnw = T("fnw")
        stg = [sbuf(st, f"stg4_{i}", [128, D], F32) for i in range(2)]
        t_stg = [T(f"stg4_{i}") for i in range(2)]
        pgw_v = pgw_d.rearrange("(c p) n -> p c n", p=128)
        on, _ = _off["nple"]
        for c in range(8):
            s_ = c % 2
            P.dma("sp", lambda e, c=c, s_=s_: e.dma_start(out=stg[s_][:], in_=pgw_v[:, c, :]), t_stg[s_], writes=[t_stg[s_]])
            if c % 2 == 0:
                V(lambda e, c=c, s_=s_: e.tensor_scalar(out=pgw[:, c, :], in0=stg[s_][:], scalar1=cst[:, on + c:on + c + 1], scalar2=None, op0=ALU.mult),
                  [t_stg[s_], t_cst], [t_pgw], n=1024)
            else:
                A(lambda e, c=c, s_=s_: e.activation(out=pgw[:, c, :], in_=stg[s_][:], func=AF.Copy, scale=cst[:, on + c:on + c + 1]),
                  [t_stg[s_], t_cst], [t_pgw], n=1024)
        P.dma("pool", lambda e: e.dma_start(out=ppw[:], in_=ppw_d.rearrange("(c p) n -> p c n", p=128)), t_ppw, writes=[t_ppw])
        fo, fw = _roff["fnw"]
        P.dma("sp", lambda e: e.dma_start(out=fnw[:], in_=rows_d[0, fo:fo + fw].partition_broadcast(128)), t_fnw, writes=[t_fnw])

        NB = 4
        bctr = [0]

        def nxt():
            v = bctr[0] % 8
            bctr[0] += 1
            return v

        def mk(name, shape, dtype, n=NB):
            return [sbuf(st, f"{name}{i}", shape, dtype) for i in range(n)], [T(f"{name}{i}") for i in range(n)]
        hb_, t_hb = mk("h4", [128, D], F32)
        pt_, t_pt = mk("p4", [128, PLE], F32)
        yk, t_yk = mk("yk", [128, 4, D], F32)
        junk, t_junk = mk("junk4", [128, D], BF16, 1)
        sq, t_sq = mk("sq4", [128, 8], F32)
        xn3, t_xn3 = mk("xn3", [128, D], BF16)
        xn3T, t_xn3T = mk("xn3T", [128, 8, 128], BF16)
        pT, t_pT = mk("pT", [128, 2, 128], BF16)
        sgm, t_sgm = mk("sgm", [128, D], F32)
        ot, t_ot = mk("ot", [128, D], F32)
        h3, _unused = mk("h3_", [128, D], F32)
        t_sgmh = [[T(f"sgmh{i}_{k}") for k in range(2)] for i in range(NB)]
        t_h3h = [[T(f"h3h{i}_{k}") for k in range(2)] for i in range(NB)]
        for i_ in range(NB):
            G(lambda e, i_=i_: e.memset(yk[i_][:], 0.0), (), [t_yk[i_]])

        for j in range(nt):
            b = j % NB
            P.dma("sp", lambda e, j=j, b=b: e.dma_start(out=hb_[b][:], in_=h_d[j * 128:(j + 1) * 128, :]), t_hb[b], reads=[t_hd[j]], writes=[t_hb[b]])
            P.dma("sp", lambda e, j=j, b=b: e.dma_start(out=pt_[b][:], in_=p_d[j * 128:(j + 1) * 128, :]), t_pt[b], writes=[t_pt[b]])
            for k in range(TOPK):
                P.dma("pool", lambda e, j=j, b=b, k=k: e.indirect_dma_start(
                    out=yk[b][:, k, :], out_offset=None, in_=ys_d, in_offset=bass.IndirectOffsetOnAxis(ap=posi[:, j, k:k + 1], axis=0),
                    bounds_check=_bcreg(e, nc), oob_is_err=False), t_yk[b], reads=[t_ysd, t_posi[j]], writes=[t_yk[b]], nbytes=int(_OPT["gx"] * 150e3))
            for k in range(TOPK):
                V(lambda e, j=j, b=b, k=k: e.scalar_tensor_tensor(out=hb_[b][:], in0=yk[b][:, k, :], scalar=gates[:, j, k:k + 1], in1=hb_[b][:],
                                                                 op0=ALU.mult, op1=ALU.add), [t_yk[b], t_gates[j], t_hb[b]], [t_hb[b]])
            if debug:
                P.dma("sp", lambda e, j=j, b=b: e.dma_start(out=dbg["h2"][j * 128:(j + 1) * 128, :], in_=hb_[b][:]), L["t_dbgs"][8 + j % 8], reads=[t_hb[b]])
            A(lambda e, b=b: e.activation(out=ot[b][:], in_=hb_[b][:], func=AF.Square, accum_out=sq[b][:, 0:1]), [t_hb[b]], [t_ot[b], t_sq[b]])
            V(lambda e, b=b: e.tensor_scalar(out=sq[b][:, 1:2], in0=sq[b][:, 0:1], scalar1=1.0 / D, scalar2=EPS, op0=ALU.mult, op1=ALU.add), [t_sq[b]], [t_sq[b]])
            G(lambda e, b=b: e.tensor_tensor(out=sq[b][:, 2:3], in0=sq[b][:, 1:2], in1=nhalf[:, 0:1], op=ALU.pow), [t_sq[b], t_nhalf], [t_sq[b]])
            A(lambda e, b=b: e.activation(out=xn3[b][:], in_=hb_[b][:], func=AF.Copy, scale=sq[b][:, 2:3]), [t_hb[b], t_sq[b]], [t_xn3[b]])
            b0_, b1_ = nxt(), nxt()
            bg_ = [nxt(), nxt()]
            bp_ = [nxt(), nxt()]
            pb0 = psb[b0_][:].bitcast(BF16)
            for c in range(8):
                M(lambda e, b=b, c=c, pb0=pb0: e.transpose(pb0[:, c * 128:(c + 1) * 128], xn3[b][:, c * 128:(c + 1) * 128], identb[:]),
                  [t_xn3[b], t_identb], [t_ps[b0_]])
            A(lambda e, b=b, pb0=pb0: e.copy(xn3T[b][:], pb0.rearrange("p (c t) -> p c t", c=8)), [t_ps[b0_]], [t_xn3T[b]], n=1024)
            for c in range(2):
                M(lambda e, b=b, c=c, b1_=b1_: e.transpose(psb[b1_][:, c * 128:(c + 1) * 128], pt_[b][:, c * 128:(c + 1) * 128], ident), [t_pt[b], t_cst], [t_ps[b1_]], n=400)
            A(lambda e, b=b, b1_=b1_: e.copy(pT[b][:], psb[b1_][:, 0:256].rearrange("p (c t) -> p c t", c=2)), [t_ps[b1_]], [t_pT[b]], n=256)
            for nb in range(2):
                for c in range(8):
                    M(lambda e, b=b, nb=nb, c=c, bg_=bg_: e.matmul(psb[bg_[nb]][:], lhsT=xn3T[b][:, c, :], rhs=pgw[:, c, nb * 512:(nb + 1) * 512], start=(c == 0), stop=(c == 7)),
                      [t_xn3T[b], t_pgw], [t_ps[bg_[nb]]], n=600)
            for nb in range(2):
                for c in range(2):
                    M(lambda e, b=b, nb=nb, c=c, bp_=bp_: e.matmul(psb[bp_[nb]][:], lhsT=pT[b][:, c, :], rhs=ppw[:, c, nb * 512:(nb + 1) * 512], start=(c == 0), stop=(c == 1)),
                      [t_pT[b], t_ppw], [t_ps[bp_[nb]]], n=600)
            for nb in range(2):
                hs = slice(nb * 512, (nb + 1) * 512)
                A(lambda e, b=b, nb=nb, bg_=bg_, hs=hs: e.activation(out=sgm[b][:, hs], in_=psb[bg_[nb]][:], func=AF.Sigmoid), [t_ps[bg_[nb]]], [t_sgmh[b][nb]], n=512)
                V(lambda e, b=b, nb=nb, bp_=bp_, hs=hs: e.tensor_tensor(out=sgm[b][:, hs], in0=sgm[b][:, hs], in1=psb[bp_[nb]][:], op=ALU.mult),
                  [t_sgmh[b][nb], t_ps[bp_[nb]]], [t_sgmh[b][nb]], n=512)
                V(lambda e, b=b, hs=hs: e.tensor_tensor(out=h3[b][:, hs], in0=hb_[b][:, hs], in1=sgm[b][:, hs], op=ALU.add), [t_hb[b], t_sgmh[b][nb]], [t_h3h[b][nb]], n=512)
            A(lambda e, b=b: e.activation(out=ot[b][:], in_=h3[b][:], func=AF.Square, accum_out=sq[b][:, 4:5]), t_h3h[b], [t_ot[b], t_sq[b]])
            V(lambda e, b=b: e.tensor_scalar(out=sq[b][:, 5:6], in0=sq[b][:, 4:5], scalar1=1.0 / D, scalar2=EPS, op0=ALU.mult, op1=ALU.add), [t_sq[b]], [t_sq[b]])
            G(lambda e, b=b: e.tensor_tensor(out=sq[b][:, 6:7], in0=sq[b][:, 5:6], in1=nhalf[:, 0:1], op=ALU.pow), [t_sq[b], t_nhalf], [t_sq[b]])
            A(lambda e, b=b: e.activation(out=ot[b][:], in_=h3[b][:], func=AF.Copy, scale=sq[b][:, 6:7]), t_h3h[b] + [t_sq[b]], [t_ot[b]], n=1024)
            G(lambda e, b=b: e.tensor_tensor(out=ot[b][:], in0=ot[b][:], in1=fnw[:], op=ALU.mult), [t_ot[b], t_fnw], [t_ot[b]], n=1024)
            P.dma("sp", lambda e, j=j, b=b: e.dma_start(out=out_d[j * 128:(j + 1) * 128, :], in_=ot[b][:]), t_ot[b], reads=[t_ot[b]])
        P.barrier(alltoks)


_CACHE = {}


def kernel(**inputs):
    inp = {k: np.asarray(v) for k, v in inputs.items()}
    if "nc" not in _CACHE:
        _CACHE["nc"] = build()[0]
    nc = _CACHE["nc"]
    maps = make_in_maps(inp, ncores=8)
    res = run_bass_kernel_spmd(nc, maps, core_ids=list(range(8)))
    out = np.stack([np.asarray(r["out"], dtype=np.float32) for r in res.results], axis=0)
    return out.reshape(8, SEQ, D)
```
